# Optimizing a Trainium2 kernel written in Bass

```python
import numpy as np
import jax
import jax.numpy as jnp
from jax import lax


D_MODEL = 1024
BATCH = 4
SEQ = 4096
DEPTH = 2

MIX_WIDTH = D_MODEL
NSA_WIDTH = MIX_WIDTH // 2
MLSTM_WIDTH = MIX_WIDTH - NSA_WIDTH
NSA_HEADS = 8
NSA_KV_HEADS = 2
NSA_HEAD_DIM = NSA_WIDTH // NSA_HEADS
NSA_BRANCHES = 3
CMP_LEN = 32
CMP_STRIDE = 16
CMP_HIDDEN = 128
SEL_BLOCK = 64
SEL_TOP = 16
WINDOW = 512
Q_BLOCK = 128
FORCE_BONUS = 1e4
NEG_INF = -1e30
MLSTM_HEADS = 4
MLSTM_HEAD_DIM = MLSTM_WIDTH // MLSTM_HEADS
MLSTM_CHUNK = 64
CONV_WIDTH = 4
D_FF = 2816
N_EXPERTS = 8
TOP_K = 2
D_FF_EXPERT = 3584
MOE_BLOCK = 256
EPS = 1e-6
N_DENSE_LAYERS = (DEPTH + 1) // 2
N_MOE_LAYERS = DEPTH // 2
NSA_KV_COLS = 2 * NSA_BRANCHES * NSA_KV_HEADS * NSA_HEAD_DIM
IN_SIZES = (NSA_WIDTH, NSA_KV_COLS, NSA_HEADS * NSA_BRANCHES, 2 * MLSTM_WIDTH, MLSTM_WIDTH, MLSTM_WIDTH, MLSTM_HEADS, MLSTM_HEADS)
IN_COLS = sum(IN_SIZES)

kernel_name = 'hybrid_nsa_mlstm_moe_block'


def _split_points():
    return [int(v) for v in np.cumsum(IN_SIZES)[:-1]]


def rmsnorm(x, g):
    x32 = x.astype(jnp.float32)
    y = x32 * lax.rsqrt(jnp.mean(x32 * x32, axis=-1, keepdims=True) + EPS)
    return (y * g.astype(jnp.float32)).astype(x.dtype)


def alibi_slopes(n):
    return jnp.asarray(2.0 ** (-8.0 * np.arange(1, n + 1) / n), dtype=jnp.float32)


def overlap_matrix(n_cmp, n_sel):
    lo_c = np.arange(n_cmp)[:, None] * CMP_STRIDE
    lo_s = np.arange(n_sel)[None, :] * SEL_BLOCK
    ov = np.minimum(lo_c + CMP_LEN, lo_s + SEL_BLOCK) - np.maximum(lo_c, lo_s)
    return jnp.asarray(np.clip(ov, 0, None) / CMP_LEN, dtype=jnp.float32)


def masked_softmax(s, mask):
    return jax.nn.softmax(jnp.where(mask, s, NEG_INF), axis=-1)


def compress_blocks(kv, pe, w1, w2):
    B, S, G, dh = kv.shape
    n_sub = CMP_LEN // CMP_STRIDE
    ch = kv.reshape(B, S // CMP_STRIDE, CMP_STRIDE, G, dh)
    n_cmp = ch.shape[1] - n_sub + 1
    blocks = jnp.concatenate([ch[:, i:i + n_cmp] for i in range(n_sub)], axis=2)
    blocks = blocks + pe[None, None, :, None, :]
    flat = blocks.transpose(0, 1, 3, 2, 4).reshape(B, n_cmp, G, CMP_LEN * dh)
    return jax.nn.silu(flat @ w1) @ w2


def nsa_attention(q, kv, gate_logits, cmp_pe, cmp_w1, cmp_w2):
    B, S, H, dh = q.shape
    G = NSA_KV_HEADS
    R = H // G
    f32 = jnp.float32
    k_c = compress_blocks(kv[:, :, 0], cmp_pe[0], cmp_w1[0], cmp_w2[0])
    v_c = compress_blocks(kv[:, :, 1], cmp_pe[1], cmp_w1[1], cmp_w2[1])
    n_cmp = k_c.shape[1]
    cmp_end = jnp.arange(n_cmp) * CMP_STRIDE + CMP_LEN - 1
    n_sel = S // SEL_BLOCK
    n_top = min(SEL_TOP, n_sel)
    k_s = kv[:, :, 2].reshape(B, n_sel, SEL_BLOCK, G, dh).transpose(0, 3, 1, 2, 4)
    v_s = kv[:, :, 3].reshape(B, n_sel, SEL_BLOCK, G, dh).transpose(0, 3, 1, 2, 4)
    pad = ((0, 0), (WINDOW, 0), (0, 0), (0, 0))
    k_w = jnp.pad(kv[:, :, 4], pad)
    v_w = jnp.pad(kv[:, :, 5], pad)
    slopes = alibi_slopes(H).reshape(G, R)
    ov = overlap_matrix(n_cmp, n_sel)
    gates = jax.nn.sigmoid(gate_logits.astype(f32)).reshape(B, S, G, R, NSA_BRANCHES)
    n_qb = S // Q_BLOCK
    q_blocks = (q * (dh ** -0.5)).reshape(B, n_qb, Q_BLOCK, G, R, dh).swapaxes(0, 1)
    g_blocks = gates.reshape(B, n_qb, Q_BLOCK, G, R, NSA_BRANCHES).swapaxes(0, 1)
    bi = jnp.arange(B)[:, None, None, None]
    gi = jnp.arange(G)[None, :, None, None]
    j_sel = jnp.arange(n_sel)

    def block_fn(args):
        qb, gb, blk = args
        t = blk * Q_BLOCK + jnp.arange(Q_BLOCK)
        s_c = jnp.einsum('bqgrd,bngd->bgrqn', qb, k_c).astype(f32)
        dist_c = (t[:, None] - cmp_end[None, :]).astype(f32)
        s_c = s_c - slopes[:, :, None, None] * dist_c
        mask_c = cmp_end[None, :] <= t[:, None]
        p_c = masked_softmax(s_c, mask_c) * jnp.any(mask_c, axis=-1)[:, None]
        o_c = jnp.einsum('bgrqn,bngd->bqgrd', p_c.astype(v_c.dtype), v_c)
        imp = jnp.einsum('bgrqn,nj->bgqj', p_c, ov)
        cur = t // SEL_BLOCK
        valid_j = j_sel[None, :] <= cur[:, None]
        forced = (j_sel[None, :] == 0) | (j_sel[None, :] == cur[:, None]) | (j_sel[None, :] == cur[:, None] - 1)
        imp = jnp.where(forced, imp + FORCE_BONUS, imp)
        imp = jnp.where(valid_j, imp, -1.0)
        _, idx = lax.top_k(imp, n_top)
        kg = k_s[bi, gi, idx]
        vg = v_s[bi, gi, idx]
        s_s = jnp.einsum('bqgrd,bgqnkd->bgrqnk', qb, kg).astype(f32)
        pos_s = idx[..., None] * SEL_BLOCK + jnp.arange(SEL_BLOCK)
        dist_s = t[None, None, :, None, None] - pos_s
        s_s = s_s - slopes[None, :, :, None, None, None] * dist_s[:, :, None].astype(f32)
        mask_s = (dist_s >= 0)[:, :, None]
        p_s = masked_softmax(s_s.reshape(B, G, R, Q_BLOCK, n_top * SEL_BLOCK),
                             mask_s.reshape(B, G, 1, Q_BLOCK, n_top * SEL_BLOCK)).reshape(s_s.shape)
        o_s = jnp.einsum('bgrqnk,bgqnkd->bqgrd', p_s.astype(vg.dtype), vg)
        k_blk = lax.dynamic_slice_in_dim(k_w, blk * Q_BLOCK, Q_BLOCK + WINDOW, axis=1)
        v_blk = lax.dynamic_slice_in_dim(v_w, blk * Q_BLOCK, Q_BLOCK + WINDOW, axis=1)
        pos_w = blk * Q_BLOCK - WINDOW + jnp.arange(Q_BLOCK + WINDOW)
        dist_w = t[:, None] - pos_w[None, :]
        mask_w = (dist_w >= 0) & (dist_w < WINDOW) & (pos_w[None, :] >= 0)
        s_w = jnp.einsum('bqgrd,bkgd->bgrqk', qb, k_blk).astype(f32) - slopes[:, :, None, None] * dist_w.astype(f32)
        p_w = masked_softmax(s_w, mask_w)
        o_w = jnp.einsum('bgrqk,bkgd->bqgrd', p_w.astype(v_blk.dtype), v_blk)
        out = gb[..., 0:1] * o_c + gb[..., 1:2] * o_s + gb[..., 2:3] * o_w
        return out.astype(q.dtype)

    out = lax.map(block_fn, (q_blocks, g_blocks, jnp.arange(n_qb)))
    return out.swapaxes(0, 1).reshape(B, S, H * dh)


def causal_conv(x, w, b):
    K = w.shape[0]
    S = x.shape[1]
    xp = jnp.pad(x, ((0, 0), (K - 1, 0), (0, 0)))
    y = b
    for i in range(K):
        y = y + xp[:, i:i + S] * w[i]
    return y


def mlstm(q, k, v, o_pre, i_pre, f_pre, norm_g):
    B, S, _ = q.shape
    H, d, L = MLSTM_HEADS, MLSTM_HEAD_DIM, MLSTM_CHUNK
    nc = S // L
    f32 = jnp.float32

    def to_chunks(a):
        return a.astype(f32).reshape(B, nc, L, H, d).transpose(1, 0, 3, 2, 4)

    def gate_chunks(a):
        return a.astype(f32).reshape(B, nc, L, H).transpose(1, 0, 3, 2)

    qc = to_chunks(q)
    kc = to_chunks(k) * (d ** -0.5)
    vc = to_chunks(v)
    lf = jax.nn.log_sigmoid(gate_chunks(f_pre))
    ig = gate_chunks(i_pre)
    causal = jnp.tril(jnp.ones((L, L), dtype=bool))

    def step(carry, inp):
        C, n, m = carry
        qq, kk, vv, lf_c, ig_c = inp
        a = jnp.cumsum(lf_c, axis=-1)
        log_d = a[..., :, None] - a[..., None, :] + ig_c[..., None, :]
        log_d = jnp.where(causal, log_d, -jnp.inf)
        m_inter = a + m[..., None]
        m_t = jnp.maximum(m_inter, jnp.max(log_d, axis=-1))
        d_mat = jnp.exp(log_d - m_t[..., None])
        inter = jnp.exp(m_inter - m_t)
        s_qk = jnp.einsum('bhjd,bhsd->bhjs', qq, kk) * d_mat
        num = inter[..., None] * jnp.einsum('bhed,bhjd->bhje', C, qq) + jnp.einsum('bhjs,bhse->bhje', s_qk, vv)
        den = inter * jnp.einsum('bhd,bhjd->bhj', n, qq) + jnp.sum(s_qk, axis=-1)
        h = num / jnp.maximum(jnp.abs(den), jnp.exp(-m_t))[..., None]
        a_last = a[..., -1]
        log_w = a_last[..., None] - a + ig_c
        m_new = jnp.maximum(a_last + m, jnp.max(log_w, axis=-1))
        w = jnp.exp(log_w - m_new[..., None])
        decay = jnp.exp(a_last + m - m_new)
        C_new = decay[..., None, None] * C + jnp.einsum('bhse,bhsd->bhed', w[..., None] * vv, kk)
        n_new = decay[..., None] * n + jnp.einsum('bhs,bhsd->bhd', w, kk)
        return (C_new, n_new, m_new), h

    init = (jnp.zeros((B, H, d, d), f32), jnp.zeros((B, H, d), f32), jnp.zeros((B, H), f32))
    _, h = lax.scan(step, init, (qc, kc, vc, lf, ig))
    h = h.transpose(1, 0, 3, 2, 4).reshape(B, S, H * d)
    h = jax.nn.sigmoid(o_pre.astype(f32)) * h
    hh = h.reshape(B, S, H, d)
    hh = hh * lax.rsqrt(jnp.mean(hh * hh, axis=-1, keepdims=True) + EPS)
    return (hh.reshape(B, S, H * d) * norm_g.astype(f32)).astype(q.dtype)


def hybrid_mixer(h, w_in, b_in, cmp_pe, cmp_w1, cmp_w2, conv_w, conv_b, mlstm_norm_g, w_out):
    B, S, _ = h.shape
    proj = h @ w_in + b_in
    q_n, kv_n, gate_n, qk_m, v_m, o_m, i_m, f_m = jnp.split(proj, _split_points(), axis=-1)
    y_nsa = nsa_attention(q_n.reshape(B, S, NSA_HEADS, NSA_HEAD_DIM),
                          kv_n.reshape(B, S, 2 * NSA_BRANCHES, NSA_KV_HEADS, NSA_HEAD_DIM),
                          gate_n, cmp_pe, cmp_w1, cmp_w2)
    qk_m = jax.nn.silu(causal_conv(qk_m, conv_w, conv_b))
    q_m, k_m = jnp.split(qk_m, 2, axis=-1)
    y_ml = mlstm(q_m, k_m, v_m, o_m, i_m, f_m, mlstm_norm_g)
    return jnp.concatenate([y_nsa, y_ml], axis=-1) @ w_out


def swiglu(h, wg, wu, wd):
    return (jax.nn.silu(h @ wg) * (h @ wu)) @ wd


def moe_swiglu(h, router_w, w_gate, w_up, w_down):
    B, S, D = h.shape
    N = B * S
    xf = h.reshape(N, D)
    logits = (xf @ router_w).astype(jnp.float32)
    top_val, top_idx = lax.top_k(logits, TOP_K)
    weights = jax.nn.softmax(top_val, axis=-1)
    e_flat = top_idx.reshape(-1)
    tok_flat = jnp.repeat(jnp.arange(N), TOP_K)
    w_flat = weights.reshape(-1)
    order = jnp.argsort(e_flat)
    e_sorted = e_flat[order]
    counts = jnp.bincount(e_flat, length=N_EXPERTS)
    starts = jnp.cumsum(counts) - counts
    padded = (counts + MOE_BLOCK - 1) // MOE_BLOCK * MOE_BLOCK
    pad_ends = jnp.cumsum(padded)
    pad_starts = pad_ends - padded
    dest = pad_starts[e_sorted] + (jnp.arange(N * TOP_K) - starts[e_sorted])
    n_rows = ((N * TOP_K + MOE_BLOCK - 1) // MOE_BLOCK + N_EXPERTS) * MOE_BLOCK
    row_tok = jnp.zeros((n_rows,), jnp.int32).at[dest].set(tok_flat[order])
    row_w = jnp.zeros((n_rows,), jnp.float32).at[dest].set(w_flat[order])
    n_blocks = n_rows // MOE_BLOCK
    blk_expert = jnp.minimum(jnp.searchsorted(pad_ends, jnp.arange(n_blocks) * MOE_BLOCK, side='right'), N_EXPERTS - 1)

    def expert_block(args):
        toks, e = args
        xb = xf[toks]
        return (jax.nn.silu(xb @ w_gate[e]) * (xb @ w_up[e])) @ w_down[e]

    y = lax.map(expert_block, (row_tok.reshape(n_blocks, MOE_BLOCK), blk_expert))
    y = y.reshape(n_rows, D) * row_w[:, None].astype(y.dtype)
    out = jnp.zeros((N, D), h.dtype).at[row_tok].add(y)
    return out.reshape(B, S, D)


def setup_inputs(seed: int = 0) -> dict:
    key = jax.random.key(seed)
    ks = jax.random.split(key, 24)
    f32 = jnp.float32

    def nrm(k, shape, scale):
        return jax.random.normal(k, shape, f32) * scale

    x = nrm(ks[0], (BATCH, SEQ, D_MODEL), 1.0)
    c = nrm(ks[1], (BATCH, D_MODEL), 1.0)
    ada_w = nrm(ks[2], (DEPTH, D_MODEL, 6 * D_MODEL), 0.5 * D_MODEL ** -0.5)
    ada_b = nrm(ks[3], (DEPTH, 6 * D_MODEL), 0.02)
    norm_mix_g = 1.0 + nrm(ks[4], (DEPTH, D_MODEL), 0.05)
    norm_ffn_g = 1.0 + nrm(ks[5], (DEPTH, D_MODEL), 0.05)
    w_in = nrm(ks[6], (DEPTH, D_MODEL, IN_COLS), D_MODEL ** -0.5)
    b_in = nrm(ks[7], (DEPTH, IN_COLS), 0.02)
    b_in = b_in.at[:, -MLSTM_HEADS:].add(jnp.linspace(3.0, 6.0, MLSTM_HEADS, dtype=f32))
    cmp_pe = nrm(ks[8], (DEPTH, 2, CMP_LEN, NSA_HEAD_DIM), 0.02)
    cmp_w1 = nrm(ks[9], (DEPTH, 2, CMP_LEN * NSA_HEAD_DIM, CMP_HIDDEN), (CMP_LEN * NSA_HEAD_DIM) ** -0.5)
    cmp_w2 = nrm(ks[10], (DEPTH, 2, CMP_HIDDEN, NSA_HEAD_DIM), CMP_HIDDEN ** -0.5)
    conv_w = nrm(ks[11], (DEPTH, CONV_WIDTH, 2 * MLSTM_WIDTH), CONV_WIDTH ** -0.5)
    conv_b = nrm(ks[12], (DEPTH, 2 * MLSTM_WIDTH), 0.02)
    mlstm_norm_g = 1.0 + nrm(ks[13], (DEPTH, MLSTM_WIDTH), 0.05)
    w_out = nrm(ks[14], (DEPTH, MIX_WIDTH, D_MODEL), MIX_WIDTH ** -0.5)
    ffn_w_gate = nrm(ks[15], (N_DENSE_LAYERS, D_MODEL, D_FF), D_MODEL ** -0.5)
    ffn_w_up = nrm(ks[16], (N_DENSE_LAYERS, D_MODEL, D_FF), D_MODEL ** -0.5)
    ffn_w_down = nrm(ks[17], (N_DENSE_LAYERS, D_FF, D_MODEL), D_FF ** -0.5)
    router_w = nrm(ks[18], (N_MOE_LAYERS, D_MODEL, N_EXPERTS), D_MODEL ** -0.5)
    moe_w_gate = nrm(ks[19], (N_MOE_LAYERS, N_EXPERTS, D_MODEL, D_FF_EXPERT), D_MODEL ** -0.5)
    moe_w_up = nrm(ks[20], (N_MOE_LAYERS, N_EXPERTS, D_MODEL, D_FF_EXPERT), D_MODEL ** -0.5)
    moe_w_down = nrm(ks[21], (N_MOE_LAYERS, N_EXPERTS, D_FF_EXPERT, D_MODEL), D_FF_EXPERT ** -0.5)
    final_norm_g = 1.0 + nrm(ks[22], (D_MODEL,), 0.05)
    return {'x': x, 'c': c, 'ada_w': ada_w, 'ada_b': ada_b, 'norm_mix_g': norm_mix_g,
            'norm_ffn_g': norm_ffn_g, 'w_in': w_in, 'b_in': b_in, 'cmp_pe': cmp_pe,
            'cmp_w1': cmp_w1, 'cmp_w2': cmp_w2, 'conv_w': conv_w, 'conv_b': conv_b,
            'mlstm_norm_g': mlstm_norm_g, 'w_out': w_out, 'ffn_w_gate': ffn_w_gate,
            'ffn_w_up': ffn_w_up, 'ffn_w_down': ffn_w_down, 'router_w': router_w,
            'moe_w_gate': moe_w_gate, 'moe_w_up': moe_w_up, 'moe_w_down': moe_w_down,
            'final_norm_g': final_norm_g}


def reference(x, c, ada_w, ada_b, norm_mix_g, norm_ffn_g, w_in, b_in, cmp_pe, cmp_w1, cmp_w2,
              conv_w, conv_b, mlstm_norm_g, w_out, ffn_w_gate, ffn_w_up, ffn_w_down, router_w,
              moe_w_gate, moe_w_up, moe_w_down, final_norm_g):
    c_act = jax.nn.silu(c)
    for l in range(DEPTH):
        mod = (c_act @ ada_w[l] + ada_b[l])[:, None, :]
        sh1, sc1, g1, sh2, sc2, g2 = jnp.split(mod, 6, axis=-1)
        h = rmsnorm(x, norm_mix_g[l]) * (1.0 + sc1) + sh1
        x = x + g1 * hybrid_mixer(h, w_in[l], b_in[l], cmp_pe[l], cmp_w1[l], cmp_w2[l],
                                  conv_w[l], conv_b[l], mlstm_norm_g[l], w_out[l])
        h = rmsnorm(x, norm_ffn_g[l]) * (1.0 + sc2) + sh2
        if l % 2 == 0:
            i = l // 2
            y = swiglu(h, ffn_w_gate[i], ffn_w_up[i], ffn_w_down[i])
        else:
            i = l // 2
            y = moe_swiglu(h, router_w[i], moe_w_gate[i], moe_w_up[i], moe_w_down[i])
        x = x + g2 * y
    return rmsnorm(x, final_norm_g)
```

```python
import contextlib
import numpy as np
import concourse.bass as bass
import concourse.mybir as mybir
from concourse.bass_utils import run_bass_kernel_spmd

F32 = mybir.dt.float32
BF16 = mybir.dt.bfloat16
AF = mybir.ActivationFunctionType
ALU = mybir.AluOpType
AX = mybir.AxisListType

D = 1024
S = 4096
NB = 4
EPS = 1e-6
IN_COLS = 3360
NEG = -30000.0


class Prog:
    ENG = ("pe", "dve", "act", "pool", "sp")

    def __init__(self, nc, stack, n_dma_sems=8):
        self.nc = nc
        self.eng = {"pe": nc.tensor, "dve": nc.vector, "act": nc.scalar, "pool": nc.gpsimd, "sp": nc.sync}
        self.sem = {}
        self.count = {}
        for e in self.ENG:
            self.sem[e] = stack.enter_context(nc.semaphore("s_" + e))
            self.count[e] = 0
        self.dsem, self.dval, self.drr = {}, {}, {}
        for q in ("sp", "pool", "act"):
            self.dsem[q] = [stack.enter_context(nc.semaphore("d_%s%d" % (q, i))) for i in range(n_dma_sems)]
            self.dval[q] = [0] * n_dma_sems
            self.drr[q] = 0
        self.seen = {e: {} for e in self.ENG}
        self.snap = {}
        self.last_w = {}
        self.readers = {}
        self.n_wait = 0
        self.n_ops = 0
        self.csem = {}
        self.ctoks = []

    def _semobj(self, key):
        if isinstance(key, str):
            return self.sem[key]
        if key[0] == "c":
            return self.csem[key]
        return self.dsem[key[1]][key[2]]

    def _wait(self, e, tok):
        key, val = tok
        if self.seen[e].get(key, 0) >= val:
            return
        self.eng[e].wait_ge(self._semobj(key), val)
        self.n_wait += 1
        self.seen[e][key] = val
        sn = self.snap.get(tok)
        if sn:
            se = self.seen[e]
            for k, v in sn.items():
                if se.get(k, 0) < v:
                    se[k] = v

    def _deps(self, reads, writes):
        deps = []
        for r in reads:
            t = self.last_w.get(r)
            if t is not None:
                deps.append(t)
        for w in writes:
            t = self.last_w.get(w)
            if t is not None:
                deps.append(t)
            deps.extend(self.readers.get(w, ()))
        return deps

    def _commit(self, tok, reads, writes):
        for r in reads:
            lst = self.readers.setdefault(r, [])
            lst.append(tok)
            if len(lst) > 64:
                best = {}
                for k, v in lst:
                    if best.get(k, 0) < v:
                        best[k] = v
                lst[:] = list(best.items())
        for w in writes:
            self.last_w[w] = tok
            self.readers[w] = []

    def op(self, e, fn, reads=(), writes=()):
        for t in self._deps(reads, writes):
            self._wait(e, t)
        ins = fn(self.eng[e])
        self.count[e] += 1
        ins.then_inc(self.sem[e], 1)
        tok = (e, self.count[e])
        sn = dict(self.seen[e])
        sn[e] = self.count[e]
        self.snap[tok] = sn
        self._commit(tok, reads, writes)
        self.n_ops += 1
        return tok

    def dma(self, q, out, in_, reads=(), writes=(), **kw):
        for t in self._deps(reads, writes):
            self._wait(q, t)
        i = self.drr[q]
        self.drr[q] = (i + 1) % len(self.dsem[q])
        key = ("d", q, i)
        if self.dval[q][i] > 0:
            self._wait(q, (key, self.dval[q][i]))
        ins = self.eng[q].dma_start(out=out, in_=in_, **kw)
        self.dval[q][i] += 16
        ins.then_inc(self.dsem[q][i], 16)
        tok = (key, self.dval[q][i])
        self.snap[tok] = dict(self.seen[q])
        self._commit(tok, reads, writes)
        return tok

    def finish(self, e="sp"):
        for t in self.ctoks:
            self._wait(e, t)
        for q in self.dsem:
            for i, v in enumerate(self.dval[q]):
                if v:
                    self._wait(e, (("d", q, i), v))
        for o in self.ENG:
            if o != e and self.count[o]:
                self._wait(e, (o, self.count[o]))


class Ctx:
    def __init__(self, name="k"):
        self.nc = bass.Bass("TRN2", target_bir_lowering=False)
        self.stack = contextlib.ExitStack()
        self.root_stack = self.stack
        self.P = Prog(self.nc, self.stack)
        self.pfx = ""
        self.bind = {}
        self.ncoll = 0

    def inp(self, name, shape, dt=F32):
        if name in self.bind:
            ap = self.bind[name]
            assert list(ap.shape) == list(shape), (name, ap.shape, shape)
            return ap
        return self.nc.dram_tensor(self.pfx + name, list(shape), dt, kind="ExternalInput").ap()

    def outp(self, name, shape, dt=F32):
        if name in self.bind:
            ap = self.bind[name]
            assert list(ap.shape) == list(shape), (name, ap.shape, shape)
            return ap
        return self.nc.dram_tensor(self.pfx + name, list(shape), dt, kind="ExternalOutput").ap()

    def scratch(self, name, shape, dt=F32):
        return self.nc.dram_tensor(name, list(shape), dt)

    def sb(self, name, shape, dt=F32):
        return self.stack.enter_context(self.nc.sbuf_tensor(self.pfx + name, list(shape), dt))

    def ps(self, name, shape, dt=F32):
        return self.stack.enter_context(self.nc.psum_tensor(self.pfx + name, list(shape), dt))

    @contextlib.contextmanager
    def phase(self, pfx, bind=None):
        old = (self.stack, self.pfx, self.bind)
        st = contextlib.ExitStack()
        self.stack, self.pfx, self.bind = st, pfx, dict(bind or {})
        try:
            yield
        finally:
            barrier(self.P)
            st.close()
            self.stack, self.pfx, self.bind = old

    def coll(self, kind, op, groups, src, dst, reads, writes):
        P = self.P
        for t in P._deps(reads, writes):
            P._wait("pool", t)
        sem = self.root_stack.enter_context(self.nc.semaphore("cc%d" % self.ncoll))
        key = ("c", self.ncoll)
        self.ncoll += 1
        P.csem[key] = sem
        self.nc.gpsimd.collective_compute(kind, op, replica_groups=groups, ins=[src.ap().opt()], outs=[dst.ap().opt()]).then_inc(sem)
        tok = (key, 1)
        P.snap[tok] = dict(P.seen["pool"])
        P.ctoks.append(tok)
        P._commit(tok, reads, writes)
        return tok

    def close(self):
        self.P.finish("sp")
        self.root_stack.close()
        return self.nc


class ModMgr:
    def __init__(self, C, nlayers):
        self.C = C
        P = C.P
        rs = C.root_stack
        nc = C.nc
        self.cT = nc.dram_tensor("MOD_cT", [128, 8], F32, kind="ExternalInput").ap()
        self.adab = nc.dram_tensor("MOD_adab", [128, 48 * nlayers], F32, kind="ExternalInput").ap()
        self.adaw = [nc.dram_tensor("MOD_adaw%d" % l, [D, 6 * D], F32, kind="ExternalInput").ap() for l in range(nlayers)]
        self.c_sb = rs.enter_context(nc.sbuf_tensor("MOD_c", [128, 8], F32))
        self.b_sb = rs.enter_context(nc.sbuf_tensor("MOD_b", [128, 48 * nlayers], F32))
        self.modall = rs.enter_context(nc.sbuf_tensor("MOD_all", [128, 48 * nlayers], F32))
        self.sealed = rs.enter_context(nc.sbuf_tensor("MOD_sealed", [128, 48 * nlayers], F32))
        P.dma("sp", self.c_sb[:], self.cT, writes=["MODc"])
        P.dma("sp", self.b_sb[:], self.adab, writes=["MODb"])
        P.op("act", lambda e: e.activation(out=self.c_sb[:], in_=self.c_sb[:], func=AF.Silu), reads=["MODc"], writes=["MODc"])
        self.c_bf = rs.enter_context(nc.sbuf_tensor("MOD_cbf", [128, 8], BF16))
        P.op("dve", lambda e: e.tensor_copy(out=self.c_bf[:], in_=self.c_sb[:]), reads=["MODc"], writes=["MODcb"])
        self.units = [(l, ch) for l in range(nlayers) for ch in range(48)]
        self.next_dma = 0
        self.next_mm = 0
        self.wts = None
        self.psum = None
        self.tick = 0

    def attach(self, wts, psum_cols):
        self.wts = wts
        self.psum = psum_cols
        self.nslot = psum_cols.shape[1]
        self.base_dma = self.next_dma
        self.next_dma = self.next_mm
        for _ in range(len(wts) - 1):
            self._dma()

    def detach(self):
        self.wts = None
        self.psum = None

    def _dma(self):
        if self.next_dma >= len(self.units):
            return
        u = self.next_dma
        l, ch = self.units[u]
        wt = self.wts[u % len(self.wts)]
        self.C.P.dma("pool", wt[:], self.adaw[l][:, ch * 128:(ch + 1) * 128].rearrange("(kc ki) m -> ki kc m", ki=128),
                     writes=["MODw%d" % (u % len(self.wts))])
        self.next_dma += 1

    def unit(self):
        if self.next_mm >= len(self.units):
            return False
        P = self.C.P
        u = self.next_mm
        l, ch = self.units[u]
        self._dma()
        nb = len(self.wts)
        wt = self.wts[u % nb]
        col = l * 48 + ch
        ps = self.psum[:, u % self.nslot:u % self.nslot + 1]
        pk = "MODp%d" % (u % self.nslot)

        def mm(e):
            ins = None
            for kc in range(8):
                ins = e.matmul(ps, lhsT=wt[:, kc, :], rhs=self.c_bf[:, kc:kc + 1], start=(kc == 0), stop=(kc == 7), skip_group_check=True)
            return ins
        P.op("pe", mm, reads=["MODw%d" % (u % nb), "MODcb"], writes=[pk])
        P.op("dve", lambda e: e.tensor_tensor(out=self.modall[:, col:col + 1], in0=ps, in1=self.b_sb[:, col:col + 1], op=ALU.add),
             reads=[pk, "MODb"], writes=["MODall"])
        self.next_mm += 1
        if ch == 15 or ch == 47:
            c0 = l * 48 + (0 if ch == 15 else 16)
            c1 = l * 48 + ch + 1
            key = "MODs%d_%d" % (l, 0 if ch == 15 else 1)
            P.op("dve", lambda e: e.tensor_copy(out=self.sealed[:, c0:c1], in_=self.modall[:, c0:c1]), reads=["MODall"], writes=[key])
        return True

    def bg_tick(self, every=6):
        every = DBG.get("bg_every", every)
        if self.wts is None:
            return
        self.tick += 1
        if self.tick % every == 0:
            self.unit()

    def need(self, l, part):
        last = l * 48 + (15 if part == 0 else 47)
        if self.next_mm <= last:
            own = self.wts is None
            if own:
                C = self.C
                wts = [C.sb("MODfw%d_%d_%d" % (l, part, i), [128, 8, 128], BF16) for i in range(4)]
                pm = C.ps("MODfp%d_%d" % (l, part), [128, 8])
                self.attach(wts, pm[:, :])
            while self.next_mm <= last:
                self.unit()
            if own:
                self.detach()
        c0 = l * 48 + (0 if part == 0 else 16)
        c1 = l * 48 + (16 if part == 0 else 48)
        return self.sealed[:, c0:c1], "MODs%d_%d" % (l, part)


def emit_mod(C, cT, adaw, adab, nch, name):
    P = C.P
    c_sb = C.sb(name + "_c", [128, 8])
    b_sb = C.sb(name + "_b", [128, nch])
    mod = C.sb(name + "_mod", [128, nch])
    pm = C.ps(name + "_pm", [128, nch])
    wts = [C.sb(name + "_w%d" % i, [128, 8, 128]) for i in range(2)]
    P.dma("sp", c_sb[:], cT, writes=[name + "c"])
    P.dma("sp", b_sb[:], adab, writes=[name + "b"])
    P.op("act", lambda e: e.activation(out=c_sb[:], in_=c_sb[:], func=AF.Silu), reads=[name + "c"], writes=[name + "c"])
    for j in range(nch):
        wt = wts[j % 2]
        wk = name + "w%d" % (j % 2)
        P.dma("sp", wt[:], adaw[:, j * 128:(j + 1) * 128].rearrange("(kc ki) m -> ki kc m", ki=128), writes=[wk])

        def mm(e, wt=wt, j=j):
            ins = None
            for kc in range(8):
                ins = e.matmul(pm[:, j:j + 1], lhsT=wt[:, kc, :], rhs=c_sb[:, kc:kc + 1], start=(kc == 0), stop=(kc == 7))
            return ins
        P.op("pe", mm, reads=[wk, name + "c"], writes=[name + "pm"])
    P.op("dve", lambda e: e.tensor_tensor(out=mod[:], in0=pm[:], in1=b_sb[:], op=ALU.add),
         reads=[name + "pm", name + "b"], writes=[name + "mod"])
    return mod, name + "mod"


def emit_norm_block(C, xt, xkey, scale, shift, skey, ones_bf, hT, hkey, tmp_bufs, name, ntok=512, h32=None):
    P = C.P
    sq, ms, rstd, tmp = tmp_bufs
    P.op("act", lambda e: e.activation(out=sq[:, :, :ntok], in_=xt, func=AF.Square), reads=[xkey], writes=[name + "sq"])

    def mm(e):
        ins = None
        for kc in range(8):
            ins = e.matmul(ms[:, :ntok], lhsT=ones_bf[:], rhs=sq[:, kc, :ntok], start=(kc == 0), stop=(kc == 7))
        return ins
    P.op("pe", mm, reads=[name + "sq", "ones"], writes=[name + "ms"])
    P.op("act", lambda e: e.activation(out=rstd[:, :ntok], in_=ms[:, :ntok], func=AF.Sqrt, bias=C.eps_ap[:], scale=1.0),
         reads=[name + "ms", "eps"], writes=[name + "rstd"])
    P.op("dve", lambda e: e.reciprocal(out=rstd[:, :ntok], in_=rstd[:, :ntok]), reads=[name + "rstd"], writes=[name + "rstd"])
    for kc in range(8):
        P.op("dve", lambda e, kc=kc: e.scalar_tensor_tensor(out=tmp[:, kc, :ntok], in0=xt[:, kc, :], scalar=scale[:, kc:kc + 1],
                                                            in1=rstd[:, :ntok], op0=ALU.mult, op1=ALU.mult),
             reads=[xkey, skey, name + "rstd"], writes=[name + "tmp%d" % kc])
        if h32 is not None:
            P.op("act", lambda e, kc=kc: e.activation(out=h32[:, kc, :ntok], in_=tmp[:, kc, :ntok], func=AF.Identity,
                                                      bias=shift[:, kc:kc + 1], scale=1.0),
                 reads=[name + "tmp%d" % kc, skey], writes=[name + "h32_%d" % kc])
            P.op("pool", lambda e, kc=kc: e.tensor_copy(out=hT[:, kc, :ntok], in_=h32[:, kc, :ntok]),
                 reads=[name + "h32_%d" % kc], writes=[hkey])
        else:
            P.op("act", lambda e, kc=kc: e.activation(out=hT[:, kc, :ntok], in_=tmp[:, kc, :ntok], func=AF.Identity,
                                                      bias=shift[:, kc:kc + 1], scale=1.0),
                 reads=[name + "tmp%d" % kc, skey], writes=[hkey])


def setup_consts(C):
    P = C.P
    if hasattr(C, "ones_bf"):
        return
    C.ones_bf = C.root_stack.enter_context(C.nc.sbuf_tensor("ones_bf", [128, 128], BF16))
    C.eps_ap = C.root_stack.enter_context(C.nc.sbuf_tensor("eps_ap", [128, 1], F32))
    P.op("dve", lambda e: e.memset(C.ones_bf[:], 1.0 / D), writes=["ones"])
    P.op("dve", lambda e: e.memset(C.eps_ap[:], EPS), writes=["eps"])


NT1 = 2048


def build_A1():
    C = Ctx()
    P = C.P
    xT = C.inp("xT", [D, NT1])
    cT = C.inp("cT", [128, 8])
    adaw = C.inp("adaw", [D, 2048])
    adab = C.inp("adab", [128, 16])
    gam = C.inp("gam", [128, 8])
    w_in = C.inp("w_in", [D, IN_COLS])
    b_in = C.inp("b_in", [128, 27])
    projT = C.outp("projT", [27 * 128, NT1])
    setup_consts(C)
    mod, mkey = emit_mod(C, cT, adaw, adab, 16, "m1")
    gam_sb = C.sb("gam_sb", [128, 8])
    bin_sb = C.sb("bin_sb", [128, 27])
    scale = C.sb("scale", [128, 8])
    P.dma("sp", gam_sb[:], gam, writes=["gam"])
    P.dma("sp", bin_sb[:], b_in, writes=["bin"])
    P.op("dve", lambda e: e.scalar_tensor_tensor(out=scale[:], in0=mod[:, 8:16], scalar=1.0, in1=gam_sb[:], op0=ALU.add, op1=ALU.mult),
         reads=[mkey, "gam"], writes=["scale"])
    wb = C.sb("wb", [128, 8, IN_COLS], BF16)
    for i in range(7):
        P.dma("pool", wb[:, :, i * 480:(i + 1) * 480], w_in[:, i * 480:(i + 1) * 480].rearrange("(kc ki) m -> ki kc m", ki=128),
              writes=["wb%d" % i])
    wkeys = ["wb%d" % i for i in range(7)]
    xts = [C.sb("xt%d" % i, [128, 8, 512]) for i in range(2)]
    hT = C.sb("hT", [128, 8, 512], BF16)
    tmpb = (C.sb("sq", [128, 8, 512], BF16), C.ps("ms", [128, 512]), C.sb("rstd", [128, 512]), C.sb("tmp", [128, 8, 512]))
    pps = [C.ps("pp%d" % i, [128, 512]) for i in range(4)]
    obs = [C.sb("ob%d" % i, [128, 512]) for i in range(4)]
    xT3 = xT.rearrange("(kc ki) t -> ki kc t", ki=128)
    for tb in range(NT1 // 512):
        xt = xts[tb % 2]
        xk = "xt%d" % (tb % 2)
        P.dma("sp", xt[:], xT3[:, :, tb * 512:(tb + 1) * 512], writes=[xk])
        emit_norm_block(C, xt[:], xk, scale, mod, "scale", C.ones_bf, hT, "hT", tmpb, "n1")
        for m in range(27):
            mw = 128 if m < 26 else IN_COLS - 26 * 128
            pp = pps[m % 4]
            ob = obs[m % 4]

            def mm(e, m=m, mw=mw, pp=pp):
                ins = None
                for kc in range(8):
                    ins = e.matmul(pp[:mw, :], lhsT=wb[:, kc, m * 128:m * 128 + mw], rhs=hT[:, kc, :], start=(kc == 0), stop=(kc == 7))
                return ins
            P.op("pe", mm, reads=["hT"] + wkeys, writes=["pp%d" % (m % 4)])
            P.op("act", lambda e, m=m, mw=mw, pp=pp, ob=ob: e.activation(out=ob[:mw, :], in_=pp[:mw, :], func=AF.Identity,
                                                                          bias=bin_sb[:mw, m:m + 1], scale=1.0),
                 reads=["pp%d" % (m % 4), "bin"], writes=["ob%d" % (m % 4)])
            P.dma("pool", projT[m * 128:m * 128 + mw, tb * 512:(tb + 1) * 512], ob[:mw, :], reads=["ob%d" % (m % 4)])
    return C.close()


def barrier(P):
    toks = list(P.ctoks)
    for q in P.dsem:
        for i, v in enumerate(P.dval[q]):
            if v:
                toks.append((("d", q, i), v))
    for o in P.ENG:
        if P.count[o]:
            toks.append((o, P.count[o]))
    for e in P.ENG:
        for t in toks:
            if t[0] != e:
                P._wait(e, t)
    for e in ("pe", "dve", "act", "pool"):
        if P.count[e]:
            P._wait(e, (e, P.count[e]))


def build_A4(moe, final):
    C = Ctx()
    emit_A4(C, moe, final)
    return C.close()


def emit_A4(C, moe, final):
    P = C.P
    NT = 2048
    NBk = NT // 512
    fused = "zT" in C.bind
    xT = C.inp("xT", [D, NT])
    if fused:
        zT = C.bind["zT"]
    else:
        yT = C.inp("yT", [D, NT])
    mm_ = getattr(C, "modmgr", None)
    if mm_ is None:
        cT = C.inp("cT", [128, 8])
        adaw = C.inp("adaw", [D, 4096])
        adab = C.inp("adab", [128, 32])
    gam = C.inp("gam", [128, 8])
    if not fused:
        w_out = C.inp("w_out", [D, D])
    if moe:
        NE, FF = 8, 3584
        rw = C.inp("rw", [D, 8])
        sel = C.inp("sel", [8, 8 * 128])
        ident = C.inp("ident", [128, 128])
    else:
        NE, FF = 1, 2816
    wg = C.inp("wg", [NE, D, FF])
    wu = C.inp("wu", [NE, D, FF])
    wd = C.inp("wd", [NE, FF, D])
    if final:
        fgam = C.inp("fgam", [128, 8])
    xoT = C.outp("xoT", [D, NT])
    setup_consts(C)
    if mm_ is None:
        mod, mkey = emit_mod(C, cT, adaw, adab, 32, "m4")
    else:
        mod, mkey = mm_.need(C.layer, 1)
    gam_sb = C.sb("gam_sb", [128, 8])
    scale = C.sb("scale", [128, 8])
    P.dma("sp", gam_sb[:], gam, writes=["gam"])
    P.op("dve", lambda e: e.scalar_tensor_tensor(out=scale[:], in0=mod[:, 16:24], scalar=1.0, in1=gam_sb[:], op0=ALU.add, op1=ALU.mult),
         reads=[mkey, "gam"], writes=["scale"])
    if final:
        fg_sb = C.sb("fg_sb", [128, 8])
        P.dma("sp", fg_sb[:], fgam, writes=["fgam"])
    xs = C.sb("xs", [128, 8, NT])
    hT = C.sb("hT", [128, 8, NT], BF16)
    xT3 = xT.rearrange("(kc ki) t -> ki kc t", ki=128)
    if not fused:
        yT3 = yT.rearrange("(kc ki) t -> ki kc t", ki=128)
    xoT3 = xoT.rearrange("(kc ki) t -> ki kc t", ki=128)
    for tb in range(NBk):
        P.dma("sp", xs[:, :, tb * 512:(tb + 1) * 512], xT3[:, :, tb * 512:(tb + 1) * 512], reads=["xo_d"], writes=["xs%d" % tb])
    if moe:
        wT = C.sb("wT", [8, NT], BF16)
    st1 = contextlib.ExitStack()
    main_stack = C.stack
    C.stack = st1
    if fused:
        ybs = [C.sb("zb%d" % i, [128, 8, 512]) for i in range(2)]
    else:
        wob = C.sb("wob", [128, 8, D], BF16)
        P.dma("pool", wob[:], w_out.rearrange("(kc ki) m -> ki kc m", ki=128), writes=["wob"])
        ybs = [C.sb("yb%d" % i, [128, 8, 512], BF16) for i in range(2)]
    tmpb = (C.sb("sq", [128, 8, 512], BF16), C.ps("ms", [128, 512]), C.sb("rstd", [128, 512]), C.sb("tmp", [128, 8, 512]))
    pzs = [C.ps("pz%d" % i, [128, 512]) for i in range(2)]
    if moe:
        h32 = C.sb("h32", [128, 8, 512])
        lgT = C.sb("lgT", [8, NT])
        rw_sb = C.sb("rw_sb", [128, 8, 8])
        P.dma("sp", rw_sb[:], rw.rearrange("(kc ki) e -> ki kc e", ki=128), writes=["rw"])
        plg = C.ps("plg", [8, 512])
    for tb in range(NBk):
        yb = ybs[tb % 2]
        yk = "yb%d" % (tb % 2)
        tsl = slice(tb * 512, (tb + 1) * 512)
        if fused:
            for gi in range(4):
                P.dma("sp", yb[:, 2 * gi:2 * gi + 2, :], zT[gi][:, tsl].rearrange("(kc ki) t -> ki kc t", ki=128), reads=["zs%d" % gi], writes=[yk])
        else:
            P.dma("pool", yb[:], yT3[:, :, tsl], writes=[yk])
        for m in range(8):
            if fused:
                P.op("dve", lambda e, m=m, yb=yb: e.scalar_tensor_tensor(out=xs[:, m, tsl], in0=yb[:, m, :], scalar=mod[:, m:m + 1],
                                                                         in1=xs[:, m, tsl], op0=ALU.mult, op1=ALU.add),
                     reads=[yk, mkey, "xs%d" % tb], writes=["xs%d" % tb])
                continue
            pz = pzs[m % 2]

            def mm(e, m=m, pz=pz, yb=yb):
                ins = None
                for kc in range(8):
                    ins = e.matmul(pz[:], lhsT=wob[:, kc, m * 128:(m + 1) * 128], rhs=yb[:, kc, :], start=(kc == 0), stop=(kc == 7))
                return ins
            P.op("pe", mm, reads=["wob", yk], writes=["pz%d" % (m % 2)])
            P.op("dve", lambda e, m=m, pz=pz: e.scalar_tensor_tensor(out=xs[:, m, tsl], in0=pz[:], scalar=mod[:, m:m + 1],
                                                                     in1=xs[:, m, tsl], op0=ALU.mult, op1=ALU.add),
                 reads=["pz%d" % (m % 2), mkey, "xs%d" % tb], writes=["xs%d" % tb])
        emit_norm_block(C, xs[:, :, tsl], "xs%d" % tb, scale, mod[:, 8:16], "scale", C.ones_bf, hT[:, :, tsl], "hT%d" % tb,
                        tmpb, "n4", h32=(h32 if moe else None))
        if moe:
            def mmr(e):
                ins = None
                for kc in range(8):
                    ins = e.matmul(plg[:], lhsT=rw_sb[:, kc, :], rhs=h32[:, kc, :], start=(kc == 0), stop=(kc == 7))
                return ins
            P.op("pe", mmr, reads=["rw"] + ["n4h32_%d" % kc for kc in range(8)], writes=["plg"])
            P.op("act", lambda e: e.copy(out=lgT[:, tsl], in_=plg[:]), reads=["plg"], writes=["lgT%d" % tb])
    if moe:
        id_sb = C.sb("id_sb", [128, 128])
        P.dma("sp", id_sb[:], ident, writes=["ident"])
        lg = C.sb("lg", [128, 16, 8])
        s8 = C.sb("s8", [128, 16, 8])
        e21 = C.sb("e21", [128, 16])
        w1 = C.sb("w1", [128, 16])
        w2 = C.sb("w2", [128, 16])
        m1 = C.sb("m1", [128, 16, 8])
        m2 = C.sb("m2", [128, 16, 8])
        ptr = C.ps("ptr", [128, 16, 8])

        def mmt(e):
            ins = None
            for tt in range(16):
                ins = e.transpose(ptr[:, tt, :], lgT[:, tt * 128:(tt + 1) * 128], id_sb[:8, :8])
            return ins
        P.op("pe", mmt, reads=["ident"] + ["lgT%d" % tb for tb in range(NBk)], writes=["ptr"])
        P.op("dve", lambda e: e.tensor_copy(out=lg[:], in_=ptr[:]), reads=["ptr"], writes=["lg"])
        for tt in range(16):
            P.op("dve", lambda e, tt=tt: e.max(out=s8[:, tt, :], in_=lg[:, tt, :]), reads=["lg"], writes=["s8_%d" % tt])
        s8k = ["s8_%d" % tt for tt in range(16)]
        P.op("dve", lambda e: e.tensor_tensor(out=e21[:], in0=s8[:, :, 1], in1=s8[:, :, 0], op=ALU.subtract), reads=s8k, writes=["e21"])
        P.op("act", lambda e: e.activation(out=e21[:], in_=e21[:], func=AF.Exp), reads=["e21"], writes=["e21"])
        P.op("dve", lambda e: e.tensor_scalar(out=w1[:], in0=e21[:], scalar1=1.0, scalar2=None, op0=ALU.add), reads=["e21"], writes=["w1"])
        P.op("dve", lambda e: e.reciprocal(out=w1[:], in_=w1[:]), reads=["w1"], writes=["w1"])
        P.op("dve", lambda e: e.tensor_tensor(out=w2[:], in0=e21[:], in1=w1[:], op=ALU.mult), reads=["e21", "w1"], writes=["w2"])
        for tt in range(16):
            P.op("dve", lambda e, tt=tt: e.tensor_scalar(out=m1[:, tt, :], in0=lg[:, tt, :], scalar1=s8[:, tt, 0:1], scalar2=w1[:, tt:tt + 1],
                                                         op0=ALU.is_equal, op1=ALU.mult), reads=["lg", "w1"] + s8k, writes=["m1_%d" % tt])
            P.op("dve", lambda e, tt=tt: e.tensor_scalar(out=m2[:, tt, :], in0=lg[:, tt, :], scalar1=s8[:, tt, 1:2], scalar2=w2[:, tt:tt + 1],
                                                         op0=ALU.is_equal, op1=ALU.mult), reads=["lg", "w2"] + s8k, writes=["m2_%d" % tt])
        P.op("dve", lambda e: e.tensor_tensor(out=m1[:], in0=m1[:], in1=m2[:], op=ALU.add),
             reads=["m1_%d" % tt for tt in range(16)] + ["m2_%d" % tt for tt in range(16)], writes=["wtok"])

        for tb in range(NBk):
            pz = pzs[tb % 2]

            def mmtb(e, tb=tb, pz=pz):
                ins = None
                for t4 in range(4):
                    tt = tb * 4 + t4
                    ins = e.transpose(pz[:8, t4 * 128:(t4 + 1) * 128], m1[:, tt, :], id_sb[:])
                return ins
            P.op("pe", mmtb, reads=["wtok", "ident"], writes=["pz%d" % (tb % 2)])
            P.op("dve", lambda e, tb=tb, pz=pz: e.tensor_copy(out=wT[:, tb * 512:(tb + 1) * 512], in_=pz[:8, :]),
                 reads=["pz%d" % (tb % 2)], writes=["wT"])
    barrier(P)
    st1.close()
    C.stack = main_stack
    st2 = contextlib.ExitStack()
    C.stack = st2
    nch = FF // 128
    groups = []
    f0 = 0
    while f0 < nch:
        nf = min(4, nch - f0)
        groups.append((f0, nf))
        f0 += nf
    gbs = [C.sb("gb%d" % i, [128, 8, 512], BF16) for i in range(2)]
    ubs = [C.sb("ub%d" % i, [128, 8, 512], BF16) for i in range(2)]
    dbs = [C.sb("db%d" % i, [128, 4, D], BF16) for i in range(2)]
    abs_ = [C.sb("ab%d" % i, [128, 4, NT], BF16) for i in range(2)]
    sgs = [C.sb("sg%d" % i, [128, 512]) for i in range(2)]
    pgs = [C.ps("pg%d" % i, [128, 512]) for i in range(2)]
    pus = [C.ps("pu%d" % i, [128, 512]) for i in range(2)]
    pds = [C.ps("pd%d" % i, [128, 512]) for i in range(2)]
    if moe:
        sel_sb = C.sb("sel_sb", [8, 8, 128], BF16)
        P.dma("pool", sel_sb[:], sel.rearrange("k (e m) -> k e m", e=8), writes=["sel"])
        wBs = [C.sb("wB%d" % i, [128, NT], BF16) for i in range(2)]
    hkeys = ["hT%d" % tb for tb in range(NBk)]
    work = [(ex, gi) for ex in range(NE) for gi in range(len(groups))]

    def emit_gu(idx):
        ex, gi = work[idx]
        f0, nf = groups[gi]
        bi = idx % 2
        gb, ub, ab = gbs[bi], ubs[bi], abs_[bi]
        cols = slice(f0 * 128, (f0 + nf) * 128)
        P.dma("pool", gb[:, :, :nf * 128], wg[ex, :, cols].rearrange("(kc ki) m -> ki kc m", ki=128), writes=["gb%d" % bi])
        P.dma("pool", ub[:, :, :nf * 128], wu[ex, :, cols].rearrange("(kc ki) m -> ki kc m", ki=128), writes=["ub%d" % bi])
        if moe and gi == 0:
            wB = wBs[ex % 2]
            for tb in range(NBk):
                pd = pds[tb % 2]
                P.op("pe", lambda e, tb=tb, pd=pd: e.matmul(pd[:], lhsT=sel_sb[:, ex, :], rhs=wT[:, tb * 512:(tb + 1) * 512], start=True, stop=True),
                     reads=["sel", "wT"], writes=["pd%d" % (tb % 2)])
                P.op("act", lambda e, tb=tb, pd=pd, wB=wB: e.copy(out=wB[:, tb * 512:(tb + 1) * 512], in_=pd[:]),
                     reads=["pd%d" % (tb % 2)], writes=["wB%d_%d" % (ex % 2, tb)])
        k = 0
        for fc in range(nf):
            for tb in range(NBk):
                pg, pu, sg = pgs[k % 2], pus[k % 2], sgs[k % 2]
                kk = k % 2
                tsl = slice(tb * 512, (tb + 1) * 512)

                def mm(e, fc=fc, tsl=tsl, pg=pg, pu=pu):
                    ins = None
                    for kc in range(8):
                        ins = e.matmul(pg[:], lhsT=gb[:, kc, fc * 128:(fc + 1) * 128], rhs=hT[:, kc, tsl], start=(kc == 0), stop=(kc == 7))
                    for kc in range(8):
                        ins = e.matmul(pu[:], lhsT=ub[:, kc, fc * 128:(fc + 1) * 128], rhs=hT[:, kc, tsl], start=(kc == 0), stop=(kc == 7))
                    return ins
                P.op("pe", mm, reads=["gb%d" % bi, "ub%d" % bi, "hT%d" % tb], writes=["pg%d" % kk, "pu%d" % kk])
                P.op("act", lambda e, pg=pg, sg=sg: e.activation(out=sg[:], in_=pg[:], func=AF.Silu), reads=["pg%d" % kk], writes=["sg%d" % kk])
                akey = "ab%d_%d_%d" % (bi, fc, tb)
                if moe:
                    P.op("dve", lambda e, sg=sg, pu=pu: e.tensor_tensor(out=sg[:], in0=sg[:], in1=pu[:], op=ALU.mult),
                         reads=["sg%d" % kk, "pu%d" % kk], writes=["sg%d" % kk])
                    P.op("dve", lambda e, sg=sg, fc=fc, tsl=tsl: e.tensor_tensor(out=ab[:, fc, tsl], in0=sg[:], in1=wBs[ex % 2][:, tsl], op=ALU.mult),
                         reads=["sg%d" % kk, "wB%d_%d" % (ex % 2, tb)], writes=[akey])
                else:
                    P.op("dve", lambda e, sg=sg, pu=pu, fc=fc, tsl=tsl: e.tensor_tensor(out=ab[:, fc, tsl], in0=sg[:], in1=pu[:], op=ALU.mult),
                         reads=["sg%d" % kk, "pu%d" % kk], writes=[akey])
                k += 1

    def emit_dn(idx):
        ex, gi = work[idx]
        f0, nf = groups[gi]
        bi = idx % 2
        db, ab = dbs[bi], abs_[bi]
        P.dma("pool", db[:, :nf, :], wd[ex, f0 * 128:(f0 + nf) * 128, :].rearrange("(fc fi) m -> fi fc m", fi=128), writes=["db%d" % bi])
        k = 0
        for m in range(8):
            for tb in range(NBk):
                pd = pds[k % 2]
                tsl = slice(tb * 512, (tb + 1) * 512)

                def mm(e, m=m, tsl=tsl, pd=pd):
                    ins = None
                    for fc in range(nf):
                        ins = e.matmul(pd[:], lhsT=db[:, fc, m * 128:(m + 1) * 128], rhs=ab[:, fc, tsl], start=(fc == 0), stop=(fc == nf - 1))
                    return ins
                P.op("pe", mm, reads=["db%d" % bi] + ["ab%d_%d_%d" % (bi, fc, tb) for fc in range(nf)], writes=["pd%d" % (k % 2)])
                P.op("dve", lambda e, m=m, tsl=tsl, pd=pd: e.scalar_tensor_tensor(out=xs[:, m, tsl], in0=pd[:], scalar=mod[:, 24 + m:25 + m],
                                                                                 in1=xs[:, m, tsl], op0=ALU.mult, op1=ALU.add),
                     reads=["pd%d" % (k % 2), mkey, "xs%d" % tb], writes=["xs%d" % tb])
                k += 1

    emit_gu(0)
    for i in range(len(work)):
        if i + 1 < len(work):
            emit_gu(i + 1)
        emit_dn(i)
    barrier(P)
    st2.close()
    C.stack = main_stack
    if final:
        tmpb = (C.sb("fsq", [128, 8, 512], BF16), C.ps("fms", [128, 512]), C.sb("frstd", [128, 512]), C.sb("ftmp", [128, 8, 512]))
        sq, ms, rstd, tmp = tmpb
        for tb in range(NBk):
            tsl = slice(tb * 512, (tb + 1) * 512)
            xk = "xs%d" % tb
            P.op("act", lambda e, tsl=tsl: e.activation(out=sq[:], in_=xs[:, :, tsl], func=AF.Square), reads=[xk], writes=["fsq"])

            def mm(e):
                ins = None
                for kc in range(8):
                    ins = e.matmul(ms[:], lhsT=C.ones_bf[:], rhs=sq[:, kc, :], start=(kc == 0), stop=(kc == 7))
                return ins
            P.op("pe", mm, reads=["fsq", "ones"], writes=["fms"])
            P.op("act", lambda e: e.activation(out=rstd[:], in_=ms[:], func=AF.Sqrt, bias=C.eps_ap[:], scale=1.0), reads=["fms", "eps"], writes=["frstd"])
            P.op("dve", lambda e: e.reciprocal(out=rstd[:], in_=rstd[:]), reads=["frstd"], writes=["frstd"])
            for kc in range(8):
                P.op("dve", lambda e, kc=kc, tsl=tsl: e.scalar_tensor_tensor(out=tmp[:, kc, :], in0=xs[:, kc, tsl], scalar=fg_sb[:, kc:kc + 1],
                                                                            in1=rstd[:], op0=ALU.mult, op1=ALU.mult),
                     reads=[xk, "fgam", "frstd"], writes=["ftmp"])
            P.dma("sp", xoT3[:, :, tsl], tmp[:], reads=["ftmp"], writes=["xo_d"])
    else:
        for tb in range(NBk):
            tsl = slice(tb * 512, (tb + 1) * 512)
            P.dma("sp", xoT3[:, :, tsl], xs[:, :, tsl], reads=["xs%d" % tb], writes=["xo_d"])


def nsa_consts(g):
    t = np.arange(S)
    ti, tl = t // 128, t % 128
    qaug = np.zeros((4, 4, S), np.float32)
    for hl in range(4):
        slope = 2.0 ** (-8.0 * (4 * g + hl + 1) / 8)
        qaug[hl, 0] = -8 * slope * 128 * ti
        qaug[hl, 1] = -8 * slope * tl
        qaug[hl, 2] = 8 * slope
        qaug[hl, 3] = 8 * slope
    kaug = np.stack([np.ones(S), np.ones(S), 128.0 * ti, 1.0 * tl]).astype(np.float32)
    n = np.arange(256)
    ce = 16 * n + 31
    kaugc = np.stack([np.ones(256), np.ones(256), 128.0 * (ce // 128), 1.0 * (ce % 128)]).astype(np.float32)
    kaugc[:, 255] = 0
    pl = np.arange(128)[:, None]
    ql = np.arange(512)[None, :]
    wmask = np.zeros((128, 8, 512), np.float32)
    for j in range(-4, 4):
        dist = ql - 128 * j - pl
        wmask[:, j + 4, :] = np.where((dist >= 0) & (dist < 512), 0.0, NEG)
    cmask = np.zeros((128, 4, 512), np.float32)
    for j in range(4):
        cmask[:, j, :] = np.where(ql - 128 * j - pl >= 0, 0.0, NEG)
    cmpmask = np.zeros((128, 2, 8, 512), np.float32)
    for c in range(2):
        nn = c * 128 + np.arange(128)[:, None]
        for Q in range(8):
            vis = (16 * nn + 31 <= 512 * Q + ql) & (nn < 255)
            cmpmask[:, c, Q, :] = np.where(vis, 0.0, NEG)
    E = np.zeros((64, 32, 128), np.float32)
    for c in range(32):
        E[2 * c, c, :64] = 1
        E[2 * c + 1, c, 64:] = 1
    lo_c = np.arange(256)[:, None] * 16
    lo_s = np.arange(64)[None, :] * 64
    ovm = np.clip(np.minimum(lo_c + 32, lo_s + 64) - np.maximum(lo_c, lo_s), 0, None) / 32.0
    ovm[255] = 0
    ov = np.ascontiguousarray(ovm.reshape(2, 128, 64).transpose(1, 0, 2)).astype(np.float32)
    cur = t // 64
    j = np.arange(64)[None, :]
    valid = j <= cur[:, None]
    forced = (j == 0) | (j == cur[:, None]) | (j == cur[:, None] - 1)
    valid01 = valid.astype(np.float32)
    addtab = np.where(valid, np.where(forced, 1e4, 0.0), -1.0).astype(np.float32)
    v01 = np.ascontiguousarray(valid01.reshape(32, 128, 64).transpose(1, 0, 2))
    adt = np.ascontiguousarray(addtab.reshape(32, 128, 64).transpose(1, 0, 2))
    return dict(qaug=qaug, kaug=kaug, kaugc=kaugc, wmask=wmask.reshape(128, -1), cmask=cmask.reshape(128, -1),
                cmpmask=cmpmask.reshape(128, -1), Emat=E.reshape(64, -1), ov=ov.reshape(128, -1), v01=v01.reshape(128, -1),
                adt=adt.reshape(128, -1), identb=np.eye(128, dtype=np.float32))


def build_A2():
    C = Ctx()
    emit_A2(C)
    return C.close()


def emit_A2(C):
    P = C.P
    qT = C.inp("qT", [4, 64, S])
    kT = C.inp("kT", [3, 64, S])
    vcT = C.inp("vcT", [64, S])
    vtok = C.inp("vtok", [2, S, 64])
    gl = C.inp("gl", [S, 12])
    w1 = C.inp("w1", [2, 64, 32 * 128])
    w2 = C.inp("w2", [2, 128, 64])
    peT = C.inp("peT", [2, 64, 32])
    qaug = C.inp("qaug", [4, 4, S])
    kaug = C.inp("kaug", [4, S])
    kaugc = C.inp("kaugc", [4, 256])
    wmask_d = C.inp("wmask", [128, 8 * 512])
    cmask_d = C.inp("cmask", [128, 4 * 512])
    cmpmask_d = C.inp("cmpmask", [128, 16 * 512])
    E_d = C.inp("Emat", [64, 32 * 128])
    ov_d = C.inp("ov", [128, 128])
    v01_d = C.inp("v01", [128, 32 * 64])
    adt_d = C.inp("adt", [128, 32 * 64])
    id_d = C.inp("identb", [128, 128])
    fused = "yT_nsa" in C.bind
    if fused:
        yT_nsa = C.bind["yT_nsa"]
    else:
        o_out = C.outp("o", [S, 256])

    qa = [C.sb("qa%d" % h, [68, S], BF16) for h in range(4)]
    ks = C.sb("ks", [68, S], BF16)
    kw = C.sb("kw", [68, S], BF16)
    kc = C.sb("kc", [68, 256], BF16)
    Vs = C.sb("Vs", [128, 32, 65], BF16)
    Vw = C.sb("Vw", [128, 32, 65], BF16)
    Vc = C.sb("Vc", [128, 2, 65], BF16)
    wmask = C.sb("wmask_s", [128, 8, 512], BF16)
    cmask = C.sb("cmask_s", [128, 4, 512], BF16)
    cmpmask = C.sb("cmpmask_s", [128, 16, 512], BF16)
    Em = C.sb("Em", [64, 32, 128], BF16)
    ov = C.sb("ov_s", [128, 2, 64], BF16)
    v01 = C.sb("v01_s", [128, 32, 64])
    adt = C.sb("adt_s", [128, 32, 64])
    idb = C.sb("idb", [128, 128], BF16)
    gates = C.sb("gates", [128, 32, 12])
    o_sb = C.sb("o_sb", [128, 32, 256])
    selbT = C.sb("selbT", [64, S], BF16)
    for h in range(4):
        P.dma("pool", qa[h][0:64, :], qT[h], writes=["qa%d" % h])
        P.dma("pool", qa[h][64:68, :], qaug[h], writes=["qa%d" % h])
    P.dma("pool", ks[0:64, :], kT[1], writes=["ks"])
    P.dma("pool", ks[64:68, :], kaug, writes=["ks"])
    P.dma("pool", kw[0:64, :], kT[2], writes=["kw"])
    P.dma("pool", kw[64:68, :], kaug, writes=["kw"])
    P.op("dve", lambda e: e.memset(kc[:], 0.0), writes=["kc"])
    P.dma("pool", kc[64:68, :], kaugc, writes=["kc"])
    P.op("dve", lambda e: e.memset(Vs[:], 1.0), writes=["Vs"])
    P.op("dve", lambda e: e.memset(Vw[:], 1.0), writes=["Vw"])
    P.op("dve", lambda e: e.memset(Vc[:], 1.0), writes=["Vc"])
    P.dma("pool", Vs[:, :, 0:64], vtok[0].rearrange("(t p) d -> p t d", p=128), writes=["Vs"])
    P.dma("pool", Vw[:, :, 0:64], vtok[1].rearrange("(t p) d -> p t d", p=128), writes=["Vw"])
    P.dma("pool", wmask[:], wmask_d.rearrange("p (a b) -> p a b", a=8), writes=["wmask"])
    P.dma("pool", cmask[:], cmask_d.rearrange("p (a b) -> p a b", a=4), writes=["cmask"])
    P.dma("pool", cmpmask[:], cmpmask_d.rearrange("p (a b) -> p a b", a=16), writes=["cmpmask"])
    P.dma("pool", Em[:], E_d.rearrange("p (a b) -> p a b", a=32), writes=["Em"])
    P.dma("pool", ov[:], ov_d.rearrange("p (a b) -> p a b", a=2), writes=["ov"])
    P.dma("pool", idb[:], id_d, writes=["idb"])
    P.dma("sp", v01[:], v01_d.rearrange("p (a b) -> p a b", a=32), writes=["v01"])
    P.dma("sp", adt[:], adt_d.rearrange("p (a b) -> p a b", a=32), writes=["adt"])
    P.dma("sp", gates[:], gl.rearrange("(t p) c -> p t c", p=128), writes=["gates"])
    P.op("act", lambda e: e.activation(out=gates[:], in_=gates[:], func=AF.Sigmoid), reads=["gates"], writes=["gates"])
    P.op("pool", lambda e: e.memset(o_sb[:], 0.0), writes=["o_sb"])

    st1 = contextlib.ExitStack()
    main_stack = C.stack
    C.stack = st1
    for kv in range(2):
        src = C.sb("csrc%d" % kv, [64, S], BF16)
        w1b = C.sb("w1b%d" % kv, [64, 32, 128], BF16)
        w2b = C.sb("w2b%d" % kv, [128, 64], BF16)
        peb = C.sb("peb%d" % kv, [64, 32], BF16)
        hid = C.sb("hid%d" % kv, [128, 256], BF16)
        cv = C.sb("cv%d" % kv, [128, 1])
        ph = C.ps("ph%d" % kv, [128, 256])
        pc = C.ps("pc%d" % kv, [128, 1])
        po = C.ps("po%d" % kv, [128, 256])
        sk = "csrc%d" % kv
        P.dma("pool", src[:], kT[0] if kv == 0 else vcT, writes=[sk])
        P.dma("pool", w1b[:], w1[kv].rearrange("d (i j) -> d i j", i=32), writes=["w1b%d" % kv])
        P.dma("pool", w2b[:], w2[kv], writes=["w2b%d" % kv])
        P.dma("pool", peb[:], peT[kv], writes=["peb%d" % kv])

        def mmh(e, src=src, w1b=w1b, ph=ph):
            ins = None
            for i in range(32):
                ins = e.matmul(ph[:, 0:255], lhsT=w1b[:, i, :], rhs=src[:, i:i + 16 * 254 + 1:16], start=(i == 0), stop=(i == 31))
            return ins
        P.op("pe", mmh, reads=[sk, "w1b%d" % kv], writes=["ph%d" % kv])

        def mmc(e, w1b=w1b, peb=peb, pc=pc):
            ins = None
            for i in range(32):
                ins = e.matmul(pc[:], lhsT=w1b[:, i, :], rhs=peb[:, i:i + 1], start=(i == 0), stop=(i == 31))
            return ins
        P.op("pe", mmc, reads=["peb%d" % kv, "w1b%d" % kv], writes=["pc%d" % kv])
        P.op("dve", lambda e, cv=cv, pc=pc: e.tensor_copy(out=cv[:], in_=pc[:]), reads=["pc%d" % kv], writes=["cv%d" % kv])
        P.op("dve", lambda e, hid=hid: e.memset(hid[:], 0.0), writes=["hid%d" % kv])
        P.op("act", lambda e, hid=hid, ph=ph, cv=cv: e.activation(out=hid[:, 0:255], in_=ph[:, 0:255], func=AF.Silu, bias=cv[:], scale=1.0),
             reads=["ph%d" % kv, "cv%d" % kv, "hid%d" % kv], writes=["hid%d" % kv])
        if kv == 0:
            P.op("pe", lambda e, w2b=w2b, hid=hid, po=po: e.matmul(po[0:64, 0:255], lhsT=w2b[:], rhs=hid[:, 0:255], start=True, stop=True),
                 reads=["w2b0", "hid0"], writes=["po0"])
            P.op("dve", lambda e, po=po: e.tensor_copy(out=kc[0:64, 0:255], in_=po[0:64, 0:255]), reads=["po0", "kc"], writes=["kc"])
        else:
            def mmv(e, w2b=w2b, hid=hid, po=po):
                ins = None
                for c in range(2):
                    ins = e.matmul(po[:, c * 64:(c + 1) * 64], lhsT=hid[:, c * 128:(c + 1) * 128], rhs=w2b[:], start=True, stop=True)
                return ins
            P.op("pe", mmv, reads=["w2b1", "hid1"], writes=["po1"])
            P.op("dve", lambda e, po=po: e.tensor_copy(out=Vc[:, :, 0:64], in_=po[:, 0:128].rearrange("p (c d) -> p c d", c=2)),
                 reads=["po1", "Vc"], writes=["Vc"])
    barrier(P)
    st1.close()
    C.stack = main_stack

    Sb = [C.ps("S%d" % i, [128, 512]) for i in range(3)]
    PT = [C.sb("PT%d" % i, [128, 512], BF16) for i in range(3)]
    oacc_f = [C.ps("oacc%d" % i, [128, 512]) for i in range(2)]
    impacc_f = [C.ps("impacc%d" % i, [128, 512]) for i in range(2)]
    oacc = [t[:, 0:260].rearrange("p (a b) -> p a b", a=4) for t in oacc_f]
    impacc = [t[:, 0:256].rearrange("p (a b) -> p a b", a=4) for t in impacc_f]
    ptr = C.ps("ptrs", [128, 1024], BF16)
    imp_sb = C.sb("imp_sb", [128, 4, 64])
    imp2 = C.sb("imp2", [128, 4, 64])
    imp3 = C.sb("imp3", [128, 4, 64])
    s8a = C.sb("s8a", [128, 4, 8])
    s8b = C.sb("s8b", [128, 4, 8])
    selb = C.sb("selb", [128, 4, 64], BF16)
    dmx = C.sb("dmx", [128, 4])
    coef = C.sb("coef", [128, 4])
    state = {"k": 0, "pass": 0}
    mm_ = getattr(C, "modmgr", None)
    if mm_ is not None and mm_.next_mm < len(mm_.units) and not DBG.get("no_bg"):
        bgw = [C.sb("bgw%d" % i, [128, 8, 128], BF16) for i in range(6)]
        if DBG.get("bg_own_bank"):
            bgp = Sb.pop()
            mm_.attach(bgw, bgp[:, 0:8])
        else:
            mm_.attach(bgw, impacc_f[0][:, 256:264])
    else:
        mm_ = None

    def attn_pass(chunks, hl, Q, br, with_imp):
        pi = state["pass"] % 2
        state["pass"] += 1
        oa = oacc[pi]
        ia = impacc[pi]
        Qsl = slice(Q * 512, (Q + 1) * 512)
        n = len(chunks)
        bufidx = []

        def emit_pv(idx):
            bi = bufidx[idx]
            _, vr, vkey, ovr = chunks[idx]

            def pv(e):
                ins = None
                for qt in range(4):
                    ins = e.matmul(oa[:, qt, :], lhsT=PT[bi][:, qt * 128:(qt + 1) * 128], rhs=vr, start=(idx == 0 and qt == 0),
                                   stop=(idx == n - 1), skip_group_check=True)
                if with_imp:
                    for qt in range(4):
                        ins = e.matmul(ia[:, qt, :], lhsT=PT[bi][:, qt * 128:(qt + 1) * 128], rhs=ovr, start=(idx == 0 and qt == 0),
                                       stop=(idx == n - 1), skip_group_check=True)
                return ins
            wr = ["oacc%d" % pi] + (["impacc%d" % pi] if with_imp else [])
            P.op("pe", pv, reads=["PT%d" % bi, vkey, "ov"], writes=wr)

        for idx in range(n):
            bi = state["k"] % len(Sb)
            state["k"] += 1
            bufidx.append(bi)
            mms = chunks[idx][0]

            def smm(e, mms=mms, bi=bi):
                ins = None
                for mi, (lt, rh, _) in enumerate(mms):
                    ins = e.matmul(Sb[bi][:], lhsT=lt, rhs=rh, start=(mi == 0), stop=(mi == len(mms) - 1))
                return ins
            rd = []
            for (_, _, r) in mms:
                rd.extend(r)
            P.op("pe", smm, reads=rd, writes=["S%d" % bi])
            P.op("act", lambda e, bi=bi: e.activation(out=PT[bi][:], in_=Sb[bi][:], func=AF.Exp, scale=0.125),
                 reads=["S%d" % bi], writes=["PT%d" % bi])
            if idx >= 1:
                emit_pv(idx - 1)
            if mm_ is not None and not with_imp:
                mm_.bg_tick()
        emit_pv(n - 1)
        P.op("dve", lambda e: e.tensor_scalar(out=dmx[:], in0=oa[:, :, 64], scalar1=1e-30, scalar2=None, op0=ALU.max),
             reads=["oacc%d" % pi], writes=["dmx"])
        P.op("dve", lambda e: e.reciprocal(out=dmx[:], in_=dmx[:]), reads=["dmx"], writes=["dmx"])
        P.op("dve", lambda e: e.tensor_tensor(out=coef[:], in0=dmx[:], in1=gates[:, Q * 4:Q * 4 + 4, hl * 3 + br], op=ALU.mult),
             reads=["dmx", "gates"], writes=["coef"])
        for qt in range(4):
            osl = o_sb[:, Q * 4 + qt, hl * 64:(hl + 1) * 64]
            P.op("dve", lambda e, qt=qt, osl=osl: e.scalar_tensor_tensor(out=osl, in0=oa[:, qt, 0:64], scalar=coef[:, qt:qt + 1], in1=osl,
                                                                         op0=ALU.mult, op1=ALU.add),
                 reads=["oacc%d" % pi, "coef", "o_sb"], writes=["o_sb"])
            if with_imp:
                if hl == 0:
                    P.op("dve", lambda e, qt=qt: e.tensor_scalar(out=imp_sb[:, qt, :], in0=ia[:, qt, :], scalar1=dmx[:, qt:qt + 1], scalar2=None,
                                                                 op0=ALU.mult), reads=["impacc%d" % pi, "dmx"], writes=["imp_sb"])
                else:
                    P.op("dve", lambda e, qt=qt: e.scalar_tensor_tensor(out=imp_sb[:, qt, :], in0=ia[:, qt, :], scalar=dmx[:, qt:qt + 1],
                                                                        in1=imp_sb[:, qt, :], op0=ALU.mult, op1=ALU.add),
                         reads=["impacc%d" % pi, "dmx", "imp_sb"], writes=["imp_sb"])

    for Q in range(8):
        Qsl = slice(Q * 512, (Q + 1) * 512)
        ncc = 2 if Q >= 4 else 1
        for hl in range(4):
            chunks = []
            for c in range(ncc):
                mms = [(kc[:, c * 128:(c + 1) * 128], qa[hl][:, Qsl], ["kc", "qa%d" % hl]),
                       (idb[:], cmpmask[:, c * 8 + Q, :], ["idb", "cmpmask"])]
                chunks.append((mms, Vc[:, c, :], "Vc", ov[:, c, :]))
            attn_pass(chunks, hl, Q, 0, True)
        for hl in range(4):
            chunks = []
            for j in range(-4, 4):
                c = 4 * Q + j
                if c < 0:
                    continue
                mms = [(kw[:, c * 128:(c + 1) * 128], qa[hl][:, Qsl], ["kw", "qa%d" % hl]),
                       (idb[:], wmask[:, j + 4, :], ["idb", "wmask"])]
                chunks.append((mms, Vw[:, c, :], "Vw", None))
            attn_pass(chunks, hl, Q, 2, False)
        P.op("dve", lambda e: e.tensor_tensor(out=imp2[:], in0=imp_sb[:], in1=v01[:, Q * 4:Q * 4 + 4, :], op=ALU.mult),
             reads=["imp_sb", "v01"], writes=["imp2"])
        P.op("dve", lambda e: e.tensor_tensor(out=imp2[:], in0=imp2[:], in1=adt[:, Q * 4:Q * 4 + 4, :], op=ALU.add),
             reads=["imp2", "adt"], writes=["imp2"])
        for qt in range(4):
            P.op("dve", lambda e, qt=qt: e.max(out=s8a[:, qt, :], in_=imp2[:, qt, :]), reads=["imp2"], writes=["s8a%d" % qt])
            P.op("dve", lambda e, qt=qt: e.match_replace(out=imp3[:, qt, :], in_to_replace=s8a[:, qt, :], in_values=imp2[:, qt, :], imm_value=-3.0e38),
                 reads=["imp2", "s8a%d" % qt], writes=["imp3_%d" % qt])
            P.op("dve", lambda e, qt=qt: e.max(out=s8b[:, qt, :], in_=imp3[:, qt, :]), reads=["imp3_%d" % qt], writes=["s8b%d" % qt])
            P.op("dve", lambda e, qt=qt: e.tensor_scalar(out=imp3[:, qt, :], in0=imp2[:, qt, :], scalar1=s8b[:, qt, 7:8], scalar2=-NEG,
                                                         op0=ALU.is_ge, op1=ALU.mult),
                 reads=["imp2", "s8b%d" % qt, "imp3_%d" % qt], writes=["imp3_%d" % qt])
            P.op("dve", lambda e, qt=qt: e.tensor_scalar(out=selb[:, qt, :], in0=imp3[:, qt, :], scalar1=NEG, scalar2=None, op0=ALU.add),
                 reads=["imp3_%d" % qt], writes=["selb%d" % qt])

        def trs(e):
            ins = None
            for qt in range(4):
                ins = e.transpose(ptr[0:64, qt * 128:(qt + 1) * 128], selb[:, qt, :], idb[:])
            return ins
        P.op("pe", trs, reads=["selb%d" % qt for qt in range(4)] + ["idb"], writes=["ptrs"])
        P.op("dve", lambda e: e.tensor_copy(out=selbT[:, Qsl], in_=ptr[0:64, 0:512]), reads=["ptrs"], writes=["selbT%d" % Q])
        for hl in range(4):
            chunks = []
            for c in range(4 * Q + 4):
                mms = [(ks[:, c * 128:(c + 1) * 128], qa[hl][:, Qsl], ["ks", "qa%d" % hl]),
                       (Em[:, c, :], selbT[:, Qsl], ["Em", "selbT%d" % Q])]
                if c >= 4 * Q:
                    mms.append((idb[:], cmask[:, c - 4 * Q, :], ["idb", "cmask"]))
                chunks.append((mms, Vs[:, c, :], "Vs", None))
            attn_pass(chunks, hl, Q, 1, False)
    if mm_ is not None:
        while mm_.unit():
            pass
        mm_.detach()
    if not fused:
        P.dma("sp", o_out.rearrange("(t p) c -> p t c", p=128), o_sb[:], reads=["o_sb"])
        return
    o_bf = C.sb("o_bf", [128, 32, 256], BF16)
    P.op("act", lambda e: e.copy(out=o_bf[:], in_=o_sb[:]), reads=["o_sb"], writes=["o_bf"])
    yst = [C.sb("yst%d" % i, [128, 2, 512], BF16) for i in range(2)]
    for t4 in range(8):
        ys = yst[t4 % 2]
        for cc in range(2):
            def trs(e, t4=t4, cc=cc):
                ins = None
                for j in range(4):
                    ins = e.transpose(ptr[:, j * 128:(j + 1) * 128], o_bf[:, t4 * 4 + j, cc * 128:(cc + 1) * 128], idb[:])
                return ins
            P.op("pe", trs, reads=["o_bf", "idb"], writes=["ptrs"])
            P.op("dve", lambda e, ys=ys, cc=cc: e.tensor_copy(out=ys[:, cc, :], in_=ptr[:, 0:512]), reads=["ptrs"], writes=["yst%d" % (t4 % 2)])
        P.dma("sp", yT_nsa[:, t4 * 512:(t4 + 1) * 512].rearrange("(cc p) t -> p cc t", p=128), ys[:], reads=["yst%d" % (t4 % 2)], writes=["yT_d"])


def build_A3():
    C = Ctx()
    emit_A3(C)
    return C.close()


def emit_A3(C):
    P = C.P
    NCH = 64
    qkT = C.inp("qkT", [2, 2, 128, S])
    convw = C.inp("convw", [128, 16])
    convb = C.inp("convb", [128, 4])
    vtok = C.inp("vtok", [2, S, 128])
    otok = C.inp("otok", [2, S, 128])
    fused = "yT_ml" in C.bind
    if fused:
        yT_ml = C.bind["yT_ml"]
        igfg = C.bind["igfg"]
    else:
        ig_d = C.inp("ig", [64, 128])
        fg_d = C.inp("fg", [64, 128])
    normg = C.inp("normg", [64, 256])
    tri_d = C.inp("tri", [64, 64])
    id_d = C.inp("ident", [128, 128])
    if not fused:
        y_out = C.outp("y", [2, S, 128])
    setup_consts(C)

    cw = C.sb("cw", [128, 16])
    cb = C.sb("cb", [128, 4])
    tri = C.sb("tri_s", [64, 64])
    idf = C.sb("idf", [128, 128])
    idb = C.sb("idb", [128, 128], BF16)
    ng = C.sb("ng", [64, 256])
    ones = C.sb("ones_f", [128, 128])
    P.dma("sp", cw[:], convw, writes=["cw"])
    P.dma("sp", cb[:], convb, writes=["cb"])
    P.dma("sp", tri[:], tri_d, writes=["tri"])
    P.dma("sp", idf[:], id_d, writes=["idf"])
    P.dma("pool", idb[:], id_d, writes=["idb"])
    P.dma("sp", ng[:], normg, writes=["ng"])
    P.op("dve", lambda e: e.memset(ones[:], 1.0), writes=["onesf"])

    T = {}
    for nm in ("u", "w", "inter", "ecl", "uew"):
        T[nm] = C.sb("T_" + nm, [64, 128])
    decay = C.sb("T_decay", [128, 128])
    ew = C.sb("T_ew", [128, 128])
    st0 = contextlib.ExitStack()
    main_stack = C.stack
    C.stack = st0
    igs = C.sb("igs", [64, 128])
    lf = C.sb("lf", [64, 128])
    a_sb = C.sb("a_sb", [64, 128])
    yv = C.sb("yv", [64, 128])
    yT = C.sb("yT", [128, 64])
    MT = C.sb("MT", [128, 64])
    Z = C.sb("Z", [128, 128])
    M = C.sb("M", [64, 128])
    M63 = C.sb("M63", [128, 128])
    aL = C.sb("aL", [128, 128])
    mB = C.sb("mB", [128, 128])
    mC = C.sb("mC", [128, 128])
    mx = C.sb("mx", [128, 128])
    tmpg = C.sb("tmpg", [128, 128])
    pa = C.ps("pa", [64, 128])
    paL = C.ps("paL", [128, 128])
    pt1 = C.ps("pt1", [128, 64])
    pt2 = C.ps("pt2", [64, 128])
    pt3 = C.ps("pt3", [128, 128])
    if fused:
        gT = C.sb("gT", [128, 2, 64])
        P.dma("sp", gT[:, 0, :], igfg[0:2].rearrange("h (c p) -> (h c) p", p=64), writes=["gT"])
        P.dma("sp", gT[:, 1, :], igfg[2:4].rearrange("h (c p) -> (h c) p", p=64), writes=["gT"])
        P.op("pe", lambda e: e.transpose(pt2[:], gT[:, 0, :], idf[:]), reads=["gT", "idf"], writes=["pt2"])
        P.op("dve", lambda e: e.tensor_copy(out=igs[:], in_=pt2[:]), reads=["pt2"], writes=["igs"])
        P.op("pe", lambda e: e.transpose(pt2[:], gT[:, 1, :], idf[:]), reads=["gT", "idf", "igs"], writes=["pt2"])
        P.op("dve", lambda e: e.tensor_copy(out=lf[:], in_=pt2[:]), reads=["pt2"], writes=["lf"])
    else:
        P.dma("sp", igs[:], ig_d, writes=["igs"])
        P.dma("sp", lf[:], fg_d, writes=["lf"])
    P.op("act", lambda e: e.activation(out=lf[:], in_=lf[:], func=AF.Exp, scale=-1.0), reads=["lf"], writes=["lf"])
    P.op("act", lambda e: e.activation(out=lf[:], in_=lf[:], func=AF.Ln, bias=ones[0:64, 0:1], scale=1.0), reads=["lf", "ones"], writes=["lf"])
    P.op("dve", lambda e: e.tensor_scalar(out=lf[:], in0=lf[:], scalar1=-1.0, scalar2=None, op0=ALU.mult), reads=["lf"], writes=["lf"])
    P.op("pe", lambda e: e.matmul(pa[:], lhsT=tri[:], rhs=lf[:], start=True, stop=True), reads=["tri", "lf"], writes=["pa"])
    P.op("pe", lambda e: e.matmul(paL[:], lhsT=ones[0:64, :], rhs=lf[:], start=True, stop=True), reads=["ones", "lf"], writes=["paL"])
    P.op("dve", lambda e: e.tensor_copy(out=a_sb[:], in_=pa[:]), reads=["pa"], writes=["a_sb"])
    P.op("dve", lambda e: e.tensor_copy(out=aL[:], in_=paL[:]), reads=["paL"], writes=["aL"])
    P.op("dve", lambda e: e.tensor_tensor(out=yv[:], in0=igs[:], in1=a_sb[:], op=ALU.subtract), reads=["igs", "a_sb"], writes=["yv"])
    P.op("act", lambda e: e.activation(out=T["u"][:], in_=yv[:], func=AF.Exp), reads=["yv"], writes=["T_u"])
    P.op("pe", lambda e: e.transpose(pt1[:], yv[:], idf[0:64, 0:64]), reads=["yv", "idf"], writes=["pt1"])
    P.op("dve", lambda e: e.tensor_copy(out=yT[:], in_=pt1[:]), reads=["pt1"], writes=["yT"])
    P.op("dve", lambda e: e.tensor_tensor_scan(out=MT[:], data0=yT[:], data1=yT[:], initial=-3.0e38, op0=ALU.max, op1=ALU.max),
         reads=["yT"], writes=["MT"])
    P.op("pe", lambda e: e.transpose(pt2[:], MT[:], idf[:]), reads=["MT", "idf"], writes=["pt2"])
    P.op("dve", lambda e: e.tensor_copy(out=M[:], in_=pt2[:]), reads=["pt2"], writes=["M"])
    P.op("dve", lambda e: e.tensor_scalar(out=Z[:], in0=ones[:], scalar1=MT[:, 63:64], scalar2=None, op0=ALU.mult), reads=["ones", "MT"], writes=["Z"])
    P.op("pe", lambda e: e.transpose(pt3[:], Z[:], idf[:]), reads=["Z", "idf"], writes=["pt3"])
    P.op("dve", lambda e: e.tensor_copy(out=M63[:], in_=pt3[:]), reads=["pt3"], writes=["M63"])
    for h in range(2):
        hs = slice(h * 64, (h + 1) * 64)
        P.op("dve", lambda e, hs=hs: e.tensor_tensor_scan(out=mB[:, hs], data0=M63[:, hs], data1=aL[:, hs], initial=0.0, op0=ALU.max, op1=ALU.add),
             reads=["M63", "aL"], writes=["mB%d" % h])
        P.op("dve", lambda e, h=h: e.memset(mC[:, h * 64:h * 64 + 1], 0.0), writes=["mC%d" % h])
        P.op("dve", lambda e, h=h: e.tensor_copy(out=mC[:, h * 64 + 1:(h + 1) * 64], in_=mB[:, h * 64:(h + 1) * 64 - 1]),
             reads=["mB%d" % h, "mC%d" % h], writes=["mC%d" % h])
    mk = ["mB0", "mB1", "mC0", "mC1"]
    P.op("dve", lambda e: e.tensor_tensor(out=mx[:], in0=mC[:], in1=M63[:], op=ALU.max), reads=mk + ["M63"], writes=["mx"])
    P.op("dve", lambda e: e.tensor_tensor(out=tmpg[:], in0=mC[:], in1=mx[:], op=ALU.subtract), reads=mk + ["mx"], writes=["tmpg"])
    P.op("act", lambda e: e.activation(out=decay[:], in_=tmpg[:], func=AF.Exp), reads=["tmpg"], writes=["T_decay"])
    P.op("act", lambda e: e.activation(out=ew[:], in_=mx[:], func=AF.Exp, scale=-1.0), reads=["mx"], writes=["T_ew"])
    P.op("dve", lambda e: e.tensor_tensor(out=tmpg[0:64, :], in0=mC[0:64, :], in1=M[:], op=ALU.max), reads=mk + ["M", "tmpg"], writes=["tmpg"])
    P.op("act", lambda e: e.activation(out=T["w"][:], in_=tmpg[0:64, :], func=AF.Exp, scale=-1.0), reads=["tmpg"], writes=["T_w"])
    P.op("dve", lambda e: e.tensor_tensor(out=yv[:], in0=mC[0:64, :], in1=tmpg[0:64, :], op=ALU.subtract), reads=mk + ["tmpg", "yv"], writes=["yv"])
    P.op("act", lambda e: e.activation(out=T["inter"][:], in_=yv[:], func=AF.Exp), reads=["yv"], writes=["T_inter"])
    P.op("act", lambda e: e.activation(out=a_sb[:], in_=a_sb[:], func=AF.Exp, scale=-1.0), reads=["a_sb"], writes=["a_sb"])
    P.op("dve", lambda e: e.tensor_tensor(out=T["ecl"][:], in0=a_sb[:], in1=T["w"][:], op=ALU.mult), reads=["a_sb", "T_w"], writes=["T_ecl"])
    P.op("dve", lambda e: e.tensor_tensor(out=T["uew"][:], in0=T["u"][:], in1=ew[0:64, :], op=ALU.mult), reads=["T_u", "T_ew"], writes=["T_uew"])
    barrier(P)
    st0.close()
    C.stack = main_stack

    qTb = C.sb("qTb", [128, S], BF16)
    kTb = C.sb("kTb", [128, S], BF16)
    ktok = C.sb("ktok", [64, NCH, 128], BF16)
    Va = C.sb("Va", [64, NCH, 129], BF16)
    Vp = C.sb("Vp", [64, NCH, 129], BF16)
    hraw = C.sb("hraw", [64, NCH, 129])
    osg = C.sb("osg", [64, NCH, 128])
    sqt = C.sb("sqt", [64, 32, 128])
    xpad = C.sb("xpad", [128, S + 3])
    acc = C.sb("acc", [128, S])
    Cf = C.sb("Cf", [128, 129])
    Cbs = [C.sb("Cb%d" % i, [128, 129], BF16) for i in range(2)]
    tKV = C.sb("tKV", [128, 129])
    Gs = [C.sb("Gs%d" % i, [64, 64], BF16) for i in range(2)]
    den = C.sb("den", [64, NCH])
    ssq = C.sb("ssq", [64, NCH])
    pG_t = C.ps("pG", [64, 2, 64])
    pin_t = C.ps("pin", [64, 2, 129])
    pit_t = C.ps("pit", [64, 2, 129])
    pG = [pG_t[:, i, :] for i in range(2)]
    pin = [pin_t[:, i, :] for i in range(2)]
    pit = [pit_t[:, i, :] for i in range(2)]
    pKV = [C.ps("pKV%d" % i, [128, 129]) for i in range(2)]
    ptk = C.ps("ptk", [64, 4, 128], BF16)
    if fused:
        yTs = C.sb("yTs", [128, S], BF16)
        pty = C.ps("pty", [128, 512], BF16)
    P.op("dve", lambda e: e.memset(xpad[:, 0:3], 0.0), writes=["xpad"])

    for h in range(2):
        for qk in range(2):
            P.dma("sp", xpad[:, 3:], qkT[h, qk], writes=["xpad"])
            wi = (h * 2 + qk) * 4
            P.op("dve", lambda e, wi=wi, h=h, qk=qk: e.tensor_scalar(out=acc[:], in0=xpad[:, 3:3 + S], scalar1=cw[:, wi + 3:wi + 4],
                                                                     scalar2=cb[:, h * 2 + qk:h * 2 + qk + 1], op0=ALU.mult, op1=ALU.add),
                 reads=["xpad", "cw", "cb"], writes=["acc"])
            for i in range(3):
                P.op("dve", lambda e, wi=wi, i=i: e.scalar_tensor_tensor(out=acc[:], in0=xpad[:, i:i + S], scalar=cw[:, wi + i:wi + i + 1],
                                                                         in1=acc[:], op0=ALU.mult, op1=ALU.add),
                     reads=["xpad", "cw", "acc"], writes=["acc"])
            P.op("act", lambda e: e.activation(out=acc[:], in_=acc[:], func=AF.Silu), reads=["acc"], writes=["acc"])
            if qk == 0:
                P.op("dve", lambda e: e.tensor_scalar(out=qTb[:], in0=acc[:], scalar1=128.0 ** -0.5, scalar2=None, op0=ALU.mult),
                     reads=["acc"], writes=["qTb"])
            else:
                P.op("pool", lambda e: e.tensor_copy(out=kTb[:], in_=acc[:]), reads=["acc"], writes=["kTb"])
        for c4 in range(NCH // 4):
            def trk(e, c4=c4):
                ins = None
                for j in range(4):
                    c = c4 * 4 + j
                    ins = e.transpose(ptk[:, j, :], kTb[:, c * 64:(c + 1) * 64], idb[:])
                return ins
            P.op("pe", trk, reads=["kTb", "idb"], writes=["ptk"])
            P.op("act", lambda e, c4=c4: e.copy(out=ktok[:, c4 * 4:(c4 + 1) * 4, :], in_=ptk[:]), reads=["ptk"], writes=["ktok"])
        P.op("pool", lambda e: e.memset(Va[:], 1.0), writes=["Va"])
        P.dma("pool", Va[:, :, 0:128], vtok[h].rearrange("(c p) e -> p c e", p=64), writes=["Va"])
        P.dma("sp", osg[:], otok[h].rearrange("(c p) e -> p c e", p=64), writes=["osg"])
        P.op("act", lambda e: e.activation(out=osg[:], in_=osg[:], func=AF.Sigmoid), reads=["osg"], writes=["osg"])
        for c in range(NCH):
            col = h * 64 + c
            P.op("act", lambda e, c=c, col=col: e.activation(out=Vp[:, c, :], in_=Va[:, c, :], func=AF.Copy, scale=T["uew"][:, col:col + 1]),
                 reads=["Va", "T_uew"], writes=["Vp%d" % c])
        P.op("dve", lambda e: e.memset(Cf[:], 0.0), writes=["Cf"])
        P.op("dve", lambda e: e.memset(Cbs[0][:], 0.0), writes=["Cb0"])
        P.op("dve", lambda e: e.memset(Cbs[1][:], 0.0), writes=["Cb1"])

        def emit_gkv(c):
            b = c % 2
            csl = slice(c * 64, (c + 1) * 64)
            P.op("pe", lambda e: e.matmul(pG[b], lhsT=kTb[:, csl], rhs=qTb[:, csl], start=True, stop=True),
                 reads=["kTb", "qTb"], writes=["pG%d" % b])
            P.op("pe", lambda e: e.matmul(pKV[b][:], lhsT=ktok[:, c, :], rhs=Vp[:, c, :], start=True, stop=True),
                 reads=["ktok", "Vp%d" % c], writes=["pKV%d" % b])

        emit_gkv(0)
        for c in range(NCH):
            b = c % 2
            col = h * 64 + c
            csl = slice(c * 64, (c + 1) * 64)
            if c + 1 < NCH:
                emit_gkv(c + 1)
            if c + 1 < NCH:
                nb_ = (c + 1) % 2
                P.op("dve", lambda e, b=b, col=col: e.scalar_tensor_tensor(out=Cf[:], in0=Cf[:], scalar=decay[:, col:col + 1], in1=pKV[b][:],
                                                                           op0=ALU.mult, op1=ALU.add), reads=["Cf", "T_decay", "pKV%d" % b], writes=["Cf"])
                P.op("act", lambda e, nb_=nb_: e.copy(out=Cbs[nb_][:], in_=Cf[:]), reads=["Cf"], writes=["Cb%d" % nb_])
            P.op("dve", lambda e, b=b, col=col: e.scalar_tensor_tensor(out=Gs[b][:], in0=pG[b], scalar=T["u"][:, col:col + 1], in1=tri[:],
                                                                       op0=ALU.mult, op1=ALU.mult),
                 reads=["pG%d" % b, "T_u", "tri"], writes=["Gs%d" % b])
            if c > 0:
                P.op("pe", lambda e, b=b, csl=csl: e.matmul(pin[b], lhsT=qTb[:, csl], rhs=Cbs[b][:], start=True, stop=True),
                     reads=["qTb", "Cb%d" % b], writes=["pin%d" % b])
            P.op("pe", lambda e, b=b, c=c: e.matmul(pit[b], lhsT=Gs[b][:], rhs=Va[:, c, :], start=True, stop=True),
                 reads=["Gs%d" % b, "Va"], writes=["pit%d" % b])
            if c > 0:
                P.op("act", lambda e, b=b, c=c, col=col: e.activation(out=hraw[:, c, :], in_=pin[b], func=AF.Copy, scale=T["inter"][:, col:col + 1]),
                     reads=["pin%d" % b, "T_inter", "hrawn"], writes=["hraw%d" % c])
                P.op("dve", lambda e, b=b, c=c, col=col: e.scalar_tensor_tensor(out=hraw[:, c, :], in0=pit[b], scalar=T["w"][:, col:col + 1],
                                                                                in1=hraw[:, c, :], op0=ALU.mult, op1=ALU.add),
                     reads=["pit%d" % b, "T_w", "hraw%d" % c], writes=["hraw%d" % c])
            else:
                P.op("dve", lambda e, b=b, c=c, col=col: e.tensor_scalar(out=hraw[:, c, :], in0=pit[b], scalar1=T["w"][:, col:col + 1], scalar2=None,
                                                                         op0=ALU.mult), reads=["pit%d" % b, "T_w", "hrawn"], writes=["hraw%d" % c])
        hk = ["hraw%d" % c for c in range(NCH)]
        hsl = slice(h * 64, (h + 1) * 64)
        P.op("act", lambda e: e.activation(out=den[:], in_=hraw[:, :, 128], func=AF.Abs), reads=hk, writes=["den"])
        P.op("dve", lambda e, hsl=hsl: e.tensor_tensor(out=den[:], in0=den[:], in1=T["ecl"][:, hsl], op=ALU.max), reads=["den", "T_ecl"], writes=["den"])
        P.op("dve", lambda e: e.reciprocal(out=den[:], in_=den[:]), reads=["den"], writes=["den"])
        P.op("dve", lambda e: e.tensor_tensor(out=hraw[:, :, 0:128], in0=hraw[:, :, 0:128], in1=den[:, :].unsqueeze(2).to_broadcast([64, NCH, 128]), op=ALU.mult),
             reads=hk + ["den"], writes=["hrawn"])
        P.op("dve", lambda e: e.tensor_tensor(out=osg[:], in0=osg[:], in1=hraw[:, :, 0:128], op=ALU.mult), reads=hk + ["hrawn", "osg"], writes=["osg"])
        for hf in range(2):
            P.op("pool", lambda e, hf=hf: e.tensor_tensor(out=sqt[:], in0=osg[:, hf * 32:(hf + 1) * 32, :], in1=osg[:, hf * 32:(hf + 1) * 32, :], op=ALU.mult),
                 reads=["osg"], writes=["sqt"])
            P.op("dve", lambda e, hf=hf: e.tensor_reduce(out=ssq[:, hf * 32:(hf + 1) * 32], in_=sqt[:], axis=AX.X, op=ALU.add), reads=["sqt"], writes=["ssq"])
        P.op("act", lambda e: e.activation(out=ssq[:], in_=ssq[:], func=AF.Sqrt, bias=C.eps_ap[0:64, :], scale=1.0 / 128), reads=["ssq", "eps"], writes=["ssq"])
        P.op("dve", lambda e: e.reciprocal(out=ssq[:], in_=ssq[:]), reads=["ssq"], writes=["ssq"])
        P.op("dve", lambda e: e.tensor_tensor(out=osg[:], in0=osg[:], in1=ssq[:, :].unsqueeze(2).to_broadcast([64, NCH, 128]), op=ALU.mult),
             reads=["osg", "ssq"], writes=["osg"])
        P.op("dve", lambda e, h=h: e.tensor_tensor(out=osg[:], in0=osg[:], in1=ng[:, h * 128:(h + 1) * 128].unsqueeze(1).to_broadcast([64, NCH, 128]), op=ALU.mult),
             reads=["osg", "ng"], writes=["osg"])
        if not fused:
            P.dma("sp", y_out[h].rearrange("(c p) e -> p c e", p=64), osg[:], reads=["osg"])
            continue
        P.op("act", lambda e: e.copy(out=Va[:, :, 0:128], in_=osg[:]), reads=["osg"], writes=["Va"])
        for c8 in range(NCH // 8):
            def trs(e, c8=c8):
                ins = None
                for j in range(8):
                    ins = e.transpose(pty[:, j * 64:(j + 1) * 64], Va[:, c8 * 8 + j, 0:128], idb[0:64, 0:64])
                return ins
            P.op("pe", trs, reads=["Va", "idb"], writes=["pty"])
            P.op("dve", lambda e, c8=c8: e.tensor_copy(out=yTs[:, c8 * 512:(c8 + 1) * 512], in_=pty[:]), reads=["pty"], writes=["yTs"])
        P.dma("sp", yT_ml[h * 128:(h + 1) * 128, :], yTs[:], reads=["yTs"], writes=["yT_d"])


DBG = {}
FMC = 1028
TMC = 652
PAIRS = [[0, 1], [2, 3], [4, 5], [6, 7]]


def emit_P1(C):
    P = C.P
    xgc = C.bind.get("xgc")
    if xgc is None:
        xg = C.inp("xg", [2 * D, 2048])
    mm_ = getattr(C, "modmgr", None)
    if mm_ is None:
        cT = C.inp("cT", [128, 8])
        adaw = C.inp("adaw", [D, 2048])
        adab = C.inp("adab", [128, 16])
    gam = C.inp("gam", [128, 8])
    wc = C.inp("wc", [D, FMC + TMC])
    bfm = C.inp("bfm", [128, 9])
    btm = C.inp("btm", [128, TMC])
    fm_d = C.bind["fm_d"]
    tm_d = C.bind["tm_d"]
    setup_consts(C)
    if mm_ is None:
        mod, mkey = emit_mod(C, cT, adaw, adab, 16, "m1")
    else:
        mod, mkey = mm_.need(C.layer, 0)
    gam_sb = C.sb("gam_sb", [128, 8])
    bfm_sb = C.sb("bfm_sb", [128, 9])
    btm_sb = C.sb("btm_sb", [128, TMC])
    scale = C.sb("scale", [128, 8])
    P.dma("sp", gam_sb[:], gam, writes=["gam"])
    P.dma("sp", bfm_sb[:], bfm, writes=["bfm"])
    P.dma("sp", btm_sb[:], btm, writes=["btm"])
    P.op("dve", lambda e: e.scalar_tensor_tensor(out=scale[:], in0=mod[:, 8:16], scalar=1.0, in1=gam_sb[:], op0=ALU.add, op1=ALU.mult),
         reads=[mkey, "gam"], writes=["scale"])
    NW = FMC + TMC
    wb = C.sb("wb", [128, 8, NW], BF16)
    for i in range(4):
        P.dma("pool", wb[:, :, i * 420:(i + 1) * 420], wc[:, i * 420:(i + 1) * 420].rearrange("(kc ki) m -> ki kc m", ki=128),
              writes=["wb%d" % i])
    wkeys = ["wb%d" % i for i in range(4)]
    xts = [C.sb("xt%d" % i, [128, 8, 512]) for i in range(2)]
    hTs = [C.sb("hT%d" % i, [128, 8, 512], BF16) for i in range(2)]
    tmpb = (C.sb("sq", [128, 8, 512], BF16), C.ps("ms", [128, 512]), C.sb("rstd", [128, 512]), C.sb("tmp", [128, 8, 512]))
    pps = [C.ps("pp%d" % i, [128, 512]) for i in range(4)]
    obs = [C.sb("ob%d" % i, [128, 512]) for i in range(4)]
    otm = [C.sb("otm%d" % i, [128, TMC]) for i in range(2)]
    k = 0
    for tb in range(8):
        half, cb = tb // 4, (tb % 4) * 512
        xt = xts[tb % 2]
        xk = "xt%d" % (tb % 2)
        if xgc is None:
            P.dma("sp", xt[:], xg[half * D:(half + 1) * D, cb:cb + 512].rearrange("(kc ki) t -> ki kc t", ki=128), writes=[xk])
        else:
            for i in range(4):
                P.dma("sp", xt[:, 2 * i:2 * i + 2, :], xgc[i][half * 256:(half + 1) * 256, cb:cb + 512].rearrange("(kc ki) t -> ki kc t", ki=128),
                      reads=["xgc%d" % i], writes=[xk])
        hT = hTs[tb % 2]
        hk_ = "hT%d" % (tb % 2)
        emit_norm_block(C, xt[:], xk, scale, mod, "scale", C.ones_bf, hT, hk_, tmpb, "n1")
        for m in range(9):
            mw = 128 if m < 8 else FMC - 1024
            pp, ob = pps[k % 4], obs[k % 4]
            kk = k % 4
            k += 1

            def mm(e, m=m, mw=mw, pp=pp, hT=hT):
                ins = None
                for kc in range(8):
                    ins = e.matmul(pp[:mw, :], lhsT=wb[:, kc, m * 128:m * 128 + mw], rhs=hT[:, kc, :], start=(kc == 0), stop=(kc == 7))
                return ins
            P.op("pe", mm, reads=[hk_] + wkeys, writes=["pp%d" % kk])
            P.op("act", lambda e, m=m, mw=mw, pp=pp, ob=ob: e.activation(out=ob[:mw, :], in_=pp[:mw, :], func=AF.Identity,
                                                                          bias=bfm_sb[:mw, m:m + 1], scale=1.0),
                 reads=["pp%d" % kk, "bfm"], writes=["ob%d" % kk])
            P.dma("pool", fm_d[m * 128:m * 128 + mw, tb * 512:(tb + 1) * 512], ob[:mw, :], reads=["ob%d" % kk], writes=["fm_d"])
        for tt in range(4):
            ot = otm[tt % 2]
            for (c0, cw) in ((0, 512), (512, TMC - 512)):
                pp = pps[k % 4]
                kk = k % 4
                k += 1

                def mm(e, tt=tt, c0=c0, cw=cw, pp=pp, hT=hT):
                    ins = None
                    for kc in range(8):
                        ins = e.matmul(pp[:, :cw], lhsT=hT[:, kc, tt * 128:(tt + 1) * 128], rhs=wb[:, kc, FMC + c0:FMC + c0 + cw],
                                       start=(kc == 0), stop=(kc == 7))
                    return ins
                P.op("pe", mm, reads=[hk_] + wkeys, writes=["pp%d" % kk])
                P.op("dve", lambda e, c0=c0, cw=cw, pp=pp, ot=ot: e.tensor_tensor(out=ot[:, c0:c0 + cw], in0=pp[:, :cw], in1=btm_sb[:, c0:c0 + cw], op=ALU.add),
                     reads=["pp%d" % kk, "btm"], writes=["otm%d" % (tt % 2)])
            r0 = tb * 512 + tt * 128
            P.dma("pool", tm_d[r0:r0 + 128, :], ot[:], reads=["otm%d" % (tt % 2)], writes=["tm_d"])


def emit_P3b(C):
    P = C.P
    wo = C.inp("wo", [512, D])
    yT_d = C.bind["yT_d"]
    zp_g = C.bind["zp_g"]
    zs_g = C.bind["zs_g"]
    yT = C.sb("yT", [128, 4, S], BF16)
    wob = C.sb("wob", [128, 4, D], BF16)
    P.dma("pool", wob[:], wo.rearrange("(kc ki) m -> ki kc m", ki=128), writes=["wob"])
    for kc in range(4):
        P.dma("sp", yT[:, kc, :], yT_d[kc * 128:(kc + 1) * 128, :], reads=["yT_d"], writes=["yT%d" % kc])
    pps = [C.ps("pz%d" % i, [128, 512]) for i in range(4)]
    obs = [C.sb("oz%d" % i, [128, 512]) for i in range(4)]
    k = 0
    for gi in range(4):
        for m in (2 * gi, 2 * gi + 1):
            for tb in range(8):
                half, cb = tb // 4, (tb % 4) * 512
                pp, ob, kk = pps[k % 4], obs[k % 4], k % 4
                k += 1

                def mm(e, m=m, tb=tb, pp=pp):
                    ins = None
                    for kc in range(4):
                        ins = e.matmul(pp[:], lhsT=wob[:, kc, m * 128:(m + 1) * 128], rhs=yT[:, kc, tb * 512:(tb + 1) * 512], start=(kc == 0), stop=(kc == 3))
                    return ins
                P.op("pe", mm, reads=["wob"] + ["yT%d" % kc for kc in range(4)], writes=["pz%d" % kk])
                if k % 2:
                    P.op("act", lambda e, pp=pp, ob=ob: e.copy(out=ob[:], in_=pp[:]), reads=["pz%d" % kk], writes=["oz%d" % kk])
                else:
                    P.op("dve", lambda e, pp=pp, ob=ob: e.tensor_copy(out=ob[:], in_=pp[:]), reads=["pz%d" % kk], writes=["oz%d" % kk])
                r0 = half * 256 + (m % 2) * 128
                P.dma("sp", zp_g[gi].ap()[r0:r0 + 128, cb:cb + 512], ob[:], reads=["oz%d" % kk], writes=["zp%d" % gi])
        C.coll("ReduceScatter", ALU.add, PAIRS, zp_g[gi], zs_g[gi], reads=["zp%d" % gi], writes=["zs%d" % gi])


def build_fused(nlayers=2, dbg_dense=False):
    C = Ctx()
    nc = C.nc
    x_own = C.inp("x_own", [D, 2048])
    xg_in = C.inp("xg_in", [2 * D, 2048])
    out = C.outp("out", [D, 2048])
    fm_d = C.scratch("fm_d", [FMC, S]).ap()
    tm_d = C.scratch("tm_d", [S, TMC]).ap()
    yT_d = C.scratch("yT_d", [512, S], BF16).ap()
    zp_g = [C.scratch("zp_g%d" % i, [512, 2048]) for i in range(4)]
    zs_g = [C.scratch("zs_g%d" % i, [256, 2048]) for i in range(4)]
    xo_t = C.scratch("xo_d", [D, 2048])
    xoc_t = [C.scratch("xoc%d" % i, [256, 2048]) for i in range(4)]
    xgc_t = [C.scratch("xgc%d" % i, [512, 2048]) for i in range(4)]
    setup_consts(C)
    C.modmgr = ModMgr(C, nlayers)
    for l in range(nlayers):
        C.layer = l
        moe = (l % 2 == 1) and not dbg_dense
        last = (l == nlayers - 1)
        final = (l == 1)
        L = "L%d_" % l
        b1 = {"fm_d": fm_d, "tm_d": tm_d}
        if l == 0:
            b1["xg"] = xg_in
        else:
            b1["xgc"] = [t.ap() for t in xgc_t]
        with C.phase(L + "P1_", b1):
            emit_P1(C)
        with C.phase(L + "A2_", {"qT": fm_d[0:256].rearrange("(h d) t -> h d t", h=4),
                                 "kT": fm_d[256:448].rearrange("(b d) t -> b d t", b=3),
                                 "vcT": fm_d[448:512],
                                 "vtok": tm_d[:, 0:128].rearrange("t (b d) -> b t d", b=2),
                                 "gl": tm_d[:, 128:140],
                                 "yT_nsa": yT_d[0:256]}):
            emit_A2(C)
        with C.phase(L + "A3_", {"qkT": fm_d[512:1024].rearrange("(h q d) t -> h q d t", h=2, q=2),
                                 "igfg": fm_d[1024:1028],
                                 "vtok": tm_d[:, 140:396].rearrange("t (h e) -> h t e", h=2),
                                 "otok": tm_d[:, 396:652].rearrange("t (h e) -> h t e", h=2),
                                 "yT_ml": yT_d[256:512]}):
            emit_A3(C)
        with C.phase(L + "P3_", {"yT_d": yT_d, "zp_g": zp_g, "zs_g": zs_g}):
            emit_P3b(C)
        with C.phase(L + "A4_", {"xT": x_own if l == 0 else xo_t.ap(), "zT": [t.ap() for t in zs_g], "xoT": out if last else xo_t.ap()}):
            emit_A4(C, moe, final)
        if not last:
            for i in range(4):
                C.P.dma("pool", xoc_t[i].ap(), xo_t.ap()[i * 256:(i + 1) * 256, :], reads=["xo_d"], writes=["xoc%d" % i])
                C.coll("AllGather", ALU.bypass, PAIRS, xoc_t[i], xgc_t[i], reads=["xoc%d" % i], writes=["xgc%d" % i])
    if DBG.get("dump_y"):
        dy = C.outp("dbg_y", [512, S])
        C.P.dma("pool", dy, yT_d, reads=["yT_d"])
    if DBG.get("dump_mod"):
        dm = C.outp("dbg_mod", [128, 48 * nlayers])
        C.P.dma("sp", dm, C.modmgr.sealed[:], reads=["MODs%d_%d" % (l, p) for l in range(nlayers) for p in range(2)])
    return C.close()


def _chunkT(v, n):
    return np.ascontiguousarray(np.asarray(v, np.float32).reshape(n, 128).T)


def _a2_inputs(proj, g, inp, l, consts):
    m = {}
    q = proj[:, 0:512].reshape(S, 8, 64)[:, 4 * g:4 * g + 4]
    m["qT"] = np.ascontiguousarray(q.transpose(1, 2, 0))
    kv = proj[:, 512:1280].reshape(S, 6, 2, 64)[:, :, g]
    m["kT"] = np.ascontiguousarray(kv[:, [0, 2, 4]].transpose(1, 2, 0))
    m["vcT"] = np.ascontiguousarray(kv[:, 1].T)
    m["vtok"] = np.ascontiguousarray(kv[:, [3, 5]].transpose(1, 0, 2))
    m["gl"] = np.ascontiguousarray(proj[:, 1280:1304].reshape(S, 8, 3)[:, 4 * g:4 * g + 4].reshape(S, 12))
    w1 = inp["cmp_w1"][l]
    m["w1"] = np.ascontiguousarray(w1.reshape(2, 32, 64, 128).transpose(0, 2, 1, 3).reshape(2, 64, 32 * 128))
    m["w2"] = np.ascontiguousarray(inp["cmp_w2"][l])
    m["peT"] = np.ascontiguousarray(inp["cmp_pe"][l].transpose(0, 2, 1))
    m.update(consts[g])
    return m


def _a3_inputs(proj, hp, inp, l):
    m = {}
    hs = [2 * hp, 2 * hp + 1]
    qk = proj[:, 1304:2328]
    qkT = np.zeros((2, 2, 128, S), np.float32)
    convw = np.zeros((128, 2, 2, 4), np.float32)
    convb = np.zeros((128, 2, 2), np.float32)
    cwl = inp["conv_w"][l]
    cbl = inp["conv_b"][l]
    for i, h in enumerate(hs):
        for j in range(2):
            cols = slice(j * 512 + h * 128, j * 512 + h * 128 + 128)
            qkT[i, j] = qk[:, cols].T
            convw[:, i, j, :] = cwl[:, cols].T
            convb[:, i, j] = cbl[cols]
    m["qkT"] = qkT
    m["convw"] = convw.reshape(128, 16)
    m["convb"] = convb.reshape(128, 4)
    v = proj[:, 2328:2840]
    o = proj[:, 2840:3352]
    ip = proj[:, 3352:3356]
    fp = proj[:, 3356:3360]
    m["vtok"] = np.ascontiguousarray(np.stack([v[:, h * 128:(h + 1) * 128] for h in hs]))
    m["otok"] = np.ascontiguousarray(np.stack([o[:, h * 128:(h + 1) * 128] for h in hs]))
    m["ig"] = np.ascontiguousarray(np.concatenate([ip[:, h].reshape(64, 64).T for h in hs], axis=1))
    m["fg"] = np.ascontiguousarray(np.concatenate([fp[:, h].reshape(64, 64).T for h in hs], axis=1))
    g = inp["mlstm_norm_g"][l]
    m["normg"] = np.ascontiguousarray(np.broadcast_to(np.concatenate([g[h * 128:(h + 1) * 128] for h in hs])[None, :], (64, 256)))
    m["tri"] = np.triu(np.ones((64, 64), np.float32))
    m["ident"] = np.eye(128, dtype=np.float32)
    return m


_PROGS = {}


def _prog(name, fn):
    if name not in _PROGS:
        _PROGS[name] = fn()
    return _PROGS[name]


def _core_cols(g):
    hs = [2 * g, 2 * g + 1]
    r = np.arange
    fm = [g * 256 + r(256)]
    for br in (0, 2, 4, 1):
        fm.append(512 + br * 128 + g * 64 + r(64))
    for h in hs:
        fm.append(1304 + h * 128 + r(128))
        fm.append(1304 + 512 + h * 128 + r(128))
    fm.append(np.array([3352 + hs[0], 3352 + hs[1], 3356 + hs[0], 3356 + hs[1]]))
    tm = [512 + 3 * 128 + g * 64 + r(64), 512 + 5 * 128 + g * 64 + r(64), 1280 + 12 * g + r(12)]
    for h in hs:
        tm.append(2328 + h * 128 + r(128))
    for h in hs:
        tm.append(2840 + h * 128 + r(128))
    fm = np.concatenate(fm)
    tm = np.concatenate(tm)
    assert fm.size == FMC and tm.size == TMC
    return fm, tm


def _fused_inputs(inp, core, nlayers, consts, sel, ident, dbg_dense=False):
    b, g = core // 2, core % 2
    x = inp["x"]
    m = {}
    xb = x[b]
    m["x_own"] = np.ascontiguousarray(xb[g * 2048:(g + 1) * 2048].T)
    m["xg_in"] = np.ascontiguousarray(xb.reshape(2, 2048, D).transpose(0, 2, 1).reshape(2 * D, 2048))
    fm, tm = _core_cols(g)
    hs = [2 * g, 2 * g + 1]
    m["MOD_cT"] = _chunkT(inp["c"][b], 8)
    m["MOD_adab"] = np.ascontiguousarray(np.concatenate([_chunkT(inp["ada_b"][l], 48) for l in range(nlayers)], axis=1))
    for l in range(nlayers):
        m["MOD_adaw%d" % l] = inp["ada_w"][l]
    for l in range(nlayers):
        L = "L%d_" % l
        p = L + "P1_"
        m[p + "gam"] = _chunkT(inp["norm_mix_g"][l], 8)
        m[p + "wc"] = np.ascontiguousarray(inp["w_in"][l][:, np.concatenate([fm, tm])])
        bf = np.zeros(9 * 128, np.float32)
        bf[:FMC] = inp["b_in"][l][fm]
        m[p + "bfm"] = _chunkT(bf, 9)
        m[p + "btm"] = np.ascontiguousarray(np.broadcast_to(inp["b_in"][l][tm][None, :], (128, TMC)))
        p = L + "A2_"
        w1 = inp["cmp_w1"][l]
        m[p + "w1"] = np.ascontiguousarray(w1.reshape(2, 32, 64, 128).transpose(0, 2, 1, 3).reshape(2, 64, 32 * 128))
        m[p + "w2"] = np.ascontiguousarray(inp["cmp_w2"][l])
        m[p + "peT"] = np.ascontiguousarray(inp["cmp_pe"][l].transpose(0, 2, 1))
        for k, v in consts[g].items():
            m[p + k] = v
        p = L + "A3_"
        convw = np.zeros((128, 2, 2, 4), np.float32)
        convb = np.zeros((128, 2, 2), np.float32)
        for i, h in enumerate(hs):
            for j in range(2):
                cols = slice(j * 512 + h * 128, j * 512 + h * 128 + 128)
                convw[:, i, j, :] = inp["conv_w"][l][:, cols].T
                convb[:, i, j] = inp["conv_b"][l][cols]
        m[p + "convw"] = convw.reshape(128, 16)
        m[p + "convb"] = convb.reshape(128, 4)
        gn = inp["mlstm_norm_g"][l]
        m[p + "normg"] = np.ascontiguousarray(np.broadcast_to(np.concatenate([gn[h * 128:(h + 1) * 128] for h in hs])[None, :], (64, 256)))
        m[p + "tri"] = np.triu(np.ones((64, 64), np.float32))
        m[p + "ident"] = ident
        p = L + "P3_"
        rows = np.concatenate([g * 256 + np.arange(256), 512 + hs[0] * 128 + np.arange(128), 512 + hs[1] * 128 + np.arange(128)])
        m[p + "wo"] = np.ascontiguousarray(inp["w_out"][l][rows])
        p = L + "A4_"
        moe = (l % 2 == 1) and not dbg_dense
        m[p + "gam"] = _chunkT(inp["norm_ffn_g"][l], 8)
        if moe:
            m.update({p + "rw": inp["router_w"][l // 2], p + "sel": sel.reshape(8, 1024), p + "ident": ident,
                      p + "wg": inp["moe_w_gate"][l // 2], p + "wu": inp["moe_w_up"][l // 2], p + "wd": inp["moe_w_down"][l // 2]})
        else:
            m.update({p + "wg": inp["ffn_w_gate"][0:1], p + "wu": inp["ffn_w_up"][0:1], p + "wd": inp["ffn_w_down"][0:1]})
        if l == 1:
            m[p + "fgam"] = _chunkT(inp["final_norm_g"], 8)
    return m


def run_fused(inputs, nlayers=2, dbg_dense=False):
    inp = {k: np.asarray(v, np.float32) for k, v in inputs.items()}
    cores = list(range(8))
    consts = [nsa_consts(0), nsa_consts(1)]
    ident = np.eye(128, dtype=np.float32)
    sel = np.zeros((8, 8, 128), np.float32)
    for e in range(8):
        sel[e, e, :] = 1
    maps = [_fused_inputs(inp, core, nlayers, consts, sel, ident, dbg_dense) for core in cores]
    nc = _prog("fused%d_%d" % (nlayers, dbg_dense), lambda: build_fused(nlayers, dbg_dense))
    if DBG.get("trace"):
        rr = run_bass_kernel_spmd(nc, maps, core_ids=cores, trace=True)
        DBG["result"] = rr
        res = rr.results
    else:
        res = run_bass_kernel_spmd(nc, maps, core_ids=cores).results
    DBG["res"] = res
    out = np.stack([np.concatenate([res[2 * b]["out"], res[2 * b + 1]["out"]], axis=1).T for b in range(NB)])
    return np.ascontiguousarray(out.astype(np.float32))


def kernel(**inputs):
    return run_fused(inputs, 2)
```

```python
import contextlib
import numpy as np
import concourse.bass as bass
import concourse.mybir as mybir
from concourse.bass_utils import run_bass_kernel_spmd

F32 = mybir.dt.float32
BF16 = mybir.dt.bfloat16
AF = mybir.ActivationFunctionType
ALU = mybir.AluOpType
AX = mybir.AxisListType

D = 1024
S = 4096
NB = 4
EPS = 1e-6
IN_COLS = 3360
NEG = -30000.0


class Prog:
    ENG = ("pe", "dve", "act", "pool", "sp")

    def __init__(self, nc, stack, n_dma_sems=8):
        self.nc = nc
        self.eng = {"pe": nc.tensor, "dve": nc.vector, "act": nc.scalar, "pool": nc.gpsimd, "sp": nc.sync}
        self.sem = {}
        self.count = {}
        for e in self.ENG:
            self.sem[e] = stack.enter_context(nc.semaphore("s_" + e))
            self.count[e] = 0
        self.dsem, self.dval, self.drr = {}, {}, {}
        for q in ("sp", "pool", "act"):
            self.dsem[q] = [stack.enter_context(nc.semaphore("d_%s%d" % (q, i))) for i in range(n_dma_sems)]
            self.dval[q] = [0] * n_dma_sems
            self.drr[q] = 0
        self.seen = {e: {} for e in self.ENG}
        self.snap = {}
        self.last_w = {}
        self.readers = {}
        self.n_wait = 0
        self.n_ops = 0
        self.csem = {}
        self.ctoks = []

    def _semobj(self, key):
        if isinstance(key, str):
            return self.sem[key]
        if key[0] == "c":
            return self.csem[key]
        return self.dsem[key[1]][key[2]]

    def _wait(self, e, tok):
        key, val = tok
        if self.seen[e].get(key, 0) >= val:
            return
        self.eng[e].wait_ge(self._semobj(key), val)
        self.n_wait += 1
        self.seen[e][key] = val
        sn = self.snap.get(tok)
        if sn:
            se = self.seen[e]
            for k, v in sn.items():
                if se.get(k, 0) < v:
                    se[k] = v

    def _deps(self, reads, writes):
        deps = []
        for r in reads:
            t = self.last_w.get(r)
            if t is not None:
                deps.append(t)
        for w in writes:
            t = self.last_w.get(w)
            if t is not None:
                deps.append(t)
            deps.extend(self.readers.get(w, ()))
        return deps

    def _commit(self, tok, reads, writes):
        for r in reads:
            lst = self.readers.setdefault(r, [])
            lst.append(tok)
            if len(lst) > 64:
                best = {}
                for k, v in lst:
                    if best.get(k, 0) < v:
                        best[k] = v
                lst[:] = list(best.items())
        for w in writes:
            self.last_w[w] = tok
            self.readers[w] = []

    def op(self, e, fn, reads=(), writes=()):
        for t in self._deps(reads, writes):
            self._wait(e, t)
        ins = fn(self.eng[e])
        self.count[e] += 1
        ins.then_inc(self.sem[e], 1)
        tok = (e, self.count[e])
        sn = dict(self.seen[e])
        sn[e] = self.count[e]
        self.snap[tok] = sn
        self._commit(tok, reads, writes)
        self.n_ops += 1
        return tok

    def dma(self, q, out, in_, reads=(), writes=(), **kw):
        for t in self._deps(reads, writes):
            self._wait(q, t)
        i = self.drr[q]
        self.drr[q] = (i + 1) % len(self.dsem[q])
        key = ("d", q, i)
        if self.dval[q][i] > 0:
            self._wait(q, (key, self.dval[q][i]))
        ins = self.eng[q].dma_start(out=out, in_=in_, **kw)
        self.dval[q][i] += 16
        ins.then_inc(self.dsem[q][i], 16)
        tok = (key, self.dval[q][i])
        self.snap[tok] = dict(self.seen[q])
        self._commit(tok, reads, writes)
        return tok

    def finish(self, e="sp"):
        for t in self.ctoks:
            self._wait(e, t)
        for q in self.dsem:
            for i, v in enumerate(self.dval[q]):
                if v:
                    self._wait(e, (("d", q, i), v))
        for o in self.ENG:
            if o != e and self.count[o]:
                self._wait(e, (o, self.count[o]))


class Ctx:
    def __init__(self, name="k"):
        self.nc = bass.Bass("TRN2", target_bir_lowering=False)
        self.stack = contextlib.ExitStack()
        self.root_stack = self.stack
        self.P = Prog(self.nc, self.stack)
        self.pfx = ""
        self.bind = {}
        self.ncoll = 0

    def inp(self, name, shape, dt=F32):
        if name in self.bind:
            ap = self.bind[name]
            assert list(ap.shape) == list(shape), (name, ap.shape, shape)
            return ap
        return self.nc.dram_tensor(self.pfx + name, list(shape), dt, kind="ExternalInput").ap()

    def outp(self, name, shape, dt=F32):
        if name in self.bind:
            ap = self.bind[name]
            assert list(ap.shape) == list(shape), (name, ap.shape, shape)
            return ap
        return self.nc.dram_tensor(self.pfx + name, list(shape), dt, kind="ExternalOutput").ap()

    def scratch(self, name, shape, dt=F32):
        return self.nc.dram_tensor(name, list(shape), dt)

    def sb(self, name, shape, dt=F32):
        return self.stack.enter_context(self.nc.sbuf_tensor(self.pfx + name, list(shape), dt))

    def ps(self, name, shape, dt=F32):
        return self.stack.enter_context(self.nc.psum_tensor(self.pfx + name, list(shape), dt))

    @contextlib.contextmanager
    def phase(self, pfx, bind=None):
        old = (self.stack, self.pfx, self.bind)
        st = contextlib.ExitStack()
        self.stack, self.pfx, self.bind = st, pfx, dict(bind or {})
        try:
            yield
        finally:
            barrier(self.P)
            st.close()
            self.stack, self.pfx, self.bind = old

    def coll(self, kind, op, groups, src, dst, reads, writes):
        P = self.P
        for t in P._deps(reads, writes):
            P._wait("pool", t)
        sem = self.root_stack.enter_context(self.nc.semaphore("cc%d" % self.ncoll))
        key = ("c", self.ncoll)
        self.ncoll += 1
        P.csem[key] = sem
        self.nc.gpsimd.collective_compute(kind, op, replica_groups=groups, ins=[src.ap().opt()], outs=[dst.ap().opt()]).then_inc(sem)
        tok = (key, 1)
        P.snap[tok] = dict(P.seen["pool"])
        P.ctoks.append(tok)
        P._commit(tok, reads, writes)
        return tok

    def close(self):
        self.P.finish("sp")
        self.root_stack.close()
        return self.nc


class ModMgr:
    def __init__(self, C, nlayers):
        self.C = C
        P = C.P
        rs = C.root_stack
        nc = C.nc
        self.cT = nc.dram_tensor("MOD_cT", [128, 8], F32, kind="ExternalInput").ap()
        self.adab = nc.dram_tensor("MOD_adab", [128, 48 * nlayers], F32, kind="ExternalInput").ap()
        self.adaw = [nc.dram_tensor("MOD_adaw%d" % l, [D, 6 * D], F32, kind="ExternalInput").ap() for l in range(nlayers)]
        self.c_sb = rs.enter_context(nc.sbuf_tensor("MOD_c", [128, 8], F32))
        self.b_sb = rs.enter_context(nc.sbuf_tensor("MOD_b", [128, 48 * nlayers], F32))
        self.modall = rs.enter_context(nc.sbuf_tensor("MOD_all", [128, 48 * nlayers], F32))
        self.sealed = rs.enter_context(nc.sbuf_tensor("MOD_sealed", [128, 48 * nlayers], F32))
        P.dma("sp", self.c_sb[:], self.cT, writes=["MODc"])
        P.dma("sp", self.b_sb[:], self.adab, writes=["MODb"])
        P.op("act", lambda e: e.activation(out=self.c_sb[:], in_=self.c_sb[:], func=AF.Silu), reads=["MODc"], writes=["MODc"])
        self.c_bf = rs.enter_context(nc.sbuf_tensor("MOD_cbf", [128, 8], BF16))
        P.op("dve", lambda e: e.tensor_copy(out=self.c_bf[:], in_=self.c_sb[:]), reads=["MODc"], writes=["MODcb"])
        self.units = [(l, ch) for l in range(nlayers) for ch in range(48)]
        self.next_dma = 0
        self.next_mm = 0
        self.wts = None
        self.psum = None
        self.tick = 0

    def attach(self, wts, psum_cols):
        self.wts = wts
        self.psum = psum_cols
        self.nslot = psum_cols.shape[1]
        self.base_dma = self.next_dma
        self.next_dma = self.next_mm
        for _ in range(len(wts) - 1):
            self._dma()

    def detach(self):
        self.wts = None
        self.psum = None

    def _dma(self):
        if self.next_dma >= len(self.units):
            return
        u = self.next_dma
        l, ch = self.units[u]
        wt = self.wts[u % len(self.wts)]
        self.C.P.dma("pool", wt[:], self.adaw[l][:, ch * 128:(ch + 1) * 128].rearrange("(kc ki) m -> ki kc m", ki=128),
                     writes=["MODw%d" % (u % len(self.wts))])
        self.next_dma += 1

    def unit(self):
        if self.next_mm >= len(self.units):
            return False
        P = self.C.P
        u = self.next_mm
        l, ch = self.units[u]
        self._dma()
        nb = len(self.wts)
        wt = self.wts[u % nb]
        col = l * 48 + ch
        ps = self.psum[:, u % self.nslot:u % self.nslot + 1]
        pk = "MODp%d" % (u % self.nslot)

        def mm(e):
            ins = None
            for kc in range(8):
                ins = e.matmul(ps, lhsT=wt[:, kc, :], rhs=self.c_bf[:, kc:kc + 1], start=(kc == 0), stop=(kc == 7), skip_group_check=True)
            return ins
        P.op("pe", mm, reads=["MODw%d" % (u % nb), "MODcb"], writes=[pk])
        P.op("dve", lambda e: e.tensor_tensor(out=self.modall[:, col:col + 1], in0=ps, in1=self.b_sb[:, col:col + 1], op=ALU.add),
             reads=[pk, "MODb"], writes=["MODall"])
        self.next_mm += 1
        if ch == 15 or ch == 47:
            c0 = l * 48 + (0 if ch == 15 else 16)
            c1 = l * 48 + ch + 1
            key = "MODs%d_%d" % (l, 0 if ch == 15 else 1)
            P.op("dve", lambda e: e.tensor_copy(out=self.sealed[:, c0:c1], in_=self.modall[:, c0:c1]), reads=["MODall"], writes=[key])
        return True

    def bg_tick(self, every=6):
        every = DBG.get("bg_every", every)
        if self.wts is None:
            return
        self.tick += 1
        if self.tick % every == 0:
            self.unit()

    def need(self, l, part):
        last = l * 48 + (15 if part == 0 else 47)
        if self.next_mm <= last:
            own = self.wts is None
            if own:
                C = self.C
                wts = [C.sb("MODfw%d_%d_%d" % (l, part, i), [128, 8, 128], BF16) for i in range(4)]
                pm = C.ps("MODfp%d_%d" % (l, part), [128, 8])
                self.attach(wts, pm[:, :])
            while self.next_mm <= last:
                self.unit()
            if own:
                self.detach()
        c0 = l * 48 + (0 if part == 0 else 16)
        c1 = l * 48 + (16 if part == 0 else 48)
        return self.sealed[:, c0:c1], "MODs%d_%d" % (l, part)


def emit_mod(C, cT, adaw, adab, nch, name):
    P = C.P
    c_sb = C.sb(name + "_c", [128, 8])
    b_sb = C.sb(name + "_b", [128, nch])
    mod = C.sb(name + "_mod", [128, nch])
    pm = C.ps(name + "_pm", [128, nch])
    wts = [C.sb(name + "_w%d" % i, [128, 8, 128]) for i in range(2)]
    P.dma("sp", c_sb[:], cT, writes=[name + "c"])
    P.dma("sp", b_sb[:], adab, writes=[name + "b"])
    P.op("act", lambda e: e.activation(out=c_sb[:], in_=c_sb[:], func=AF.Silu), reads=[name + "c"], writes=[name + "c"])
    for j in range(nch):
        wt = wts[j % 2]
        wk = name + "w%d" % (j % 2)
        P.dma("sp", wt[:], adaw[:, j * 128:(j + 1) * 128].rearrange("(kc ki) m -> ki kc m", ki=128), writes=[wk])

        def mm(e, wt=wt, j=j):
            ins = None
            for kc in range(8):
                ins = e.matmul(pm[:, j:j + 1], lhsT=wt[:, kc, :], rhs=c_sb[:, kc:kc + 1], start=(kc == 0), stop=(kc == 7))
            return ins
        P.op("pe", mm, reads=[wk, name + "c"], writes=[name + "pm"])
    P.op("dve", lambda e: e.tensor_tensor(out=mod[:], in0=pm[:], in1=b_sb[:], op=ALU.add),
         reads=[name + "pm", name + "b"], writes=[name + "mod"])
    return mod, name + "mod"


def emit_norm_block(C, xt, xkey, scale, shift, skey, ones_bf, hT, hkey, tmp_bufs, name, ntok=512, h32=None):
    P = C.P
    sq, ms, rstd, tmp = tmp_bufs
    P.op("act", lambda e: e.activation(out=sq[:, :, :ntok], in_=xt, func=AF.Square), reads=[xkey], writes=[name + "sq"])

    def mm(e):
        ins = None
        for kc in range(8):
            ins = e.matmul(ms[:, :ntok], lhsT=ones_bf[:], rhs=sq[:, kc, :ntok], start=(kc == 0), stop=(kc == 7))
        return ins
    P.op("pe", mm, reads=[name + "sq", "ones"], writes=[name + "ms"])
    P.op("act", lambda e: e.activation(out=rstd[:, :ntok], in_=ms[:, :ntok], func=AF.Sqrt, bias=C.eps_ap[:], scale=1.0),
         reads=[name + "ms", "eps"], writes=[name + "rstd"])
    P.op("dve", lambda e: e.reciprocal(out=rstd[:, :ntok], in_=rstd[:, :ntok]), reads=[name + "rstd"], writes=[name + "rstd"])
    for kc in range(8):
        P.op("dve", lambda e, kc=kc: e.scalar_tensor_tensor(out=tmp[:, kc, :ntok], in0=xt[:, kc, :], scalar=scale[:, kc:kc + 1],
                                                            in1=rstd[:, :ntok], op0=ALU.mult, op1=ALU.mult),
             reads=[xkey, skey, name + "rstd"], writes=[name + "tmp%d" % kc])
        if h32 is not None:
            P.op("act", lambda e, kc=kc: e.activation(out=h32[:, kc, :ntok], in_=tmp[:, kc, :ntok], func=AF.Identity,
                                                      bias=shift[:, kc:kc + 1], scale=1.0),
                 reads=[name + "tmp%d" % kc, skey], writes=[name + "h32_%d" % kc])
            P.op("pool", lambda e, kc=kc: e.tensor_copy(out=hT[:, kc, :ntok], in_=h32[:, kc, :ntok]),
                 reads=[name + "h32_%d" % kc], writes=[hkey])
        else:
            P.op("act", lambda e, kc=kc: e.activation(out=hT[:, kc, :ntok], in_=tmp[:, kc, :ntok], func=AF.Identity,
                                                      bias=shift[:, kc:kc + 1], scale=1.0),
                 reads=[name + "tmp%d" % kc, skey], writes=[hkey])


def setup_consts(C):
    P = C.P
    if hasattr(C, "ones_bf"):
        return
    C.ones_bf = C.root_stack.enter_context(C.nc.sbuf_tensor("ones_bf", [128, 128], BF16))
    C.eps_ap = C.root_stack.enter_context(C.nc.sbuf_tensor("eps_ap", [128, 1], F32))
    P.op("dve", lambda e: e.memset(C.ones_bf[:], 1.0 / D), writes=["ones"])
    P.op("dve", lambda e: e.memset(C.eps_ap[:], EPS), writes=["eps"])


NT1 = 2048


def build_A1():
    C = Ctx()
    P = C.P
    xT = C.inp("xT", [D, NT1])
    cT = C.inp("cT", [128, 8])
    adaw = C.inp("adaw", [D, 2048])
    adab = C.inp("adab", [128, 16])
    gam = C.inp("gam", [128, 8])
    w_in = C.inp("w_in", [D, IN_COLS])
    b_in = C.inp("b_in", [128, 27])
    projT = C.outp("projT", [27 * 128, NT1])
    setup_consts(C)
    mod, mkey = emit_mod(C, cT, adaw, adab, 16, "m1")
    gam_sb = C.sb("gam_sb", [128, 8])
    bin_sb = C.sb("bin_sb", [128, 27])
    scale = C.sb("scale", [128, 8])
    P.dma("sp", gam_sb[:], gam, writes=["gam"])
    P.dma("sp", bin_sb[:], b_in, writes=["bin"])
    P.op("dve", lambda e: e.scalar_tensor_tensor(out=scale[:], in0=mod[:, 8:16], scalar=1.0, in1=gam_sb[:], op0=ALU.add, op1=ALU.mult),
         reads=[mkey, "gam"], writes=["scale"])
    wb = C.sb("wb", [128, 8, IN_COLS], BF16)
    for i in range(7):
        P.dma("pool", wb[:, :, i * 480:(i + 1) * 480], w_in[:, i * 480:(i + 1) * 480].rearrange("(kc ki) m -> ki kc m", ki=128),
              writes=["wb%d" % i])
    wkeys = ["wb%d" % i for i in range(7)]
    xts = [C.sb("xt%d" % i, [128, 8, 512]) for i in range(2)]
    hT = C.sb("hT", [128, 8, 512], BF16)
    tmpb = (C.sb("sq", [128, 8, 512], BF16), C.ps("ms", [128, 512]), C.sb("rstd", [128, 512]), C.sb("tmp", [128, 8, 512]))
    pps = [C.ps("pp%d" % i, [128, 512]) for i in range(4)]
    obs = [C.sb("ob%d" % i, [128, 512]) for i in range(4)]
    xT3 = xT.rearrange("(kc ki) t -> ki kc t", ki=128)
    for tb in range(NT1 // 512):
        xt = xts[tb % 2]
        xk = "xt%d" % (tb % 2)
        P.dma("sp", xt[:], xT3[:, :, tb * 512:(tb + 1) * 512], writes=[xk])
        emit_norm_block(C, xt[:], xk, scale, mod, "scale", C.ones_bf, hT, "hT", tmpb, "n1")
        for m in range(27):
            mw = 128 if m < 26 else IN_COLS - 26 * 128
            pp = pps[m % 4]
            ob = obs[m % 4]

            def mm(e, m=m, mw=mw, pp=pp):
                ins = None
                for kc in range(8):
                    ins = e.matmul(pp[:mw, :], lhsT=wb[:, kc, m * 128:m * 128 + mw], rhs=hT[:, kc, :], start=(kc == 0), stop=(kc == 7))
                return ins
            P.op("pe", mm, reads=["hT"] + wkeys, writes=["pp%d" % (m % 4)])
            P.op("act", lambda e, m=m, mw=mw, pp=pp, ob=ob: e.activation(out=ob[:mw, :], in_=pp[:mw, :], func=AF.Identity,
                                                                          bias=bin_sb[:mw, m:m + 1], scale=1.0),
                 reads=["pp%d" % (m % 4), "bin"], writes=["ob%d" % (m % 4)])
            P.dma("pool", projT[m * 128:m * 128 + mw, tb * 512:(tb + 1) * 512], ob[:mw, :], reads=["ob%d" % (m % 4)])
    return C.close()


def barrier(P):
    toks = list(P.ctoks)
    for q in P.dsem:
        for i, v in enumerate(P.dval[q]):
            if v:
                toks.append((("d", q, i), v))
    for o in P.ENG:
        if P.count[o]:
            toks.append((o, P.count[o]))
    for e in P.ENG:
        for t in toks:
            if t[0] != e:
                P._wait(e, t)
    for e in ("pe", "dve", "act", "pool"):
        if P.count[e]:
            P._wait(e, (e, P.count[e]))


def build_A4(moe, final):
    C = Ctx()
    emit_A4(C, moe, final)
    return C.close()


def emit_A4(C, moe, final):
    P = C.P
    NT = 2048
    NBk = NT // 512
    fused = "zT" in C.bind
    xT = C.inp("xT", [D, NT])
    if fused:
        zT = C.bind["zT"]
    else:
        yT = C.inp("yT", [D, NT])
    mm_ = getattr(C, "modmgr", None)
    if mm_ is None:
        cT = C.inp("cT", [128, 8])
        adaw = C.inp("adaw", [D, 4096])
        adab = C.inp("adab", [128, 32])
    gam = C.inp("gam", [128, 8])
    if not fused:
        w_out = C.inp("w_out", [D, D])
    if moe:
        NE, FF = 8, 3584
        rw = C.inp("rw", [D, 8])
        sel = C.inp("sel", [8, 8 * 128])
        ident = C.inp("ident", [128, 128])
    else:
        NE, FF = 1, 2816
    wg = C.inp("wg", [NE, D, FF])
    wu = C.inp("wu", [NE, D, FF])
    wd = C.inp("wd", [NE, FF, D])
    if final:
        fgam = C.inp("fgam", [128, 8])
    xoT = C.outp("xoT", [D, NT])
    setup_consts(C)
    if mm_ is None:
        mod, mkey = emit_mod(C, cT, adaw, adab, 32, "m4")
    else:
        mod, mkey = mm_.need(C.layer, 1)
    gam_sb = C.sb("gam_sb", [128, 8])
    scale = C.sb("scale", [128, 8])
    P.dma("sp", gam_sb[:], gam, writes=["gam"])
    P.op("dve", lambda e: e.scalar_tensor_tensor(out=scale[:], in0=mod[:, 16:24], scalar=1.0, in1=gam_sb[:], op0=ALU.add, op1=ALU.mult),
         reads=[mkey, "gam"], writes=["scale"])
    if final:
        fg_sb = C.sb("fg_sb", [128, 8])
        P.dma("sp", fg_sb[:], fgam, writes=["fgam"])
    xs = C.sb("xs", [128, 8, NT])
    hT = C.sb("hT", [128, 8, NT], BF16)
    xT3 = xT.rearrange("(kc ki) t -> ki kc t", ki=128)
    if not fused:
        yT3 = yT.rearrange("(kc ki) t -> ki kc t", ki=128)
    xoT3 = xoT.rearrange("(kc ki) t -> ki kc t", ki=128)
    for tb in range(NBk):
        P.dma("sp", xs[:, :, tb * 512:(tb + 1) * 512], xT3[:, :, tb * 512:(tb + 1) * 512], reads=["xo_d"], writes=["xs%d" % tb])
    if moe:
        wT = C.sb("wT", [8, NT], BF16)
    st1 = contextlib.ExitStack()
    main_stack = C.stack
    C.stack = st1
    if fused:
        ybs = [C.sb("zb%d" % i, [128, 8, 512]) for i in range(2)]
    else:
        wob = C.sb("wob", [128, 8, D], BF16)
        P.dma("pool", wob[:], w_out.rearrange("(kc ki) m -> ki kc m", ki=128), writes=["wob"])
        ybs = [C.sb("yb%d" % i, [128, 8, 512], BF16) for i in range(2)]
    tmpb = (C.sb("sq", [128, 8, 512], BF16), C.ps("ms", [128, 512]), C.sb("rstd", [128, 512]), C.sb("tmp", [128, 8, 512]))
    pzs = [C.ps("pz%d" % i, [128, 512]) for i in range(2)]
    if moe:
        h32 = C.sb("h32", [128, 8, 512])
        lgT = C.sb("lgT", [8, NT])
        rw_sb = C.sb("rw_sb", [128, 8, 8])
        P.dma("sp", rw_sb[:], rw.rearrange("(kc ki) e -> ki kc e", ki=128), writes=["rw"])
        plg = C.ps("plg", [8, 512])
    for tb in range(NBk):
        yb = ybs[tb % 2]
        yk = "yb%d" % (tb % 2)
        tsl = slice(tb * 512, (tb + 1) * 512)
        if fused:
            for gi in range(4):
                P.dma("sp", yb[:, 2 * gi:2 * gi + 2, :], zT[gi][:, tsl].rearrange("(kc ki) t -> ki kc t", ki=128), reads=["zs%d" % gi], writes=[yk])
        else:
            P.dma("pool", yb[:], yT3[:, :, tsl], writes=[yk])
        for m in range(8):
            if fused:
                P.op("dve", lambda e, m=m, yb=yb: e.scalar_tensor_tensor(out=xs[:, m, tsl], in0=yb[:, m, :], scalar=mod[:, m:m + 1],
                                                                         in1=xs[:, m, tsl], op0=ALU.mult, op1=ALU.add),
                     reads=[yk, mkey, "xs%d" % tb], writes=["xs%d" % tb])
                continue
            pz = pzs[m % 2]

            def mm(e, m=m, pz=pz, yb=yb):
                ins = None
                for kc in range(8):
                    ins = e.matmul(pz[:], lhsT=wob[:, kc, m * 128:(m + 1) * 128], rhs=yb[:, kc, :], start=(kc == 0), stop=(kc == 7))
                return ins
            P.op("pe", mm, reads=["wob", yk], writes=["pz%d" % (m % 2)])
            P.op("dve", lambda e, m=m, pz=pz: e.scalar_tensor_tensor(out=xs[:, m, tsl], in0=pz[:], scalar=mod[:, m:m + 1],
                                                                     in1=xs[:, m, tsl], op0=ALU.mult, op1=ALU.add),
                 reads=["pz%d" % (m % 2), mkey, "xs%d" % tb], writes=["xs%d" % tb])
        emit_norm_block(C, xs[:, :, tsl], "xs%d" % tb, scale, mod[:, 8:16], "scale", C.ones_bf, hT[:, :, tsl], "hT%d" % tb,
                        tmpb, "n4", h32=(h32 if moe else None))
        if moe:
            def mmr(e):
                ins = None
                for kc in range(8):
                    ins = e.matmul(plg[:], lhsT=rw_sb[:, kc, :], rhs=h32[:, kc, :], start=(kc == 0), stop=(kc == 7))
                return ins
            P.op("pe", mmr, reads=["rw"] + ["n4h32_%d" % kc for kc in range(8)], writes=["plg"])
            P.op("act", lambda e: e.copy(out=lgT[:, tsl], in_=plg[:]), reads=["plg"], writes=["lgT%d" % tb])
    if moe:
        id_sb = C.sb("id_sb", [128, 128])
        P.dma("sp", id_sb[:], ident, writes=["ident"])
        lg = C.sb("lg", [128, 16, 8])
        s8 = C.sb("s8", [128, 16, 8])
        e21 = C.sb("e21", [128, 16])
        w1 = C.sb("w1", [128, 16])
        w2 = C.sb("w2", [128, 16])
        m1 = C.sb("m1", [128, 16, 8])
        m2 = C.sb("m2", [128, 16, 8])
        ptr = C.ps("ptr", [128, 16, 8])

        def mmt(e):
            ins = None
            for tt in range(16):
                ins = e.transpose(ptr[:, tt, :], lgT[:, tt * 128:(tt + 1) * 128], id_sb[:8, :8])
            return ins
        P.op("pe", mmt, reads=["ident"] + ["lgT%d" % tb for tb in range(NBk)], writes=["ptr"])
        P.op("dve", lambda e: e.tensor_copy(out=lg[:], in_=ptr[:]), reads=["ptr"], writes=["lg"])
        for tt in range(16):
            P.op("dve", lambda e, tt=tt: e.max(out=s8[:, tt, :], in_=lg[:, tt, :]), reads=["lg"], writes=["s8_%d" % tt])
        s8k = ["s8_%d" % tt for tt in range(16)]
        P.op("dve", lambda e: e.tensor_tensor(out=e21[:], in0=s8[:, :, 1], in1=s8[:, :, 0], op=ALU.subtract), reads=s8k, writes=["e21"])
        P.op("act", lambda e: e.activation(out=e21[:], in_=e21[:], func=AF.Exp), reads=["e21"], writes=["e21"])
        P.op("dve", lambda e: e.tensor_scalar(out=w1[:], in0=e21[:], scalar1=1.0, scalar2=None, op0=ALU.add), reads=["e21"], writes=["w1"])
        P.op("dve", lambda e: e.reciprocal(out=w1[:], in_=w1[:]), reads=["w1"], writes=["w1"])
        P.op("dve", lambda e: e.tensor_tensor(out=w2[:], in0=e21[:], in1=w1[:], op=ALU.mult), reads=["e21", "w1"], writes=["w2"])
        for tt in range(16):
            P.op("dve", lambda e, tt=tt: e.tensor_scalar(out=m1[:, tt, :], in0=lg[:, tt, :], scalar1=s8[:, tt, 0:1], scalar2=w1[:, tt:tt + 1],
                                                         op0=ALU.is_equal, op1=ALU.mult), reads=["lg", "w1"] + s8k, writes=["m1_%d" % tt])
            P.op("dve", lambda e, tt=tt: e.tensor_scalar(out=m2[:, tt, :], in0=lg[:, tt, :], scalar1=s8[:, tt, 1:2], scalar2=w2[:, tt:tt + 1],
                                                         op0=ALU.is_equal, op1=ALU.mult), reads=["lg", "w2"] + s8k, writes=["m2_%d" % tt])
        P.op("dve", lambda e: e.tensor_tensor(out=m1[:], in0=m1[:], in1=m2[:], op=ALU.add),
             reads=["m1_%d" % tt for tt in range(16)] + ["m2_%d" % tt for tt in range(16)], writes=["wtok"])

        for tb in range(NBk):
            pz = pzs[tb % 2]

            def mmtb(e, tb=tb, pz=pz):
                ins = None
                for t4 in range(4):
                    tt = tb * 4 + t4
                    ins = e.transpose(pz[:8, t4 * 128:(t4 + 1) * 128], m1[:, tt, :], id_sb[:])
                return ins
            P.op("pe", mmtb, reads=["wtok", "ident"], writes=["pz%d" % (tb % 2)])
            P.op("dve", lambda e, tb=tb, pz=pz: e.tensor_copy(out=wT[:, tb * 512:(tb + 1) * 512], in_=pz[:8, :]),
                 reads=["pz%d" % (tb % 2)], writes=["wT"])
    barrier(P)
    st1.close()
    C.stack = main_stack
    st2 = contextlib.ExitStack()
    C.stack = st2
    nch = FF // 128
    groups = []
    f0 = 0
    while f0 < nch:
        nf = min(4, nch - f0)
        groups.append((f0, nf))
        f0 += nf
    gbs = [C.sb("gb%d" % i, [128, 8, 512], BF16) for i in range(2)]
    ubs = [C.sb("ub%d" % i, [128, 8, 512], BF16) for i in range(2)]
    dbs = [C.sb("db%d" % i, [128, 4, D], BF16) for i in range(2)]
    abs_ = [C.sb("ab%d" % i, [128, 4, NT], BF16) for i in range(2)]
    sgs = [C.sb("sg%d" % i, [128, 512]) for i in range(2)]
    pgs = [C.ps("pg%d" % i, [128, 512]) for i in range(2)]
    pus = [C.ps("pu%d" % i, [128, 512]) for i in range(2)]
    pds = [C.ps("pd%d" % i, [128, 512]) for i in range(2)]
    if moe:
        sel_sb = C.sb("sel_sb", [8, 8, 128], BF16)
        P.dma("pool", sel_sb[:], sel.rearrange("k (e m) -> k e m", e=8), writes=["sel"])
        wBs = [C.sb("wB%d" % i, [128, NT], BF16) for i in range(2)]
    hkeys = ["hT%d" % tb for tb in range(NBk)]
    work = [(ex, gi) for ex in range(NE) for gi in range(len(groups))]

    def emit_gu(idx):
        ex, gi = work[idx]
        f0, nf = groups[gi]
        bi = idx % 2
        gb, ub, ab = gbs[bi], ubs[bi], abs_[bi]
        cols = slice(f0 * 128, (f0 + nf) * 128)
        P.dma("pool", gb[:, :, :nf * 128], wg[ex, :, cols].rearrange("(kc ki) m -> ki kc m", ki=128), writes=["gb%d" % bi])
        P.dma("pool", ub[:, :, :nf * 128], wu[ex, :, cols].rearrange("(kc ki) m -> ki kc m", ki=128), writes=["ub%d" % bi])
        if moe and gi == 0:
            wB = wBs[ex % 2]
            for tb in range(NBk):
                pd = pds[tb % 2]
                P.op("pe", lambda e, tb=tb, pd=pd: e.matmul(pd[:], lhsT=sel_sb[:, ex, :], rhs=wT[:, tb * 512:(tb + 1) * 512], start=True, stop=True),
                     reads=["sel", "wT"], writes=["pd%d" % (tb % 2)])
                P.op("act", lambda e, tb=tb, pd=pd, wB=wB: e.copy(out=wB[:, tb * 512:(tb + 1) * 512], in_=pd[:]),
                     reads=["pd%d" % (tb % 2)], writes=["wB%d_%d" % (ex % 2, tb)])
        k = 0
        for fc in range(nf):
            for tb in range(NBk):
                pg, pu, sg = pgs[k % 2], pus[k % 2], sgs[k % 2]
                kk = k % 2
                tsl = slice(tb * 512, (tb + 1) * 512)

                def mm(e, fc=fc, tsl=tsl, pg=pg, pu=pu):
                    ins = None
                    for kc in range(8):
                        ins = e.matmul(pg[:], lhsT=gb[:, kc, fc * 128:(fc + 1) * 128], rhs=hT[:, kc, tsl], start=(kc == 0), stop=(kc == 7))
                    for kc in range(8):
                        ins = e.matmul(pu[:], lhsT=ub[:, kc, fc * 128:(fc + 1) * 128], rhs=hT[:, kc, tsl], start=(kc == 0), stop=(kc == 7))
                    return ins
                P.op("pe", mm, reads=["gb%d" % bi, "ub%d" % bi, "hT%d" % tb], writes=["pg%d" % kk, "pu%d" % kk])
                P.op("act", lambda e, pg=pg, sg=sg: e.activation(out=sg[:], in_=pg[:], func=AF.Silu), reads=["pg%d" % kk], writes=["sg%d" % kk])
                akey = "ab%d_%d_%d" % (bi, fc, tb)
                if moe:
                    P.op("dve", lambda e, sg=sg, pu=pu: e.tensor_tensor(out=sg[:], in0=sg[:], in1=pu[:], op=ALU.mult),
                         reads=["sg%d" % kk, "pu%d" % kk], writes=["sg%d" % kk])
                    P.op("dve", lambda e, sg=sg, fc=fc, tsl=tsl: e.tensor_tensor(out=ab[:, fc, tsl], in0=sg[:], in1=wBs[ex % 2][:, tsl], op=ALU.mult),
                         reads=["sg%d" % kk, "wB%d_%d" % (ex % 2, tb)], writes=[akey])
                else:
                    P.op("dve", lambda e, sg=sg, pu=pu, fc=fc, tsl=tsl: e.tensor_tensor(out=ab[:, fc, tsl], in0=sg[:], in1=pu[:], op=ALU.mult),
                         reads=["sg%d" % kk, "pu%d" % kk], writes=[akey])
                k += 1

    def emit_dn(idx):
        ex, gi = work[idx]
        f0, nf = groups[gi]
        bi = idx % 2
        db, ab = dbs[bi], abs_[bi]
        P.dma("pool", db[:, :nf, :], wd[ex, f0 * 128:(f0 + nf) * 128, :].rearrange("(fc fi) m -> fi fc m", fi=128), writes=["db%d" % bi])
        k = 0
        for m in range(8):
            for tb in range(NBk):
                pd = pds[k % 2]
                tsl = slice(tb * 512, (tb + 1) * 512)

                def mm(e, m=m, tsl=tsl, pd=pd):
                    ins = None
                    for fc in range(nf):
                        ins = e.matmul(pd[:], lhsT=db[:, fc, m * 128:(m + 1) * 128], rhs=ab[:, fc, tsl], start=(fc == 0), stop=(fc == nf - 1))
                    return ins
                P.op("pe", mm, reads=["db%d" % bi] + ["ab%d_%d_%d" % (bi, fc, tb) for fc in range(nf)], writes=["pd%d" % (k % 2)])
                P.op("dve", lambda e, m=m, tsl=tsl, pd=pd: e.scalar_tensor_tensor(out=xs[:, m, tsl], in0=pd[:], scalar=mod[:, 24 + m:25 + m],
                                                                                 in1=xs[:, m, tsl], op0=ALU.mult, op1=ALU.add),
                     reads=["pd%d" % (k % 2), mkey, "xs%d" % tb], writes=["xs%d" % tb])
                k += 1

    emit_gu(0)
    for i in range(len(work)):
        if i + 1 < len(work):
            emit_gu(i + 1)
        emit_dn(i)
    barrier(P)
    st2.close()
    C.stack = main_stack
    if final:
        tmpb = (C.sb("fsq", [128, 8, 512], BF16), C.ps("fms", [128, 512]), C.sb("frstd", [128, 512]), C.sb("ftmp", [128, 8, 512]))
        sq, ms, rstd, tmp = tmpb
        for tb in range(NBk):
            tsl = slice(tb * 512, (tb + 1) * 512)
            xk = "xs%d" % tb
            P.op("act", lambda e, tsl=tsl: e.activation(out=sq[:], in_=xs[:, :, tsl], func=AF.Square), reads=[xk], writes=["fsq"])

            def mm(e):
                ins = None
                for kc in range(8):
                    ins = e.matmul(ms[:], lhsT=C.ones_bf[:], rhs=sq[:, kc, :], start=(kc == 0), stop=(kc == 7))
                return ins
            P.op("pe", mm, reads=["fsq", "ones"], writes=["fms"])
            P.op("act", lambda e: e.activation(out=rstd[:], in_=ms[:], func=AF.Sqrt, bias=C.eps_ap[:], scale=1.0), reads=["fms", "eps"], writes=["frstd"])
            P.op("dve", lambda e: e.reciprocal(out=rstd[:], in_=rstd[:]), reads=["frstd"], writes=["frstd"])
            for kc in range(8):
                P.op("dve", lambda e, kc=kc, tsl=tsl: e.scalar_tensor_tensor(out=tmp[:, kc, :], in0=xs[:, kc, tsl], scalar=fg_sb[:, kc:kc + 1],
                                                                            in1=rstd[:], op0=ALU.mult, op1=ALU.mult),
                     reads=[xk, "fgam", "frstd"], writes=["ftmp"])
            P.dma("sp", xoT3[:, :, tsl], tmp[:], reads=["ftmp"], writes=["xo_d"])
    else:
        for tb in range(NBk):
            tsl = slice(tb * 512, (tb + 1) * 512)
            P.dma("sp", xoT3[:, :, tsl], xs[:, :, tsl], reads=["xs%d" % tb], writes=["xo_d"])


def nsa_consts(g):
    t = np.arange(S)
    ti, tl = t // 128, t % 128
    qaug = np.zeros((4, 4, S), np.float32)
    for hl in range(4):
        slope = 2.0 ** (-8.0 * (4 * g + hl + 1) / 8)
        qaug[hl, 0] = -8 * slope * 128 * ti
        qaug[hl, 1] = -8 * slope * tl
        qaug[hl, 2] = 8 * slope
        qaug[hl, 3] = 8 * slope
    kaug = np.stack([np.ones(S), np.ones(S), 128.0 * ti, 1.0 * tl]).astype(np.float32)
    n = np.arange(256)
    ce = 16 * n + 31
    kaugc = np.stack([np.ones(256), np.ones(256), 128.0 * (ce // 128), 1.0 * (ce % 128)]).astype(np.float32)
    kaugc[:, 255] = 0
    pl = np.arange(128)[:, None]
    ql = np.arange(512)[None, :]
    wmask = np.zeros((128, 8, 512), np.float32)
    for j in range(-4, 4):
        dist = ql - 128 * j - pl
        wmask[:, j + 4, :] = np.where((dist >= 0) & (dist < 512), 0.0, NEG)
    cmask = np.zeros((128, 4, 512), np.float32)
    for j in range(4):
        cmask[:, j, :] = np.where(ql - 128 * j - pl >= 0, 0.0, NEG)
    cmpmask = np.zeros((128, 2, 8, 512), np.float32)
    for c in range(2):
        nn = c * 128 + np.arange(128)[:, None]
        for Q in range(8):
            vis = (16 * nn + 31 <= 512 * Q + ql) & (nn < 255)
            cmpmask[:, c, Q, :] = np.where(vis, 0.0, NEG)
    E = np.zeros((64, 32, 128), np.float32)
    for c in range(32):
        E[2 * c, c, :64] = 1
        E[2 * c + 1, c, 64:] = 1
    lo_c = np.arange(256)[:, None] * 16
    lo_s = np.arange(64)[None, :] * 64
    ovm = np.clip(np.minimum(lo_c + 32, lo_s + 64) - np.maximum(lo_c, lo_s), 0, None) / 32.0
    ovm[255] = 0
    ov = np.ascontiguousarray(ovm.reshape(2, 128, 64).transpose(1, 0, 2)).astype(np.float32)
    cur = t // 64
    j = np.arange(64)[None, :]
    valid = j <= cur[:, None]
    forced = (j == 0) | (j == cur[:, None]) | (j == cur[:, None] - 1)
    valid01 = valid.astype(np.float32)
    addtab = np.where(valid, np.where(forced, 1e4, 0.0), -1.0).astype(np.float32)
    v01 = np.ascontiguousarray(valid01.reshape(32, 128, 64).transpose(1, 0, 2))
    adt = np.ascontiguousarray(addtab.reshape(32, 128, 64).transpose(1, 0, 2))
    return dict(qaug=qaug, kaug=kaug, kaugc=kaugc, wmask=wmask.reshape(128, -1), cmask=cmask.reshape(128, -1),
                cmpmask=cmpmask.reshape(128, -1), Emat=E.reshape(64, -1), ov=ov.reshape(128, -1), v01=v01.reshape(128, -1),
                adt=adt.reshape(128, -1), identb=np.eye(128, dtype=np.float32))


def build_A2():
    C = Ctx()
    emit_A2(C)
    return C.close()


def emit_A2(C):
    P = C.P
    qT = C.inp("qT", [4, 64, S])
    kT = C.inp("kT", [3, 64, S])
    vcT = C.inp("vcT", [64, S])
    vtok = C.inp("vtok", [2, S, 64])
    gl = C.inp("gl", [S, 12])
    w1 = C.inp("w1", [2, 64, 32 * 128])
    w2 = C.inp("w2", [2, 128, 64])
    peT = C.inp("peT", [2, 64, 32])
    qaug = C.inp("qaug", [4, 4, S])
    kaug = C.inp("kaug", [4, S])
    kaugc = C.inp("kaugc", [4, 256])
    wmask_d = C.inp("wmask", [128, 8 * 512])
    cmask_d = C.inp("cmask", [128, 4 * 512])
    cmpmask_d = C.inp("cmpmask", [128, 16 * 512])
    E_d = C.inp("Emat", [64, 32 * 128])
    ov_d = C.inp("ov", [128, 128])
    v01_d = C.inp("v01", [128, 32 * 64])
    adt_d = C.inp("adt", [128, 32 * 64])
    id_d = C.inp("identb", [128, 128])
    fused = "yT_nsa" in C.bind
    if fused:
        yT_nsa = C.bind["yT_nsa"]
    else:
        o_out = C.outp("o", [S, 256])

    qa = [C.sb("qa%d" % h, [68, S], BF16) for h in range(4)]
    ks = C.sb("ks", [68, S], BF16)
    kw = C.sb("kw", [68, S], BF16)
    kc = C.sb("kc", [68, 256], BF16)
    Vs = C.sb("Vs", [128, 32, 65], BF16)
    Vw = C.sb("Vw", [128, 32, 65], BF16)
    Vc = C.sb("Vc", [128, 2, 65], BF16)
    wmask = C.sb("wmask_s", [128, 8, 512], BF16)
    cmask = C.sb("cmask_s", [128, 4, 512], BF16)
    cmpmask = C.sb("cmpmask_s", [128, 16, 512], BF16)
    Em = C.sb("Em", [64, 32, 128], BF16)
    ov = C.sb("ov_s", [128, 2, 64], BF16)
    v01 = C.sb("v01_s", [128, 32, 64])
    adt = C.sb("adt_s", [128, 32, 64])
    idb = C.sb("idb", [128, 128], BF16)
    gates = C.sb("gates", [128, 32, 12])
    o_sb = C.sb("o_sb", [128, 32, 256])
    selbT = C.sb("selbT", [64, S], BF16)
    for h in range(4):
        P.dma("pool", qa[h][0:64, :], qT[h], writes=["qa%d" % h])
        P.dma("pool", qa[h][64:68, :], qaug[h], writes=["qa%d" % h])
    P.dma("pool", ks[0:64, :], kT[1], writes=["ks"])
    P.dma("pool", ks[64:68, :], kaug, writes=["ks"])
    P.dma("pool", kw[0:64, :], kT[2], writes=["kw"])
    P.dma("pool", kw[64:68, :], kaug, writes=["kw"])
    P.op("dve", lambda e: e.memset(kc[:], 0.0), writes=["kc"])
    P.dma("pool", kc[64:68, :], kaugc, writes=["kc"])
    P.op("dve", lambda e: e.memset(Vs[:], 1.0), writes=["Vs"])
    P.op("dve", lambda e: e.memset(Vw[:], 1.0), writes=["Vw"])
    P.op("dve", lambda e: e.memset(Vc[:], 1.0), writes=["Vc"])
    P.dma("pool", Vs[:, :, 0:64], vtok[0].rearrange("(t p) d -> p t d", p=128), writes=["Vs"])
    P.dma("pool", Vw[:, :, 0:64], vtok[1].rearrange("(t p) d -> p t d", p=128), writes=["Vw"])
    P.dma("pool", wmask[:], wmask_d.rearrange("p (a b) -> p a b", a=8), writes=["wmask"])
    P.dma("pool", cmask[:], cmask_d.rearrange("p (a b) -> p a b", a=4), writes=["cmask"])
    P.dma("pool", cmpmask[:], cmpmask_d.rearrange("p (a b) -> p a b", a=16), writes=["cmpmask"])
    P.dma("pool", Em[:], E_d.rearrange("p (a b) -> p a b", a=32), writes=["Em"])
    P.dma("pool", ov[:], ov_d.rearrange("p (a b) -> p a b", a=2), writes=["ov"])
    P.dma("pool", idb[:], id_d, writes=["idb"])
    P.dma("sp", v01[:], v01_d.rearrange("p (a b) -> p a b", a=32), writes=["v01"])
    P.dma("sp", adt[:], adt_d.rearrange("p (a b) -> p a b", a=32), writes=["adt"])
    P.dma("sp", gates[:], gl.rearrange("(t p) c -> p t c", p=128), writes=["gates"])
    P.op("act", lambda e: e.activation(out=gates[:], in_=gates[:], func=AF.Sigmoid), reads=["gates"], writes=["gates"])
    P.op("pool", lambda e: e.memset(o_sb[:], 0.0), writes=["o_sb"])

    st1 = contextlib.ExitStack()
    main_stack = C.stack
    C.stack = st1
    for kv in range(2):
        src = C.sb("csrc%d" % kv, [64, S], BF16)
        w1b = C.sb("w1b%d" % kv, [64, 32, 128], BF16)
        w2b = C.sb("w2b%d" % kv, [128, 64], BF16)
        peb = C.sb("peb%d" % kv, [64, 32], BF16)
        hid = C.sb("hid%d" % kv, [128, 256], BF16)
        cv = C.sb("cv%d" % kv, [128, 1])
        ph = C.ps("ph%d" % kv, [128, 256])
        pc = C.ps("pc%d" % kv, [128, 1])
        po = C.ps("po%d" % kv, [128, 256])
        sk = "csrc%d" % kv
        P.dma("pool", src[:], kT[0] if kv == 0 else vcT, writes=[sk])
        P.dma("pool", w1b[:], w1[kv].rearrange("d (i j) -> d i j", i=32), writes=["w1b%d" % kv])
        P.dma("pool", w2b[:], w2[kv], writes=["w2b%d" % kv])
        P.dma("pool", peb[:], peT[kv], writes=["peb%d" % kv])

        def mmh(e, src=src, w1b=w1b, ph=ph):
            ins = None
            for i in range(32):
                ins = e.matmul(ph[:, 0:255], lhsT=w1b[:, i, :], rhs=src[:, i:i + 16 * 254 + 1:16], start=(i == 0), stop=(i == 31))
            return ins
        P.op("pe", mmh, reads=[sk, "w1b%d" % kv], writes=["ph%d" % kv])

        def mmc(e, w1b=w1b, peb=peb, pc=pc):
            ins = None
            for i in range(32):
                ins = e.matmul(pc[:], lhsT=w1b[:, i, :], rhs=peb[:, i:i + 1], start=(i == 0), stop=(i == 31))
            return ins
        P.op("pe", mmc, reads=["peb%d" % kv, "w1b%d" % kv], writes=["pc%d" % kv])
        P.op("dve", lambda e, cv=cv, pc=pc: e.tensor_copy(out=cv[:], in_=pc[:]), reads=["pc%d" % kv], writes=["cv%d" % kv])
        P.op("dve", lambda e, hid=hid: e.memset(hid[:], 0.0), writes=["hid%d" % kv])
        P.op("act", lambda e, hid=hid, ph=ph, cv=cv: e.activation(out=hid[:, 0:255], in_=ph[:, 0:255], func=AF.Silu, bias=cv[:], scale=1.0),
             reads=["ph%d" % kv, "cv%d" % kv, "hid%d" % kv], writes=["hid%d" % kv])
        if kv == 0:
            P.op("pe", lambda e, w2b=w2b, hid=hid, po=po: e.matmul(po[0:64, 0:255], lhsT=w2b[:], rhs=hid[:, 0:255], start=True, stop=True),
                 reads=["w2b0", "hid0"], writes=["po0"])
            P.op("dve", lambda e, po=po: e.tensor_copy(out=kc[0:64, 0:255], in_=po[0:64, 0:255]), reads=["po0", "kc"], writes=["kc"])
        else:
            def mmv(e, w2b=w2b, hid=hid, po=po):
                ins = None
                for c in range(2):
                    ins = e.matmul(po[:, c * 64:(c + 1) * 64], lhsT=hid[:, c * 128:(c + 1) * 128], rhs=w2b[:], start=True, stop=True)
                return ins
            P.op("pe", mmv, reads=["w2b1", "hid1"], writes=["po1"])
            P.op("dve", lambda e, po=po: e.tensor_copy(out=Vc[:, :, 0:64], in_=po[:, 0:128].rearrange("p (c d) -> p c d", c=2)),
                 reads=["po1", "Vc"], writes=["Vc"])
    barrier(P)
    st1.close()
    C.stack = main_stack

    Sb = [C.ps("S%d" % i, [128, 512]) for i in range(3)]
    PT = [C.sb("PT%d" % i, [128, 512], BF16) for i in range(3)]
    oacc_f = [C.ps("oacc%d" % i, [128, 512]) for i in range(2)]
    impacc_f = [C.ps("impacc%d" % i, [128, 512]) for i in range(2)]
    oacc = [t[:, 0:260].rearrange("p (a b) -> p a b", a=4) for t in oacc_f]
    impacc = [t[:, 0:256].rearrange("p (a b) -> p a b", a=4) for t in impacc_f]
    ptr = C.ps("ptrs", [128, 1024], BF16)
    imp_sb = C.sb("imp_sb", [128, 4, 64])
    imp2 = C.sb("imp2", [128, 4, 64])
    imp3 = C.sb("imp3", [128, 4, 64])
    s8a = C.sb("s8a", [128, 4, 8])
    s8b = C.sb("s8b", [128, 4, 8])
    selb = C.sb("selb", [128, 4, 64], BF16)
    dmx = C.sb("dmx", [128, 4])
    coef = C.sb("coef", [128, 4])
    state = {"k": 0, "pass": 0}
    mm_ = getattr(C, "modmgr", None)
    if mm_ is not None and mm_.next_mm < len(mm_.units) and not DBG.get("no_bg"):
        bgw = [C.sb("bgw%d" % i, [128, 8, 128], BF16) for i in range(6)]
        if DBG.get("bg_own_bank"):
            bgp = Sb.pop()
            mm_.attach(bgw, bgp[:, 0:8])
        else:
            mm_.attach(bgw, impacc_f[0][:, 256:264])
    else:
        mm_ = None

    def attn_pass(chunks, hl, Q, br, with_imp):
        pi = state["pass"] % 2
        state["pass"] += 1
        oa = oacc[pi]
        ia = impacc[pi]
        Qsl = slice(Q * 512, (Q + 1) * 512)
        n = len(chunks)
        bufidx = []

        def emit_pv(idx):
            bi = bufidx[idx]
            _, vr, vkey, ovr = chunks[idx]

            def pv(e):
                ins = None
                for qt in range(4):
                    ins = e.matmul(oa[:, qt, :], lhsT=PT[bi][:, qt * 128:(qt + 1) * 128], rhs=vr, start=(idx == 0 and qt == 0),
                                   stop=(idx == n - 1), skip_group_check=True)
                if with_imp:
                    for qt in range(4):
                        ins = e.matmul(ia[:, qt, :], lhsT=PT[bi][:, qt * 128:(qt + 1) * 128], rhs=ovr, start=(idx == 0 and qt == 0),
                                       stop=(idx == n - 1), skip_group_check=True)
                return ins
            wr = ["oacc%d" % pi] + (["impacc%d" % pi] if with_imp else [])
            P.op("pe", pv, reads=["PT%d" % bi, vkey, "ov"], writes=wr)

        for idx in range(n):
            bi = state["k"] % len(Sb)
            state["k"] += 1
            bufidx.append(bi)
            mms = chunks[idx][0]

            def smm(e, mms=mms, bi=bi):
                ins = None
                for mi, (lt, rh, _) in enumerate(mms):
                    ins = e.matmul(Sb[bi][:], lhsT=lt, rhs=rh, start=(mi == 0), stop=(mi == len(mms) - 1))
                return ins
            rd = []
            for (_, _, r) in mms:
                rd.extend(r)
            P.op("pe", smm, reads=rd, writes=["S%d" % bi])
            P.op("act", lambda e, bi=bi: e.activation(out=PT[bi][:], in_=Sb[bi][:], func=AF.Exp, scale=0.125),
                 reads=["S%d" % bi], writes=["PT%d" % bi])
            if idx >= 1:
                emit_pv(idx - 1)
            if mm_ is not None and not with_imp:
                mm_.bg_tick()
        emit_pv(n - 1)
        P.op("dve", lambda e: e.tensor_scalar(out=dmx[:], in0=oa[:, :, 64], scalar1=1e-30, scalar2=None, op0=ALU.max),
             reads=["oacc%d" % pi], writes=["dmx"])
        P.op("dve", lambda e: e.reciprocal(out=dmx[:], in_=dmx[:]), reads=["dmx"], writes=["dmx"])
        P.op("dve", lambda e: e.tensor_tensor(out=coef[:], in0=dmx[:], in1=gates[:, Q * 4:Q * 4 + 4, hl * 3 + br], op=ALU.mult),
             reads=["dmx", "gates"], writes=["coef"])
        for qt in range(4):
            osl = o_sb[:, Q * 4 + qt, hl * 64:(hl + 1) * 64]
            P.op("dve", lambda e, qt=qt, osl=osl: e.scalar_tensor_tensor(out=osl, in0=oa[:, qt, 0:64], scalar=coef[:, qt:qt + 1], in1=osl,
                                                                         op0=ALU.mult, op1=ALU.add),
                 reads=["oacc%d" % pi, "coef", "o_sb"], writes=["o_sb"])
            if with_imp:
                if hl == 0:
                    P.op("dve", lambda e, qt=qt: e.tensor_scalar(out=imp_sb[:, qt, :], in0=ia[:, qt, :], scalar1=dmx[:, qt:qt + 1], scalar2=None,
                                                                 op0=ALU.mult), reads=["impacc%d" % pi, "dmx"], writes=["imp_sb"])
                else:
                    P.op("dve", lambda e, qt=qt: e.scalar_tensor_tensor(out=imp_sb[:, qt, :], in0=ia[:, qt, :], scalar=dmx[:, qt:qt + 1],
                                                                        in1=imp_sb[:, qt, :], op0=ALU.mult, op1=ALU.add),
                         reads=["impacc%d" % pi, "dmx", "imp_sb"], writes=["imp_sb"])

    for Q in range(8):
        Qsl = slice(Q * 512, (Q + 1) * 512)
        ncc = 2 if Q >= 4 else 1
        for hl in range(4):
            chunks = []
            for c in range(ncc):
                mms = [(kc[:, c * 128:(c + 1) * 128], qa[hl][:, Qsl], ["kc", "qa%d" % hl]),
                       (idb[:], cmpmask[:, c * 8 + Q, :], ["idb", "cmpmask"])]
                chunks.append((mms, Vc[:, c, :], "Vc", ov[:, c, :]))
            attn_pass(chunks, hl, Q, 0, True)
        for hl in range(4):
            chunks = []
            for j in range(-4, 4):
                c = 4 * Q + j
                if c < 0:
                    continue
                mms = [(kw[:, c * 128:(c + 1) * 128], qa[hl][:, Qsl], ["kw", "qa%d" % hl]),
                       (idb[:], wmask[:, j + 4, :], ["idb", "wmask"])]
                chunks.append((mms, Vw[:, c, :], "Vw", None))
            attn_pass(chunks, hl, Q, 2, False)
        P.op("dve", lambda e: e.tensor_tensor(out=imp2[:], in0=imp_sb[:], in1=v01[:, Q * 4:Q * 4 + 4, :], op=ALU.mult),
             reads=["imp_sb", "v01"], writes=["imp2"])
        P.op("dve", lambda e: e.tensor_tensor(out=imp2[:], in0=imp2[:], in1=adt[:, Q * 4:Q * 4 + 4, :], op=ALU.add),
             reads=["imp2", "adt"], writes=["imp2"])
        for qt in range(4):
            P.op("dve", lambda e, qt=qt: e.max(out=s8a[:, qt, :], in_=imp2[:, qt, :]), reads=["imp2"], writes=["s8a%d" % qt])
            P.op("dve", lambda e, qt=qt: e.match_replace(out=imp3[:, qt, :], in_to_replace=s8a[:, qt, :], in_values=imp2[:, qt, :], imm_value=-3.0e38),
                 reads=["imp2", "s8a%d" % qt], writes=["imp3_%d" % qt])
            P.op("dve", lambda e, qt=qt: e.max(out=s8b[:, qt, :], in_=imp3[:, qt, :]), reads=["imp3_%d" % qt], writes=["s8b%d" % qt])
            P.op("dve", lambda e, qt=qt: e.tensor_scalar(out=imp3[:, qt, :], in0=imp2[:, qt, :], scalar1=s8b[:, qt, 7:8], scalar2=-NEG,
                                                         op0=ALU.is_ge, op1=ALU.mult),
                 reads=["imp2", "s8b%d" % qt, "imp3_%d" % qt], writes=["imp3_%d" % qt])
            P.op("dve", lambda e, qt=qt: e.tensor_scalar(out=selb[:, qt, :], in0=imp3[:, qt, :], scalar1=NEG, scalar2=None, op0=ALU.add),
                 reads=["imp3_%d" % qt], writes=["selb%d" % qt])

        def trs(e):
            ins = None
            for qt in range(4):
                ins = e.transpose(ptr[0:64, qt * 128:(qt + 1) * 128], selb[:, qt, :], idb[:])
            return ins
        P.op("pe", trs, reads=["selb%d" % qt for qt in range(4)] + ["idb"], writes=["ptrs"])
        P.op("dve", lambda e: e.tensor_copy(out=selbT[:, Qsl], in_=ptr[0:64, 0:512]), reads=["ptrs"], writes=["selbT%d" % Q])
        for hl in range(4):
            chunks = []
            for c in range(4 * Q + 4):
                mms = [(ks[:, c * 128:(c + 1) * 128], qa[hl][:, Qsl], ["ks", "qa%d" % hl]),
                       (Em[:, c, :], selbT[:, Qsl], ["Em", "selbT%d" % Q])]
                if c >= 4 * Q:
                    mms.append((idb[:], cmask[:, c - 4 * Q, :], ["idb", "cmask"]))
                chunks.append((mms, Vs[:, c, :], "Vs", None))
            attn_pass(chunks, hl, Q, 1, False)
    if mm_ is not None:
        while mm_.unit():
            pass
        mm_.detach()
    if not fused:
        P.dma("sp", o_out.rearrange("(t p) c -> p t c", p=128), o_sb[:], reads=["o_sb"])
        return
    o_bf = C.sb("o_bf", [128, 32, 256], BF16)
    P.op("act", lambda e: e.copy(out=o_bf[:], in_=o_sb[:]), reads=["o_sb"], writes=["o_bf"])
    yst = [C.sb("yst%d" % i, [128, 2, 512], BF16) for i in range(2)]
    for t4 in range(8):
        ys = yst[t4 % 2]
        for cc in range(2):
            def trs(e, t4=t4, cc=cc):
                ins = None
                for j in range(4):
                    ins = e.transpose(ptr[:, j * 128:(j + 1) * 128], o_bf[:, t4 * 4 + j, cc * 128:(cc + 1) * 128], idb[:])
                return ins
            P.op("pe", trs, reads=["o_bf", "idb"], writes=["ptrs"])
            P.op("dve", lambda e, ys=ys, cc=cc: e.tensor_copy(out=ys[:, cc, :], in_=ptr[:, 0:512]), reads=["ptrs"], writes=["yst%d" % (t4 % 2)])
        P.dma("sp", yT_nsa[:, t4 * 512:(t4 + 1) * 512].rearrange("(cc p) t -> p cc t", p=128), ys[:], reads=["yst%d" % (t4 % 2)], writes=["yT_d"])


def build_A3():
    C = Ctx()
    emit_A3(C)
    return C.close()


def emit_A3(C):
    P = C.P
    NCH = 64
    qkT = C.inp("qkT", [2, 2, 128, S])
    convw = C.inp("convw", [128, 16])
    convb = C.inp("convb", [128, 4])
    vtok = C.inp("vtok", [2, S, 128])
    otok = C.inp("otok", [2, S, 128])
    fused = "yT_ml" in C.bind
    if fused:
        yT_ml = C.bind["yT_ml"]
        igfg = C.bind["igfg"]
    else:
        ig_d = C.inp("ig", [64, 128])
        fg_d = C.inp("fg", [64, 128])
    normg = C.inp("normg", [64, 256])
    tri_d = C.inp("tri", [64, 64])
    id_d = C.inp("ident", [128, 128])
    if not fused:
        y_out = C.outp("y", [2, S, 128])
    setup_consts(C)

    cw = C.sb("cw", [128, 16])
    cb = C.sb("cb", [128, 4])
    tri = C.sb("tri_s", [64, 64])
    idf = C.sb("idf", [128, 128])
    idb = C.sb("idb", [128, 128], BF16)
    ng = C.sb("ng", [64, 256])
    ones = C.sb("ones_f", [128, 128])
    P.dma("sp", cw[:], convw, writes=["cw"])
    P.dma("sp", cb[:], convb, writes=["cb"])
    P.dma("sp", tri[:], tri_d, writes=["tri"])
    P.dma("sp", idf[:], id_d, writes=["idf"])
    P.dma("pool", idb[:], id_d, writes=["idb"])
    P.dma("sp", ng[:], normg, writes=["ng"])
    P.op("dve", lambda e: e.memset(ones[:], 1.0), writes=["onesf"])

    T = {}
    for nm in ("u", "w", "inter", "ecl", "uew"):
        T[nm] = C.sb("T_" + nm, [64, 128])
    decay = C.sb("T_decay", [128, 128])
    ew = C.sb("T_ew", [128, 128])
    st0 = contextlib.ExitStack()
    main_stack = C.stack
    C.stack = st0
    igs = C.sb("igs", [64, 128])
    lf = C.sb("lf", [64, 128])
    a_sb = C.sb("a_sb", [64, 128])
    yv = C.sb("yv", [64, 128])
    yT = C.sb("yT", [128, 64])
    MT = C.sb("MT", [128, 64])
    Z = C.sb("Z", [128, 128])
    M = C.sb("M", [64, 128])
    M63 = C.sb("M63", [128, 128])
    aL = C.sb("aL", [128, 128])
    mB = C.sb("mB", [128, 128])
    mC = C.sb("mC", [128, 128])
    mx = C.sb("mx", [128, 128])
    tmpg = C.sb("tmpg", [128, 128])
    pa = C.ps("pa", [64, 128])
    paL = C.ps("paL", [128, 128])
    pt1 = C.ps("pt1", [128, 64])
    pt2 = C.ps("pt2", [64, 128])
    pt3 = C.ps("pt3", [128, 128])
    if fused:
        gT = C.sb("gT", [128, 2, 64])
        P.dma("sp", gT[:, 0, :], igfg[0:2].rearrange("h (c p) -> (h c) p", p=64), writes=["gT"])
        P.dma("sp", gT[:, 1, :], igfg[2:4].rearrange("h (c p) -> (h c) p", p=64), writes=["gT"])
        P.op("pe", lambda e: e.transpose(pt2[:], gT[:, 0, :], idf[:]), reads=["gT", "idf"], writes=["pt2"])
        P.op("dve", lambda e: e.tensor_copy(out=igs[:], in_=pt2[:]), reads=["pt2"], writes=["igs"])
        P.op("pe", lambda e: e.transpose(pt2[:], gT[:, 1, :], idf[:]), reads=["gT", "idf", "igs"], writes=["pt2"])
        P.op("dve", lambda e: e.tensor_copy(out=lf[:], in_=pt2[:]), reads=["pt2"], writes=["lf"])
    else:
        P.dma("sp", igs[:], ig_d, writes=["igs"])
        P.dma("sp", lf[:], fg_d, writes=["lf"])
    P.op("act", lambda e: e.activation(out=lf[:], in_=lf[:], func=AF.Exp, scale=-1.0), reads=["lf"], writes=["lf"])
    P.op("act", lambda e: e.activation(out=lf[:], in_=lf[:], func=AF.Ln, bias=ones[0:64, 0:1], scale=1.0), reads=["lf", "ones"], writes=["lf"])
    P.op("dve", lambda e: e.tensor_scalar(out=lf[:], in0=lf[:], scalar1=-1.0, scalar2=None, op0=ALU.mult), reads=["lf"], writes=["lf"])
    P.op("pe", lambda e: e.matmul(pa[:], lhsT=tri[:], rhs=lf[:], start=True, stop=True), reads=["tri", "lf"], writes=["pa"])
    P.op("pe", lambda e: e.matmul(paL[:], lhsT=ones[0:64, :], rhs=lf[:], start=True, stop=True), reads=["ones", "lf"], writes=["paL"])
    P.op("dve", lambda e: e.tensor_copy(out=a_sb[:], in_=pa[:]), reads=["pa"], writes=["a_sb"])
    P.op("dve", lambda e: e.tensor_copy(out=aL[:], in_=paL[:]), reads=["paL"], writes=["aL"])
    P.op("dve", lambda e: e.tensor_tensor(out=yv[:], in0=igs[:], in1=a_sb[:], op=ALU.subtract), reads=["igs", "a_sb"], writes=["yv"])
    P.op("act", lambda e: e.activation(out=T["u"][:], in_=yv[:], func=AF.Exp), reads=["yv"], writes=["T_u"])
    P.op("pe", lambda e: e.transpose(pt1[:], yv[:], idf[0:64, 0:64]), reads=["yv", "idf"], writes=["pt1"])
    P.op("dve", lambda e: e.tensor_copy(out=yT[:], in_=pt1[:]), reads=["pt1"], writes=["yT"])
    P.op("dve", lambda e: e.tensor_tensor_scan(out=MT[:], data0=yT[:], data1=yT[:], initial=-3.0e38, op0=ALU.max, op1=ALU.max),
         reads=["yT"], writes=["MT"])
    P.op("pe", lambda e: e.transpose(pt2[:], MT[:], idf[:]), reads=["MT", "idf"], writes=["pt2"])
    P.op("dve", lambda e: e.tensor_copy(out=M[:], in_=pt2[:]), reads=["pt2"], writes=["M"])
    P.op("dve", lambda e: e.tensor_scalar(out=Z[:], in0=ones[:], scalar1=MT[:, 63:64], scalar2=None, op0=ALU.mult), reads=["ones", "MT"], writes=["Z"])
    P.op("pe", lambda e: e.transpose(pt3[:], Z[:], idf[:]), reads=["Z", "idf"], writes=["pt3"])
    P.op("dve", lambda e: e.tensor_copy(out=M63[:], in_=pt3[:]), reads=["pt3"], writes=["M63"])
    for h in range(2):
        hs = slice(h * 64, (h + 1) * 64)
        P.op("dve", lambda e, hs=hs: e.tensor_tensor_scan(out=mB[:, hs], data0=M63[:, hs], data1=aL[:, hs], initial=0.0, op0=ALU.max, op1=ALU.add),
             reads=["M63", "aL"], writes=["mB%d" % h])
        P.op("dve", lambda e, h=h: e.memset(mC[:, h * 64:h * 64 + 1], 0.0), writes=["mC%d" % h])
        P.op("dve", lambda e, h=h: e.tensor_copy(out=mC[:, h * 64 + 1:(h + 1) * 64], in_=mB[:, h * 64:(h + 1) * 64 - 1]),
             reads=["mB%d" % h, "mC%d" % h], writes=["mC%d" % h])
    mk = ["mB0", "mB1", "mC0", "mC1"]
    P.op("dve", lambda e: e.tensor_tensor(out=mx[:], in0=mC[:], in1=M63[:], op=ALU.max), reads=mk + ["M63"], writes=["mx"])
    P.op("dve", lambda e: e.tensor_tensor(out=tmpg[:], in0=mC[:], in1=mx[:], op=ALU.subtract), reads=mk + ["mx"], writes=["tmpg"])
    P.op("act", lambda e: e.activation(out=decay[:], in_=tmpg[:], func=AF.Exp), reads=["tmpg"], writes=["T_decay"])
    P.op("act", lambda e: e.activation(out=ew[:], in_=mx[:], func=AF.Exp, scale=-1.0), reads=["mx"], writes=["T_ew"])
    P.op("dve", lambda e: e.tensor_tensor(out=tmpg[0:64, :], in0=mC[0:64, :], in1=M[:], op=ALU.max), reads=mk + ["M", "tmpg"], writes=["tmpg"])
    P.op("act", lambda e: e.activation(out=T["w"][:], in_=tmpg[0:64, :], func=AF.Exp, scale=-1.0), reads=["tmpg"], writes=["T_w"])
    P.op("dve", lambda e: e.tensor_tensor(out=yv[:], in0=mC[0:64, :], in1=tmpg[0:64, :], op=ALU.subtract), reads=mk + ["tmpg", "yv"], writes=["yv"])
    P.op("act", lambda e: e.activation(out=T["inter"][:], in_=yv[:], func=AF.Exp), reads=["yv"], writes=["T_inter"])
    P.op("act", lambda e: e.activation(out=a_sb[:], in_=a_sb[:], func=AF.Exp, scale=-1.0), reads=["a_sb"], writes=["a_sb"])
    P.op("dve", lambda e: e.tensor_tensor(out=T["ecl"][:], in0=a_sb[:], in1=T["w"][:], op=ALU.mult), reads=["a_sb", "T_w"], writes=["T_ecl"])
    P.op("dve", lambda e: e.tensor_tensor(out=T["uew"][:], in0=T["u"][:], in1=ew[0:64, :], op=ALU.mult), reads=["T_u", "T_ew"], writes=["T_uew"])
    barrier(P)
    st0.close()
    C.stack = main_stack

    qTb = C.sb("qTb", [128, S], BF16)
    kTb = C.sb("kTb", [128, S], BF16)
    ktok = C.sb("ktok", [64, NCH, 128], BF16)
    Va = C.sb("Va", [64, NCH, 129], BF16)
    Vp = C.sb("Vp", [64, NCH, 129], BF16)
    hraw = C.sb("hraw", [64, NCH, 129])
    osg = C.sb("osg", [64, NCH, 128])
    sqt = C.sb("sqt", [64, 16, 128])
    xpads = [C.sb("xpad%d" % i, [128, S + 3]) for i in range(2)]
    acc = C.sb("acc", [128, S])
    Cf = C.sb("Cf", [128, 129])
    Cbs = [C.sb("Cb%d" % i, [128, 129], BF16) for i in range(2)]
    tKV = C.sb("tKV", [128, 129])
    Gs = [C.sb("Gs%d" % i, [64, 64], BF16) for i in range(2)]
    den = C.sb("den", [64, NCH])
    ssq = C.sb("ssq", [64, NCH])
    pG_t = C.ps("pG", [64, 2, 64])
    pin_t = C.ps("pin", [64, 2, 129])
    pit_t = C.ps("pit", [64, 2, 129])
    pG = [pG_t[:, i, :] for i in range(2)]
    pin = [pin_t[:, i, :] for i in range(2)]
    pit = [pit_t[:, i, :] for i in range(2)]
    pKV = [C.ps("pKV%d" % i, [128, 129]) for i in range(2)]
    ptk = C.ps("ptk", [64, 4, 128], BF16)
    if fused:
        yTs = C.sb("yTs", [128, S], BF16)
        pty = C.ps("pty", [128, 512], BF16)
    for i in range(2):
        P.op("dve", lambda e, i=i: e.memset(xpads[i][:, 0:3], 0.0), writes=["xpad%d" % i])
    P.op("pool", lambda e: e.memset(Va[:, :, 128:129], 1.0), writes=["Va"])

    for h in range(2):
        for qk in range(2):
            xpad = xpads[qk]
            xkey = "xpad%d" % qk
            P.dma("sp", xpad[:, 3:], qkT[h, qk], writes=[xkey])
            wi = (h * 2 + qk) * 4
            P.op("dve", lambda e, wi=wi, h=h, qk=qk, xpad=xpad: e.tensor_scalar(out=acc[:], in0=xpad[:, 3:3 + S], scalar1=cw[:, wi + 3:wi + 4],
                                                                                scalar2=cb[:, h * 2 + qk:h * 2 + qk + 1], op0=ALU.mult, op1=ALU.add),
                 reads=[xkey, "cw", "cb"], writes=["acc"])
            for i in range(3):
                P.op("dve", lambda e, wi=wi, i=i, xpad=xpad: e.scalar_tensor_tensor(out=acc[:], in0=xpad[:, i:i + S], scalar=cw[:, wi + i:wi + i + 1],
                                                                                    in1=acc[:], op0=ALU.mult, op1=ALU.add),
                     reads=[xkey, "cw", "acc"], writes=["acc"])
            if qk == 0:
                P.op("act", lambda e: e.activation(out=acc[:], in_=acc[:], func=AF.Silu), reads=["acc"], writes=["acc"])
                P.op("dve", lambda e: e.tensor_scalar(out=qTb[:], in0=acc[:], scalar1=128.0 ** -0.5, scalar2=None, op0=ALU.mult),
                     reads=["acc"], writes=["qTb"])
            else:
                P.op("act", lambda e: e.activation(out=kTb[:], in_=acc[:], func=AF.Silu), reads=["acc"], writes=["kTb"])
        for c4 in range(NCH // 4):
            def trk(e, c4=c4):
                ins = None
                for j in range(4):
                    c = c4 * 4 + j
                    ins = e.transpose(ptk[:, j, :], kTb[:, c * 64:(c + 1) * 64], idb[:])
                return ins
            P.op("pe", trk, reads=["kTb", "idb"], writes=["ptk"])
            P.op("act", lambda e, c4=c4: e.copy(out=ktok[:, c4 * 4:(c4 + 1) * 4, :], in_=ptk[:]), reads=["ptk"], writes=["ktok"])
        P.dma("pool", Va[:, :, 0:128], vtok[h].rearrange("(c p) e -> p c e", p=64), writes=["Va"])
        P.dma("sp", osg[:], otok[h].rearrange("(c p) e -> p c e", p=64), writes=["osg"])
        P.op("act", lambda e: e.activation(out=osg[:], in_=osg[:], func=AF.Sigmoid), reads=["osg"], writes=["osg"])
        P.op("pool", lambda e, h=h: e.tensor_tensor(out=Vp[:], in0=Va[:], in1=T["uew"][:, h * 64:(h + 1) * 64].unsqueeze(2).to_broadcast([64, NCH, 129]), op=ALU.mult),
             reads=["Va", "T_uew"], writes=["Vp%d" % c for c in range(NCH)])
        P.op("dve", lambda e: e.memset(Cf[:], 0.0), writes=["Cf"])
        P.op("dve", lambda e: e.memset(Cbs[0][:], 0.0), writes=["Cb0"])
        P.op("dve", lambda e: e.memset(Cbs[1][:], 0.0), writes=["Cb1"])

        def emit_gkv(c):
            b = c % 2
            csl = slice(c * 64, (c + 1) * 64)
            P.op("pe", lambda e: e.matmul(pG[b], lhsT=kTb[:, csl], rhs=qTb[:, csl], start=True, stop=True),
                 reads=["kTb", "qTb"], writes=["pG%d" % b])
            P.op("pe", lambda e: e.matmul(pKV[b][:], lhsT=ktok[:, c, :], rhs=Vp[:, c, :], start=True, stop=True),
                 reads=["ktok", "Vp%d" % c], writes=["pKV%d" % b])

        emit_gkv(0)
        for c in range(NCH):
            b = c % 2
            col = h * 64 + c
            csl = slice(c * 64, (c + 1) * 64)
            if c + 1 < NCH:
                emit_gkv(c + 1)
            if c + 1 < NCH:
                nb_ = (c + 1) % 2
                P.op("dve", lambda e, b=b, col=col: e.scalar_tensor_tensor(out=Cf[:], in0=Cf[:], scalar=decay[:, col:col + 1], in1=pKV[b][:],
                                                                           op0=ALU.mult, op1=ALU.add), reads=["Cf", "T_decay", "pKV%d" % b], writes=["Cf"])
                P.op("act", lambda e, nb_=nb_: e.copy(out=Cbs[nb_][:], in_=Cf[:]), reads=["Cf"], writes=["Cb%d" % nb_])
            P.op("dve", lambda e, b=b, col=col: e.scalar_tensor_tensor(out=Gs[b][:], in0=pG[b], scalar=T["u"][:, col:col + 1], in1=tri[:],
                                                                       op0=ALU.mult, op1=ALU.mult),
                 reads=["pG%d" % b, "T_u", "tri"], writes=["Gs%d" % b])
            if c > 0:
                P.op("pe", lambda e, b=b, csl=csl: e.matmul(pin[b], lhsT=qTb[:, csl], rhs=Cbs[b][:], start=True, stop=True),
                     reads=["qTb", "Cb%d" % b], writes=["pin%d" % b])
            P.op("pe", lambda e, b=b, c=c: e.matmul(pit[b], lhsT=Gs[b][:], rhs=Va[:, c, :], start=True, stop=True),
                 reads=["Gs%d" % b, "Va"], writes=["pit%d" % b])
            if c > 0:
                P.op("act", lambda e, b=b, c=c, col=col: e.activation(out=hraw[:, c, :], in_=pin[b], func=AF.Copy, scale=T["inter"][:, col:col + 1]),
                     reads=["pin%d" % b, "T_inter", "hrawn"], writes=["hraw%d" % c])
                P.op("dve", lambda e, b=b, c=c, col=col: e.scalar_tensor_tensor(out=hraw[:, c, :], in0=pit[b], scalar=T["w"][:, col:col + 1],
                                                                                in1=hraw[:, c, :], op0=ALU.mult, op1=ALU.add),
                     reads=["pit%d" % b, "T_w", "hraw%d" % c], writes=["hraw%d" % c])
            else:
                P.op("dve", lambda e, b=b, c=c, col=col: e.tensor_scalar(out=hraw[:, c, :], in0=pit[b], scalar1=T["w"][:, col:col + 1], scalar2=None,
                                                                         op0=ALU.mult), reads=["pit%d" % b, "T_w", "hrawn"], writes=["hraw%d" % c])
        hk = ["hraw%d" % c for c in range(NCH)]
        hsl = slice(h * 64, (h + 1) * 64)
        P.op("act", lambda e: e.activation(out=den[:], in_=hraw[:, :, 128], func=AF.Abs), reads=hk, writes=["den"])
        P.op("dve", lambda e, hsl=hsl: e.tensor_tensor(out=den[:], in0=den[:], in1=T["ecl"][:, hsl], op=ALU.max), reads=["den", "T_ecl"], writes=["den"])
        P.op("dve", lambda e: e.reciprocal(out=den[:], in_=den[:]), reads=["den"], writes=["den"])
        P.op("dve", lambda e: e.tensor_tensor(out=hraw[:, :, 0:128], in0=hraw[:, :, 0:128], in1=den[:, :].unsqueeze(2).to_broadcast([64, NCH, 128]), op=ALU.mult),
             reads=hk + ["den"], writes=["hrawn"])
        P.op("dve", lambda e: e.tensor_tensor(out=osg[:], in0=osg[:], in1=hraw[:, :, 0:128], op=ALU.mult), reads=hk + ["hrawn", "osg"], writes=["osg"])
        for hf in range(4):
            P.op("pool", lambda e, hf=hf: e.tensor_tensor(out=sqt[:], in0=osg[:, hf * 16:(hf + 1) * 16, :], in1=osg[:, hf * 16:(hf + 1) * 16, :], op=ALU.mult),
                 reads=["osg"], writes=["sqt"])
            P.op("dve", lambda e, hf=hf: e.tensor_reduce(out=ssq[:, hf * 16:(hf + 1) * 16], in_=sqt[:], axis=AX.X, op=ALU.add), reads=["sqt"], writes=["ssq"])
        P.op("act", lambda e: e.activation(out=ssq[:], in_=ssq[:], func=AF.Sqrt, bias=C.eps_ap[0:64, :], scale=1.0 / 128), reads=["ssq", "eps"], writes=["ssq"])
        P.op("dve", lambda e: e.reciprocal(out=ssq[:], in_=ssq[:]), reads=["ssq"], writes=["ssq"])
        P.op("dve", lambda e: e.tensor_tensor(out=osg[:], in0=osg[:], in1=ssq[:, :].unsqueeze(2).to_broadcast([64, NCH, 128]), op=ALU.mult),
             reads=["osg", "ssq"], writes=["osg"])
        P.op("dve", lambda e, h=h: e.tensor_tensor(out=osg[:], in0=osg[:], in1=ng[:, h * 128:(h + 1) * 128].unsqueeze(1).to_broadcast([64, NCH, 128]), op=ALU.mult),
             reads=["osg", "ng"], writes=["osg"])
        if not fused:
            P.dma("sp", y_out[h].rearrange("(c p) e -> p c e", p=64), osg[:], reads=["osg"])
            continue
        P.op("act", lambda e: e.copy(out=Va[:, :, 0:128], in_=osg[:]), reads=["osg"], writes=["Va"])
        for c8 in range(NCH // 8):
            def trs(e, c8=c8):
                ins = None
                for j in range(8):
                    ins = e.transpose(pty[:, j * 64:(j + 1) * 64], Va[:, c8 * 8 + j, 0:128], idb[0:64, 0:64])
                return ins
            P.op("pe", trs, reads=["Va", "idb"], writes=["pty"])
            P.op("dve", lambda e, c8=c8: e.tensor_copy(out=yTs[:, c8 * 512:(c8 + 1) * 512], in_=pty[:]), reads=["pty"], writes=["yTs"])
        P.dma("sp", yT_ml[h * 128:(h + 1) * 128, :], yTs[:], reads=["yTs"], writes=["yT_d"])


DBG = {}
FMC = 1028
TMC = 652
PAIRS = [[0, 1], [2, 3], [4, 5], [6, 7]]


def emit_P1(C):
    P = C.P
    xgc = C.bind.get("xgc")
    if xgc is None:
        xg = C.inp("xg", [2 * D, 2048])
    mm_ = getattr(C, "modmgr", None)
    if mm_ is None:
        cT = C.inp("cT", [128, 8])
        adaw = C.inp("adaw", [D, 2048])
        adab = C.inp("adab", [128, 16])
    gam = C.inp("gam", [128, 8])
    wc = C.inp("wc", [D, FMC + TMC])
    bfm = C.inp("bfm", [128, 9])
    btm = C.inp("btm", [128, TMC])
    fm_d = C.bind["fm_d"]
    tm_d = C.bind["tm_d"]
    setup_consts(C)
    if mm_ is None:
        mod, mkey = emit_mod(C, cT, adaw, adab, 16, "m1")
    else:
        mod, mkey = mm_.need(C.layer, 0)
    gam_sb = C.sb("gam_sb", [128, 8])
    bfm_sb = C.sb("bfm_sb", [128, 9])
    btm_sb = C.sb("btm_sb", [128, TMC])
    scale = C.sb("scale", [128, 8])
    P.dma("sp", gam_sb[:], gam, writes=["gam"])
    P.dma("sp", bfm_sb[:], bfm, writes=["bfm"])
    P.dma("sp", btm_sb[:], btm, writes=["btm"])
    P.op("dve", lambda e: e.scalar_tensor_tensor(out=scale[:], in0=mod[:, 8:16], scalar=1.0, in1=gam_sb[:], op0=ALU.add, op1=ALU.mult),
         reads=[mkey, "gam"], writes=["scale"])
    NW = FMC + TMC
    wb = C.sb("wb", [128, 8, NW], BF16)
    for i in range(4):
        P.dma("pool", wb[:, :, i * 420:(i + 1) * 420], wc[:, i * 420:(i + 1) * 420].rearrange("(kc ki) m -> ki kc m", ki=128),
              writes=["wb%d" % i])
    wkeys = ["wb%d" % i for i in range(4)]
    xts = [C.sb("xt%d" % i, [128, 8, 512]) for i in range(2)]
    hTs = [C.sb("hT%d" % i, [128, 8, 512], BF16) for i in range(2)]
    tmpb = (C.sb("sq", [128, 8, 512], BF16), C.ps("ms", [128, 512]), C.sb("rstd", [128, 512]), C.sb("tmp", [128, 8, 512]))
    pps = [C.ps("pp%d" % i, [128, 512]) for i in range(4)]
    obs = [C.sb("ob%d" % i, [128, 512]) for i in range(4)]
    otm = [C.sb("otm%d" % i, [128, TMC]) for i in range(2)]
    k = 0
    for tb in range(8):
        half, cb = tb // 4, (tb % 4) * 512
        xt = xts[tb % 2]
        xk = "xt%d" % (tb % 2)
        if xgc is None:
            P.dma("sp", xt[:], xg[half * D:(half + 1) * D, cb:cb + 512].rearrange("(kc ki) t -> ki kc t", ki=128), writes=[xk])
        else:
            for i in range(4):
                P.dma("sp", xt[:, 2 * i:2 * i + 2, :], xgc[i][half * 256:(half + 1) * 256, cb:cb + 512].rearrange("(kc ki) t -> ki kc t", ki=128),
                      reads=["xgc%d" % i], writes=[xk])
        hT = hTs[tb % 2]
        hk_ = "hT%d" % (tb % 2)
        emit_norm_block(C, xt[:], xk, scale, mod, "scale", C.ones_bf, hT, hk_, tmpb, "n1")
        for m in range(9):
            mw = 128 if m < 8 else FMC - 1024
            pp, ob = pps[k % 4], obs[k % 4]
            kk = k % 4
            k += 1

            def mm(e, m=m, mw=mw, pp=pp, hT=hT):
                ins = None
                for kc in range(8):
                    ins = e.matmul(pp[:mw, :], lhsT=wb[:, kc, m * 128:m * 128 + mw], rhs=hT[:, kc, :], start=(kc == 0), stop=(kc == 7))
                return ins
            P.op("pe", mm, reads=[hk_] + wkeys, writes=["pp%d" % kk])
            P.op("act", lambda e, m=m, mw=mw, pp=pp, ob=ob: e.activation(out=ob[:mw, :], in_=pp[:mw, :], func=AF.Identity,
                                                                          bias=bfm_sb[:mw, m:m + 1], scale=1.0),
                 reads=["pp%d" % kk, "bfm"], writes=["ob%d" % kk])
            P.dma("act", fm_d[m * 128:m * 128 + mw, tb * 512:(tb + 1) * 512], ob[:mw, :], reads=["ob%d" % kk], writes=["fm_d"])
        for tt in range(4):
            ot = otm[tt % 2]
            for (c0, cw) in ((0, 512), (512, TMC - 512)):
                pp = pps[k % 4]
                kk = k % 4
                k += 1

                def mm(e, tt=tt, c0=c0, cw=cw, pp=pp, hT=hT):
                    ins = None
                    for kc in range(8):
                        ins = e.matmul(pp[:, :cw], lhsT=hT[:, kc, tt * 128:(tt + 1) * 128], rhs=wb[:, kc, FMC + c0:FMC + c0 + cw],
                                       start=(kc == 0), stop=(kc == 7))
                    return ins
                P.op("pe", mm, reads=[hk_] + wkeys, writes=["pp%d" % kk])
                P.op("dve", lambda e, c0=c0, cw=cw, pp=pp, ot=ot: e.tensor_tensor(out=ot[:, c0:c0 + cw], in0=pp[:, :cw], in1=btm_sb[:, c0:c0 + cw], op=ALU.add),
                     reads=["pp%d" % kk, "btm"], writes=["otm%d" % (tt % 2)])
            r0 = tb * 512 + tt * 128
            P.dma("pool", tm_d[r0:r0 + 128, :], ot[:], reads=["otm%d" % (tt % 2)], writes=["tm_d"])


def emit_P3b(C):
    P = C.P
    wo = C.inp("wo", [512, D])
    yT_d = C.bind["yT_d"]
    zp_g = C.bind["zp_g"]
    zs_g = C.bind["zs_g"]
    yT = C.sb("yT", [128, 4, S], BF16)
    wob = C.sb("wob", [128, 4, D], BF16)
    P.dma("pool", wob[:], wo.rearrange("(kc ki) m -> ki kc m", ki=128), writes=["wob"])
    for kc in range(4):
        P.dma("sp", yT[:, kc, :], yT_d[kc * 128:(kc + 1) * 128, :], reads=["yT_d"], writes=["yT%d" % kc])
    pps = [C.ps("pz%d" % i, [128, 512]) for i in range(4)]
    obs = [C.sb("oz%d" % i, [128, 512]) for i in range(4)]
    k = 0
    for gi in range(4):
        for m in (2 * gi, 2 * gi + 1):
            for tb in range(8):
                half, cb = tb // 4, (tb % 4) * 512
                pp, ob, kk = pps[k % 4], obs[k % 4], k % 4
                k += 1

                def mm(e, m=m, tb=tb, pp=pp):
                    ins = None
                    for kc in range(4):
                        ins = e.matmul(pp[:], lhsT=wob[:, kc, m * 128:(m + 1) * 128], rhs=yT[:, kc, tb * 512:(tb + 1) * 512], start=(kc == 0), stop=(kc == 3))
                    return ins
                P.op("pe", mm, reads=["wob"] + ["yT%d" % kc for kc in range(4)], writes=["pz%d" % kk])
                if k % 2:
                    P.op("act", lambda e, pp=pp, ob=ob: e.copy(out=ob[:], in_=pp[:]), reads=["pz%d" % kk], writes=["oz%d" % kk])
                else:
                    P.op("dve", lambda e, pp=pp, ob=ob: e.tensor_copy(out=ob[:], in_=pp[:]), reads=["pz%d" % kk], writes=["oz%d" % kk])
                r0 = half * 256 + (m % 2) * 128
                P.dma("sp", zp_g[gi].ap()[r0:r0 + 128, cb:cb + 512], ob[:], reads=["oz%d" % kk], writes=["zp%d" % gi])
        C.coll("ReduceScatter", ALU.add, PAIRS, zp_g[gi], zs_g[gi], reads=["zp%d" % gi], writes=["zs%d" % gi])


def build_fused(nlayers=2, dbg_dense=False):
    C = Ctx()
    nc = C.nc
    x_own = C.inp("x_own", [D, 2048])
    xg_in = C.inp("xg_in", [2 * D, 2048])
    out = C.outp("out", [D, 2048])
    fm_d = C.scratch("fm_d", [FMC, S]).ap()
    tm_d = C.scratch("tm_d", [S, TMC]).ap()
    yT_d = C.scratch("yT_d", [512, S], BF16).ap()
    zp_g = [C.scratch("zp_g%d" % i, [512, 2048]) for i in range(4)]
    zs_g = [C.scratch("zs_g%d" % i, [256, 2048]) for i in range(4)]
    xo_t = C.scratch("xo_d", [D, 2048])
    xoc_t = [C.scratch("xoc%d" % i, [256, 2048]) for i in range(4)]
    xgc_t = [C.scratch("xgc%d" % i, [512, 2048]) for i in range(4)]
    setup_consts(C)
    C.modmgr = ModMgr(C, nlayers)
    for l in range(nlayers):
        C.layer = l
        moe = (l % 2 == 1) and not dbg_dense
        last = (l == nlayers - 1)
        final = (l == 1)
        L = "L%d_" % l
        b1 = {"fm_d": fm_d, "tm_d": tm_d}
        if l == 0:
            b1["xg"] = xg_in
        else:
            b1["xgc"] = [t.ap() for t in xgc_t]
        with C.phase(L + "P1_", b1):
            emit_P1(C)
        with C.phase(L + "A2_", {"qT": fm_d[0:256].rearrange("(h d) t -> h d t", h=4),
                                 "kT": fm_d[256:448].rearrange("(b d) t -> b d t", b=3),
                                 "vcT": fm_d[448:512],
                                 "vtok": tm_d[:, 0:128].rearrange("t (b d) -> b t d", b=2),
                                 "gl": tm_d[:, 128:140],
                                 "yT_nsa": yT_d[0:256]}):
            emit_A2(C)
        with C.phase(L + "A3_", {"qkT": fm_d[512:1024].rearrange("(h q d) t -> h q d t", h=2, q=2),
                                 "igfg": fm_d[1024:1028],
                                 "vtok": tm_d[:, 140:396].rearrange("t (h e) -> h t e", h=2),
                                 "otok": tm_d[:, 396:652].rearrange("t (h e) -> h t e", h=2),
                                 "yT_ml": yT_d[256:512]}):
            emit_A3(C)
        with C.phase(L + "P3_", {"yT_d": yT_d, "zp_g": zp_g, "zs_g": zs_g}):
            emit_P3b(C)
        with C.phase(L + "A4_", {"xT": x_own if l == 0 else xo_t.ap(), "zT": [t.ap() for t in zs_g], "xoT": out if last else xo_t.ap()}):
            emit_A4(C, moe, final)
        if not last:
            for i in range(4):
                C.P.dma("pool", xoc_t[i].ap(), xo_t.ap()[i * 256:(i + 1) * 256, :], reads=["xo_d"], writes=["xoc%d" % i])
                C.coll("AllGather", ALU.bypass, PAIRS, xoc_t[i], xgc_t[i], reads=["xoc%d" % i], writes=["xgc%d" % i])
    if DBG.get("dump_y"):
        dy = C.outp("dbg_y", [512, S])
        C.P.dma("pool", dy, yT_d, reads=["yT_d"])
    if DBG.get("dump_mod"):
        dm = C.outp("dbg_mod", [128, 48 * nlayers])
        C.P.dma("sp", dm, C.modmgr.sealed[:], reads=["MODs%d_%d" % (l, p) for l in range(nlayers) for p in range(2)])
    return C.close()


def _chunkT(v, n):
    return np.ascontiguousarray(np.asarray(v, np.float32).reshape(n, 128).T)


def _a2_inputs(proj, g, inp, l, consts):
    m = {}
    q = proj[:, 0:512].reshape(S, 8, 64)[:, 4 * g:4 * g + 4]
    m["qT"] = np.ascontiguousarray(q.transpose(1, 2, 0))
    kv = proj[:, 512:1280].reshape(S, 6, 2, 64)[:, :, g]
    m["kT"] = np.ascontiguousarray(kv[:, [0, 2, 4]].transpose(1, 2, 0))
    m["vcT"] = np.ascontiguousarray(kv[:, 1].T)
    m["vtok"] = np.ascontiguousarray(kv[:, [3, 5]].transpose(1, 0, 2))
    m["gl"] = np.ascontiguousarray(proj[:, 1280:1304].reshape(S, 8, 3)[:, 4 * g:4 * g + 4].reshape(S, 12))
    w1 = inp["cmp_w1"][l]
    m["w1"] = np.ascontiguousarray(w1.reshape(2, 32, 64, 128).transpose(0, 2, 1, 3).reshape(2, 64, 32 * 128))
    m["w2"] = np.ascontiguousarray(inp["cmp_w2"][l])
    m["peT"] = np.ascontiguousarray(inp["cmp_pe"][l].transpose(0, 2, 1))
    m.update(consts[g])
    return m


def _a3_inputs(proj, hp, inp, l):
    m = {}
    hs = [2 * hp, 2 * hp + 1]
    qk = proj[:, 1304:2328]
    qkT = np.zeros((2, 2, 128, S), np.float32)
    convw = np.zeros((128, 2, 2, 4), np.float32)
    convb = np.zeros((128, 2, 2), np.float32)
    cwl = inp["conv_w"][l]
    cbl = inp["conv_b"][l]
    for i, h in enumerate(hs):
        for j in range(2):
            cols = slice(j * 512 + h * 128, j * 512 + h * 128 + 128)
            qkT[i, j] = qk[:, cols].T
            convw[:, i, j, :] = cwl[:, cols].T
            convb[:, i, j] = cbl[cols]
    m["qkT"] = qkT
    m["convw"] = convw.reshape(128, 16)
    m["convb"] = convb.reshape(128, 4)
    v = proj[:, 2328:2840]
    o = proj[:, 2840:3352]
    ip = proj[:, 3352:3356]
    fp = proj[:, 3356:3360]
    m["vtok"] = np.ascontiguousarray(np.stack([v[:, h * 128:(h + 1) * 128] for h in hs]))
    m["otok"] = np.ascontiguousarray(np.stack([o[:, h * 128:(h + 1) * 128] for h in hs]))
    m["ig"] = np.ascontiguousarray(np.concatenate([ip[:, h].reshape(64, 64).T for h in hs], axis=1))
    m["fg"] = np.ascontiguousarray(np.concatenate([fp[:, h].reshape(64, 64).T for h in hs], axis=1))
    g = inp["mlstm_norm_g"][l]
    m["normg"] = np.ascontiguousarray(np.broadcast_to(np.concatenate([g[h * 128:(h + 1) * 128] for h in hs])[None, :], (64, 256)))
    m["tri"] = np.triu(np.ones((64, 64), np.float32))
    m["ident"] = np.eye(128, dtype=np.float32)
    return m


_PROGS = {}


def _prog(name, fn):
    if name not in _PROGS:
        _PROGS[name] = fn()
    return _PROGS[name]


def _core_cols(g):
    hs = [2 * g, 2 * g + 1]
    r = np.arange
    fm = [g * 256 + r(256)]
    for br in (0, 2, 4, 1):
        fm.append(512 + br * 128 + g * 64 + r(64))
    for h in hs:
        fm.append(1304 + h * 128 + r(128))
        fm.append(1304 + 512 + h * 128 + r(128))
    fm.append(np.array([3352 + hs[0], 3352 + hs[1], 3356 + hs[0], 3356 + hs[1]]))
    tm = [512 + 3 * 128 + g * 64 + r(64), 512 + 5 * 128 + g * 64 + r(64), 1280 + 12 * g + r(12)]
    for h in hs:
        tm.append(2328 + h * 128 + r(128))
    for h in hs:
        tm.append(2840 + h * 128 + r(128))
    fm = np.concatenate(fm)
    tm = np.concatenate(tm)
    assert fm.size == FMC and tm.size == TMC
    return fm, tm


def _fused_inputs(inp, core, nlayers, consts, sel, ident, dbg_dense=False):
    b, g = core // 2, core % 2
    x = inp["x"]
    m = {}
    xb = x[b]
    m["x_own"] = np.ascontiguousarray(xb[g * 2048:(g + 1) * 2048].T)
    m["xg_in"] = np.ascontiguousarray(xb.reshape(2, 2048, D).transpose(0, 2, 1).reshape(2 * D, 2048))
    fm, tm = _core_cols(g)
    hs = [2 * g, 2 * g + 1]
    m["MOD_cT"] = _chunkT(inp["c"][b], 8)
    m["MOD_adab"] = np.ascontiguousarray(np.concatenate([_chunkT(inp["ada_b"][l], 48) for l in range(nlayers)], axis=1))
    for l in range(nlayers):
        m["MOD_adaw%d" % l] = inp["ada_w"][l]
    for l in range(nlayers):
        L = "L%d_" % l
        p = L + "P1_"
        m[p + "gam"] = _chunkT(inp["norm_mix_g"][l], 8)
        m[p + "wc"] = np.ascontiguousarray(inp["w_in"][l][:, np.concatenate([fm, tm])])
        bf = np.zeros(9 * 128, np.float32)
        bf[:FMC] = inp["b_in"][l][fm]
        m[p + "bfm"] = _chunkT(bf, 9)
        m[p + "btm"] = np.ascontiguousarray(np.broadcast_to(inp["b_in"][l][tm][None, :], (128, TMC)))
        p = L + "A2_"
        w1 = inp["cmp_w1"][l]
        m[p + "w1"] = np.ascontiguousarray(w1.reshape(2, 32, 64, 128).transpose(0, 2, 1, 3).reshape(2, 64, 32 * 128))
        m[p + "w2"] = np.ascontiguousarray(inp["cmp_w2"][l])
        m[p + "peT"] = np.ascontiguousarray(inp["cmp_pe"][l].transpose(0, 2, 1))
        for k, v in consts[g].items():
            m[p + k] = v
        p = L + "A3_"
        convw = np.zeros((128, 2, 2, 4), np.float32)
        convb = np.zeros((128, 2, 2), np.float32)
        for i, h in enumerate(hs):
            for j in range(2):
                cols = slice(j * 512 + h * 128, j * 512 + h * 128 + 128)
                convw[:, i, j, :] = inp["conv_w"][l][:, cols].T
                convb[:, i, j] = inp["conv_b"][l][cols]
        m[p + "convw"] = convw.reshape(128, 16)
        m[p + "convb"] = convb.reshape(128, 4)
        gn = inp["mlstm_norm_g"][l]
        m[p + "normg"] = np.ascontiguousarray(np.broadcast_to(np.concatenate([gn[h * 128:(h + 1) * 128] for h in hs])[None, :], (64, 256)))
        m[p + "tri"] = np.triu(np.ones((64, 64), np.float32))
        m[p + "ident"] = ident
        p = L + "P3_"
        rows = np.concatenate([g * 256 + np.arange(256), 512 + hs[0] * 128 + np.arange(128), 512 + hs[1] * 128 + np.arange(128)])
        m[p + "wo"] = np.ascontiguousarray(inp["w_out"][l][rows])
        p = L + "A4_"
        moe = (l % 2 == 1) and not dbg_dense
        m[p + "gam"] = _chunkT(inp["norm_ffn_g"][l], 8)
        if moe:
            m.update({p + "rw": inp["router_w"][l // 2], p + "sel": sel.reshape(8, 1024), p + "ident": ident,
                      p + "wg": inp["moe_w_gate"][l // 2], p + "wu": inp["moe_w_up"][l // 2], p + "wd": inp["moe_w_down"][l // 2]})
        else:
            m.update({p + "wg": inp["ffn_w_gate"][0:1], p + "wu": inp["ffn_w_up"][0:1], p + "wd": inp["ffn_w_down"][0:1]})
        if l == 1:
            m[p + "fgam"] = _chunkT(inp["final_norm_g"], 8)
    return m


def run_fused(inputs, nlayers=2, dbg_dense=False):
    inp = {k: np.asarray(v, np.float32) for k, v in inputs.items()}
    cores = list(range(8))
    consts = [nsa_consts(0), nsa_consts(1)]
    ident = np.eye(128, dtype=np.float32)
    sel = np.zeros((8, 8, 128), np.float32)
    for e in range(8):
        sel[e, e, :] = 1
    maps = [_fused_inputs(inp, core, nlayers, consts, sel, ident, dbg_dense) for core in cores]
    nc = _prog("fused%d_%d" % (nlayers, dbg_dense), lambda: build_fused(nlayers, dbg_dense))
    if DBG.get("trace"):
        rr = run_bass_kernel_spmd(nc, maps, core_ids=cores, trace=True)
        DBG["result"] = rr
        res = rr.results
    else:
        res = run_bass_kernel_spmd(nc, maps, core_ids=cores).results
    DBG["res"] = res
    out = np.stack([np.concatenate([res[2 * b]["out"], res[2 * b + 1]["out"]], axis=1).T for b in range(NB)])
    return np.ascontiguousarray(out.astype(np.float32))


def kernel(**inputs):
    return run_fused(inputs, 2)
```

```python
import contextlib
import numpy as np
import ml_dtypes
import concourse.bass as bass
import concourse.mybir as mybir
from concourse.bass_utils import run_bass_kernel_spmd

F32 = mybir.dt.float32
BF16 = mybir.dt.bfloat16
AF = mybir.ActivationFunctionType
ALU = mybir.AluOpType
AX = mybir.AxisListType

D = 1024
S = 4096
NB = 4
EPS = 1e-6
IN_COLS = 3360
NEG = -30000.0


class Prog:
    ENG = ("pe", "dve", "act", "pool", "sp")

    def __init__(self, nc, stack, n_dma_sems=8):
        self.nc = nc
        self.eng = {"pe": nc.tensor, "dve": nc.vector, "act": nc.scalar, "pool": nc.gpsimd, "sp": nc.sync}
        self.sem = {}
        self.count = {}
        for e in self.ENG:
            self.sem[e] = stack.enter_context(nc.semaphore("s_" + e))
            self.count[e] = 0
        self.dsem, self.dval, self.drr = {}, {}, {}
        for q in ("sp", "pool", "act"):
            self.dsem[q] = [stack.enter_context(nc.semaphore("d_%s%d" % (q, i))) for i in range(n_dma_sems)]
            self.dval[q] = [0] * n_dma_sems
            self.drr[q] = 0
        self.seen = {e: {} for e in self.ENG}
        self.snap = {}
        self.last_w = {}
        self.readers = {}
        self.n_wait = 0
        self.n_ops = 0
        self.csem = {}
        self.ctoks = []

    def _semobj(self, key):
        if isinstance(key, str):
            return self.sem[key]
        if key[0] == "c":
            return self.csem[key]
        return self.dsem[key[1]][key[2]]

    def _wait(self, e, tok):
        key, val = tok
        if self.seen[e].get(key, 0) >= val:
            return
        self.eng[e].wait_ge(self._semobj(key), val)
        self.n_wait += 1
        self.seen[e][key] = val
        sn = self.snap.get(tok)
        if sn:
            se = self.seen[e]
            for k, v in sn.items():
                if se.get(k, 0) < v:
                    se[k] = v

    def _deps(self, reads, writes):
        deps = []
        for r in reads:
            t = self.last_w.get(r)
            if t is not None:
                deps.append(t)
        for w in writes:
            t = self.last_w.get(w)
            if t is not None:
                deps.append(t)
            deps.extend(self.readers.get(w, ()))
        return deps

    def _commit(self, tok, reads, writes):
        for r in reads:
            lst = self.readers.setdefault(r, [])
            lst.append(tok)
            if len(lst) > 64:
                best = {}
                for k, v in lst:
                    if best.get(k, 0) < v:
                        best[k] = v
                lst[:] = list(best.items())
        for w in writes:
            self.last_w[w] = tok
            self.readers[w] = []

    def op(self, e, fn, reads=(), writes=()):
        for t in self._deps(reads, writes):
            self._wait(e, t)
        ins = fn(self.eng[e])
        self.count[e] += 1
        ins.then_inc(self.sem[e], 1)
        tok = (e, self.count[e])
        sn = dict(self.seen[e])
        sn[e] = self.count[e]
        self.snap[tok] = sn
        self._commit(tok, reads, writes)
        self.n_ops += 1
        return tok

    def dma(self, q, out, in_, reads=(), writes=(), **kw):
        for t in self._deps(reads, writes):
            self._wait(q, t)
        i = self.drr[q]
        self.drr[q] = (i + 1) % len(self.dsem[q])
        key = ("d", q, i)
        if self.dval[q][i] > 0:
            self._wait(q, (key, self.dval[q][i]))
        ins = self.eng[q].dma_start(out=out, in_=in_, **kw)
        self.dval[q][i] += 16
        ins.then_inc(self.dsem[q][i], 16)
        tok = (key, self.dval[q][i])
        self.snap[tok] = dict(self.seen[q])
        self._commit(tok, reads, writes)
        return tok

    def finish(self, e="sp"):
        for t in self.ctoks:
            self._wait(e, t)
        for q in self.dsem:
            for i, v in enumerate(self.dval[q]):
                if v:
                    self._wait(e, (("d", q, i), v))
        for o in self.ENG:
            if o != e and self.count[o]:
                self._wait(e, (o, self.count[o]))


class Ctx:
    def __init__(self, name="k"):
        self.nc = bass.Bass("TRN2", target_bir_lowering=False)
        self.stack = contextlib.ExitStack()
        self.root_stack = self.stack
        self.P = Prog(self.nc, self.stack)
        self.pfx = ""
        self.bind = {}
        self.ncoll = 0

    def inp(self, name, shape, dt=F32):
        if name in self.bind:
            ap = self.bind[name]
            assert list(ap.shape) == list(shape), (name, ap.shape, shape)
            return ap
        return self.nc.dram_tensor(self.pfx + name, list(shape), dt, kind="ExternalInput").ap()

    def outp(self, name, shape, dt=F32):
        if name in self.bind:
            ap = self.bind[name]
            assert list(ap.shape) == list(shape), (name, ap.shape, shape)
            return ap
        return self.nc.dram_tensor(self.pfx + name, list(shape), dt, kind="ExternalOutput").ap()

    def scratch(self, name, shape, dt=F32):
        return self.nc.dram_tensor(name, list(shape), dt)

    def sb(self, name, shape, dt=F32):
        return self.stack.enter_context(self.nc.sbuf_tensor(self.pfx + name, list(shape), dt))

    def ps(self, name, shape, dt=F32):
        return self.stack.enter_context(self.nc.psum_tensor(self.pfx + name, list(shape), dt))

    @contextlib.contextmanager
    def phase(self, pfx, bind=None):
        old = (self.stack, self.pfx, self.bind)
        st = contextlib.ExitStack()
        self.stack, self.pfx, self.bind = st, pfx, dict(bind or {})
        try:
            yield
        finally:
            barrier(self.P)
            st.close()
            self.stack, self.pfx, self.bind = old

    def coll(self, kind, op, groups, src, dst, reads, writes):
        P = self.P
        for t in P._deps(reads, writes):
            P._wait("pool", t)
        sem = self.root_stack.enter_context(self.nc.semaphore("cc%d" % self.ncoll))
        key = ("c", self.ncoll)
        self.ncoll += 1
        P.csem[key] = sem
        self.nc.gpsimd.collective_compute(kind, op, replica_groups=groups, ins=[src.ap().opt()], outs=[dst.ap().opt()]).then_inc(sem)
        tok = (key, 1)
        P.snap[tok] = dict(P.seen["pool"])
        P.ctoks.append(tok)
        P._commit(tok, reads, writes)
        return tok

    def close(self):
        self.P.finish("sp")
        self.root_stack.close()
        return self.nc


class ModMgr:
    def __init__(self, C, nlayers):
        self.C = C
        P = C.P
        rs = C.root_stack
        nc = C.nc
        self.cT = nc.dram_tensor("MOD_cT", [128, 8], F32, kind="ExternalInput").ap()
        self.adab = nc.dram_tensor("MOD_adab", [128, 48 * nlayers], F32, kind="ExternalInput").ap()
        self.adaw = [nc.dram_tensor("MOD_adaw%d" % l, [D, 6 * D], F32, kind="ExternalInput").ap() for l in range(nlayers)]
        self.c_sb = rs.enter_context(nc.sbuf_tensor("MOD_c", [128, 8], F32))
        self.b_sb = rs.enter_context(nc.sbuf_tensor("MOD_b", [128, 48 * nlayers], F32))
        self.modall = rs.enter_context(nc.sbuf_tensor("MOD_all", [128, 48 * nlayers], F32))
        self.sealed = rs.enter_context(nc.sbuf_tensor("MOD_sealed", [128, 48 * nlayers], F32))
        P.dma("sp", self.c_sb[:], self.cT, writes=["MODc"])
        P.dma("sp", self.b_sb[:], self.adab, writes=["MODb"])
        P.op("act", lambda e: e.activation(out=self.c_sb[:], in_=self.c_sb[:], func=AF.Silu), reads=["MODc"], writes=["MODc"])
        self.c_bf = rs.enter_context(nc.sbuf_tensor("MOD_cbf", [128, 8], BF16))
        P.op("dve", lambda e: e.tensor_copy(out=self.c_bf[:], in_=self.c_sb[:]), reads=["MODc"], writes=["MODcb"])
        self.units = [(l, ch) for l in range(nlayers) for ch in range(48)]
        self.next_dma = 0
        self.next_mm = 0
        self.wts = None
        self.psum = None
        self.tick = 0

    def attach(self, wts, psum_cols):
        self.wts = wts
        self.psum = psum_cols
        self.nslot = psum_cols.shape[1]
        self.base_dma = self.next_dma
        self.next_dma = self.next_mm
        for _ in range(len(wts) - 1):
            self._dma()

    def detach(self):
        self.wts = None
        self.psum = None

    def _dma(self):
        if self.next_dma >= len(self.units):
            return
        u = self.next_dma
        l, ch = self.units[u]
        wt = self.wts[u % len(self.wts)]
        self.C.P.dma("pool", wt[:], self.adaw[l][:, ch * 128:(ch + 1) * 128].rearrange("(kc ki) m -> ki kc m", ki=128),
                     writes=["MODw%d" % (u % len(self.wts))])
        self.next_dma += 1

    def unit(self):
        if self.next_mm >= len(self.units):
            return False
        P = self.C.P
        u = self.next_mm
        l, ch = self.units[u]
        self._dma()
        nb = len(self.wts)
        wt = self.wts[u % nb]
        col = l * 48 + ch
        ps = self.psum[:, u % self.nslot:u % self.nslot + 1]
        pk = "MODp%d" % (u % self.nslot)

        def mm(e):
            ins = None
            for kc in range(8):
                ins = e.matmul(ps, lhsT=wt[:, kc, :], rhs=self.c_bf[:, kc:kc + 1], start=(kc == 0), stop=(kc == 7), skip_group_check=True)
            return ins
        P.op("pe", mm, reads=["MODw%d" % (u % nb), "MODcb"], writes=[pk])
        P.op("dve", lambda e: e.tensor_tensor(out=self.modall[:, col:col + 1], in0=ps, in1=self.b_sb[:, col:col + 1], op=ALU.add),
             reads=[pk, "MODb"], writes=["MODall"])
        self.next_mm += 1
        if ch == 15 or ch == 47:
            c0 = l * 48 + (0 if ch == 15 else 16)
            c1 = l * 48 + ch + 1
            key = "MODs%d_%d" % (l, 0 if ch == 15 else 1)
            P.op("dve", lambda e: e.tensor_copy(out=self.sealed[:, c0:c1], in_=self.modall[:, c0:c1]), reads=["MODall"], writes=[key])
        return True

    def bg_tick(self, every=6):
        every = DBG.get("bg_every", every)
        if self.wts is None:
            return
        self.tick += 1
        if self.tick % every == 0:
            self.unit()

    def need(self, l, part):
        last = l * 48 + (15 if part == 0 else 47)
        if self.next_mm <= last:
            own = self.wts is None
            if own:
                C = self.C
                wts = [C.sb("MODfw%d_%d_%d" % (l, part, i), [128, 8, 128], BF16) for i in range(4)]
                pm = C.ps("MODfp%d_%d" % (l, part), [128, 8])
                self.attach(wts, pm[:, :])
            while self.next_mm <= last:
                self.unit()
            if own:
                self.detach()
        c0 = l * 48 + (0 if part == 0 else 16)
        c1 = l * 48 + (16 if part == 0 else 48)
        return self.sealed[:, c0:c1], "MODs%d_%d" % (l, part)


def emit_mod(C, cT, adaw, adab, nch, name):
    P = C.P
    c_sb = C.sb(name + "_c", [128, 8])
    b_sb = C.sb(name + "_b", [128, nch])
    mod = C.sb(name + "_mod", [128, nch])
    pm = C.ps(name + "_pm", [128, nch])
    wts = [C.sb(name + "_w%d" % i, [128, 8, 128]) for i in range(2)]
    P.dma("sp", c_sb[:], cT, writes=[name + "c"])
    P.dma("sp", b_sb[:], adab, writes=[name + "b"])
    P.op("act", lambda e: e.activation(out=c_sb[:], in_=c_sb[:], func=AF.Silu), reads=[name + "c"], writes=[name + "c"])
    for j in range(nch):
        wt = wts[j % 2]
        wk = name + "w%d" % (j % 2)
        P.dma("sp", wt[:], adaw[:, j * 128:(j + 1) * 128].rearrange("(kc ki) m -> ki kc m", ki=128), writes=[wk])

        def mm(e, wt=wt, j=j):
            ins = None
            for kc in range(8):
                ins = e.matmul(pm[:, j:j + 1], lhsT=wt[:, kc, :], rhs=c_sb[:, kc:kc + 1], start=(kc == 0), stop=(kc == 7))
            return ins
        P.op("pe", mm, reads=[wk, name + "c"], writes=[name + "pm"])
    P.op("dve", lambda e: e.tensor_tensor(out=mod[:], in0=pm[:], in1=b_sb[:], op=ALU.add),
         reads=[name + "pm", name + "b"], writes=[name + "mod"])
    return mod, name + "mod"


def emit_norm_block(C, xt, xkey, scale, shift, skey, ones_bf, hT, hkey, tmp_bufs, name, ntok=512, h32=None):
    P = C.P
    sq, ms, rstd, tmp = tmp_bufs
    P.op("act", lambda e: e.activation(out=sq[:, :, :ntok], in_=xt, func=AF.Square), reads=[xkey], writes=[name + "sq"])

    def mm(e):
        ins = None
        for kc in range(8):
            ins = e.matmul(ms[:, :ntok], lhsT=ones_bf[:], rhs=sq[:, kc, :ntok], start=(kc == 0), stop=(kc == 7))
        return ins
    P.op("pe", mm, reads=[name + "sq", "ones"], writes=[name + "ms"])
    P.op("act", lambda e: e.activation(out=rstd[:, :ntok], in_=ms[:, :ntok], func=AF.Sqrt, bias=C.eps_ap[:], scale=1.0),
         reads=[name + "ms", "eps"], writes=[name + "rstd"])
    P.op("dve", lambda e: e.reciprocal(out=rstd[:, :ntok], in_=rstd[:, :ntok]), reads=[name + "rstd"], writes=[name + "rstd"])
    for kc in range(8):
        P.op("dve", lambda e, kc=kc: e.scalar_tensor_tensor(out=tmp[:, kc, :ntok], in0=xt[:, kc, :], scalar=scale[:, kc:kc + 1],
                                                            in1=rstd[:, :ntok], op0=ALU.mult, op1=ALU.mult),
             reads=[xkey, skey, name + "rstd"], writes=[name + "tmp%d" % kc])
        if h32 is not None:
            P.op("act", lambda e, kc=kc: e.activation(out=h32[:, kc, :ntok], in_=tmp[:, kc, :ntok], func=AF.Identity,
                                                      bias=shift[:, kc:kc + 1], scale=1.0),
                 reads=[name + "tmp%d" % kc, skey], writes=[name + "h32_%d" % kc])
            P.op("pool", lambda e, kc=kc: e.tensor_copy(out=hT[:, kc, :ntok], in_=h32[:, kc, :ntok]),
                 reads=[name + "h32_%d" % kc], writes=[hkey])
        else:
            P.op("act", lambda e, kc=kc: e.activation(out=hT[:, kc, :ntok], in_=tmp[:, kc, :ntok], func=AF.Identity,
                                                      bias=shift[:, kc:kc + 1], scale=1.0),
                 reads=[name + "tmp%d" % kc, skey], writes=[hkey])


def setup_consts(C):
    P = C.P
    if hasattr(C, "ones_bf"):
        return
    C.ones_bf = C.root_stack.enter_context(C.nc.sbuf_tensor("ones_bf", [128, 128], BF16))
    C.eps_ap = C.root_stack.enter_context(C.nc.sbuf_tensor("eps_ap", [128, 1], F32))
    P.op("dve", lambda e: e.memset(C.ones_bf[:], 1.0 / D), writes=["ones"])
    P.op("dve", lambda e: e.memset(C.eps_ap[:], EPS), writes=["eps"])


NT1 = 2048


def build_A1():
    C = Ctx()
    P = C.P
    xT = C.inp("xT", [D, NT1])
    cT = C.inp("cT", [128, 8])
    adaw = C.inp("adaw", [D, 2048])
    adab = C.inp("adab", [128, 16])
    gam = C.inp("gam", [128, 8])
    w_in = C.inp("w_in", [D, IN_COLS])
    b_in = C.inp("b_in", [128, 27])
    projT = C.outp("projT", [27 * 128, NT1])
    setup_consts(C)
    mod, mkey = emit_mod(C, cT, adaw, adab, 16, "m1")
    gam_sb = C.sb("gam_sb", [128, 8])
    bin_sb = C.sb("bin_sb", [128, 27])
    scale = C.sb("scale", [128, 8])
    P.dma("sp", gam_sb[:], gam, writes=["gam"])
    P.dma("sp", bin_sb[:], b_in, writes=["bin"])
    P.op("dve", lambda e: e.scalar_tensor_tensor(out=scale[:], in0=mod[:, 8:16], scalar=1.0, in1=gam_sb[:], op0=ALU.add, op1=ALU.mult),
         reads=[mkey, "gam"], writes=["scale"])
    wb = C.sb("wb", [128, 8, IN_COLS], BF16)
    for i in range(7):
        P.dma("pool", wb[:, :, i * 480:(i + 1) * 480], w_in[:, i * 480:(i + 1) * 480].rearrange("(kc ki) m -> ki kc m", ki=128),
              writes=["wb%d" % i])
    wkeys = ["wb%d" % i for i in range(7)]
    xts = [C.sb("xt%d" % i, [128, 8, 512]) for i in range(2)]
    hT = C.sb("hT", [128, 8, 512], BF16)
    tmpb = (C.sb("sq", [128, 8, 512], BF16), C.ps("ms", [128, 512]), C.sb("rstd", [128, 512]), C.sb("tmp", [128, 8, 512]))
    pps = [C.ps("pp%d" % i, [128, 512]) for i in range(4)]
    obs = [C.sb("ob%d" % i, [128, 512]) for i in range(4)]
    if xT_chunks is None:
        xT3 = xT.rearrange("(kc ki) t -> ki kc t", ki=128)
    for tb in range(NT1 // 512):
        xt = xts[tb % 2]
        xk = "xt%d" % (tb % 2)
        P.dma("sp", xt[:], xT3[:, :, tb * 512:(tb + 1) * 512], writes=[xk])
        emit_norm_block(C, xt[:], xk, scale, mod, "scale", C.ones_bf, hT, "hT", tmpb, "n1")
        for m in range(27):
            mw = 128 if m < 26 else IN_COLS - 26 * 128
            pp = pps[m % 4]
            ob = obs[m % 4]

            def mm(e, m=m, mw=mw, pp=pp):
                ins = None
                for kc in range(8):
                    ins = e.matmul(pp[:mw, :], lhsT=wb[:, kc, m * 128:m * 128 + mw], rhs=hT[:, kc, :], start=(kc == 0), stop=(kc == 7))
                return ins
            P.op("pe", mm, reads=["hT"] + wkeys, writes=["pp%d" % (m % 4)])
            P.op("act", lambda e, m=m, mw=mw, pp=pp, ob=ob: e.activation(out=ob[:mw, :], in_=pp[:mw, :], func=AF.Identity,
                                                                          bias=bin_sb[:mw, m:m + 1], scale=1.0),
                 reads=["pp%d" % (m % 4), "bin"], writes=["ob%d" % (m % 4)])
            P.dma("pool", projT[m * 128:m * 128 + mw, tb * 512:(tb + 1) * 512], ob[:mw, :], reads=["ob%d" % (m % 4)])
    return C.close()


def barrier(P):
    toks = list(P.ctoks)
    for q in P.dsem:
        for i, v in enumerate(P.dval[q]):
            if v:
                toks.append((("d", q, i), v))
    for o in P.ENG:
        if P.count[o]:
            toks.append((o, P.count[o]))
    for e in P.ENG:
        for t in toks:
            if t[0] != e:
                P._wait(e, t)
    for e in ("pe", "dve", "act", "pool"):
        if P.count[e]:
            P._wait(e, (e, P.count[e]))


def build_A4(moe, final):
    C = Ctx()
    emit_A4(C, moe, final)
    return C.close()


def emit_A4(C, moe, final):
    P = C.P
    NT = 2048
    NBk = NT // 512
    fused = "zT" in C.bind
    xT_chunks = C.bind.get("xT_chunks")
    if xT_chunks is None:
        xT = C.inp("xT", [D, NT])
    xo_chunks = C.bind.get("xo_chunks")
    if fused:
        zT = C.bind["zT"]
    else:
        yT = C.inp("yT", [D, NT])
    mm_ = getattr(C, "modmgr", None)
    if mm_ is None:
        cT = C.inp("cT", [128, 8])
        adaw = C.inp("adaw", [D, 4096])
        adab = C.inp("adab", [128, 32])
    gam = C.inp("gam", [128, 8])
    if not fused:
        w_out = C.inp("w_out", [D, D])
    if moe:
        NE, FF = 8, 3584
        rw = C.inp("rw", [D, 8])
        sel = C.inp("sel", [8, 8 * 128])
        ident = C.inp("ident", [128, 128])
    else:
        NE, FF = 1, 2816
    wg = C.inp("wg", [NE, D, FF])
    wu = C.inp("wu", [NE, D, FF])
    wd = C.inp("wd", [NE, FF, D])
    if final:
        fgam = C.inp("fgam", [128, 8])
    if xo_chunks is None:
        xoT = C.outp("xoT", [D, NT])
    setup_consts(C)
    if mm_ is None:
        mod, mkey = emit_mod(C, cT, adaw, adab, 32, "m4")
    else:
        mod, mkey = mm_.need(C.layer, 1)
    gam_sb = C.sb("gam_sb", [128, 8])
    scale = C.sb("scale", [128, 8])
    P.dma("sp", gam_sb[:], gam, writes=["gam"])
    P.op("dve", lambda e: e.scalar_tensor_tensor(out=scale[:], in0=mod[:, 16:24], scalar=1.0, in1=gam_sb[:], op0=ALU.add, op1=ALU.mult),
         reads=[mkey, "gam"], writes=["scale"])
    if final:
        fg_sb = C.sb("fg_sb", [128, 8])
        P.dma("sp", fg_sb[:], fgam, writes=["fgam"])
    xs = C.sb("xs", [128, 8, NT])
    hT = C.sb("hT", [128, 8, NT], BF16)
    if xT_chunks is None:
        xT3 = xT.rearrange("(kc ki) t -> ki kc t", ki=128)
    if not fused:
        yT3 = yT.rearrange("(kc ki) t -> ki kc t", ki=128)
    if xo_chunks is None:
        xoT3 = xoT.rearrange("(kc ki) t -> ki kc t", ki=128)
    for tb in range(NBk):
        if xT_chunks is None:
            P.dma("sp", xs[:, :, tb * 512:(tb + 1) * 512], xT3[:, :, tb * 512:(tb + 1) * 512], reads=["xo_d"], writes=["xs%d" % tb])
        else:
            for i in range(4):
                P.dma("sp", xs[:, 2 * i:2 * i + 2, tb * 512:(tb + 1) * 512],
                      xT_chunks[i][:, tb * 512:(tb + 1) * 512].rearrange("(kc ki) t -> ki kc t", ki=128), reads=["xoc%d" % i], writes=["xs%d" % tb])
    if moe:
        wT = C.sb("wT", [8, NT], BF16)
    st1 = contextlib.ExitStack()
    main_stack = C.stack
    C.stack = st1
    if fused:
        ybs = [C.sb("zb%d" % i, [128, 8, 512]) for i in range(2)]
    else:
        wob = C.sb("wob", [128, 8, D], BF16)
        P.dma("pool", wob[:], w_out.rearrange("(kc ki) m -> ki kc m", ki=128), writes=["wob"])
        ybs = [C.sb("yb%d" % i, [128, 8, 512], BF16) for i in range(2)]
    tmpb = (C.sb("sq", [128, 8, 512], BF16), C.ps("ms", [128, 512]), C.sb("rstd", [128, 512]), C.sb("tmp", [128, 8, 512]))
    pzs = [C.ps("pz%d" % i, [128, 512]) for i in range(2)]
    if moe:
        h32 = C.sb("h32", [128, 8, 512])
        lgT = C.sb("lgT", [8, NT])
        rw_sb = C.sb("rw_sb", [128, 8, 8])
        P.dma("sp", rw_sb[:], rw.rearrange("(kc ki) e -> ki kc e", ki=128), writes=["rw"])
        plg = C.ps("plg", [8, 512])
    for tb in range(NBk):
        yb = ybs[tb % 2]
        yk = "yb%d" % (tb % 2)
        tsl = slice(tb * 512, (tb + 1) * 512)
        if fused:
            for gi in range(4):
                P.dma("sp", yb[:, 2 * gi:2 * gi + 2, :], zT[gi][:, tsl].rearrange("(kc ki) t -> ki kc t", ki=128), reads=["zs%d" % gi], writes=[yk])
        else:
            P.dma("pool", yb[:], yT3[:, :, tsl], writes=[yk])
        for m in range(8):
            if fused:
                P.op("dve", lambda e, m=m, yb=yb: e.scalar_tensor_tensor(out=xs[:, m, tsl], in0=yb[:, m, :], scalar=mod[:, m:m + 1],
                                                                         in1=xs[:, m, tsl], op0=ALU.mult, op1=ALU.add),
                     reads=[yk, mkey, "xs%d" % tb], writes=["xs%d" % tb])
                continue
            pz = pzs[m % 2]

            def mm(e, m=m, pz=pz, yb=yb):
                ins = None
                for kc in range(8):
                    ins = e.matmul(pz[:], lhsT=wob[:, kc, m * 128:(m + 1) * 128], rhs=yb[:, kc, :], start=(kc == 0), stop=(kc == 7))
                return ins
            P.op("pe", mm, reads=["wob", yk], writes=["pz%d" % (m % 2)])
            P.op("dve", lambda e, m=m, pz=pz: e.scalar_tensor_tensor(out=xs[:, m, tsl], in0=pz[:], scalar=mod[:, m:m + 1],
                                                                     in1=xs[:, m, tsl], op0=ALU.mult, op1=ALU.add),
                 reads=["pz%d" % (m % 2), mkey, "xs%d" % tb], writes=["xs%d" % tb])
        emit_norm_block(C, xs[:, :, tsl], "xs%d" % tb, scale, mod[:, 8:16], "scale", C.ones_bf, hT[:, :, tsl], "hT%d" % tb,
                        tmpb, "n4", h32=(h32 if moe else None))
        if moe:
            def mmr(e):
                ins = None
                for kc in range(8):
                    ins = e.matmul(plg[:], lhsT=rw_sb[:, kc, :], rhs=h32[:, kc, :], start=(kc == 0), stop=(kc == 7))
                return ins
            P.op("pe", mmr, reads=["rw"] + ["n4h32_%d" % kc for kc in range(8)], writes=["plg"])
            P.op("act", lambda e: e.copy(out=lgT[:, tsl], in_=plg[:]), reads=["plg"], writes=["lgT%d" % tb])
    if moe:
        id_sb = C.sb("id_sb", [128, 128])
        P.dma("sp", id_sb[:], ident, writes=["ident"])
        lg = C.sb("lg", [128, 16, 8])
        s8 = C.sb("s8", [128, 16, 8])
        e21 = C.sb("e21", [128, 16])
        w1 = C.sb("w1", [128, 16])
        w2 = C.sb("w2", [128, 16])
        m1 = C.sb("m1", [128, 16, 8])
        m2 = C.sb("m2", [128, 16, 8])
        ptr = C.ps("ptr", [128, 16, 8])

        def mmt(e):
            ins = None
            for tt in range(16):
                ins = e.transpose(ptr[:, tt, :], lgT[:, tt * 128:(tt + 1) * 128], id_sb[:8, :8])
            return ins
        P.op("pe", mmt, reads=["ident"] + ["lgT%d" % tb for tb in range(NBk)], writes=["ptr"])
        P.op("dve", lambda e: e.tensor_copy(out=lg[:], in_=ptr[:]), reads=["ptr"], writes=["lg"])
        for tt in range(16):
            P.op("dve", lambda e, tt=tt: e.max(out=s8[:, tt, :], in_=lg[:, tt, :]), reads=["lg"], writes=["s8_%d" % tt])
        s8k = ["s8_%d" % tt for tt in range(16)]
        P.op("dve", lambda e: e.tensor_tensor(out=e21[:], in0=s8[:, :, 1], in1=s8[:, :, 0], op=ALU.subtract), reads=s8k, writes=["e21"])
        P.op("act", lambda e: e.activation(out=e21[:], in_=e21[:], func=AF.Exp), reads=["e21"], writes=["e21"])
        P.op("dve", lambda e: e.tensor_scalar(out=w1[:], in0=e21[:], scalar1=1.0, scalar2=None, op0=ALU.add), reads=["e21"], writes=["w1"])
        P.op("dve", lambda e: e.reciprocal(out=w1[:], in_=w1[:]), reads=["w1"], writes=["w1"])
        P.op("dve", lambda e: e.tensor_tensor(out=w2[:], in0=e21[:], in1=w1[:], op=ALU.mult), reads=["e21", "w1"], writes=["w2"])
        for tt in range(16):
            P.op("dve", lambda e, tt=tt: e.tensor_scalar(out=m1[:, tt, :], in0=lg[:, tt, :], scalar1=s8[:, tt, 0:1], scalar2=w1[:, tt:tt + 1],
                                                         op0=ALU.is_equal, op1=ALU.mult), reads=["lg", "w1"] + s8k, writes=["m1_%d" % tt])
            P.op("dve", lambda e, tt=tt: e.tensor_scalar(out=m2[:, tt, :], in0=lg[:, tt, :], scalar1=s8[:, tt, 1:2], scalar2=w2[:, tt:tt + 1],
                                                         op0=ALU.is_equal, op1=ALU.mult), reads=["lg", "w2"] + s8k, writes=["m2_%d" % tt])
        P.op("dve", lambda e: e.tensor_tensor(out=m1[:], in0=m1[:], in1=m2[:], op=ALU.add),
             reads=["m1_%d" % tt for tt in range(16)] + ["m2_%d" % tt for tt in range(16)], writes=["wtok"])

        for tb in range(NBk):
            pz = pzs[tb % 2]

            def mmtb(e, tb=tb, pz=pz):
                ins = None
                for t4 in range(4):
                    tt = tb * 4 + t4
                    ins = e.transpose(pz[:8, t4 * 128:(t4 + 1) * 128], m1[:, tt, :], id_sb[:])
                return ins
            P.op("pe", mmtb, reads=["wtok", "ident"], writes=["pz%d" % (tb % 2)])
            P.op("dve", lambda e, tb=tb, pz=pz: e.tensor_copy(out=wT[:, tb * 512:(tb + 1) * 512], in_=pz[:8, :]),
                 reads=["pz%d" % (tb % 2)], writes=["wT"])
    barrier(P)
    st1.close()
    C.stack = main_stack
    st2 = contextlib.ExitStack()
    C.stack = st2
    nch = FF // 128
    groups = []
    f0 = 0
    while f0 < nch:
        nf = min(4, nch - f0)
        groups.append((f0, nf))
        f0 += nf
    gbs = [C.sb("gb%d" % i, [128, 8, 512], BF16) for i in range(2)]
    ubs = [C.sb("ub%d" % i, [128, 8, 512], BF16) for i in range(2)]
    dbs = [C.sb("db%d" % i, [128, 4, D], BF16) for i in range(2)]
    abs_ = [C.sb("ab%d" % i, [128, 4, NT], BF16) for i in range(2)]
    sgs = [C.sb("sg%d" % i, [128, 512]) for i in range(2)]
    pgs = [C.ps("pg%d" % i, [128, 512]) for i in range(2)]
    pus = [C.ps("pu%d" % i, [128, 512]) for i in range(2)]
    pds = [C.ps("pd%d" % i, [128, 512]) for i in range(2)]
    if moe:
        sel_sb = C.sb("sel_sb", [8, 8, 128], BF16)
        P.dma("pool", sel_sb[:], sel.rearrange("k (e m) -> k e m", e=8), writes=["sel"])
        wBs = [C.sb("wB%d" % i, [128, NT], BF16) for i in range(2)]
    hkeys = ["hT%d" % tb for tb in range(NBk)]
    work = [(ex, gi) for ex in range(NE) for gi in range(len(groups))]

    def emit_gu(idx):
        ex, gi = work[idx]
        f0, nf = groups[gi]
        bi = idx % 2
        gb, ub, ab = gbs[bi], ubs[bi], abs_[bi]
        cols = slice(f0 * 128, (f0 + nf) * 128)
        P.dma("pool", gb[:, :, :nf * 128], wg[ex, :, cols].rearrange("(kc ki) m -> ki kc m", ki=128), writes=["gb%d" % bi])
        P.dma("pool", ub[:, :, :nf * 128], wu[ex, :, cols].rearrange("(kc ki) m -> ki kc m", ki=128), writes=["ub%d" % bi])
        if moe and gi == 0:
            wB = wBs[ex % 2]
            for tb in range(NBk):
                pd = pds[tb % 2]
                P.op("pe", lambda e, tb=tb, pd=pd: e.matmul(pd[:], lhsT=sel_sb[:, ex, :], rhs=wT[:, tb * 512:(tb + 1) * 512], start=True, stop=True),
                     reads=["sel", "wT"], writes=["pd%d" % (tb % 2)])
                P.op("act", lambda e, tb=tb, pd=pd, wB=wB: e.copy(out=wB[:, tb * 512:(tb + 1) * 512], in_=pd[:]),
                     reads=["pd%d" % (tb % 2)], writes=["wB%d_%d" % (ex % 2, tb)])
        k = 0
        for fc in range(nf):
            for tb in range(NBk):
                pg, pu, sg = pgs[k % 2], pus[k % 2], sgs[k % 2]
                kk = k % 2
                tsl = slice(tb * 512, (tb + 1) * 512)

                def mm(e, fc=fc, tsl=tsl, pg=pg, pu=pu):
                    ins = None
                    for kc in range(8):
                        ins = e.matmul(pg[:], lhsT=gb[:, kc, fc * 128:(fc + 1) * 128], rhs=hT[:, kc, tsl], start=(kc == 0), stop=(kc == 7))
                    for kc in range(8):
                        ins = e.matmul(pu[:], lhsT=ub[:, kc, fc * 128:(fc + 1) * 128], rhs=hT[:, kc, tsl], start=(kc == 0), stop=(kc == 7))
                    return ins
                P.op("pe", mm, reads=["gb%d" % bi, "ub%d" % bi, "hT%d" % tb], writes=["pg%d" % kk, "pu%d" % kk])
                P.op("act", lambda e, pg=pg, sg=sg: e.activation(out=sg[:], in_=pg[:], func=AF.Silu), reads=["pg%d" % kk], writes=["sg%d" % kk])
                akey = "ab%d_%d_%d" % (bi, fc, tb)
                if moe:
                    P.op("dve", lambda e, sg=sg, pu=pu: e.tensor_tensor(out=sg[:], in0=sg[:], in1=pu[:], op=ALU.mult),
                         reads=["sg%d" % kk, "pu%d" % kk], writes=["sg%d" % kk])
                    P.op("dve", lambda e, sg=sg, fc=fc, tsl=tsl: e.tensor_tensor(out=ab[:, fc, tsl], in0=sg[:], in1=wBs[ex % 2][:, tsl], op=ALU.mult),
                         reads=["sg%d" % kk, "wB%d_%d" % (ex % 2, tb)], writes=[akey])
                else:
                    P.op("dve", lambda e, sg=sg, pu=pu, fc=fc, tsl=tsl: e.tensor_tensor(out=ab[:, fc, tsl], in0=sg[:], in1=pu[:], op=ALU.mult),
                         reads=["sg%d" % kk, "pu%d" % kk], writes=[akey])
                k += 1

    def emit_dn(idx):
        ex, gi = work[idx]
        f0, nf = groups[gi]
        bi = idx % 2
        db, ab = dbs[bi], abs_[bi]
        P.dma("pool", db[:, :nf, :], wd[ex, f0 * 128:(f0 + nf) * 128, :].rearrange("(fc fi) m -> fi fc m", fi=128), writes=["db%d" % bi])
        k = 0
        for m in range(8):
            for tb in range(NBk):
                pd = pds[k % 2]
                tsl = slice(tb * 512, (tb + 1) * 512)

                def mm(e, m=m, tsl=tsl, pd=pd):
                    ins = None
                    for fc in range(nf):
                        ins = e.matmul(pd[:], lhsT=db[:, fc, m * 128:(m + 1) * 128], rhs=ab[:, fc, tsl], start=(fc == 0), stop=(fc == nf - 1))
                    return ins
                P.op("pe", mm, reads=["db%d" % bi] + ["ab%d_%d_%d" % (bi, fc, tb) for fc in range(nf)], writes=["pd%d" % (k % 2)])
                P.op("dve", lambda e, m=m, tsl=tsl, pd=pd: e.scalar_tensor_tensor(out=xs[:, m, tsl], in0=pd[:], scalar=mod[:, 24 + m:25 + m],
                                                                                 in1=xs[:, m, tsl], op0=ALU.mult, op1=ALU.add),
                     reads=["pd%d" % (k % 2), mkey, "xs%d" % tb], writes=["xs%d" % tb])
                k += 1

    emit_gu(0)
    for i in range(len(work)):
        if i + 1 < len(work):
            emit_gu(i + 1)
        emit_dn(i)
    barrier(P)
    st2.close()
    C.stack = main_stack
    if final:
        tmpb = (C.sb("fsq", [128, 8, 512], BF16), C.ps("fms", [128, 512]), C.sb("frstd", [128, 512]), C.sb("ftmp", [128, 8, 512]))
        sq, ms, rstd, tmp = tmpb
        for tb in range(NBk):
            tsl = slice(tb * 512, (tb + 1) * 512)
            xk = "xs%d" % tb
            P.op("act", lambda e, tsl=tsl: e.activation(out=sq[:], in_=xs[:, :, tsl], func=AF.Square), reads=[xk], writes=["fsq"])

            def mm(e):
                ins = None
                for kc in range(8):
                    ins = e.matmul(ms[:], lhsT=C.ones_bf[:], rhs=sq[:, kc, :], start=(kc == 0), stop=(kc == 7))
                return ins
            P.op("pe", mm, reads=["fsq", "ones"], writes=["fms"])
            P.op("act", lambda e: e.activation(out=rstd[:], in_=ms[:], func=AF.Sqrt, bias=C.eps_ap[:], scale=1.0), reads=["fms", "eps"], writes=["frstd"])
            P.op("dve", lambda e: e.reciprocal(out=rstd[:], in_=rstd[:]), reads=["frstd"], writes=["frstd"])
            for kc in range(8):
                P.op("dve", lambda e, kc=kc, tsl=tsl: e.scalar_tensor_tensor(out=tmp[:, kc, :], in0=xs[:, kc, tsl], scalar=fg_sb[:, kc:kc + 1],
                                                                            in1=rstd[:], op0=ALU.mult, op1=ALU.mult),
                     reads=[xk, "fgam", "frstd"], writes=["ftmp"])
            P.dma("sp", xoT3[:, :, tsl], tmp[:], reads=["ftmp"], writes=["xo_d"])
    else:
        for tb in range(NBk):
            tsl = slice(tb * 512, (tb + 1) * 512)
            if xo_chunks is None:
                P.dma("sp", xoT3[:, :, tsl], xs[:, :, tsl], reads=["xs%d" % tb], writes=["xo_d"])
            else:
                for i in range(4):
                    P.dma("sp", xo_chunks[i][:, tsl].rearrange("(kc ki) t -> ki kc t", ki=128), xs[:, 2 * i:2 * i + 2, tsl],
                          reads=["xs%d" % tb], writes=["xoc%d" % i])


def nsa_consts(g):
    t = np.arange(S)
    ti, tl = t // 128, t % 128
    qaug = np.zeros((4, 4, S), np.float32)
    for hl in range(4):
        slope = 2.0 ** (-8.0 * (4 * g + hl + 1) / 8)
        qaug[hl, 0] = -8 * slope * 128 * ti
        qaug[hl, 1] = -8 * slope * tl
        qaug[hl, 2] = 8 * slope
        qaug[hl, 3] = 8 * slope
    kaug = np.stack([np.ones(S), np.ones(S), 128.0 * ti, 1.0 * tl]).astype(np.float32)
    n = np.arange(256)
    ce = 16 * n + 31
    kaugc = np.stack([np.ones(256), np.ones(256), 128.0 * (ce // 128), 1.0 * (ce % 128)]).astype(np.float32)
    kaugc[:, 255] = 0
    pl = np.arange(128)[:, None]
    ql = np.arange(512)[None, :]
    wmask = np.zeros((128, 8, 512), np.float32)
    for j in range(-4, 4):
        dist = ql - 128 * j - pl
        wmask[:, j + 4, :] = np.where((dist >= 0) & (dist < 512), 0.0, NEG)
    cmask = np.zeros((128, 4, 512), np.float32)
    for j in range(4):
        cmask[:, j, :] = np.where(ql - 128 * j - pl >= 0, 0.0, NEG)
    cmpmask = np.zeros((128, 2, 8, 512), np.float32)
    for c in range(2):
        nn = c * 128 + np.arange(128)[:, None]
        for Q in range(8):
            vis = (16 * nn + 31 <= 512 * Q + ql) & (nn < 255)
            cmpmask[:, c, Q, :] = np.where(vis, 0.0, NEG)
    E = np.zeros((64, 32, 128), np.float32)
    for c in range(32):
        E[2 * c, c, :64] = 1
        E[2 * c + 1, c, 64:] = 1
    lo_c = np.arange(256)[:, None] * 16
    lo_s = np.arange(64)[None, :] * 64
    ovm = np.clip(np.minimum(lo_c + 32, lo_s + 64) - np.maximum(lo_c, lo_s), 0, None) / 32.0
    ovm[255] = 0
    ov = np.ascontiguousarray(ovm.reshape(2, 128, 64).transpose(1, 0, 2)).astype(np.float32)
    cur = t // 64
    j = np.arange(64)[None, :]
    valid = j <= cur[:, None]
    forced = (j == 0) | (j == cur[:, None]) | (j == cur[:, None] - 1)
    valid01 = valid.astype(np.float32)
    addtab = np.where(valid, np.where(forced, 1e4, 0.0), -1.0).astype(np.float32)
    v01 = np.ascontiguousarray(valid01.reshape(32, 128, 64).transpose(1, 0, 2))
    adt = np.ascontiguousarray(addtab.reshape(32, 128, 64).transpose(1, 0, 2))
    bf = ml_dtypes.bfloat16
    return dict(qaug=qaug, kaug=kaug, kaugc=kaugc, wmask=wmask.reshape(128, -1).astype(bf), cmask=cmask.reshape(128, -1).astype(bf),
                cmpmask=cmpmask.reshape(128, -1).astype(bf), Emat=E.reshape(64, -1).astype(bf), ov=ov.reshape(128, -1), v01=v01.reshape(128, -1),
                adt=adt.reshape(128, -1), identb=np.eye(128, dtype=np.float32))


def build_A2():
    C = Ctx()
    emit_A2(C)
    return C.close()


def emit_A2(C):
    P = C.P
    qT = C.inp("qT", [4, 64, S])
    kT = C.inp("kT", [3, 64, S])
    vcT = C.inp("vcT", [64, S])
    vtok = C.inp("vtok", [2, S, 64])
    gl = C.inp("gl", [S, 12])
    w1 = C.inp("w1", [2, 64, 32 * 128])
    w2 = C.inp("w2", [2, 128, 64])
    peT = C.inp("peT", [2, 64, 32])
    qaug = C.inp("qaug", [4, 4, S])
    kaug = C.inp("kaug", [4, S])
    kaugc = C.inp("kaugc", [4, 256])
    wmask_d = C.inp("wmask", [128, 8 * 512], BF16)
    cmask_d = C.inp("cmask", [128, 4 * 512], BF16)
    cmpmask_d = C.inp("cmpmask", [128, 16 * 512], BF16)
    E_d = C.inp("Emat", [64, 32 * 128], BF16)
    ov_d = C.inp("ov", [128, 128])
    v01_d = C.inp("v01", [128, 32 * 64])
    adt_d = C.inp("adt", [128, 32 * 64])
    id_d = C.inp("identb", [128, 128])
    fused = "yT_nsa" in C.bind
    if fused:
        yT_nsa = C.bind["yT_nsa"]
    else:
        o_out = C.outp("o", [S, 256])

    qa = [C.sb("qa%d" % h, [68, S], BF16) for h in range(4)]
    ks = C.sb("ks", [68, S], BF16)
    kw = C.sb("kw", [68, S], BF16)
    kc = C.sb("kc", [68, 256], BF16)
    Vs = C.sb("Vs", [128, 32, 65], BF16)
    Vw = C.sb("Vw", [128, 32, 65], BF16)
    Vc = C.sb("Vc", [128, 2, 65], BF16)
    wmask = C.sb("wmask_s", [128, 8, 512], BF16)
    cmask = C.sb("cmask_s", [128, 4, 512], BF16)
    cmpmask = C.sb("cmpmask_s", [128, 16, 512], BF16)
    Em = C.sb("Em", [64, 32, 128], BF16)
    ov = C.sb("ov_s", [128, 2, 64], BF16)
    v01 = C.sb("v01_s", [128, 32, 64])
    adt = C.sb("adt_s", [128, 32, 64])
    idb = C.sb("idb", [128, 128], BF16)
    gates = C.sb("gates", [128, 32, 12])
    o_sb = C.sb("o_sb", [128, 32, 256])
    selbT = C.sb("selbT", [64, S], BF16)
    qq = "sp" if fused else "pool"
    for h in range(4):
        P.dma(qq, qa[h][0:64, :], qT[h], writes=["qa%d" % h])
        P.dma("pool", qa[h][64:68, :], qaug[h], writes=["qa%d" % h])
    P.dma(qq, ks[0:64, :], kT[1], writes=["ks"])
    P.dma("pool", ks[64:68, :], kaug, writes=["ks"])
    P.dma(qq, kw[0:64, :], kT[2], writes=["kw"])
    P.dma("pool", kw[64:68, :], kaug, writes=["kw"])
    P.op("dve", lambda e: e.memset(kc[:], 0.0), writes=["kc"])
    P.dma("pool", kc[64:68, :], kaugc, writes=["kc"])
    P.op("dve", lambda e: e.memset(Vs[:], 1.0), writes=["Vs"])
    P.op("dve", lambda e: e.memset(Vw[:], 1.0), writes=["Vw"])
    P.op("dve", lambda e: e.memset(Vc[:], 1.0), writes=["Vc"])
    P.dma("pool", Vs[:, :, 0:64], vtok[0].rearrange("(t p) d -> p t d", p=128), writes=["Vs"])
    P.dma("pool", Vw[:, :, 0:64], vtok[1].rearrange("(t p) d -> p t d", p=128), writes=["Vw"])
    P.dma("act", cmpmask[:], cmpmask_d.rearrange("p (a b) -> p a b", a=16), writes=["cmpmask"])
    P.dma("act", wmask[:], wmask_d.rearrange("p (a b) -> p a b", a=8), writes=["wmask"])
    P.dma("act", cmask[:], cmask_d.rearrange("p (a b) -> p a b", a=4), writes=["cmask"])
    P.dma("act", Em[:], E_d.rearrange("p (a b) -> p a b", a=32), writes=["Em"])
    P.dma("pool", ov[:], ov_d.rearrange("p (a b) -> p a b", a=2), writes=["ov"])
    P.dma("pool", idb[:], id_d, writes=["idb"])
    P.dma("sp", v01[:], v01_d.rearrange("p (a b) -> p a b", a=32), writes=["v01"])
    P.dma("sp", adt[:], adt_d.rearrange("p (a b) -> p a b", a=32), writes=["adt"])
    P.dma("sp", gates[:], gl.rearrange("(t p) c -> p t c", p=128), writes=["gates"])
    P.op("act", lambda e: e.activation(out=gates[:], in_=gates[:], func=AF.Sigmoid), reads=["gates"], writes=["gates"])
    P.op("pool", lambda e: e.memset(o_sb[:], 0.0), writes=["o_sb"])

    st1 = contextlib.ExitStack()
    main_stack = C.stack
    C.stack = st1
    for kv in range(2):
        src = C.sb("csrc%d" % kv, [64, S], BF16)
        w1b = C.sb("w1b%d" % kv, [64, 32, 128], BF16)
        w2b = C.sb("w2b%d" % kv, [128, 64], BF16)
        peb = C.sb("peb%d" % kv, [64, 32], BF16)
        hid = C.sb("hid%d" % kv, [128, 256], BF16)
        cv = C.sb("cv%d" % kv, [128, 1])
        ph = C.ps("ph%d" % kv, [128, 256])
        pc = C.ps("pc%d" % kv, [128, 1])
        po = C.ps("po%d" % kv, [128, 256])
        sk = "csrc%d" % kv
        P.dma("sp" if fused else "pool", src[:], kT[0] if kv == 0 else vcT, writes=[sk])
        P.dma("pool", w1b[:], w1[kv].rearrange("d (i j) -> d i j", i=32), writes=["w1b%d" % kv])
        P.dma("pool", w2b[:], w2[kv], writes=["w2b%d" % kv])
        P.dma("pool", peb[:], peT[kv], writes=["peb%d" % kv])

        def mmh(e, src=src, w1b=w1b, ph=ph):
            ins = None
            for i in range(32):
                ins = e.matmul(ph[:, 0:255], lhsT=w1b[:, i, :], rhs=src[:, i:i + 16 * 254 + 1:16], start=(i == 0), stop=(i == 31))
            return ins
        P.op("pe", mmh, reads=[sk, "w1b%d" % kv], writes=["ph%d" % kv])

        def mmc(e, w1b=w1b, peb=peb, pc=pc):
            ins = None
            for i in range(32):
                ins = e.matmul(pc[:], lhsT=w1b[:, i, :], rhs=peb[:, i:i + 1], start=(i == 0), stop=(i == 31))
            return ins
        P.op("pe", mmc, reads=["peb%d" % kv, "w1b%d" % kv], writes=["pc%d" % kv])
        P.op("dve", lambda e, cv=cv, pc=pc: e.tensor_copy(out=cv[:], in_=pc[:]), reads=["pc%d" % kv], writes=["cv%d" % kv])
        P.op("dve", lambda e, hid=hid: e.memset(hid[:], 0.0), writes=["hid%d" % kv])
        P.op("act", lambda e, hid=hid, ph=ph, cv=cv: e.activation(out=hid[:, 0:255], in_=ph[:, 0:255], func=AF.Silu, bias=cv[:], scale=1.0),
             reads=["ph%d" % kv, "cv%d" % kv, "hid%d" % kv], writes=["hid%d" % kv])
        if kv == 0:
            P.op("pe", lambda e, w2b=w2b, hid=hid, po=po: e.matmul(po[0:64, 0:255], lhsT=w2b[:], rhs=hid[:, 0:255], start=True, stop=True),
                 reads=["w2b0", "hid0"], writes=["po0"])
            P.op("dve", lambda e, po=po: e.tensor_copy(out=kc[0:64, 0:255], in_=po[0:64, 0:255]), reads=["po0", "kc"], writes=["kc"])
        else:
            def mmv(e, w2b=w2b, hid=hid, po=po):
                ins = None
                for c in range(2):
                    ins = e.matmul(po[:, c * 64:(c + 1) * 64], lhsT=hid[:, c * 128:(c + 1) * 128], rhs=w2b[:], start=True, stop=True)
                return ins
            P.op("pe", mmv, reads=["w2b1", "hid1"], writes=["po1"])
            P.op("dve", lambda e, po=po: e.tensor_copy(out=Vc[:, :, 0:64], in_=po[:, 0:128].rearrange("p (c d) -> p c d", c=2)),
                 reads=["po1", "Vc"], writes=["Vc"])
    barrier(P)
    st1.close()
    C.stack = main_stack

    Sb = [C.ps("S%d" % i, [128, 512]) for i in range(3)]
    PT = [C.sb("PT%d" % i, [128, 512], BF16) for i in range(3)]
    oacc_f = [C.ps("oacc%d" % i, [128, 512]) for i in range(2)]
    impacc_f = [C.ps("impacc%d" % i, [128, 512]) for i in range(2)]
    oacc = [t[:, 0:260].rearrange("p (a b) -> p a b", a=4) for t in oacc_f]
    impacc = [t[:, 0:256].rearrange("p (a b) -> p a b", a=4) for t in impacc_f]
    ptr = C.ps("ptrs", [128, 1024], BF16)
    imp_sb = C.sb("imp_sb", [128, 4, 64])
    imp2 = C.sb("imp2", [128, 4, 64])
    imp3 = C.sb("imp3", [128, 4, 64])
    s8a = C.sb("s8a", [128, 4, 8])
    s8b = C.sb("s8b", [128, 4, 8])
    selb = C.sb("selb", [128, 4, 64], BF16)
    dmx = C.sb("dmx", [128, 4])
    coef = C.sb("coef", [128, 4])
    state = {"k": 0, "pass": 0}
    mm_ = getattr(C, "modmgr", None)
    if mm_ is not None and mm_.next_mm < len(mm_.units) and not DBG.get("no_bg"):
        bgw = [C.sb("bgw%d" % i, [128, 8, 128], BF16) for i in range(6)]
        if DBG.get("bg_own_bank"):
            bgp = Sb.pop()
            mm_.attach(bgw, bgp[:, 0:8])
        else:
            mm_.attach(bgw, impacc_f[0][:, 256:264])
    else:
        mm_ = None

    def attn_pass(chunks, hl, Q, br, with_imp):
        pi = state["pass"] % 2
        state["pass"] += 1
        oa = oacc[pi]
        ia = impacc[pi]
        Qsl = slice(Q * 512, (Q + 1) * 512)
        n = len(chunks)
        bufidx = []

        def emit_pv(idx):
            bi = bufidx[idx]
            _, vr, vkey, ovr = chunks[idx]

            def pv(e):
                ins = None
                for qt in range(4):
                    ins = e.matmul(oa[:, qt, :], lhsT=PT[bi][:, qt * 128:(qt + 1) * 128], rhs=vr, start=(idx == 0 and qt == 0),
                                   stop=(idx == n - 1), skip_group_check=True)
                if with_imp:
                    for qt in range(4):
                        ins = e.matmul(ia[:, qt, :], lhsT=PT[bi][:, qt * 128:(qt + 1) * 128], rhs=ovr, start=(idx == 0 and qt == 0),
                                       stop=(idx == n - 1), skip_group_check=True)
                return ins
            wr = ["oacc%d" % pi] + (["impacc%d" % pi] if with_imp else [])
            P.op("pe", pv, reads=["PT%d" % bi, vkey, "ov"], writes=wr)

        for idx in range(n):
            bi = state["k"] % len(Sb)
            state["k"] += 1
            bufidx.append(bi)
            mms = chunks[idx][0]

            def smm(e, mms=mms, bi=bi):
                ins = None
                for mi, (lt, rh, _) in enumerate(mms):
                    ins = e.matmul(Sb[bi][:], lhsT=lt, rhs=rh, start=(mi == 0), stop=(mi == len(mms) - 1))
                return ins
            rd = []
            for (_, _, r) in mms:
                rd.extend(r)
            P.op("pe", smm, reads=rd, writes=["S%d" % bi])
            P.op("act", lambda e, bi=bi: e.activation(out=PT[bi][:], in_=Sb[bi][:], func=AF.Exp, scale=0.125),
                 reads=["S%d" % bi], writes=["PT%d" % bi])
            if idx >= 1:
                emit_pv(idx - 1)
            if mm_ is not None and not with_imp:
                mm_.bg_tick()
        emit_pv(n - 1)
        P.op("dve", lambda e: e.tensor_scalar(out=dmx[:], in0=oa[:, :, 64], scalar1=1e-30, scalar2=None, op0=ALU.max),
             reads=["oacc%d" % pi], writes=["dmx"])
        P.op("dve", lambda e: e.reciprocal(out=dmx[:], in_=dmx[:]), reads=["dmx"], writes=["dmx"])
        P.op("dve", lambda e: e.tensor_tensor(out=coef[:], in0=dmx[:], in1=gates[:, Q * 4:Q * 4 + 4, hl * 3 + br], op=ALU.mult),
             reads=["dmx", "gates"], writes=["coef"])
        for qt in range(4):
            osl = o_sb[:, Q * 4 + qt, hl * 64:(hl + 1) * 64]
            P.op("dve", lambda e, qt=qt, osl=osl: e.scalar_tensor_tensor(out=osl, in0=oa[:, qt, 0:64], scalar=coef[:, qt:qt + 1], in1=osl,
                                                                         op0=ALU.mult, op1=ALU.add),
                 reads=["oacc%d" % pi, "coef", "o_sb"], writes=["o_sb"])
            if with_imp:
                if hl == 0:
                    P.op("dve", lambda e, qt=qt: e.tensor_scalar(out=imp_sb[:, qt, :], in0=ia[:, qt, :], scalar1=dmx[:, qt:qt + 1], scalar2=None,
                                                                 op0=ALU.mult), reads=["impacc%d" % pi, "dmx"], writes=["imp_sb"])
                else:
                    P.op("dve", lambda e, qt=qt: e.scalar_tensor_tensor(out=imp_sb[:, qt, :], in0=ia[:, qt, :], scalar=dmx[:, qt:qt + 1],
                                                                        in1=imp_sb[:, qt, :], op0=ALU.mult, op1=ALU.add),
                         reads=["impacc%d" % pi, "dmx", "imp_sb"], writes=["imp_sb"])

    for Q in range(8):
        Qsl = slice(Q * 512, (Q + 1) * 512)
        ncc = 2 if Q >= 4 else 1
        for hl in range(4):
            chunks = []
            for c in range(ncc):
                mms = [(kc[:, c * 128:(c + 1) * 128], qa[hl][:, Qsl], ["kc", "qa%d" % hl]),
                       (idb[:], cmpmask[:, c * 8 + Q, :], ["idb", "cmpmask"])]
                chunks.append((mms, Vc[:, c, :], "Vc", ov[:, c, :]))
            attn_pass(chunks, hl, Q, 0, True)
        for hl in range(4):
            chunks = []
            for j in range(-4, 4):
                c = 4 * Q + j
                if c < 0:
                    continue
                mms = [(kw[:, c * 128:(c + 1) * 128], qa[hl][:, Qsl], ["kw", "qa%d" % hl]),
                       (idb[:], wmask[:, j + 4, :], ["idb", "wmask"])]
                chunks.append((mms, Vw[:, c, :], "Vw", None))
            attn_pass(chunks, hl, Q, 2, False)
        P.op("dve", lambda e: e.tensor_tensor(out=imp2[:], in0=imp_sb[:], in1=v01[:, Q * 4:Q * 4 + 4, :], op=ALU.mult),
             reads=["imp_sb", "v01"], writes=["imp2"])
        P.op("dve", lambda e: e.tensor_tensor(out=imp2[:], in0=imp2[:], in1=adt[:, Q * 4:Q * 4 + 4, :], op=ALU.add),
             reads=["imp2", "adt"], writes=["imp2"])
        for qt in range(4):
            P.op("dve", lambda e, qt=qt: e.max(out=s8a[:, qt, :], in_=imp2[:, qt, :]), reads=["imp2"], writes=["s8a%d" % qt])
            P.op("dve", lambda e, qt=qt: e.match_replace(out=imp3[:, qt, :], in_to_replace=s8a[:, qt, :], in_values=imp2[:, qt, :], imm_value=-3.0e38),
                 reads=["imp2", "s8a%d" % qt], writes=["imp3_%d" % qt])
            P.op("dve", lambda e, qt=qt: e.max(out=s8b[:, qt, :], in_=imp3[:, qt, :]), reads=["imp3_%d" % qt], writes=["s8b%d" % qt])
            P.op("dve", lambda e, qt=qt: e.tensor_scalar(out=imp3[:, qt, :], in0=imp2[:, qt, :], scalar1=s8b[:, qt, 7:8], scalar2=-NEG,
                                                         op0=ALU.is_ge, op1=ALU.mult),
                 reads=["imp2", "s8b%d" % qt, "imp3_%d" % qt], writes=["imp3_%d" % qt])
            P.op("dve", lambda e, qt=qt: e.tensor_scalar(out=selb[:, qt, :], in0=imp3[:, qt, :], scalar1=NEG, scalar2=None, op0=ALU.add),
                 reads=["imp3_%d" % qt], writes=["selb%d" % qt])

        def trs(e):
            ins = None
            for qt in range(4):
                ins = e.transpose(ptr[0:64, qt * 128:(qt + 1) * 128], selb[:, qt, :], idb[:])
            return ins
        P.op("pe", trs, reads=["selb%d" % qt for qt in range(4)] + ["idb"], writes=["ptrs"])
        P.op("dve", lambda e: e.tensor_copy(out=selbT[:, Qsl], in_=ptr[0:64, 0:512]), reads=["ptrs"], writes=["selbT%d" % Q])
        for hl in range(4):
            chunks = []
            for c in range(4 * Q + 4):
                mms = [(ks[:, c * 128:(c + 1) * 128], qa[hl][:, Qsl], ["ks", "qa%d" % hl]),
                       (Em[:, c, :], selbT[:, Qsl], ["Em", "selbT%d" % Q])]
                if c >= 4 * Q:
                    mms.append((idb[:], cmask[:, c - 4 * Q, :], ["idb", "cmask"]))
                chunks.append((mms, Vs[:, c, :], "Vs", None))
            attn_pass(chunks, hl, Q, 1, False)
    if mm_ is not None:
        while mm_.unit():
            pass
        mm_.detach()
    if not fused:
        P.dma("sp", o_out.rearrange("(t p) c -> p t c", p=128), o_sb[:], reads=["o_sb"])
        return
    o_bf = C.sb("o_bf", [128, 32, 256], BF16)
    P.op("act", lambda e: e.copy(out=o_bf[:], in_=o_sb[:]), reads=["o_sb"], writes=["o_bf"])
    yst = [C.sb("yst%d" % i, [128, 2, 512], BF16) for i in range(2)]
    for t4 in range(8):
        ys = yst[t4 % 2]
        for cc in range(2):
            def trs(e, t4=t4, cc=cc):
                ins = None
                for j in range(4):
                    ins = e.transpose(ptr[:, j * 128:(j + 1) * 128], o_bf[:, t4 * 4 + j, cc * 128:(cc + 1) * 128], idb[:])
                return ins
            P.op("pe", trs, reads=["o_bf", "idb"], writes=["ptrs"])
            P.op("dve", lambda e, ys=ys, cc=cc: e.tensor_copy(out=ys[:, cc, :], in_=ptr[:, 0:512]), reads=["ptrs"], writes=["yst%d" % (t4 % 2)])
        P.dma("sp", yT_nsa[:, t4 * 512:(t4 + 1) * 512].rearrange("(cc p) t -> p cc t", p=128), ys[:], reads=["yst%d" % (t4 % 2)], writes=["yT_d"])


def build_A3():
    C = Ctx()
    emit_A3(C)
    return C.close()


def emit_A3(C):
    P = C.P
    NCH = 64
    qkT = C.inp("qkT", [2, 2, 128, S])
    convw = C.inp("convw", [128, 16])
    convb = C.inp("convb", [128, 4])
    vtok = C.inp("vtok", [2, S, 128])
    otok = C.inp("otok", [2, S, 128])
    fused = "yT_ml" in C.bind
    if fused:
        yT_ml = C.bind["yT_ml"]
        igfg = C.bind["igfg"]
    else:
        ig_d = C.inp("ig", [64, 128])
        fg_d = C.inp("fg", [64, 128])
    normg = C.inp("normg", [64, 256])
    tri_d = C.inp("tri", [64, 64])
    id_d = C.inp("ident", [128, 128])
    if not fused:
        y_out = C.outp("y", [2, S, 128])
    setup_consts(C)

    cw = C.sb("cw", [128, 16])
    cb = C.sb("cb", [128, 4])
    tri = C.sb("tri_s", [64, 64])
    idf = C.sb("idf", [128, 128])
    idb = C.sb("idb", [128, 128], BF16)
    ng = C.sb("ng", [64, 256])
    ones = C.sb("ones_f", [128, 128])
    P.dma("sp", cw[:], convw, writes=["cw"])
    P.dma("sp", cb[:], convb, writes=["cb"])
    P.dma("sp", tri[:], tri_d, writes=["tri"])
    P.dma("sp", idf[:], id_d, writes=["idf"])
    P.dma("pool", idb[:], id_d, writes=["idb"])
    P.dma("sp", ng[:], normg, writes=["ng"])
    P.op("dve", lambda e: e.memset(ones[:], 1.0), writes=["onesf"])

    T = {}
    for nm in ("u", "w", "inter", "ecl", "uew"):
        T[nm] = C.sb("T_" + nm, [64, 128])
    decay = C.sb("T_decay", [128, 128])
    ew = C.sb("T_ew", [128, 128])
    st0 = contextlib.ExitStack()
    main_stack = C.stack
    C.stack = st0
    igs = C.sb("igs", [64, 128])
    lf = C.sb("lf", [64, 128])
    a_sb = C.sb("a_sb", [64, 128])
    yv = C.sb("yv", [64, 128])
    yT = C.sb("yT", [128, 64])
    MT = C.sb("MT", [128, 64])
    Z = C.sb("Z", [128, 128])
    M = C.sb("M", [64, 128])
    M63 = C.sb("M63", [128, 128])
    aL = C.sb("aL", [128, 128])
    mB = C.sb("mB", [128, 128])
    mC = C.sb("mC", [128, 128])
    mx = C.sb("mx", [128, 128])
    tmpg = C.sb("tmpg", [128, 128])
    pa = C.ps("pa", [64, 128])
    paL = C.ps("paL", [128, 128])
    pt1 = C.ps("pt1", [128, 64])
    pt2 = C.ps("pt2", [64, 128])
    pt3 = C.ps("pt3", [128, 128])
    if fused:
        gT = C.sb("gT", [128, 2, 64])
        P.dma("sp", gT[:, 0, :], igfg[0:2].rearrange("h (c p) -> (h c) p", p=64), writes=["gT"])
        P.dma("sp", gT[:, 1, :], igfg[2:4].rearrange("h (c p) -> (h c) p", p=64), writes=["gT"])
        P.op("pe", lambda e: e.transpose(pt2[:], gT[:, 0, :], idf[:]), reads=["gT", "idf"], writes=["pt2"])
        P.op("dve", lambda e: e.tensor_copy(out=igs[:], in_=pt2[:]), reads=["pt2"], writes=["igs"])
        P.op("pe", lambda e: e.transpose(pt2[:], gT[:, 1, :], idf[:]), reads=["gT", "idf", "igs"], writes=["pt2"])
        P.op("dve", lambda e: e.tensor_copy(out=lf[:], in_=pt2[:]), reads=["pt2"], writes=["lf"])
    else:
        P.dma("sp", igs[:], ig_d, writes=["igs"])
        P.dma("sp", lf[:], fg_d, writes=["lf"])
    P.op("act", lambda e: e.activation(out=lf[:], in_=lf[:], func=AF.Exp, scale=-1.0), reads=["lf"], writes=["lf"])
    P.op("act", lambda e: e.activation(out=lf[:], in_=lf[:], func=AF.Ln, bias=ones[0:64, 0:1], scale=1.0), reads=["lf", "ones"], writes=["lf"])
    P.op("dve", lambda e: e.tensor_scalar(out=lf[:], in0=lf[:], scalar1=-1.0, scalar2=None, op0=ALU.mult), reads=["lf"], writes=["lf"])
    P.op("pe", lambda e: e.matmul(pa[:], lhsT=tri[:], rhs=lf[:], start=True, stop=True), reads=["tri", "lf"], writes=["pa"])
    P.op("pe", lambda e: e.matmul(paL[:], lhsT=ones[0:64, :], rhs=lf[:], start=True, stop=True), reads=["ones", "lf"], writes=["paL"])
    P.op("dve", lambda e: e.tensor_copy(out=a_sb[:], in_=pa[:]), reads=["pa"], writes=["a_sb"])
    P.op("dve", lambda e: e.tensor_copy(out=aL[:], in_=paL[:]), reads=["paL"], writes=["aL"])
    P.op("dve", lambda e: e.tensor_tensor(out=yv[:], in0=igs[:], in1=a_sb[:], op=ALU.subtract), reads=["igs", "a_sb"], writes=["yv"])
    P.op("act", lambda e: e.activation(out=T["u"][:], in_=yv[:], func=AF.Exp), reads=["yv"], writes=["T_u"])
    P.op("pe", lambda e: e.transpose(pt1[:], yv[:], idf[0:64, 0:64]), reads=["yv", "idf"], writes=["pt1"])
    P.op("dve", lambda e: e.tensor_copy(out=yT[:], in_=pt1[:]), reads=["pt1"], writes=["yT"])
    P.op("dve", lambda e: e.tensor_tensor_scan(out=MT[:], data0=yT[:], data1=yT[:], initial=-3.0e38, op0=ALU.max, op1=ALU.max),
         reads=["yT"], writes=["MT"])
    P.op("pe", lambda e: e.transpose(pt2[:], MT[:], idf[:]), reads=["MT", "idf"], writes=["pt2"])
    P.op("dve", lambda e: e.tensor_copy(out=M[:], in_=pt2[:]), reads=["pt2"], writes=["M"])
    P.op("dve", lambda e: e.tensor_scalar(out=Z[:], in0=ones[:], scalar1=MT[:, 63:64], scalar2=None, op0=ALU.mult), reads=["ones", "MT"], writes=["Z"])
    P.op("pe", lambda e: e.transpose(pt3[:], Z[:], idf[:]), reads=["Z", "idf"], writes=["pt3"])
    P.op("dve", lambda e: e.tensor_copy(out=M63[:], in_=pt3[:]), reads=["pt3"], writes=["M63"])
    for h in range(2):
        hs = slice(h * 64, (h + 1) * 64)
        P.op("dve", lambda e, hs=hs: e.tensor_tensor_scan(out=mB[:, hs], data0=M63[:, hs], data1=aL[:, hs], initial=0.0, op0=ALU.max, op1=ALU.add),
             reads=["M63", "aL"], writes=["mB%d" % h])
        P.op("dve", lambda e, h=h: e.memset(mC[:, h * 64:h * 64 + 1], 0.0), writes=["mC%d" % h])
        P.op("dve", lambda e, h=h: e.tensor_copy(out=mC[:, h * 64 + 1:(h + 1) * 64], in_=mB[:, h * 64:(h + 1) * 64 - 1]),
             reads=["mB%d" % h, "mC%d" % h], writes=["mC%d" % h])
    mk = ["mB0", "mB1", "mC0", "mC1"]
    P.op("dve", lambda e: e.tensor_tensor(out=mx[:], in0=mC[:], in1=M63[:], op=ALU.max), reads=mk + ["M63"], writes=["mx"])
    P.op("dve", lambda e: e.tensor_tensor(out=tmpg[:], in0=mC[:], in1=mx[:], op=ALU.subtract), reads=mk + ["mx"], writes=["tmpg"])
    P.op("act", lambda e: e.activation(out=decay[:], in_=tmpg[:], func=AF.Exp), reads=["tmpg"], writes=["T_decay"])
    P.op("act", lambda e: e.activation(out=ew[:], in_=mx[:], func=AF.Exp, scale=-1.0), reads=["mx"], writes=["T_ew"])
    P.op("dve", lambda e: e.tensor_tensor(out=tmpg[0:64, :], in0=mC[0:64, :], in1=M[:], op=ALU.max), reads=mk + ["M", "tmpg"], writes=["tmpg"])
    P.op("act", lambda e: e.activation(out=T["w"][:], in_=tmpg[0:64, :], func=AF.Exp, scale=-1.0), reads=["tmpg"], writes=["T_w"])
    P.op("dve", lambda e: e.tensor_tensor(out=yv[:], in0=mC[0:64, :], in1=tmpg[0:64, :], op=ALU.subtract), reads=mk + ["tmpg", "yv"], writes=["yv"])
    P.op("act", lambda e: e.activation(out=T["inter"][:], in_=yv[:], func=AF.Exp), reads=["yv"], writes=["T_inter"])
    P.op("act", lambda e: e.activation(out=a_sb[:], in_=a_sb[:], func=AF.Exp, scale=-1.0), reads=["a_sb"], writes=["a_sb"])
    P.op("dve", lambda e: e.tensor_tensor(out=T["ecl"][:], in0=a_sb[:], in1=T["w"][:], op=ALU.mult), reads=["a_sb", "T_w"], writes=["T_ecl"])
    P.op("dve", lambda e: e.tensor_tensor(out=T["uew"][:], in0=T["u"][:], in1=ew[0:64, :], op=ALU.mult), reads=["T_u", "T_ew"], writes=["T_uew"])
    barrier(P)
    st0.close()
    C.stack = main_stack

    qTb = C.sb("qTb", [128, S], BF16)
    kTb = C.sb("kTb", [128, S], BF16)
    ktok = C.sb("ktok", [64, NCH, 128], BF16)
    Va = C.sb("Va", [64, NCH, 129], BF16)
    Vp = C.sb("Vp", [64, NCH, 129], BF16)
    hraw = C.sb("hraw", [64, NCH, 129])
    osg = C.sb("osg", [64, NCH, 128])
    sqt = C.sb("sqt", [64, 16, 128])
    xpads = [C.sb("xpad%d" % i, [128, S + 3]) for i in range(2)]
    acc = C.sb("acc", [128, S])
    Cf = C.sb("Cf", [128, 129])
    Cbs = [C.sb("Cb%d" % i, [128, 129], BF16) for i in range(2)]
    tKV = C.sb("tKV", [128, 129])
    Gs = [C.sb("Gs%d" % i, [64, 64], BF16) for i in range(2)]
    den = C.sb("den", [64, NCH])
    ssq = C.sb("ssq", [64, NCH])
    pG_t = C.ps("pG", [64, 2, 64])
    pin_t = C.ps("pin", [64, 2, 129])
    pit_t = C.ps("pit", [64, 2, 129])
    pG = [pG_t[:, i, :] for i in range(2)]
    pin = [pin_t[:, i, :] for i in range(2)]
    pit = [pit_t[:, i, :] for i in range(2)]
    pKV = [C.ps("pKV%d" % i, [128, 129]) for i in range(2)]
    ptk = C.ps("ptk", [64, 4, 128], BF16)
    if fused:
        yTs = C.sb("yTs", [128, S], BF16)
        pty = C.ps("pty", [128, 512], BF16)
    for i in range(2):
        P.op("dve", lambda e, i=i: e.memset(xpads[i][:, 0:3], 0.0), writes=["xpad%d" % i])
    P.op("pool", lambda e: e.memset(Va[:, :, 128:129], 1.0), writes=["Va"])

    for h in range(2):
        for qk in range(2):
            xpad = xpads[qk]
            xkey = "xpad%d" % qk
            P.dma("sp", xpad[:, 3:], qkT[h, qk], writes=[xkey])
            wi = (h * 2 + qk) * 4
            P.op("dve", lambda e, wi=wi, h=h, qk=qk, xpad=xpad: e.tensor_scalar(out=acc[:], in0=xpad[:, 3:3 + S], scalar1=cw[:, wi + 3:wi + 4],
                                                                                scalar2=cb[:, h * 2 + qk:h * 2 + qk + 1], op0=ALU.mult, op1=ALU.add),
                 reads=[xkey, "cw", "cb"], writes=["acc"])
            for i in range(3):
                P.op("dve", lambda e, wi=wi, i=i, xpad=xpad: e.scalar_tensor_tensor(out=acc[:], in0=xpad[:, i:i + S], scalar=cw[:, wi + i:wi + i + 1],
                                                                                    in1=acc[:], op0=ALU.mult, op1=ALU.add),
                     reads=[xkey, "cw", "acc"], writes=["acc"])
            if qk == 0:
                P.op("act", lambda e: e.activation(out=acc[:], in_=acc[:], func=AF.Silu), reads=["acc"], writes=["acc"])
                P.op("dve", lambda e: e.tensor_scalar(out=qTb[:], in0=acc[:], scalar1=128.0 ** -0.5, scalar2=None, op0=ALU.mult),
                     reads=["acc"], writes=["qTb"])
            else:
                P.op("act", lambda e: e.activation(out=kTb[:], in_=acc[:], func=AF.Silu), reads=["acc"], writes=["kTb"])
        for c4 in range(NCH // 4):
            def trk(e, c4=c4):
                ins = None
                for j in range(4):
                    c = c4 * 4 + j
                    ins = e.transpose(ptk[:, j, :], kTb[:, c * 64:(c + 1) * 64], idb[:])
                return ins
            P.op("pe", trk, reads=["kTb", "idb"], writes=["ptk"])
            P.op("act", lambda e, c4=c4: e.copy(out=ktok[:, c4 * 4:(c4 + 1) * 4, :], in_=ptk[:]), reads=["ptk"], writes=["ktok"])
        P.dma("pool", Va[:, :, 0:128], vtok[h].rearrange("(c p) e -> p c e", p=64), writes=["Va"])
        P.dma("sp", osg[:], otok[h].rearrange("(c p) e -> p c e", p=64), writes=["osg"])
        P.op("act", lambda e: e.activation(out=osg[:], in_=osg[:], func=AF.Sigmoid), reads=["osg"], writes=["osg"])
        P.op("pool", lambda e, h=h: e.tensor_tensor(out=Vp[:], in0=Va[:], in1=T["uew"][:, h * 64:(h + 1) * 64].unsqueeze(2).to_broadcast([64, NCH, 129]), op=ALU.mult),
             reads=["Va", "T_uew"], writes=["Vp%d" % c for c in range(NCH)])
        P.op("dve", lambda e: e.memset(Cf[:], 0.0), writes=["Cf"])
        P.op("dve", lambda e: e.memset(Cbs[0][:], 0.0), writes=["Cb0"])
        P.op("dve", lambda e: e.memset(Cbs[1][:], 0.0), writes=["Cb1"])

        def emit_gkv(c):
            b = c % 2
            csl = slice(c * 64, (c + 1) * 64)
            P.op("pe", lambda e: e.matmul(pG[b], lhsT=kTb[:, csl], rhs=qTb[:, csl], start=True, stop=True),
                 reads=["kTb", "qTb"], writes=["pG%d" % b])
            P.op("pe", lambda e: e.matmul(pKV[b][:], lhsT=ktok[:, c, :], rhs=Vp[:, c, :], start=True, stop=True),
                 reads=["ktok", "Vp%d" % c], writes=["pKV%d" % b])

        emit_gkv(0)
        for c in range(NCH):
            b = c % 2
            col = h * 64 + c
            csl = slice(c * 64, (c + 1) * 64)
            if c + 1 < NCH:
                emit_gkv(c + 1)
            if c + 1 < NCH:
                nb_ = (c + 1) % 2
                P.op("dve", lambda e, b=b, col=col: e.scalar_tensor_tensor(out=Cf[:], in0=Cf[:], scalar=decay[:, col:col + 1], in1=pKV[b][:],
                                                                           op0=ALU.mult, op1=ALU.add), reads=["Cf", "T_decay", "pKV%d" % b], writes=["Cf"])
                P.op("act", lambda e, nb_=nb_: e.copy(out=Cbs[nb_][:], in_=Cf[:]), reads=["Cf"], writes=["Cb%d" % nb_])
            P.op("dve", lambda e, b=b, col=col: e.scalar_tensor_tensor(out=Gs[b][:], in0=pG[b], scalar=T["u"][:, col:col + 1], in1=tri[:],
                                                                       op0=ALU.mult, op1=ALU.mult),
                 reads=["pG%d" % b, "T_u", "tri"], writes=["Gs%d" % b])
            if c > 0:
                P.op("pe", lambda e, b=b, csl=csl: e.matmul(pin[b], lhsT=qTb[:, csl], rhs=Cbs[b][:], start=True, stop=True),
                     reads=["qTb", "Cb%d" % b], writes=["pin%d" % b])
            P.op("pe", lambda e, b=b, c=c: e.matmul(pit[b], lhsT=Gs[b][:], rhs=Va[:, c, :], start=True, stop=True),
                 reads=["Gs%d" % b, "Va"], writes=["pit%d" % b])
            if c > 0:
                P.op("act", lambda e, b=b, c=c, col=col: e.activation(out=hraw[:, c, :], in_=pin[b], func=AF.Copy, scale=T["inter"][:, col:col + 1]),
                     reads=["pin%d" % b, "T_inter", "hrawn"], writes=["hraw%d" % c])
                P.op("dve", lambda e, b=b, c=c, col=col: e.scalar_tensor_tensor(out=hraw[:, c, :], in0=pit[b], scalar=T["w"][:, col:col + 1],
                                                                                in1=hraw[:, c, :], op0=ALU.mult, op1=ALU.add),
                     reads=["pit%d" % b, "T_w", "hraw%d" % c], writes=["hraw%d" % c])
            else:
                P.op("dve", lambda e, b=b, c=c, col=col: e.tensor_scalar(out=hraw[:, c, :], in0=pit[b], scalar1=T["w"][:, col:col + 1], scalar2=None,
                                                                         op0=ALU.mult), reads=["pit%d" % b, "T_w", "hrawn"], writes=["hraw%d" % c])
        hk = ["hraw%d" % c for c in range(NCH)]
        hsl = slice(h * 64, (h + 1) * 64)
        P.op("act", lambda e: e.activation(out=den[:], in_=hraw[:, :, 128], func=AF.Abs), reads=hk, writes=["den"])
        P.op("dve", lambda e, hsl=hsl: e.tensor_tensor(out=den[:], in0=den[:], in1=T["ecl"][:, hsl], op=ALU.max), reads=["den", "T_ecl"], writes=["den"])
        P.op("dve", lambda e: e.reciprocal(out=den[:], in_=den[:]), reads=["den"], writes=["den"])
        P.op("dve", lambda e: e.tensor_tensor(out=hraw[:, :, 0:128], in0=hraw[:, :, 0:128], in1=den[:, :].unsqueeze(2).to_broadcast([64, NCH, 128]), op=ALU.mult),
             reads=hk + ["den"], writes=["hrawn"])
        P.op("dve", lambda e: e.tensor_tensor(out=osg[:], in0=osg[:], in1=hraw[:, :, 0:128], op=ALU.mult), reads=hk + ["hrawn", "osg"], writes=["osg"])
        for hf in range(4):
            P.op("pool", lambda e, hf=hf: e.tensor_tensor(out=sqt[:], in0=osg[:, hf * 16:(hf + 1) * 16, :], in1=osg[:, hf * 16:(hf + 1) * 16, :], op=ALU.mult),
                 reads=["osg"], writes=["sqt"])
            P.op("dve", lambda e, hf=hf: e.tensor_reduce(out=ssq[:, hf * 16:(hf + 1) * 16], in_=sqt[:], axis=AX.X, op=ALU.add), reads=["sqt"], writes=["ssq"])
        P.op("act", lambda e: e.activation(out=ssq[:], in_=ssq[:], func=AF.Sqrt, bias=C.eps_ap[0:64, :], scale=1.0 / 128), reads=["ssq", "eps"], writes=["ssq"])
        P.op("dve", lambda e: e.reciprocal(out=ssq[:], in_=ssq[:]), reads=["ssq"], writes=["ssq"])
        P.op("dve", lambda e: e.tensor_tensor(out=osg[:], in0=osg[:], in1=ssq[:, :].unsqueeze(2).to_broadcast([64, NCH, 128]), op=ALU.mult),
             reads=["osg", "ssq"], writes=["osg"])
        P.op("dve", lambda e, h=h: e.tensor_tensor(out=osg[:], in0=osg[:], in1=ng[:, h * 128:(h + 1) * 128].unsqueeze(1).to_broadcast([64, NCH, 128]), op=ALU.mult),
             reads=["osg", "ng"], writes=["osg"])
        if not fused:
            P.dma("sp", y_out[h].rearrange("(c p) e -> p c e", p=64), osg[:], reads=["osg"])
            continue
        P.op("act", lambda e: e.copy(out=Va[:, :, 0:128], in_=osg[:]), reads=["osg"], writes=["Va"])
        for c8 in range(NCH // 8):
            def trs(e, c8=c8):
                ins = None
                for j in range(8):
                    ins = e.transpose(pty[:, j * 64:(j + 1) * 64], Va[:, c8 * 8 + j, 0:128], idb[0:64, 0:64])
                return ins
            P.op("pe", trs, reads=["Va", "idb"], writes=["pty"])
            P.op("dve", lambda e, c8=c8: e.tensor_copy(out=yTs[:, c8 * 512:(c8 + 1) * 512], in_=pty[:]), reads=["pty"], writes=["yTs"])
        P.dma("sp", yT_ml[h * 128:(h + 1) * 128, :], yTs[:], reads=["yTs"], writes=["yT_d"])


DBG = {}
FMC = 1028
TMC = 652
PAIRS = [[0, 1], [2, 3], [4, 5], [6, 7]]


def emit_P1(C):
    P = C.P
    xgc = C.bind.get("xgc")
    if xgc is None:
        xg = C.inp("xg", [2 * D, 2048])
    mm_ = getattr(C, "modmgr", None)
    if mm_ is None:
        cT = C.inp("cT", [128, 8])
        adaw = C.inp("adaw", [D, 2048])
        adab = C.inp("adab", [128, 16])
    gam = C.inp("gam", [128, 8])
    wc = C.inp("wc", [D, FMC + TMC])
    bfm = C.inp("bfm", [128, 9])
    btm = C.inp("btm", [128, TMC])
    fm_d = C.bind["fm_d"]
    fmA_d = C.bind["fmA_d"]
    tm_d = C.bind["tm_d"]
    setup_consts(C)
    if mm_ is None:
        mod, mkey = emit_mod(C, cT, adaw, adab, 16, "m1")
    else:
        mod, mkey = mm_.need(C.layer, 0)
    gam_sb = C.sb("gam_sb", [128, 8])
    bfm_sb = C.sb("bfm_sb", [128, 9])
    btm_sb = C.sb("btm_sb", [128, TMC])
    scale = C.sb("scale", [128, 8])
    P.dma("sp", gam_sb[:], gam, writes=["gam"])
    P.dma("sp", bfm_sb[:], bfm, writes=["bfm"])
    P.dma("sp", btm_sb[:], btm, writes=["btm"])
    P.op("dve", lambda e: e.scalar_tensor_tensor(out=scale[:], in0=mod[:, 8:16], scalar=1.0, in1=gam_sb[:], op0=ALU.add, op1=ALU.mult),
         reads=[mkey, "gam"], writes=["scale"])
    NW = FMC + TMC
    wb = C.sb("wb", [128, 8, NW], BF16)
    for i in range(4):
        P.dma("pool", wb[:, :, i * 420:(i + 1) * 420], wc[:, i * 420:(i + 1) * 420].rearrange("(kc ki) m -> ki kc m", ki=128),
              writes=["wb%d" % i])
    wkeys = ["wb%d" % i for i in range(4)]
    xts = [C.sb("xt%d" % i, [128, 8, 512]) for i in range(2)]
    hTs = [C.sb("hT%d" % i, [128, 8, 512], BF16) for i in range(2)]
    tmpb = (C.sb("sq", [128, 8, 512], BF16), C.ps("ms", [128, 512]), C.sb("rstd", [128, 512]), C.sb("tmp", [128, 8, 512]))
    pps = [C.ps("pp%d" % i, [128, 512]) for i in range(4)]
    obs = [C.sb("ob%d" % i, [128, 512]) for i in range(4)]
    obb = [C.sb("obb%d" % i, [128, 512], BF16) for i in range(4)]
    otm = [C.sb("otm%d" % i, [128, TMC]) for i in range(2)]
    k = 0
    for tb in range(8):
        half, cb = tb // 4, (tb % 4) * 512
        xt = xts[tb % 2]
        xk = "xt%d" % (tb % 2)
        if xgc is None:
            P.dma("sp", xt[:], xg[half * D:(half + 1) * D, cb:cb + 512].rearrange("(kc ki) t -> ki kc t", ki=128), writes=[xk])
        else:
            for i in range(4):
                P.dma("sp", xt[:, 2 * i:2 * i + 2, :], xgc[i][half * 256:(half + 1) * 256, cb:cb + 512].rearrange("(kc ki) t -> ki kc t", ki=128),
                      reads=["xgc%d" % i], writes=[xk])
        hT = hTs[tb % 2]
        hk_ = "hT%d" % (tb % 2)
        emit_norm_block(C, xt[:], xk, scale, mod, "scale", C.ones_bf, hT, hk_, tmpb, "n1")
        for m in range(9):
            mw = 128 if m < 8 else FMC - 1024
            pp, ob = pps[k % 4], obs[k % 4]
            kk = k % 4
            k += 1

            def mm(e, m=m, mw=mw, pp=pp, hT=hT):
                ins = None
                for kc in range(8):
                    ins = e.matmul(pp[:mw, :], lhsT=wb[:, kc, m * 128:m * 128 + mw], rhs=hT[:, kc, :], start=(kc == 0), stop=(kc == 7))
                return ins
            P.op("pe", mm, reads=[hk_] + wkeys, writes=["pp%d" % kk])
            if m < 4:
                ob = obb[kk]
                P.op("act", lambda e, m=m, pp=pp, ob=ob: e.activation(out=ob[:], in_=pp[:], func=AF.Identity, bias=bfm_sb[:, m:m + 1], scale=1.0),
                     reads=["pp%d" % kk, "bfm"], writes=["obb%d" % kk])
                P.dma("act", fmA_d[m * 128:(m + 1) * 128, tb * 512:(tb + 1) * 512], ob[:], reads=["obb%d" % kk], writes=["fm_d"])
                continue
            P.op("act", lambda e, m=m, mw=mw, pp=pp, ob=ob: e.activation(out=ob[:mw, :], in_=pp[:mw, :], func=AF.Identity,
                                                                          bias=bfm_sb[:mw, m:m + 1], scale=1.0),
                 reads=["pp%d" % kk, "bfm"], writes=["ob%d" % kk])
            P.dma("act", fm_d[m * 128:m * 128 + mw, tb * 512:(tb + 1) * 512], ob[:mw, :], reads=["ob%d" % kk], writes=["fm_d"])
        for tt in range(4):
            ot = otm[tt % 2]
            for (c0, cw) in ((0, 512), (512, TMC - 512)):
                pp = pps[k % 4]
                kk = k % 4
                k += 1

                def mm(e, tt=tt, c0=c0, cw=cw, pp=pp, hT=hT):
                    ins = None
                    for kc in range(8):
                        ins = e.matmul(pp[:, :cw], lhsT=hT[:, kc, tt * 128:(tt + 1) * 128], rhs=wb[:, kc, FMC + c0:FMC + c0 + cw],
                                       start=(kc == 0), stop=(kc == 7))
                    return ins
                P.op("pe", mm, reads=[hk_] + wkeys, writes=["pp%d" % kk])
                P.op("dve", lambda e, c0=c0, cw=cw, pp=pp, ot=ot: e.tensor_tensor(out=ot[:, c0:c0 + cw], in0=pp[:, :cw], in1=btm_sb[:, c0:c0 + cw], op=ALU.add),
                     reads=["pp%d" % kk, "btm"], writes=["otm%d" % (tt % 2)])
            r0 = tb * 512 + tt * 128
            P.dma("pool", tm_d[r0:r0 + 128, :], ot[:], reads=["otm%d" % (tt % 2)], writes=["tm_d"])


def emit_P3b(C):
    P = C.P
    wo = C.inp("wo", [512, D])
    yT_d = C.bind["yT_d"]
    zp_g = C.bind["zp_g"]
    zs_g = C.bind["zs_g"]
    yT = C.sb("yT", [128, 4, S], BF16)
    wob = C.sb("wob", [128, 4, D], BF16)
    P.dma("pool", wob[:], wo.rearrange("(kc ki) m -> ki kc m", ki=128), writes=["wob"])
    for kc in range(4):
        P.dma("sp", yT[:, kc, :], yT_d[kc * 128:(kc + 1) * 128, :], reads=["yT_d"], writes=["yT%d" % kc])
    pps = [C.ps("pz%d" % i, [128, 512]) for i in range(4)]
    obs = [C.sb("oz%d" % i, [128, 512]) for i in range(4)]
    k = 0
    for gi in range(4):
        for m in (2 * gi, 2 * gi + 1):
            for tb in range(8):
                half, cb = tb // 4, (tb % 4) * 512
                pp, ob, kk = pps[k % 4], obs[k % 4], k % 4
                k += 1

                def mm(e, m=m, tb=tb, pp=pp):
                    ins = None
                    for kc in range(4):
                        ins = e.matmul(pp[:], lhsT=wob[:, kc, m * 128:(m + 1) * 128], rhs=yT[:, kc, tb * 512:(tb + 1) * 512], start=(kc == 0), stop=(kc == 3))
                    return ins
                P.op("pe", mm, reads=["wob"] + ["yT%d" % kc for kc in range(4)], writes=["pz%d" % kk])
                if k % 2:
                    P.op("act", lambda e, pp=pp, ob=ob: e.copy(out=ob[:], in_=pp[:]), reads=["pz%d" % kk], writes=["oz%d" % kk])
                else:
                    P.op("dve", lambda e, pp=pp, ob=ob: e.tensor_copy(out=ob[:], in_=pp[:]), reads=["pz%d" % kk], writes=["oz%d" % kk])
                r0 = half * 256 + (m % 2) * 128
                P.dma("sp", zp_g[gi].ap()[r0:r0 + 128, cb:cb + 512], ob[:], reads=["oz%d" % kk], writes=["zp%d" % gi])
        C.coll("ReduceScatter", ALU.add, PAIRS, zp_g[gi], zs_g[gi], reads=["zp%d" % gi], writes=["zs%d" % gi])


def build_fused(nlayers=2, dbg_dense=False):
    C = Ctx()
    nc = C.nc
    x_own = C.inp("x_own", [D, 2048])
    xg_in = C.inp("xg_in", [2 * D, 2048])
    out = C.outp("out", [D, 2048])
    fm_d = C.scratch("fm_d", [FMC, S]).ap()
    fmA_d = C.scratch("fmA_d", [512, S], BF16).ap()
    tm_d = C.scratch("tm_d", [S, TMC]).ap()
    yT_d = C.scratch("yT_d", [512, S], BF16).ap()
    zp_g = [C.scratch("zp_g%d" % i, [512, 2048]) for i in range(4)]
    zs_g = [C.scratch("zs_g%d" % i, [256, 2048]) for i in range(4)]
    xo_t = C.scratch("xo_d", [D, 2048])
    xoc_t = [C.scratch("xoc%d" % i, [256, 2048]) for i in range(4)]
    xgc_t = [C.scratch("xgc%d" % i, [512, 2048]) for i in range(4)]
    setup_consts(C)
    C.modmgr = ModMgr(C, nlayers)
    for l in range(nlayers):
        C.layer = l
        moe = (l % 2 == 1) and not dbg_dense
        last = (l == nlayers - 1)
        final = (l == 1)
        L = "L%d_" % l
        b1 = {"fm_d": fm_d, "fmA_d": fmA_d, "tm_d": tm_d}
        if l == 0:
            b1["xg"] = xg_in
        else:
            b1["xgc"] = [t.ap() for t in xgc_t]
        with C.phase(L + "P1_", b1):
            emit_P1(C)
        with C.phase(L + "A2_", {"qT": fmA_d[0:256].rearrange("(h d) t -> h d t", h=4),
                                 "kT": fmA_d[256:448].rearrange("(b d) t -> b d t", b=3),
                                 "vcT": fmA_d[448:512],
                                 "vtok": tm_d[:, 0:128].rearrange("t (b d) -> b t d", b=2),
                                 "gl": tm_d[:, 128:140],
                                 "yT_nsa": yT_d[0:256]}):
            emit_A2(C)
        with C.phase(L + "A3_", {"qkT": fm_d[512:1024].rearrange("(h q d) t -> h q d t", h=2, q=2),
                                 "igfg": fm_d[1024:1028],
                                 "vtok": tm_d[:, 140:396].rearrange("t (h e) -> h t e", h=2),
                                 "otok": tm_d[:, 396:652].rearrange("t (h e) -> h t e", h=2),
                                 "yT_ml": yT_d[256:512]}):
            emit_A3(C)
        with C.phase(L + "P3_", {"yT_d": yT_d, "zp_g": zp_g, "zs_g": zs_g}):
            emit_P3b(C)
        b4 = {"zT": [t.ap() for t in zs_g]}
        if l == 0:
            b4["xT"] = x_own
        else:
            b4["xT_chunks"] = [t.ap() for t in xoc_t]
        if last:
            b4["xoT"] = out
        else:
            b4["xo_chunks"] = [t.ap() for t in xoc_t]
        with C.phase(L + "A4_", b4):
            emit_A4(C, moe, final)
        if not last:
            for i in range(4):
                C.coll("AllGather", ALU.bypass, PAIRS, xoc_t[i], xgc_t[i], reads=["xoc%d" % i], writes=["xgc%d" % i])
    if DBG.get("dump_y"):
        dy = C.outp("dbg_y", [512, S])
        C.P.dma("pool", dy, yT_d, reads=["yT_d"])
    if DBG.get("dump_mod"):
        dm = C.outp("dbg_mod", [128, 48 * nlayers])
        C.P.dma("sp", dm, C.modmgr.sealed[:], reads=["MODs%d_%d" % (l, p) for l in range(nlayers) for p in range(2)])
    return C.close()


def _chunkT(v, n):
    return np.ascontiguousarray(np.asarray(v, np.float32).reshape(n, 128).T)


def _a2_inputs(proj, g, inp, l, consts):
    m = {}
    q = proj[:, 0:512].reshape(S, 8, 64)[:, 4 * g:4 * g + 4]
    m["qT"] = np.ascontiguousarray(q.transpose(1, 2, 0))
    kv = proj[:, 512:1280].reshape(S, 6, 2, 64)[:, :, g]
    m["kT"] = np.ascontiguousarray(kv[:, [0, 2, 4]].transpose(1, 2, 0))
    m["vcT"] = np.ascontiguousarray(kv[:, 1].T)
    m["vtok"] = np.ascontiguousarray(kv[:, [3, 5]].transpose(1, 0, 2))
    m["gl"] = np.ascontiguousarray(proj[:, 1280:1304].reshape(S, 8, 3)[:, 4 * g:4 * g + 4].reshape(S, 12))
    w1 = inp["cmp_w1"][l]
    m["w1"] = np.ascontiguousarray(w1.reshape(2, 32, 64, 128).transpose(0, 2, 1, 3).reshape(2, 64, 32 * 128))
    m["w2"] = np.ascontiguousarray(inp["cmp_w2"][l])
    m["peT"] = np.ascontiguousarray(inp["cmp_pe"][l].transpose(0, 2, 1))
    m.update(consts[g])
    return m


def _a3_inputs(proj, hp, inp, l):
    m = {}
    hs = [2 * hp, 2 * hp + 1]
    qk = proj[:, 1304:2328]
    qkT = np.zeros((2, 2, 128, S), np.float32)
    convw = np.zeros((128, 2, 2, 4), np.float32)
    convb = np.zeros((128, 2, 2), np.float32)
    cwl = inp["conv_w"][l]
    cbl = inp["conv_b"][l]
    for i, h in enumerate(hs):
        for j in range(2):
            cols = slice(j * 512 + h * 128, j * 512 + h * 128 + 128)
            qkT[i, j] = qk[:, cols].T
            convw[:, i, j, :] = cwl[:, cols].T
            convb[:, i, j] = cbl[cols]
    m["qkT"] = qkT
    m["convw"] = convw.reshape(128, 16)
    m["convb"] = convb.reshape(128, 4)
    v = proj[:, 2328:2840]
    o = proj[:, 2840:3352]
    ip = proj[:, 3352:3356]
    fp = proj[:, 3356:3360]
    m["vtok"] = np.ascontiguousarray(np.stack([v[:, h * 128:(h + 1) * 128] for h in hs]))
    m["otok"] = np.ascontiguousarray(np.stack([o[:, h * 128:(h + 1) * 128] for h in hs]))
    m["ig"] = np.ascontiguousarray(np.concatenate([ip[:, h].reshape(64, 64).T for h in hs], axis=1))
    m["fg"] = np.ascontiguousarray(np.concatenate([fp[:, h].reshape(64, 64).T for h in hs], axis=1))
    g = inp["mlstm_norm_g"][l]
    m["normg"] = np.ascontiguousarray(np.broadcast_to(np.concatenate([g[h * 128:(h + 1) * 128] for h in hs])[None, :], (64, 256)))
    m["tri"] = np.triu(np.ones((64, 64), np.float32))
    m["ident"] = np.eye(128, dtype=np.float32)
    return m


_PROGS = {}


def _prog(name, fn):
    if name not in _PROGS:
        _PROGS[name] = fn()
    return _PROGS[name]


def _core_cols(g):
    hs = [2 * g, 2 * g + 1]
    r = np.arange
    fm = [g * 256 + r(256)]
    for br in (0, 2, 4, 1):
        fm.append(512 + br * 128 + g * 64 + r(64))
    for h in hs:
        fm.append(1304 + h * 128 + r(128))
        fm.append(1304 + 512 + h * 128 + r(128))
    fm.append(np.array([3352 + hs[0], 3352 + hs[1], 3356 + hs[0], 3356 + hs[1]]))
    tm = [512 + 3 * 128 + g * 64 + r(64), 512 + 5 * 128 + g * 64 + r(64), 1280 + 12 * g + r(12)]
    for h in hs:
        tm.append(2328 + h * 128 + r(128))
    for h in hs:
        tm.append(2840 + h * 128 + r(128))
    fm = np.concatenate(fm)
    tm = np.concatenate(tm)
    assert fm.size == FMC and tm.size == TMC
    return fm, tm


def _fused_inputs(inp, core, nlayers, consts, sel, ident, dbg_dense=False):
    b, g = core // 2, core % 2
    x = inp["x"]
    m = {}
    xb = x[b]
    m["x_own"] = np.ascontiguousarray(xb[g * 2048:(g + 1) * 2048].T)
    m["xg_in"] = np.ascontiguousarray(xb.reshape(2, 2048, D).transpose(0, 2, 1).reshape(2 * D, 2048))
    fm, tm = _core_cols(g)
    hs = [2 * g, 2 * g + 1]
    m["MOD_cT"] = _chunkT(inp["c"][b], 8)
    m["MOD_adab"] = np.ascontiguousarray(np.concatenate([_chunkT(inp["ada_b"][l], 48) for l in range(nlayers)], axis=1))
    for l in range(nlayers):
        m["MOD_adaw%d" % l] = inp["ada_w"][l]
    for l in range(nlayers):
        L = "L%d_" % l
        p = L + "P1_"
        m[p + "gam"] = _chunkT(inp["norm_mix_g"][l], 8)
        m[p + "wc"] = np.ascontiguousarray(inp["w_in"][l][:, np.concatenate([fm, tm])])
        bf = np.zeros(9 * 128, np.float32)
        bf[:FMC] = inp["b_in"][l][fm]
        m[p + "bfm"] = _chunkT(bf, 9)
        m[p + "btm"] = np.ascontiguousarray(np.broadcast_to(inp["b_in"][l][tm][None, :], (128, TMC)))
        p = L + "A2_"
        w1 = inp["cmp_w1"][l]
        m[p + "w1"] = np.ascontiguousarray(w1.reshape(2, 32, 64, 128).transpose(0, 2, 1, 3).reshape(2, 64, 32 * 128))
        m[p + "w2"] = np.ascontiguousarray(inp["cmp_w2"][l])
        m[p + "peT"] = np.ascontiguousarray(inp["cmp_pe"][l].transpose(0, 2, 1))
        for k, v in consts[g].items():
            m[p + k] = v
        p = L + "A3_"
        convw = np.zeros((128, 2, 2, 4), np.float32)
        convb = np.zeros((128, 2, 2), np.float32)
        for i, h in enumerate(hs):
            for j in range(2):
                cols = slice(j * 512 + h * 128, j * 512 + h * 128 + 128)
                convw[:, i, j, :] = inp["conv_w"][l][:, cols].T
                convb[:, i, j] = inp["conv_b"][l][cols]
        m[p + "convw"] = convw.reshape(128, 16)
        m[p + "convb"] = convb.reshape(128, 4)
        gn = inp["mlstm_norm_g"][l]
        m[p + "normg"] = np.ascontiguousarray(np.broadcast_to(np.concatenate([gn[h * 128:(h + 1) * 128] for h in hs])[None, :], (64, 256)))
        m[p + "tri"] = np.triu(np.ones((64, 64), np.float32))
        m[p + "ident"] = ident
        p = L + "P3_"
        rows = np.concatenate([g * 256 + np.arange(256), 512 + hs[0] * 128 + np.arange(128), 512 + hs[1] * 128 + np.arange(128)])
        m[p + "wo"] = np.ascontiguousarray(inp["w_out"][l][rows])
        p = L + "A4_"
        moe = (l % 2 == 1) and not dbg_dense
        m[p + "gam"] = _chunkT(inp["norm_ffn_g"][l], 8)
        if moe:
            m.update({p + "rw": inp["router_w"][l // 2], p + "sel": sel.reshape(8, 1024), p + "ident": ident,
                      p + "wg": inp["moe_w_gate"][l // 2], p + "wu": inp["moe_w_up"][l // 2], p + "wd": inp["moe_w_down"][l // 2]})
        else:
            m.update({p + "wg": inp["ffn_w_gate"][0:1], p + "wu": inp["ffn_w_up"][0:1], p + "wd": inp["ffn_w_down"][0:1]})
        if l == 1:
            m[p + "fgam"] = _chunkT(inp["final_norm_g"], 8)
    return m


def run_fused(inputs, nlayers=2, dbg_dense=False):
    inp = {k: np.asarray(v, np.float32) for k, v in inputs.items()}
    cores = list(range(8))
    consts = [nsa_consts(0), nsa_consts(1)]
    ident = np.eye(128, dtype=np.float32)
    sel = np.zeros((8, 8, 128), np.float32)
    for e in range(8):
        sel[e, e, :] = 1
    maps = [_fused_inputs(inp, core, nlayers, consts, sel, ident, dbg_dense) for core in cores]
    nc = _prog("fused%d_%d" % (nlayers, dbg_dense), lambda: build_fused(nlayers, dbg_dense))
    if DBG.get("trace"):
        rr = run_bass_kernel_spmd(nc, maps, core_ids=cores, trace=True)
        DBG["result"] = rr
        res = rr.results
    else:
        res = run_bass_kernel_spmd(nc, maps, core_ids=cores).results
    DBG["res"] = res
    out = np.stack([np.concatenate([res[2 * b]["out"], res[2 * b + 1]["out"]], axis=1).T for b in range(NB)])
    return np.ascontiguousarray(out.astype(np.float32))


def kernel(**inputs):
    return run_fused(inputs, 2)
```

```python
import contextlib
import numpy as np
import ml_dtypes
import concourse.bass as bass
import concourse.mybir as mybir
from concourse.bass_utils import run_bass_kernel_spmd

F32 = mybir.dt.float32
BF16 = mybir.dt.bfloat16
AF = mybir.ActivationFunctionType
ALU = mybir.AluOpType
AX = mybir.AxisListType

D = 1024
S = 4096
NB = 4
EPS = 1e-6
IN_COLS = 3360
NEG = -30000.0


class Prog:
    ENG = ("pe", "dve", "act", "pool", "sp")

    def __init__(self, nc, stack, n_dma_sems=8):
        self.nc = nc
        self.eng = {"pe": nc.tensor, "dve": nc.vector, "act": nc.scalar, "pool": nc.gpsimd, "sp": nc.sync}
        self.sem = {}
        self.count = {}
        for e in self.ENG:
            self.sem[e] = stack.enter_context(nc.semaphore("s_" + e))
            self.count[e] = 0
        self.dsem, self.dval, self.drr = {}, {}, {}
        for q in ("sp", "pool", "act"):
            self.dsem[q] = [stack.enter_context(nc.semaphore("d_%s%d" % (q, i))) for i in range(n_dma_sems)]
            self.dval[q] = [0] * n_dma_sems
            self.drr[q] = 0
        self.seen = {e: {} for e in self.ENG}
        self.snap = {}
        self.last_w = {}
        self.readers = {}
        self.n_wait = 0
        self.n_ops = 0
        self.csem = {}
        self.ctoks = []

    def _semobj(self, key):
        if isinstance(key, str):
            return self.sem[key]
        if key[0] == "c":
            return self.csem[key]
        return self.dsem[key[1]][key[2]]

    def _wait(self, e, tok):
        key, val = tok
        if self.seen[e].get(key, 0) >= val:
            return
        self.eng[e].wait_ge(self._semobj(key), val)
        self.n_wait += 1
        self.seen[e][key] = val
        sn = self.snap.get(tok)
        if sn:
            se = self.seen[e]
            for k, v in sn.items():
                if se.get(k, 0) < v:
                    se[k] = v

    def _deps(self, reads, writes):
        deps = []
        for r in reads:
            t = self.last_w.get(r)
            if t is not None:
                deps.append(t)
        for w in writes:
            t = self.last_w.get(w)
            if t is not None:
                deps.append(t)
            deps.extend(self.readers.get(w, ()))
        return deps

    def _commit(self, tok, reads, writes):
        for r in reads:
            lst = self.readers.setdefault(r, [])
            lst.append(tok)
            if len(lst) > 64:
                best = {}
                for k, v in lst:
                    if best.get(k, 0) < v:
                        best[k] = v
                lst[:] = list(best.items())
        for w in writes:
            self.last_w[w] = tok
            self.readers[w] = []

    def op(self, e, fn, reads=(), writes=()):
        for t in self._deps(reads, writes):
            self._wait(e, t)
        ins = fn(self.eng[e])
        self.count[e] += 1
        ins.then_inc(self.sem[e], 1)
        tok = (e, self.count[e])
        sn = dict(self.seen[e])
        sn[e] = self.count[e]
        self.snap[tok] = sn
        self._commit(tok, reads, writes)
        self.n_ops += 1
        return tok

    def dma(self, q, out, in_, reads=(), writes=(), **kw):
        for t in self._deps(reads, writes):
            self._wait(q, t)
        i = self.drr[q]
        self.drr[q] = (i + 1) % len(self.dsem[q])
        key = ("d", q, i)
        if self.dval[q][i] > 0:
            self._wait(q, (key, self.dval[q][i]))
        ins = self.eng[q].dma_start(out=out, in_=in_, **kw)
        self.dval[q][i] += 16
        ins.then_inc(self.dsem[q][i], 16)
        tok = (key, self.dval[q][i])
        self.snap[tok] = dict(self.seen[q])
        self._commit(tok, reads, writes)
        return tok

    def finish(self, e="sp"):
        for t in self.ctoks:
            self._wait(e, t)
        for q in self.dsem:
            for i, v in enumerate(self.dval[q]):
                if v:
                    self._wait(e, (("d", q, i), v))
        for o in self.ENG:
            if o != e and self.count[o]:
                self._wait(e, (o, self.count[o]))


class Ctx:
    def __init__(self, name="k"):
        self.nc = bass.Bass("TRN2", target_bir_lowering=False)
        self.stack = contextlib.ExitStack()
        self.root_stack = self.stack
        self.P = Prog(self.nc, self.stack)
        self.pfx = ""
        self.bind = {}
        self.ncoll = 0

    def inp(self, name, shape, dt=F32):
        if name in self.bind:
            ap = self.bind[name]
            assert list(ap.shape) == list(shape), (name, ap.shape, shape)
            return ap
        return self.nc.dram_tensor(self.pfx + name, list(shape), dt, kind="ExternalInput").ap()

    def outp(self, name, shape, dt=F32):
        if name in self.bind:
            ap = self.bind[name]
            assert list(ap.shape) == list(shape), (name, ap.shape, shape)
            return ap
        return self.nc.dram_tensor(self.pfx + name, list(shape), dt, kind="ExternalOutput").ap()

    def scratch(self, name, shape, dt=F32):
        return self.nc.dram_tensor(name, list(shape), dt)

    def sb(self, name, shape, dt=F32):
        return self.stack.enter_context(self.nc.sbuf_tensor(self.pfx + name, list(shape), dt))

    def ps(self, name, shape, dt=F32):
        return self.stack.enter_context(self.nc.psum_tensor(self.pfx + name, list(shape), dt))

    @contextlib.contextmanager
    def phase(self, pfx, bind=None):
        old = (self.stack, self.pfx, self.bind)
        st = contextlib.ExitStack()
        self.stack, self.pfx, self.bind = st, pfx, dict(bind or {})
        try:
            yield
        finally:
            barrier(self.P)
            st.close()
            self.stack, self.pfx, self.bind = old

    def coll(self, kind, op, groups, src, dst, reads, writes):
        P = self.P
        for t in P._deps(reads, writes):
            P._wait("pool", t)
        sem = self.root_stack.enter_context(self.nc.semaphore("cc%d" % self.ncoll))
        key = ("c", self.ncoll)
        self.ncoll += 1
        P.csem[key] = sem
        self.nc.gpsimd.collective_compute(kind, op, replica_groups=groups, ins=[src.ap().opt()], outs=[dst.ap().opt()]).then_inc(sem)
        tok = (key, 1)
        P.snap[tok] = dict(P.seen["pool"])
        P.ctoks.append(tok)
        P._commit(tok, reads, writes)
        return tok

    def close(self):
        self.P.finish("sp")
        self.root_stack.close()
        return self.nc


class ModMgr:
    def __init__(self, C, nlayers):
        self.C = C
        P = C.P
        rs = C.root_stack
        nc = C.nc
        self.cT = nc.dram_tensor("MOD_cT", [128, 8], F32, kind="ExternalInput").ap()
        self.adab = nc.dram_tensor("MOD_adab", [128, 48 * nlayers], F32, kind="ExternalInput").ap()
        self.adaw = [nc.dram_tensor("MOD_adaw%d" % l, [D, 6 * D], F32, kind="ExternalInput").ap() for l in range(nlayers)]
        self.c_sb = rs.enter_context(nc.sbuf_tensor("MOD_c", [128, 8], F32))
        self.b_sb = rs.enter_context(nc.sbuf_tensor("MOD_b", [128, 48 * nlayers], F32))
        self.modall = rs.enter_context(nc.sbuf_tensor("MOD_all", [128, 48 * nlayers], F32))
        self.sealed = rs.enter_context(nc.sbuf_tensor("MOD_sealed", [128, 48 * nlayers], F32))
        P.dma("sp", self.c_sb[:], self.cT, writes=["MODc"])
        P.dma("sp", self.b_sb[:], self.adab, writes=["MODb"])
        P.op("act", lambda e: e.activation(out=self.c_sb[:], in_=self.c_sb[:], func=AF.Silu), reads=["MODc"], writes=["MODc"])
        self.c_bf = rs.enter_context(nc.sbuf_tensor("MOD_cbf", [128, 8], BF16))
        P.op("dve", lambda e: e.tensor_copy(out=self.c_bf[:], in_=self.c_sb[:]), reads=["MODc"], writes=["MODcb"])
        self.units = [(l, ch) for l in range(nlayers) for ch in range(48)]
        self.next_dma = 0
        self.next_mm = 0
        self.wts = None
        self.psum = None
        self.tick = 0

    def attach(self, wts, psum_cols):
        self.wts = wts
        self.psum = psum_cols
        self.nslot = psum_cols.shape[1]
        self.base_dma = self.next_dma
        self.next_dma = self.next_mm
        for _ in range(len(wts) - 1):
            self._dma()

    def detach(self):
        self.wts = None
        self.psum = None

    def _dma(self):
        if self.next_dma >= len(self.units):
            return
        u = self.next_dma
        l, ch = self.units[u]
        wt = self.wts[u % len(self.wts)]
        self.C.P.dma("pool", wt[:], self.adaw[l][:, ch * 128:(ch + 1) * 128].rearrange("(kc ki) m -> ki kc m", ki=128),
                     writes=["MODw%d" % (u % len(self.wts))])
        self.next_dma += 1

    def unit(self):
        if self.next_mm >= len(self.units):
            return False
        P = self.C.P
        u = self.next_mm
        l, ch = self.units[u]
        self._dma()
        nb = len(self.wts)
        wt = self.wts[u % nb]
        col = l * 48 + ch
        ps = self.psum[:, u % self.nslot:u % self.nslot + 1]
        pk = "MODp%d" % (u % self.nslot)

        def mm(e):
            ins = None
            for kc in range(8):
                ins = e.matmul(ps, lhsT=wt[:, kc, :], rhs=self.c_bf[:, kc:kc + 1], start=(kc == 0), stop=(kc == 7), skip_group_check=True)
            return ins
        P.op("pe", mm, reads=["MODw%d" % (u % nb), "MODcb"], writes=[pk])
        P.op("dve", lambda e: e.tensor_tensor(out=self.modall[:, col:col + 1], in0=ps, in1=self.b_sb[:, col:col + 1], op=ALU.add),
             reads=[pk, "MODb"], writes=["MODall"])
        self.next_mm += 1
        if ch == 15 or ch == 47:
            c0 = l * 48 + (0 if ch == 15 else 16)
            c1 = l * 48 + ch + 1
            key = "MODs%d_%d" % (l, 0 if ch == 15 else 1)
            P.op("dve", lambda e: e.tensor_copy(out=self.sealed[:, c0:c1], in_=self.modall[:, c0:c1]), reads=["MODall"], writes=[key])
        return True

    def bg_tick(self, every=6):
        every = DBG.get("bg_every", every)
        if self.wts is None:
            return
        self.tick += 1
        if self.tick % every == 0:
            self.unit()

    def need(self, l, part):
        last = l * 48 + (15 if part == 0 else 47)
        if self.next_mm <= last:
            own = self.wts is None
            if own:
                C = self.C
                wts = [C.sb("MODfw%d_%d_%d" % (l, part, i), [128, 8, 128], BF16) for i in range(4)]
                pm = C.ps("MODfp%d_%d" % (l, part), [128, 8])
                self.attach(wts, pm[:, :])
            while self.next_mm <= last:
                self.unit()
            if own:
                self.detach()
        c0 = l * 48 + (0 if part == 0 else 16)
        c1 = l * 48 + (16 if part == 0 else 48)
        return self.sealed[:, c0:c1], "MODs%d_%d" % (l, part)


def emit_mod(C, cT, adaw, adab, nch, name):
    P = C.P
    c_sb = C.sb(name + "_c", [128, 8])
    b_sb = C.sb(name + "_b", [128, nch])
    mod = C.sb(name + "_mod", [128, nch])
    pm = C.ps(name + "_pm", [128, nch])
    wts = [C.sb(name + "_w%d" % i, [128, 8, 128]) for i in range(2)]
    P.dma("sp", c_sb[:], cT, writes=[name + "c"])
    P.dma("sp", b_sb[:], adab, writes=[name + "b"])
    P.op("act", lambda e: e.activation(out=c_sb[:], in_=c_sb[:], func=AF.Silu), reads=[name + "c"], writes=[name + "c"])
    for j in range(nch):
        wt = wts[j % 2]
        wk = name + "w%d" % (j % 2)
        P.dma("sp", wt[:], adaw[:, j * 128:(j + 1) * 128].rearrange("(kc ki) m -> ki kc m", ki=128), writes=[wk])

        def mm(e, wt=wt, j=j):
            ins = None
            for kc in range(8):
                ins = e.matmul(pm[:, j:j + 1], lhsT=wt[:, kc, :], rhs=c_sb[:, kc:kc + 1], start=(kc == 0), stop=(kc == 7))
            return ins
        P.op("pe", mm, reads=[wk, name + "c"], writes=[name + "pm"])
    P.op("dve", lambda e: e.tensor_tensor(out=mod[:], in0=pm[:], in1=b_sb[:], op=ALU.add),
         reads=[name + "pm", name + "b"], writes=[name + "mod"])
    return mod, name + "mod"


def emit_norm_block(C, xt, xkey, scale, shift, skey, ones_bf, hT, hkey, tmp_bufs, name, ntok=512, h32=None):
    P = C.P
    sq, ms, rstd, tmp = tmp_bufs
    P.op("act", lambda e: e.activation(out=sq[:, :, :ntok], in_=xt, func=AF.Square), reads=[xkey], writes=[name + "sq"])

    def mm(e):
        ins = None
        for kc in range(8):
            ins = e.matmul(ms[:, :ntok], lhsT=ones_bf[:], rhs=sq[:, kc, :ntok], start=(kc == 0), stop=(kc == 7))
        return ins
    P.op("pe", mm, reads=[name + "sq", "ones"], writes=[name + "ms"])
    P.op("act", lambda e: e.activation(out=rstd[:, :ntok], in_=ms[:, :ntok], func=AF.Sqrt, bias=C.eps_ap[:], scale=1.0),
         reads=[name + "ms", "eps"], writes=[name + "rstd"])
    P.op("dve", lambda e: e.reciprocal(out=rstd[:, :ntok], in_=rstd[:, :ntok]), reads=[name + "rstd"], writes=[name + "rstd"])
    for kc in range(8):
        P.op("dve", lambda e, kc=kc: e.scalar_tensor_tensor(out=tmp[:, kc, :ntok], in0=xt[:, kc, :], scalar=scale[:, kc:kc + 1],
                                                            in1=rstd[:, :ntok], op0=ALU.mult, op1=ALU.mult),
             reads=[xkey, skey, name + "rstd"], writes=[name + "tmp%d" % kc])
        if h32 is not None:
            P.op("act", lambda e, kc=kc: e.activation(out=h32[:, kc, :ntok], in_=tmp[:, kc, :ntok], func=AF.Identity,
                                                      bias=shift[:, kc:kc + 1], scale=1.0),
                 reads=[name + "tmp%d" % kc, skey], writes=[name + "h32_%d" % kc])
            P.op("pool", lambda e, kc=kc: e.tensor_copy(out=hT[:, kc, :ntok], in_=h32[:, kc, :ntok]),
                 reads=[name + "h32_%d" % kc], writes=[hkey])
        else:
            P.op("act", lambda e, kc=kc: e.activation(out=hT[:, kc, :ntok], in_=tmp[:, kc, :ntok], func=AF.Identity,
                                                      bias=shift[:, kc:kc + 1], scale=1.0),
                 reads=[name + "tmp%d" % kc, skey], writes=[hkey])


def setup_consts(C):
    P = C.P
    if hasattr(C, "ones_bf"):
        return
    C.ones_bf = C.root_stack.enter_context(C.nc.sbuf_tensor("ones_bf", [128, 128], BF16))
    C.eps_ap = C.root_stack.enter_context(C.nc.sbuf_tensor("eps_ap", [128, 1], F32))
    P.op("dve", lambda e: e.memset(C.ones_bf[:], 1.0 / D), writes=["ones"])
    P.op("dve", lambda e: e.memset(C.eps_ap[:], EPS), writes=["eps"])


NT1 = 2048


def build_A1():
    C = Ctx()
    P = C.P
    xT = C.inp("xT", [D, NT1])
    cT = C.inp("cT", [128, 8])
    adaw = C.inp("adaw", [D, 2048])
    adab = C.inp("adab", [128, 16])
    gam = C.inp("gam", [128, 8])
    w_in = C.inp("w_in", [D, IN_COLS])
    b_in = C.inp("b_in", [128, 27])
    projT = C.outp("projT", [27 * 128, NT1])
    setup_consts(C)
    mod, mkey = emit_mod(C, cT, adaw, adab, 16, "m1")
    gam_sb = C.sb("gam_sb", [128, 8])
    bin_sb = C.sb("bin_sb", [128, 27])
    scale = C.sb("scale", [128, 8])
    P.dma("sp", gam_sb[:], gam, writes=["gam"])
    P.dma("sp", bin_sb[:], b_in, writes=["bin"])
    P.op("dve", lambda e: e.scalar_tensor_tensor(out=scale[:], in0=mod[:, 8:16], scalar=1.0, in1=gam_sb[:], op0=ALU.add, op1=ALU.mult),
         reads=[mkey, "gam"], writes=["scale"])
    wb = C.sb("wb", [128, 8, IN_COLS], BF16)
    for i in range(7):
        P.dma("pool", wb[:, :, i * 480:(i + 1) * 480], w_in[:, i * 480:(i + 1) * 480].rearrange("(kc ki) m -> ki kc m", ki=128),
              writes=["wb%d" % i])
    wkeys = ["wb%d" % i for i in range(7)]
    xts = [C.sb("xt%d" % i, [128, 8, 512]) for i in range(2)]
    hT = C.sb("hT", [128, 8, 512], BF16)
    tmpb = (C.sb("sq", [128, 8, 512], BF16), C.ps("ms", [128, 512]), C.sb("rstd", [128, 512]), C.sb("tmp", [128, 8, 512]))
    pps = [C.ps("pp%d" % i, [128, 512]) for i in range(4)]
    obs = [C.sb("ob%d" % i, [128, 512]) for i in range(4)]
    if xT_chunks is None:
        xT3 = xT.rearrange("(kc ki) t -> ki kc t", ki=128)
    for tb in range(NT1 // 512):
        xt = xts[tb % 2]
        xk = "xt%d" % (tb % 2)
        P.dma("sp", xt[:], xT3[:, :, tb * 512:(tb + 1) * 512], writes=[xk])
        emit_norm_block(C, xt[:], xk, scale, mod, "scale", C.ones_bf, hT, "hT", tmpb, "n1")
        for m in range(27):
            mw = 128 if m < 26 else IN_COLS - 26 * 128
            pp = pps[m % 4]
            ob = obs[m % 4]

            def mm(e, m=m, mw=mw, pp=pp):
                ins = None
                for kc in range(8):
                    ins = e.matmul(pp[:mw, :], lhsT=wb[:, kc, m * 128:m * 128 + mw], rhs=hT[:, kc, :], start=(kc == 0), stop=(kc == 7))
                return ins
            P.op("pe", mm, reads=["hT"] + wkeys, writes=["pp%d" % (m % 4)])
            P.op("act", lambda e, m=m, mw=mw, pp=pp, ob=ob: e.activation(out=ob[:mw, :], in_=pp[:mw, :], func=AF.Identity,
                                                                          bias=bin_sb[:mw, m:m + 1], scale=1.0),
                 reads=["pp%d" % (m % 4), "bin"], writes=["ob%d" % (m % 4)])
            P.dma("pool", projT[m * 128:m * 128 + mw, tb * 512:(tb + 1) * 512], ob[:mw, :], reads=["ob%d" % (m % 4)])
    return C.close()


def barrier(P):
    toks = list(P.ctoks)
    for q in P.dsem:
        for i, v in enumerate(P.dval[q]):
            if v:
                toks.append((("d", q, i), v))
    for o in P.ENG:
        if P.count[o]:
            toks.append((o, P.count[o]))
    for e in P.ENG:
        for t in toks:
            if t[0] != e:
                P._wait(e, t)
    for e in ("pe", "dve", "act", "pool"):
        if P.count[e]:
            P._wait(e, (e, P.count[e]))


def build_A4(moe, final):
    C = Ctx()
    emit_A4(C, moe, final)
    return C.close()


def emit_A4(C, moe, final):
    P = C.P
    NT = 2048
    NBk = NT // 512
    fused = "zT" in C.bind
    xT_chunks = C.bind.get("xT_chunks")
    if xT_chunks is None:
        xT = C.inp("xT", [D, NT])
    xo_chunks = C.bind.get("xo_chunks")
    if fused:
        zT = C.bind["zT"]
    else:
        yT = C.inp("yT", [D, NT])
    mm_ = getattr(C, "modmgr", None)
    if mm_ is None:
        cT = C.inp("cT", [128, 8])
        adaw = C.inp("adaw", [D, 4096])
        adab = C.inp("adab", [128, 32])
    gam = C.inp("gam", [128, 8])
    if not fused:
        w_out = C.inp("w_out", [D, D])
    if moe:
        NE, FF = 8, 3584
        rw = C.inp("rw", [D, 8])
        sel = C.inp("sel", [8, 8 * 128])
        ident = C.inp("ident", [128, 128])
    else:
        NE, FF = 1, 2816
    wg = C.inp("wg", [NE, D, FF])
    wu = C.inp("wu", [NE, D, FF])
    wd = C.inp("wd", [NE, FF, D])
    if final:
        fgam = C.inp("fgam", [128, 8])
    if xo_chunks is None:
        xoT = C.outp("xoT", [D, NT])
    setup_consts(C)
    if mm_ is None:
        mod, mkey = emit_mod(C, cT, adaw, adab, 32, "m4")
    else:
        mod, mkey = mm_.need(C.layer, 1)
    gam_sb = C.sb("gam_sb", [128, 8])
    scale = C.sb("scale", [128, 8])
    P.dma("sp", gam_sb[:], gam, writes=["gam"])
    P.op("dve", lambda e: e.scalar_tensor_tensor(out=scale[:], in0=mod[:, 16:24], scalar=1.0, in1=gam_sb[:], op0=ALU.add, op1=ALU.mult),
         reads=[mkey, "gam"], writes=["scale"])
    if final:
        fg_sb = C.sb("fg_sb", [128, 8])
        P.dma("sp", fg_sb[:], fgam, writes=["fgam"])
    xs = C.sb("xs", [128, 8, NT])
    hT = C.sb("hT", [128, 8, NT], BF16)
    if xT_chunks is None:
        xT3 = xT.rearrange("(kc ki) t -> ki kc t", ki=128)
    if not fused:
        yT3 = yT.rearrange("(kc ki) t -> ki kc t", ki=128)
    if xo_chunks is None:
        xoT3 = xoT.rearrange("(kc ki) t -> ki kc t", ki=128)
    for tb in range(NBk):
        if xT_chunks is None:
            P.dma("sp", xs[:, :, tb * 512:(tb + 1) * 512], xT3[:, :, tb * 512:(tb + 1) * 512], reads=["xo_d"], writes=["xs%d" % tb])
        else:
            for i in range(4):
                P.dma("sp", xs[:, 2 * i:2 * i + 2, tb * 512:(tb + 1) * 512],
                      xT_chunks[i][:, tb * 512:(tb + 1) * 512].rearrange("(kc ki) t -> ki kc t", ki=128), reads=["xoc%d" % i], writes=["xs%d" % tb])
    if moe:
        wT = C.sb("wT", [8, NT], BF16)
    st1 = contextlib.ExitStack()
    main_stack = C.stack
    C.stack = st1
    if fused:
        ybs = [C.sb("zb%d" % i, [128, 8, 512]) for i in range(2)]
    else:
        wob = C.sb("wob", [128, 8, D], BF16)
        P.dma("pool", wob[:], w_out.rearrange("(kc ki) m -> ki kc m", ki=128), writes=["wob"])
        ybs = [C.sb("yb%d" % i, [128, 8, 512], BF16) for i in range(2)]
    tmpb = (C.sb("sq", [128, 8, 512], BF16), C.ps("ms", [128, 512]), C.sb("rstd", [128, 512]), C.sb("tmp", [128, 8, 512]))
    pzs = [C.ps("pz%d" % i, [128, 512]) for i in range(2)]
    if moe:
        h32 = C.sb("h32", [128, 8, 512])
        lgT = C.sb("lgT", [8, NT])
        rw_sb = C.sb("rw_sb", [128, 8, 8])
        P.dma("sp", rw_sb[:], rw.rearrange("(kc ki) e -> ki kc e", ki=128), writes=["rw"])
        plg = C.ps("plg", [8, 512])
    for tb in range(NBk):
        yb = ybs[tb % 2]
        yk = "yb%d" % (tb % 2)
        tsl = slice(tb * 512, (tb + 1) * 512)
        if fused:
            for gi in range(4):
                P.dma("sp", yb[:, 2 * gi:2 * gi + 2, :], zT[gi][:, tsl].rearrange("(kc ki) t -> ki kc t", ki=128), reads=["zs%d" % gi], writes=[yk])
        else:
            P.dma("pool", yb[:], yT3[:, :, tsl], writes=[yk])
        for m in range(8):
            if fused:
                P.op("dve", lambda e, m=m, yb=yb: e.scalar_tensor_tensor(out=xs[:, m, tsl], in0=yb[:, m, :], scalar=mod[:, m:m + 1],
                                                                         in1=xs[:, m, tsl], op0=ALU.mult, op1=ALU.add),
                     reads=[yk, mkey, "xs%d" % tb], writes=["xs%d" % tb])
                continue
            pz = pzs[m % 2]

            def mm(e, m=m, pz=pz, yb=yb):
                ins = None
                for kc in range(8):
                    ins = e.matmul(pz[:], lhsT=wob[:, kc, m * 128:(m + 1) * 128], rhs=yb[:, kc, :], start=(kc == 0), stop=(kc == 7))
                return ins
            P.op("pe", mm, reads=["wob", yk], writes=["pz%d" % (m % 2)])
            P.op("dve", lambda e, m=m, pz=pz: e.scalar_tensor_tensor(out=xs[:, m, tsl], in0=pz[:], scalar=mod[:, m:m + 1],
                                                                     in1=xs[:, m, tsl], op0=ALU.mult, op1=ALU.add),
                 reads=["pz%d" % (m % 2), mkey, "xs%d" % tb], writes=["xs%d" % tb])
        emit_norm_block(C, xs[:, :, tsl], "xs%d" % tb, scale, mod[:, 8:16], "scale", C.ones_bf, hT[:, :, tsl], "hT%d" % tb,
                        tmpb, "n4", h32=(h32 if moe else None))
        if moe:
            def mmr(e):
                ins = None
                for kc in range(8):
                    ins = e.matmul(plg[:], lhsT=rw_sb[:, kc, :], rhs=h32[:, kc, :], start=(kc == 0), stop=(kc == 7))
                return ins
            P.op("pe", mmr, reads=["rw"] + ["n4h32_%d" % kc for kc in range(8)], writes=["plg"])
            P.op("act", lambda e: e.copy(out=lgT[:, tsl], in_=plg[:]), reads=["plg"], writes=["lgT%d" % tb])
    if moe:
        id_sb = C.sb("id_sb", [128, 128])
        P.dma("sp", id_sb[:], ident, writes=["ident"])
        lg = C.sb("lg", [128, 16, 8])
        s8 = C.sb("s8", [128, 16, 8])
        e21 = C.sb("e21", [128, 16])
        w1 = C.sb("w1", [128, 16])
        w2 = C.sb("w2", [128, 16])
        m1 = C.sb("m1", [128, 16, 8])
        m2 = C.sb("m2", [128, 16, 8])
        ptr = C.ps("ptr", [128, 16, 8])

        def mmt(e):
            ins = None
            for tt in range(16):
                ins = e.transpose(ptr[:, tt, :], lgT[:, tt * 128:(tt + 1) * 128], id_sb[:8, :8])
            return ins
        P.op("pe", mmt, reads=["ident"] + ["lgT%d" % tb for tb in range(NBk)], writes=["ptr"])
        P.op("dve", lambda e: e.tensor_copy(out=lg[:], in_=ptr[:]), reads=["ptr"], writes=["lg"])
        for tt in range(16):
            P.op("dve", lambda e, tt=tt: e.max(out=s8[:, tt, :], in_=lg[:, tt, :]), reads=["lg"], writes=["s8_%d" % tt])
        s8k = ["s8_%d" % tt for tt in range(16)]
        P.op("dve", lambda e: e.tensor_tensor(out=e21[:], in0=s8[:, :, 1], in1=s8[:, :, 0], op=ALU.subtract), reads=s8k, writes=["e21"])
        P.op("act", lambda e: e.activation(out=e21[:], in_=e21[:], func=AF.Exp), reads=["e21"], writes=["e21"])
        P.op("dve", lambda e: e.tensor_scalar(out=w1[:], in0=e21[:], scalar1=1.0, scalar2=None, op0=ALU.add), reads=["e21"], writes=["w1"])
        P.op("dve", lambda e: e.reciprocal(out=w1[:], in_=w1[:]), reads=["w1"], writes=["w1"])
        P.op("dve", lambda e: e.tensor_tensor(out=w2[:], in0=e21[:], in1=w1[:], op=ALU.mult), reads=["e21", "w1"], writes=["w2"])
        for tt in range(16):
            P.op("dve", lambda e, tt=tt: e.tensor_scalar(out=m1[:, tt, :], in0=lg[:, tt, :], scalar1=s8[:, tt, 0:1], scalar2=w1[:, tt:tt + 1],
                                                         op0=ALU.is_equal, op1=ALU.mult), reads=["lg", "w1"] + s8k, writes=["m1_%d" % tt])
            P.op("dve", lambda e, tt=tt: e.tensor_scalar(out=m2[:, tt, :], in0=lg[:, tt, :], scalar1=s8[:, tt, 1:2], scalar2=w2[:, tt:tt + 1],
                                                         op0=ALU.is_equal, op1=ALU.mult), reads=["lg", "w2"] + s8k, writes=["m2_%d" % tt])
        P.op("dve", lambda e: e.tensor_tensor(out=m1[:], in0=m1[:], in1=m2[:], op=ALU.add),
             reads=["m1_%d" % tt for tt in range(16)] + ["m2_%d" % tt for tt in range(16)], writes=["wtok"])

        for tb in range(NBk):
            pz = pzs[tb % 2]

            def mmtb(e, tb=tb, pz=pz):
                ins = None
                for t4 in range(4):
                    tt = tb * 4 + t4
                    ins = e.transpose(pz[:8, t4 * 128:(t4 + 1) * 128], m1[:, tt, :], id_sb[:])
                return ins
            P.op("pe", mmtb, reads=["wtok", "ident"], writes=["pz%d" % (tb % 2)])
            P.op("dve", lambda e, tb=tb, pz=pz: e.tensor_copy(out=wT[:, tb * 512:(tb + 1) * 512], in_=pz[:8, :]),
                 reads=["pz%d" % (tb % 2)], writes=["wT"])
    barrier(P)
    st1.close()
    C.stack = main_stack
    st2 = contextlib.ExitStack()
    C.stack = st2
    nch = FF // 128
    groups = []
    f0 = 0
    while f0 < nch:
        nf = min(4, nch - f0)
        groups.append((f0, nf))
        f0 += nf
    gbs = [C.sb("gb%d" % i, [128, 8, 512], BF16) for i in range(2)]
    ubs = [C.sb("ub%d" % i, [128, 8, 512], BF16) for i in range(2)]
    dbs = [C.sb("db%d" % i, [128, 4, D], BF16) for i in range(2)]
    abs_ = [C.sb("ab%d" % i, [128, 4, NT], BF16) for i in range(2)]
    sgs = [C.sb("sg%d" % i, [128, 512]) for i in range(2)]
    pgs = [C.ps("pg%d" % i, [128, 512]) for i in range(2)]
    pus = [C.ps("pu%d" % i, [128, 512]) for i in range(2)]
    pds = [C.ps("pd%d" % i, [128, 512]) for i in range(2)]
    if moe:
        sel_sb = C.sb("sel_sb", [8, 8, 128], BF16)
        P.dma("pool", sel_sb[:], sel.rearrange("k (e m) -> k e m", e=8), writes=["sel"])
        wBs = [C.sb("wB%d" % i, [128, NT], BF16) for i in range(2)]
    hkeys = ["hT%d" % tb for tb in range(NBk)]
    work = [(ex, gi) for ex in range(NE) for gi in range(len(groups))]

    def emit_gu(idx):
        ex, gi = work[idx]
        f0, nf = groups[gi]
        bi = idx % 2
        gb, ub, ab = gbs[bi], ubs[bi], abs_[bi]
        cols = slice(f0 * 128, (f0 + nf) * 128)
        P.dma("pool", gb[:, :, :nf * 128], wg[ex, :, cols].rearrange("(kc ki) m -> ki kc m", ki=128), writes=["gb%d" % bi])
        P.dma("pool", ub[:, :, :nf * 128], wu[ex, :, cols].rearrange("(kc ki) m -> ki kc m", ki=128), writes=["ub%d" % bi])
        if moe and gi == 0:
            wB = wBs[ex % 2]
            for tb in range(NBk):
                pd = pds[tb % 2]
                P.op("pe", lambda e, tb=tb, pd=pd: e.matmul(pd[:], lhsT=sel_sb[:, ex, :], rhs=wT[:, tb * 512:(tb + 1) * 512], start=True, stop=True),
                     reads=["sel", "wT"], writes=["pd%d" % (tb % 2)])
                P.op("act", lambda e, tb=tb, pd=pd, wB=wB: e.copy(out=wB[:, tb * 512:(tb + 1) * 512], in_=pd[:]),
                     reads=["pd%d" % (tb % 2)], writes=["wB%d_%d" % (ex % 2, tb)])
        k = 0
        for fc in range(nf):
            for tb in range(NBk):
                pg, pu, sg = pgs[k % 2], pus[k % 2], sgs[k % 2]
                kk = k % 2
                tsl = slice(tb * 512, (tb + 1) * 512)

                def mm(e, fc=fc, tsl=tsl, pg=pg, pu=pu):
                    ins = None
                    for kc in range(8):
                        ins = e.matmul(pg[:], lhsT=gb[:, kc, fc * 128:(fc + 1) * 128], rhs=hT[:, kc, tsl], start=(kc == 0), stop=(kc == 7))
                    for kc in range(8):
                        ins = e.matmul(pu[:], lhsT=ub[:, kc, fc * 128:(fc + 1) * 128], rhs=hT[:, kc, tsl], start=(kc == 0), stop=(kc == 7))
                    return ins
                P.op("pe", mm, reads=["gb%d" % bi, "ub%d" % bi, "hT%d" % tb], writes=["pg%d" % kk, "pu%d" % kk])
                P.op("act", lambda e, pg=pg, sg=sg: e.activation(out=sg[:], in_=pg[:], func=AF.Silu), reads=["pg%d" % kk], writes=["sg%d" % kk])
                akey = "ab%d_%d_%d" % (bi, fc, tb)
                if moe:
                    P.op("dve", lambda e, sg=sg, pu=pu: e.tensor_tensor(out=sg[:], in0=sg[:], in1=pu[:], op=ALU.mult),
                         reads=["sg%d" % kk, "pu%d" % kk], writes=["sg%d" % kk])
                    P.op("dve", lambda e, sg=sg, fc=fc, tsl=tsl: e.tensor_tensor(out=ab[:, fc, tsl], in0=sg[:], in1=wBs[ex % 2][:, tsl], op=ALU.mult),
                         reads=["sg%d" % kk, "wB%d_%d" % (ex % 2, tb)], writes=[akey])
                else:
                    P.op("dve", lambda e, sg=sg, pu=pu, fc=fc, tsl=tsl: e.tensor_tensor(out=ab[:, fc, tsl], in0=sg[:], in1=pu[:], op=ALU.mult),
                         reads=["sg%d" % kk, "pu%d" % kk], writes=[akey])
                k += 1

    def emit_dn(idx):
        ex, gi = work[idx]
        f0, nf = groups[gi]
        bi = idx % 2
        db, ab = dbs[bi], abs_[bi]
        P.dma("pool", db[:, :nf, :], wd[ex, f0 * 128:(f0 + nf) * 128, :].rearrange("(fc fi) m -> fi fc m", fi=128), writes=["db%d" % bi])
        k = 0
        for m in range(8):
            for tb in range(NBk):
                pd = pds[k % 2]
                tsl = slice(tb * 512, (tb + 1) * 512)

                def mm(e, m=m, tsl=tsl, pd=pd):
                    ins = None
                    for fc in range(nf):
                        ins = e.matmul(pd[:], lhsT=db[:, fc, m * 128:(m + 1) * 128], rhs=ab[:, fc, tsl], start=(fc == 0), stop=(fc == nf - 1))
                    return ins
                P.op("pe", mm, reads=["db%d" % bi] + ["ab%d_%d_%d" % (bi, fc, tb) for fc in range(nf)], writes=["pd%d" % (k % 2)])
                P.op("dve", lambda e, m=m, tsl=tsl, pd=pd: e.scalar_tensor_tensor(out=xs[:, m, tsl], in0=pd[:], scalar=mod[:, 24 + m:25 + m],
                                                                                 in1=xs[:, m, tsl], op0=ALU.mult, op1=ALU.add),
                     reads=["pd%d" % (k % 2), mkey, "xs%d" % tb], writes=["xs%d" % tb])
                k += 1

    emit_gu(0)
    for i in range(len(work)):
        if i + 1 < len(work):
            emit_gu(i + 1)
        emit_dn(i)
    barrier(P)
    st2.close()
    C.stack = main_stack
    if final:
        tmpb = (C.sb("fsq", [128, 8, 512], BF16), C.ps("fms", [128, 512]), C.sb("frstd", [128, 512]), C.sb("ftmp", [128, 8, 512]))
        sq, ms, rstd, tmp = tmpb
        for tb in range(NBk):
            tsl = slice(tb * 512, (tb + 1) * 512)
            xk = "xs%d" % tb
            P.op("act", lambda e, tsl=tsl: e.activation(out=sq[:], in_=xs[:, :, tsl], func=AF.Square), reads=[xk], writes=["fsq"])

            def mm(e):
                ins = None
                for kc in range(8):
                    ins = e.matmul(ms[:], lhsT=C.ones_bf[:], rhs=sq[:, kc, :], start=(kc == 0), stop=(kc == 7))
                return ins
            P.op("pe", mm, reads=["fsq", "ones"], writes=["fms"])
            P.op("act", lambda e: e.activation(out=rstd[:], in_=ms[:], func=AF.Sqrt, bias=C.eps_ap[:], scale=1.0), reads=["fms", "eps"], writes=["frstd"])
            P.op("dve", lambda e: e.reciprocal(out=rstd[:], in_=rstd[:]), reads=["frstd"], writes=["frstd"])
            for kc in range(8):
                P.op("dve", lambda e, kc=kc, tsl=tsl: e.scalar_tensor_tensor(out=tmp[:, kc, :], in0=xs[:, kc, tsl], scalar=fg_sb[:, kc:kc + 1],
                                                                            in1=rstd[:], op0=ALU.mult, op1=ALU.mult),
                     reads=[xk, "fgam", "frstd"], writes=["ftmp"])
            P.dma("sp", xoT3[:, :, tsl], tmp[:], reads=["ftmp"], writes=["xo_d"])
    else:
        for tb in range(NBk):
            tsl = slice(tb * 512, (tb + 1) * 512)
            if xo_chunks is None:
                P.dma("sp", xoT3[:, :, tsl], xs[:, :, tsl], reads=["xs%d" % tb], writes=["xo_d"])
            else:
                for i in range(4):
                    P.dma("sp", xo_chunks[i][:, tsl].rearrange("(kc ki) t -> ki kc t", ki=128), xs[:, 2 * i:2 * i + 2, tsl],
                          reads=["xs%d" % tb], writes=["xoc%d" % i])


def nsa_consts(g):
    t = np.arange(S)
    ti, tl = t // 128, t % 128
    qaug = np.zeros((4, 4, S), np.float32)
    for hl in range(4):
        slope = 2.0 ** (-8.0 * (4 * g + hl + 1) / 8)
        qaug[hl, 0] = -8 * slope * 128 * ti
        qaug[hl, 1] = -8 * slope * tl
        qaug[hl, 2] = 8 * slope
        qaug[hl, 3] = 8 * slope
    kaug = np.stack([np.ones(S), np.ones(S), 128.0 * ti, 1.0 * tl]).astype(np.float32)
    n = np.arange(256)
    ce = 16 * n + 31
    kaugc = np.stack([np.ones(256), np.ones(256), 128.0 * (ce // 128), 1.0 * (ce % 128)]).astype(np.float32)
    kaugc[:, 255] = 0
    pl = np.arange(128)[:, None]
    ql = np.arange(512)[None, :]
    wmask = np.zeros((128, 8, 512), np.float32)
    for j in range(-4, 4):
        dist = ql - 128 * j - pl
        wmask[:, j + 4, :] = np.where((dist >= 0) & (dist < 512), 0.0, NEG)
    cmask = np.zeros((128, 4, 512), np.float32)
    for j in range(4):
        cmask[:, j, :] = np.where(ql - 128 * j - pl >= 0, 0.0, NEG)
    cmpmask = np.zeros((128, 2, 8, 512), np.float32)
    for c in range(2):
        nn = c * 128 + np.arange(128)[:, None]
        for Q in range(8):
            vis = (16 * nn + 31 <= 512 * Q + ql) & (nn < 255)
            cmpmask[:, c, Q, :] = np.where(vis, 0.0, NEG)
    E = np.zeros((64, 32, 128), np.float32)
    for c in range(32):
        E[2 * c, c, :64] = 1
        E[2 * c + 1, c, 64:] = 1
    lo_c = np.arange(256)[:, None] * 16
    lo_s = np.arange(64)[None, :] * 64
    ovm = np.clip(np.minimum(lo_c + 32, lo_s + 64) - np.maximum(lo_c, lo_s), 0, None) / 32.0
    ovm[255] = 0
    ov = np.ascontiguousarray(ovm.reshape(2, 128, 64).transpose(1, 0, 2)).astype(np.float32)
    cur = t // 64
    j = np.arange(64)[None, :]
    valid = j <= cur[:, None]
    forced = (j == 0) | (j == cur[:, None]) | (j == cur[:, None] - 1)
    valid01 = valid.astype(np.float32)
    addtab = np.where(valid, np.where(forced, 1e4, 0.0), -1.0).astype(np.float32)
    v01 = np.ascontiguousarray(valid01.reshape(32, 128, 64).transpose(1, 0, 2))
    adt = np.ascontiguousarray(addtab.reshape(32, 128, 64).transpose(1, 0, 2))
    bf = ml_dtypes.bfloat16
    return dict(qaug=qaug, kaug=kaug, kaugc=kaugc, wmask=wmask.reshape(128, -1).astype(bf), cmask=cmask.reshape(128, -1).astype(bf),
                cmpmask=cmpmask.reshape(128, -1).astype(bf), Emat=E.reshape(64, -1).astype(bf), ov=ov.reshape(128, -1), v01=v01.reshape(128, -1),
                adt=adt.reshape(128, -1), identb=np.eye(128, dtype=np.float32))


def build_A2():
    C = Ctx()
    emit_A2(C)
    return C.close()


def emit_A2(C):
    P = C.P
    qT = C.inp("qT", [4, 64, S])
    kT = C.inp("kT", [3, 64, S])
    vcT = C.inp("vcT", [64, S])
    vtok = C.inp("vtok", [2, S, 64])
    gl = C.inp("gl", [S, 12])
    w1 = C.inp("w1", [2, 64, 32 * 128])
    w2 = C.inp("w2", [2, 128, 64])
    peT = C.inp("peT", [2, 64, 32])
    qaug = C.inp("qaug", [4, 4, S])
    kaug = C.inp("kaug", [4, S])
    kaugc = C.inp("kaugc", [4, 256])
    wmask_d = C.inp("wmask", [128, 8 * 512], BF16)
    cmask_d = C.inp("cmask", [128, 4 * 512], BF16)
    cmpmask_d = C.inp("cmpmask", [128, 16 * 512], BF16)
    E_d = C.inp("Emat", [64, 32 * 128], BF16)
    ov_d = C.inp("ov", [128, 128])
    v01_d = C.inp("v01", [128, 32 * 64])
    adt_d = C.inp("adt", [128, 32 * 64])
    id_d = C.inp("identb", [128, 128])
    fused = "yT_nsa" in C.bind
    if fused:
        yT_nsa = C.bind["yT_nsa"]
    else:
        o_out = C.outp("o", [S, 256])

    qa = [C.sb("qa%d" % h, [68, S], BF16) for h in range(4)]
    ks = C.sb("ks", [68, S], BF16)
    kw = C.sb("kw", [68, S], BF16)
    kc = C.sb("kc", [68, 256], BF16)
    Vs = C.sb("Vs", [128, 32, 65], BF16)
    Vw = C.sb("Vw", [128, 32, 65], BF16)
    Vc = C.sb("Vc", [128, 2, 65], BF16)
    wmask = C.sb("wmask_s", [128, 8, 512], BF16)
    cmask = C.sb("cmask_s", [128, 4, 512], BF16)
    cmpmask = C.sb("cmpmask_s", [128, 16, 512], BF16)
    Em = C.sb("Em", [64, 32, 128], BF16)
    ov = C.sb("ov_s", [128, 2, 64], BF16)
    v01 = C.sb("v01_s", [128, 32, 64])
    adt = C.sb("adt_s", [128, 32, 64])
    idb = C.sb("idb", [128, 128], BF16)
    gates = C.sb("gates", [128, 32, 12])
    o_sb = C.sb("o_sb", [128, 32, 256])
    selbT = C.sb("selbT", [64, S], BF16)
    qq = "sp" if fused else "pool"
    for h in range(4):
        P.dma(qq, qa[h][0:64, :], qT[h], writes=["qa%d" % h])
        P.dma("pool", qa[h][64:68, :], qaug[h], writes=["qa%d" % h])
    P.dma(qq, ks[0:64, :], kT[1], writes=["ks"])
    P.dma("pool", ks[64:68, :], kaug, writes=["ks"])
    P.dma(qq, kw[0:64, :], kT[2], writes=["kw"])
    P.dma("pool", kw[64:68, :], kaug, writes=["kw"])
    P.op("dve", lambda e: e.memset(kc[:], 0.0), writes=["kc"])
    P.dma("pool", kc[64:68, :], kaugc, writes=["kc"])
    P.op("dve", lambda e: e.memset(Vs[:], 1.0), writes=["Vs"])
    P.op("dve", lambda e: e.memset(Vw[:], 1.0), writes=["Vw"])
    P.op("dve", lambda e: e.memset(Vc[:], 1.0), writes=["Vc"])
    P.dma("pool", Vs[:, :, 0:64], vtok[0].rearrange("(t p) d -> p t d", p=128), writes=["Vs"])
    P.dma("pool", Vw[:, :, 0:64], vtok[1].rearrange("(t p) d -> p t d", p=128), writes=["Vw"])
    P.dma("act", cmpmask[:], cmpmask_d.rearrange("p (a b) -> p a b", a=16), writes=["cmpmask"])
    P.dma("act", wmask[:], wmask_d.rearrange("p (a b) -> p a b", a=8), writes=["wmask"])
    P.dma("act", cmask[:], cmask_d.rearrange("p (a b) -> p a b", a=4), writes=["cmask"])
    P.dma("act", Em[:], E_d.rearrange("p (a b) -> p a b", a=32), writes=["Em"])
    P.dma("pool", ov[:], ov_d.rearrange("p (a b) -> p a b", a=2), writes=["ov"])
    P.dma("pool", idb[:], id_d, writes=["idb"])
    P.dma("sp", v01[:], v01_d.rearrange("p (a b) -> p a b", a=32), writes=["v01"])
    P.dma("sp", adt[:], adt_d.rearrange("p (a b) -> p a b", a=32), writes=["adt"])
    P.dma("sp", gates[:], gl.rearrange("(t p) c -> p t c", p=128), writes=["gates"])
    P.op("act", lambda e: e.activation(out=gates[:], in_=gates[:], func=AF.Sigmoid), reads=["gates"], writes=["gates"])
    P.op("pool", lambda e: e.memset(o_sb[:], 0.0), writes=["o_sb"])

    st1 = contextlib.ExitStack()
    main_stack = C.stack
    C.stack = st1
    for kv in range(2):
        src = C.sb("csrc%d" % kv, [64, S], BF16)
        w1b = C.sb("w1b%d" % kv, [64, 32, 128], BF16)
        w2b = C.sb("w2b%d" % kv, [128, 64], BF16)
        peb = C.sb("peb%d" % kv, [64, 32], BF16)
        hid = C.sb("hid%d" % kv, [128, 256], BF16)
        cv = C.sb("cv%d" % kv, [128, 1])
        ph = C.ps("ph%d" % kv, [128, 256])
        pc = C.ps("pc%d" % kv, [128, 1])
        po = C.ps("po%d" % kv, [128, 256])
        sk = "csrc%d" % kv
        P.dma("sp" if fused else "pool", src[:], kT[0] if kv == 0 else vcT, writes=[sk])
        P.dma("pool", w1b[:], w1[kv].rearrange("d (i j) -> d i j", i=32), writes=["w1b%d" % kv])
        P.dma("pool", w2b[:], w2[kv], writes=["w2b%d" % kv])
        P.dma("pool", peb[:], peT[kv], writes=["peb%d" % kv])

        def mmh(e, src=src, w1b=w1b, ph=ph):
            ins = None
            for i in range(32):
                ins = e.matmul(ph[:, 0:255], lhsT=w1b[:, i, :], rhs=src[:, i:i + 16 * 254 + 1:16], start=(i == 0), stop=(i == 31))
            return ins
        P.op("pe", mmh, reads=[sk, "w1b%d" % kv], writes=["ph%d" % kv])

        def mmc(e, w1b=w1b, peb=peb, pc=pc):
            ins = None
            for i in range(32):
                ins = e.matmul(pc[:], lhsT=w1b[:, i, :], rhs=peb[:, i:i + 1], start=(i == 0), stop=(i == 31))
            return ins
        P.op("pe", mmc, reads=["peb%d" % kv, "w1b%d" % kv], writes=["pc%d" % kv])
        P.op("dve", lambda e, cv=cv, pc=pc: e.tensor_copy(out=cv[:], in_=pc[:]), reads=["pc%d" % kv], writes=["cv%d" % kv])
        P.op("dve", lambda e, hid=hid: e.memset(hid[:], 0.0), writes=["hid%d" % kv])
        P.op("act", lambda e, hid=hid, ph=ph, cv=cv: e.activation(out=hid[:, 0:255], in_=ph[:, 0:255], func=AF.Silu, bias=cv[:], scale=1.0),
             reads=["ph%d" % kv, "cv%d" % kv, "hid%d" % kv], writes=["hid%d" % kv])
        if kv == 0:
            P.op("pe", lambda e, w2b=w2b, hid=hid, po=po: e.matmul(po[0:64, 0:255], lhsT=w2b[:], rhs=hid[:, 0:255], start=True, stop=True),
                 reads=["w2b0", "hid0"], writes=["po0"])
            P.op("dve", lambda e, po=po: e.tensor_copy(out=kc[0:64, 0:255], in_=po[0:64, 0:255]), reads=["po0", "kc"], writes=["kc"])
        else:
            def mmv(e, w2b=w2b, hid=hid, po=po):
                ins = None
                for c in range(2):
                    ins = e.matmul(po[:, c * 64:(c + 1) * 64], lhsT=hid[:, c * 128:(c + 1) * 128], rhs=w2b[:], start=True, stop=True)
                return ins
            P.op("pe", mmv, reads=["w2b1", "hid1"], writes=["po1"])
            P.op("dve", lambda e, po=po: e.tensor_copy(out=Vc[:, :, 0:64], in_=po[:, 0:128].rearrange("p (c d) -> p c d", c=2)),
                 reads=["po1", "Vc"], writes=["Vc"])
    barrier(P)
    st1.close()
    C.stack = main_stack

    Sb = [C.ps("S%d" % i, [128, 512]) for i in range(3)]
    PT = [C.sb("PT%d" % i, [128, 512], BF16) for i in range(3)]
    oacc_f = [C.ps("oacc%d" % i, [128, 512]) for i in range(2)]
    impacc_f = [C.ps("impacc%d" % i, [128, 512]) for i in range(2)]
    oacc = [t[:, 0:260].rearrange("p (a b) -> p a b", a=4) for t in oacc_f]
    impacc = [t[:, 0:256].rearrange("p (a b) -> p a b", a=4) for t in impacc_f]
    ptr = C.ps("ptrs", [128, 1024], BF16)
    imp_sb = C.sb("imp_sb", [128, 4, 64])
    imp2 = C.sb("imp2", [128, 4, 64])
    imp3 = C.sb("imp3", [128, 4, 64])
    s8a = C.sb("s8a", [128, 4, 8])
    s8b = C.sb("s8b", [128, 4, 8])
    selb = C.sb("selb", [128, 4, 64], BF16)
    dmx = C.sb("dmx", [128, 4])
    coef = C.sb("coef", [128, 4])
    state = {"k": 0, "pass": 0}
    mm_ = getattr(C, "modmgr", None)
    if mm_ is not None and mm_.next_mm < len(mm_.units) and not DBG.get("no_bg"):
        bgw = [C.sb("bgw%d" % i, [128, 8, 128], BF16) for i in range(6)]
        if DBG.get("bg_own_bank"):
            bgp = Sb.pop()
            mm_.attach(bgw, bgp[:, 0:8])
        else:
            mm_.attach(bgw, impacc_f[0][:, 256:264])
    else:
        mm_ = None

    def attn_pass(chunks, hl, Q, br, with_imp):
        pi = state["pass"] % 2
        state["pass"] += 1
        oa = oacc[pi]
        ia = impacc[pi]
        Qsl = slice(Q * 512, (Q + 1) * 512)
        n = len(chunks)
        bufidx = []

        def emit_pv(idx):
            bi = bufidx[idx]
            _, vr, vkey, ovr = chunks[idx]

            def pv(e):
                ins = None
                for qt in range(4):
                    ins = e.matmul(oa[:, qt, :], lhsT=PT[bi][:, qt * 128:(qt + 1) * 128], rhs=vr, start=(idx == 0 and qt == 0),
                                   stop=(idx == n - 1), skip_group_check=True)
                if with_imp:
                    for qt in range(4):
                        ins = e.matmul(ia[:, qt, :], lhsT=PT[bi][:, qt * 128:(qt + 1) * 128], rhs=ovr, start=(idx == 0 and qt == 0),
                                       stop=(idx == n - 1), skip_group_check=True)
                return ins
            wr = ["oacc%d" % pi] + (["impacc%d" % pi] if with_imp else [])
            P.op("pe", pv, reads=["PT%d" % bi, vkey, "ov"], writes=wr)

        for idx in range(n):
            bi = state["k"] % len(Sb)
            state["k"] += 1
            bufidx.append(bi)
            mms = chunks[idx][0]

            def smm(e, mms=mms, bi=bi):
                ins = None
                for mi, (lt, rh, _) in enumerate(mms):
                    ins = e.matmul(Sb[bi][:], lhsT=lt, rhs=rh, start=(mi == 0), stop=(mi == len(mms) - 1))
                return ins
            rd = []
            for (_, _, r) in mms:
                rd.extend(r)
            P.op("pe", smm, reads=rd, writes=["S%d" % bi])
            P.op("act", lambda e, bi=bi: e.activation(out=PT[bi][:], in_=Sb[bi][:], func=AF.Exp, scale=0.125),
                 reads=["S%d" % bi], writes=["PT%d" % bi])
            if idx >= 1:
                emit_pv(idx - 1)
            if mm_ is not None and br == 2:
                mm_.bg_tick(2)
        emit_pv(n - 1)
        finalize(oa, "oacc%d" % pi, hl, Q, br, with_imp, ia, "impacc%d" % pi)

    def finalize(oa, okey, hl, Q, br, with_imp, ia=None, ikey=None):
        P.op("dve", lambda e: e.tensor_scalar(out=dmx[:], in0=oa[:, :, 64], scalar1=1e-30, scalar2=None, op0=ALU.max),
             reads=[okey], writes=["dmx"])
        P.op("dve", lambda e: e.reciprocal(out=dmx[:], in_=dmx[:]), reads=["dmx"], writes=["dmx"])
        P.op("dve", lambda e: e.tensor_tensor(out=coef[:], in0=dmx[:], in1=gates[:, Q * 4:Q * 4 + 4, hl * 3 + br], op=ALU.mult),
             reads=["dmx", "gates"], writes=["coef"])
        for qt in range(4):
            osl = o_sb[:, Q * 4 + qt, hl * 64:(hl + 1) * 64]
            P.op("dve", lambda e, qt=qt, osl=osl: e.scalar_tensor_tensor(out=osl, in0=oa[:, qt, 0:64], scalar=coef[:, qt:qt + 1], in1=osl,
                                                                         op0=ALU.mult, op1=ALU.add),
                 reads=[okey, "coef", "o_sb"], writes=["o_sb"])
            if with_imp:
                if hl == 0:
                    P.op("dve", lambda e, qt=qt: e.tensor_scalar(out=imp_sb[:, qt, :], in0=ia[:, qt, :], scalar1=dmx[:, qt:qt + 1], scalar2=None,
                                                                 op0=ALU.mult), reads=[ikey, "dmx"], writes=["imp_sb"])
                else:
                    P.op("dve", lambda e, qt=qt: e.scalar_tensor_tensor(out=imp_sb[:, qt, :], in0=ia[:, qt, :], scalar=dmx[:, qt:qt + 1],
                                                                        in1=imp_sb[:, qt, :], op0=ALU.mult, op1=ALU.add),
                         reads=[ikey, "dmx", "imp_sb"], writes=["imp_sb"])

    acc4 = [(oacc[0], "oacc0"), (oacc[1], "oacc1"),
            (impacc_f[0][:, 0:260].rearrange("p (a b) -> p a b", a=4), "impacc0"),
            (impacc_f[1][:, 0:260].rearrange("p (a b) -> p a b", a=4), "impacc1")]
    Msel = [C.sb("Msel%d" % i, [128, 512], BF16) for i in range(3)]
    PT4 = [C.sb("PTs%d" % i, [128, 512], BF16) for i in range(4)]
    st4 = {"m": 0, "p": 0}

    def selected_all_heads(Q):
        Qsl = slice(Q * 512, (Q + 1) * 512)
        nch = 4 * Q + 4
        pend = []

        def emit_pv(item):
            c, hl, pj = item
            oa, okey = acc4[hl]

            def pv(e):
                ins = None
                for qt in range(4):
                    ins = e.matmul(oa[:, qt, :], lhsT=PT4[pj][:, qt * 128:(qt + 1) * 128], rhs=Vs[:, c, :], start=(c == 0 and qt == 0),
                                   stop=(c == nch - 1), skip_group_check=True)
                return ins
            P.op("pe", pv, reads=["PTs%d" % pj, "Vs"], writes=[okey])

        for c in range(nch):
            bi = state["k"] % len(Sb)
            state["k"] += 1
            mi = st4["m"] % 3
            st4["m"] += 1
            P.op("pe", lambda e, bi=bi, c=c: e.matmul(Sb[bi][:], lhsT=Em[:, c, :], rhs=selbT[:, Qsl], start=True, stop=True),
                 reads=["Em", "selbT%d" % Q], writes=["S%d" % bi])
            P.op("act", lambda e, bi=bi, mi=mi: e.copy(out=Msel[mi][:], in_=Sb[bi][:]), reads=["S%d" % bi], writes=["Msel%d" % mi])
            for hl in range(4):
                bi = state["k"] % len(Sb)
                state["k"] += 1
                pj = st4["p"] % 4
                st4["p"] += 1
                if c >= 4 * Q:
                    def smm(e, bi=bi, c=c, hl=hl):
                        e.matmul(Sb[bi][:], lhsT=ks[:, c * 128:(c + 1) * 128], rhs=qa[hl][:, Qsl], start=True, stop=False)
                        return e.matmul(Sb[bi][:], lhsT=idb[:], rhs=cmask[:, c - 4 * Q, :], start=False, stop=True)
                    P.op("pe", smm, reads=["ks", "qa%d" % hl, "idb", "cmask"], writes=["S%d" % bi])
                else:
                    P.op("pe", lambda e, bi=bi, c=c, hl=hl: e.matmul(Sb[bi][:], lhsT=ks[:, c * 128:(c + 1) * 128], rhs=qa[hl][:, Qsl], start=True, stop=True),
                         reads=["ks", "qa%d" % hl], writes=["S%d" % bi])
                P.op("act", lambda e, bi=bi, pj=pj: e.activation(out=PT4[pj][:], in_=Sb[bi][:], func=AF.Exp, scale=0.125),
                     reads=["S%d" % bi], writes=["PTs%d" % pj])
                P.op("dve", lambda e, pj=pj, mi=mi: e.tensor_tensor(out=PT4[pj][:], in0=PT4[pj][:], in1=Msel[mi][:], op=ALU.mult),
                     reads=["PTs%d" % pj, "Msel%d" % mi], writes=["PTs%d" % pj])
                pend.append((c, hl, pj))
                if len(pend) > 2:
                    emit_pv(pend.pop(0))
        while pend:
            emit_pv(pend.pop(0))
        for hl in range(4):
            oa, okey = acc4[hl]
            finalize(oa, okey, hl, Q, 1, False)

    for Q in range(8):
        Qsl = slice(Q * 512, (Q + 1) * 512)
        ncc = 2 if Q >= 4 else 1
        for hl in range(4):
            chunks = []
            for c in range(ncc):
                mms = [(kc[:, c * 128:(c + 1) * 128], qa[hl][:, Qsl], ["kc", "qa%d" % hl]),
                       (idb[:], cmpmask[:, c * 8 + Q, :], ["idb", "cmpmask"])]
                chunks.append((mms, Vc[:, c, :], "Vc", ov[:, c, :]))
            attn_pass(chunks, hl, Q, 0, True)
        for hl in range(4):
            chunks = []
            for j in range(-4, 4):
                c = 4 * Q + j
                if c < 0:
                    continue
                mms = [(kw[:, c * 128:(c + 1) * 128], qa[hl][:, Qsl], ["kw", "qa%d" % hl]),
                       (idb[:], wmask[:, j + 4, :], ["idb", "wmask"])]
                chunks.append((mms, Vw[:, c, :], "Vw", None))
            attn_pass(chunks, hl, Q, 2, False)
        P.op("dve", lambda e: e.tensor_tensor(out=imp2[:], in0=imp_sb[:], in1=v01[:, Q * 4:Q * 4 + 4, :], op=ALU.mult),
             reads=["imp_sb", "v01"], writes=["imp2"])
        P.op("dve", lambda e: e.tensor_tensor(out=imp2[:], in0=imp2[:], in1=adt[:, Q * 4:Q * 4 + 4, :], op=ALU.add),
             reads=["imp2", "adt"], writes=["imp2"])
        for qt in range(4):
            P.op("dve", lambda e, qt=qt: e.max(out=s8a[:, qt, :], in_=imp2[:, qt, :]), reads=["imp2"], writes=["s8a%d" % qt])
            P.op("dve", lambda e, qt=qt: e.match_replace(out=imp3[:, qt, :], in_to_replace=s8a[:, qt, :], in_values=imp2[:, qt, :], imm_value=-3.0e38),
                 reads=["imp2", "s8a%d" % qt], writes=["imp3_%d" % qt])
            P.op("dve", lambda e, qt=qt: e.max(out=s8b[:, qt, :], in_=imp3[:, qt, :]), reads=["imp3_%d" % qt], writes=["s8b%d" % qt])
            P.op("dve", lambda e, qt=qt: e.tensor_scalar(out=selb[:, qt, :], in0=imp2[:, qt, :], scalar1=s8b[:, qt, 7:8], scalar2=None,
                                                         op0=ALU.is_ge),
                 reads=["imp2", "s8b%d" % qt, "imp3_%d" % qt], writes=["selb%d" % qt])

        def trs(e):
            ins = None
            for qt in range(4):
                ins = e.transpose(ptr[0:64, qt * 128:(qt + 1) * 128], selb[:, qt, :], idb[:])
            return ins
        P.op("pe", trs, reads=["selb%d" % qt for qt in range(4)] + ["idb"], writes=["ptrs"])
        P.op("dve", lambda e: e.tensor_copy(out=selbT[:, Qsl], in_=ptr[0:64, 0:512]), reads=["ptrs"], writes=["selbT%d" % Q])
        selected_all_heads(Q)
    if mm_ is not None:
        while mm_.unit():
            pass
        mm_.detach()
    if not fused:
        P.dma("sp", o_out.rearrange("(t p) c -> p t c", p=128), o_sb[:], reads=["o_sb"])
        return
    o_bf = C.sb("o_bf", [128, 32, 256], BF16)
    P.op("act", lambda e: e.copy(out=o_bf[:], in_=o_sb[:]), reads=["o_sb"], writes=["o_bf"])
    yst = [C.sb("yst%d" % i, [128, 2, 512], BF16) for i in range(2)]
    for t4 in range(8):
        ys = yst[t4 % 2]
        for cc in range(2):
            def trs(e, t4=t4, cc=cc):
                ins = None
                for j in range(4):
                    ins = e.transpose(ptr[:, j * 128:(j + 1) * 128], o_bf[:, t4 * 4 + j, cc * 128:(cc + 1) * 128], idb[:])
                return ins
            P.op("pe", trs, reads=["o_bf", "idb"], writes=["ptrs"])
            P.op("dve", lambda e, ys=ys, cc=cc: e.tensor_copy(out=ys[:, cc, :], in_=ptr[:, 0:512]), reads=["ptrs"], writes=["yst%d" % (t4 % 2)])
        P.dma("sp", yT_nsa[:, t4 * 512:(t4 + 1) * 512].rearrange("(cc p) t -> p cc t", p=128), ys[:], reads=["yst%d" % (t4 % 2)], writes=["yT_d"])


def build_A3():
    C = Ctx()
    emit_A3(C)
    return C.close()


def emit_A3(C):
    P = C.P
    NCH = 64
    qkT = C.inp("qkT", [2, 2, 128, S])
    convw = C.inp("convw", [128, 16])
    convb = C.inp("convb", [128, 4])
    vtok = C.inp("vtok", [2, S, 128])
    otok = C.inp("otok", [2, S, 128])
    fused = "yT_ml" in C.bind
    if fused:
        yT_ml = C.bind["yT_ml"]
        igfg = C.bind["igfg"]
    else:
        ig_d = C.inp("ig", [64, 128])
        fg_d = C.inp("fg", [64, 128])
    normg = C.inp("normg", [64, 256])
    tri_d = C.inp("tri", [64, 64])
    id_d = C.inp("ident", [128, 128])
    if not fused:
        y_out = C.outp("y", [2, S, 128])
    setup_consts(C)

    cw = C.sb("cw", [128, 16])
    cb = C.sb("cb", [128, 4])
    tri = C.sb("tri_s", [64, 64])
    idf = C.sb("idf", [128, 128])
    idb = C.sb("idb", [128, 128], BF16)
    ng = C.sb("ng", [64, 256])
    ones = C.sb("ones_f", [128, 128])
    P.dma("sp", cw[:], convw, writes=["cw"])
    P.dma("sp", cb[:], convb, writes=["cb"])
    P.dma("sp", tri[:], tri_d, writes=["tri"])
    P.dma("sp", idf[:], id_d, writes=["idf"])
    P.dma("pool", idb[:], id_d, writes=["idb"])
    P.dma("sp", ng[:], normg, writes=["ng"])
    P.op("dve", lambda e: e.memset(ones[:], 1.0), writes=["onesf"])

    T = {}
    for nm in ("u", "w", "inter", "ecl", "uew"):
        T[nm] = C.sb("T_" + nm, [64, 128])
    decay = C.sb("T_decay", [128, 128])
    ew = C.sb("T_ew", [128, 128])
    st0 = contextlib.ExitStack()
    main_stack = C.stack
    C.stack = st0
    igs = C.sb("igs", [64, 128])
    lf = C.sb("lf", [64, 128])
    a_sb = C.sb("a_sb", [64, 128])
    yv = C.sb("yv", [64, 128])
    yT = C.sb("yT", [128, 64])
    MT = C.sb("MT", [128, 64])
    Z = C.sb("Z", [128, 128])
    M = C.sb("M", [64, 128])
    M63 = C.sb("M63", [128, 128])
    aL = C.sb("aL", [128, 128])
    mB = C.sb("mB", [128, 128])
    mC = C.sb("mC", [128, 128])
    mx = C.sb("mx", [128, 128])
    tmpg = C.sb("tmpg", [128, 128])
    pa = C.ps("pa", [64, 128])
    paL = C.ps("paL", [128, 128])
    pt1 = C.ps("pt1", [128, 64])
    pt2 = C.ps("pt2", [64, 128])
    pt3 = C.ps("pt3", [128, 128])
    if fused:
        gT = C.sb("gT", [128, 2, 64])
        P.dma("sp", gT[:, 0, :], igfg[0:2].rearrange("h (c p) -> (h c) p", p=64), writes=["gT"])
        P.dma("sp", gT[:, 1, :], igfg[2:4].rearrange("h (c p) -> (h c) p", p=64), writes=["gT"])
        P.op("pe", lambda e: e.transpose(pt2[:], gT[:, 0, :], idf[:]), reads=["gT", "idf"], writes=["pt2"])
        P.op("dve", lambda e: e.tensor_copy(out=igs[:], in_=pt2[:]), reads=["pt2"], writes=["igs"])
        P.op("pe", lambda e: e.transpose(pt2[:], gT[:, 1, :], idf[:]), reads=["gT", "idf", "igs"], writes=["pt2"])
        P.op("dve", lambda e: e.tensor_copy(out=lf[:], in_=pt2[:]), reads=["pt2"], writes=["lf"])
    else:
        P.dma("sp", igs[:], ig_d, writes=["igs"])
        P.dma("sp", lf[:], fg_d, writes=["lf"])
    P.op("act", lambda e: e.activation(out=lf[:], in_=lf[:], func=AF.Exp, scale=-1.0), reads=["lf"], writes=["lf"])
    P.op("act", lambda e: e.activation(out=lf[:], in_=lf[:], func=AF.Ln, bias=ones[0:64, 0:1], scale=1.0), reads=["lf", "ones"], writes=["lf"])
    P.op("dve", lambda e: e.tensor_scalar(out=lf[:], in0=lf[:], scalar1=-1.0, scalar2=None, op0=ALU.mult), reads=["lf"], writes=["lf"])
    P.op("pe", lambda e: e.matmul(pa[:], lhsT=tri[:], rhs=lf[:], start=True, stop=True), reads=["tri", "lf"], writes=["pa"])
    P.op("pe", lambda e: e.matmul(paL[:], lhsT=ones[0:64, :], rhs=lf[:], start=True, stop=True), reads=["ones", "lf"], writes=["paL"])
    P.op("dve", lambda e: e.tensor_copy(out=a_sb[:], in_=pa[:]), reads=["pa"], writes=["a_sb"])
    P.op("dve", lambda e: e.tensor_copy(out=aL[:], in_=paL[:]), reads=["paL"], writes=["aL"])
    P.op("dve", lambda e: e.tensor_tensor(out=yv[:], in0=igs[:], in1=a_sb[:], op=ALU.subtract), reads=["igs", "a_sb"], writes=["yv"])
    P.op("act", lambda e: e.activation(out=T["u"][:], in_=yv[:], func=AF.Exp), reads=["yv"], writes=["T_u"])
    P.op("pe", lambda e: e.transpose(pt1[:], yv[:], idf[0:64, 0:64]), reads=["yv", "idf"], writes=["pt1"])
    P.op("dve", lambda e: e.tensor_copy(out=yT[:], in_=pt1[:]), reads=["pt1"], writes=["yT"])
    P.op("dve", lambda e: e.tensor_tensor_scan(out=MT[:], data0=yT[:], data1=yT[:], initial=-3.0e38, op0=ALU.max, op1=ALU.max),
         reads=["yT"], writes=["MT"])
    P.op("pe", lambda e: e.transpose(pt2[:], MT[:], idf[:]), reads=["MT", "idf"], writes=["pt2"])
    P.op("dve", lambda e: e.tensor_copy(out=M[:], in_=pt2[:]), reads=["pt2"], writes=["M"])
    P.op("dve", lambda e: e.tensor_scalar(out=Z[:], in0=ones[:], scalar1=MT[:, 63:64], scalar2=None, op0=ALU.mult), reads=["ones", "MT"], writes=["Z"])
    P.op("pe", lambda e: e.transpose(pt3[:], Z[:], idf[:]), reads=["Z", "idf"], writes=["pt3"])
    P.op("dve", lambda e: e.tensor_copy(out=M63[:], in_=pt3[:]), reads=["pt3"], writes=["M63"])
    for h in range(2):
        hs = slice(h * 64, (h + 1) * 64)
        P.op("dve", lambda e, hs=hs: e.tensor_tensor_scan(out=mB[:, hs], data0=M63[:, hs], data1=aL[:, hs], initial=0.0, op0=ALU.max, op1=ALU.add),
             reads=["M63", "aL"], writes=["mB%d" % h])
        P.op("dve", lambda e, h=h: e.memset(mC[:, h * 64:h * 64 + 1], 0.0), writes=["mC%d" % h])
        P.op("dve", lambda e, h=h: e.tensor_copy(out=mC[:, h * 64 + 1:(h + 1) * 64], in_=mB[:, h * 64:(h + 1) * 64 - 1]),
             reads=["mB%d" % h, "mC%d" % h], writes=["mC%d" % h])
    mk = ["mB0", "mB1", "mC0", "mC1"]
    P.op("dve", lambda e: e.tensor_tensor(out=mx[:], in0=mC[:], in1=M63[:], op=ALU.max), reads=mk + ["M63"], writes=["mx"])
    P.op("dve", lambda e: e.tensor_tensor(out=tmpg[:], in0=mC[:], in1=mx[:], op=ALU.subtract), reads=mk + ["mx"], writes=["tmpg"])
    P.op("act", lambda e: e.activation(out=decay[:], in_=tmpg[:], func=AF.Exp), reads=["tmpg"], writes=["T_decay"])
    P.op("act", lambda e: e.activation(out=ew[:], in_=mx[:], func=AF.Exp, scale=-1.0), reads=["mx"], writes=["T_ew"])
    P.op("dve", lambda e: e.tensor_tensor(out=tmpg[0:64, :], in0=mC[0:64, :], in1=M[:], op=ALU.max), reads=mk + ["M", "tmpg"], writes=["tmpg"])
    P.op("act", lambda e: e.activation(out=T["w"][:], in_=tmpg[0:64, :], func=AF.Exp, scale=-1.0), reads=["tmpg"], writes=["T_w"])
    P.op("dve", lambda e: e.tensor_tensor(out=yv[:], in0=mC[0:64, :], in1=tmpg[0:64, :], op=ALU.subtract), reads=mk + ["tmpg", "yv"], writes=["yv"])
    P.op("act", lambda e: e.activation(out=T["inter"][:], in_=yv[:], func=AF.Exp), reads=["yv"], writes=["T_inter"])
    P.op("act", lambda e: e.activation(out=a_sb[:], in_=a_sb[:], func=AF.Exp, scale=-1.0), reads=["a_sb"], writes=["a_sb"])
    P.op("dve", lambda e: e.tensor_tensor(out=T["ecl"][:], in0=a_sb[:], in1=T["w"][:], op=ALU.mult), reads=["a_sb", "T_w"], writes=["T_ecl"])
    P.op("dve", lambda e: e.tensor_tensor(out=T["uew"][:], in0=T["u"][:], in1=ew[0:64, :], op=ALU.mult), reads=["T_u", "T_ew"], writes=["T_uew"])
    barrier(P)
    st0.close()
    C.stack = main_stack

    qTb = C.sb("qTb", [128, S], BF16)
    kTb = C.sb("kTb", [128, S], BF16)
    ktok = C.sb("ktok", [64, NCH, 128], BF16)
    Va = C.sb("Va", [64, NCH, 129], BF16)
    Vp = C.sb("Vp", [64, NCH, 129], BF16)
    hraw = C.sb("hraw", [64, NCH, 129])
    osg = C.sb("osg", [64, NCH, 128])
    sqt = C.sb("sqt", [64, 16, 128])
    xpads = [C.sb("xpad%d" % i, [128, S + 3]) for i in range(2)]
    acc = C.sb("acc", [128, S])
    Cf = C.sb("Cf", [128, 129])
    Cbs = [C.sb("Cb%d" % i, [128, 129], BF16) for i in range(2)]
    tKV = C.sb("tKV", [128, 129])
    Gs = [C.sb("Gs%d" % i, [64, 64], BF16) for i in range(2)]
    den = C.sb("den", [64, NCH])
    ssq = C.sb("ssq", [64, NCH])
    pG_t = C.ps("pG", [64, 2, 64])
    pin_t = C.ps("pin", [64, 2, 129])
    pit_t = C.ps("pit", [64, 2, 129])
    pG = [pG_t[:, i, :] for i in range(2)]
    pin = [pin_t[:, i, :] for i in range(2)]
    pit = [pit_t[:, i, :] for i in range(2)]
    pKV = [C.ps("pKV%d" % i, [128, 129]) for i in range(2)]
    ptk = C.ps("ptk", [64, 4, 128], BF16)
    if fused:
        yTs = C.sb("yTs", [128, S], BF16)
        pty = C.ps("pty", [128, 512], BF16)
    for i in range(2):
        P.op("dve", lambda e, i=i: e.memset(xpads[i][:, 0:3], 0.0), writes=["xpad%d" % i])
    P.op("pool", lambda e: e.memset(Va[:, :, 128:129], 1.0), writes=["Va"])

    for h in range(2):
        for qk in range(2):
            xpad = xpads[qk]
            xkey = "xpad%d" % qk
            P.dma("sp", xpad[:, 3:], qkT[h, qk], writes=[xkey])
            wi = (h * 2 + qk) * 4
            P.op("dve", lambda e, wi=wi, h=h, qk=qk, xpad=xpad: e.tensor_scalar(out=acc[:], in0=xpad[:, 3:3 + S], scalar1=cw[:, wi + 3:wi + 4],
                                                                                scalar2=cb[:, h * 2 + qk:h * 2 + qk + 1], op0=ALU.mult, op1=ALU.add),
                 reads=[xkey, "cw", "cb"], writes=["acc"])
            for i in range(3):
                P.op("dve", lambda e, wi=wi, i=i, xpad=xpad: e.scalar_tensor_tensor(out=acc[:], in0=xpad[:, i:i + S], scalar=cw[:, wi + i:wi + i + 1],
                                                                                    in1=acc[:], op0=ALU.mult, op1=ALU.add),
                     reads=[xkey, "cw", "acc"], writes=["acc"])
            if qk == 0:
                P.op("act", lambda e: e.activation(out=acc[:], in_=acc[:], func=AF.Silu), reads=["acc"], writes=["acc"])
                P.op("dve", lambda e: e.tensor_scalar(out=qTb[:], in0=acc[:], scalar1=128.0 ** -0.5, scalar2=None, op0=ALU.mult),
                     reads=["acc"], writes=["qTb"])
            else:
                P.op("act", lambda e: e.activation(out=kTb[:], in_=acc[:], func=AF.Silu), reads=["acc"], writes=["kTb"])
        for c4 in range(NCH // 4):
            def trk(e, c4=c4):
                ins = None
                for j in range(4):
                    c = c4 * 4 + j
                    ins = e.transpose(ptk[:, j, :], kTb[:, c * 64:(c + 1) * 64], idb[:])
                return ins
            P.op("pe", trk, reads=["kTb", "idb"], writes=["ptk"])
            P.op("act", lambda e, c4=c4: e.copy(out=ktok[:, c4 * 4:(c4 + 1) * 4, :], in_=ptk[:]), reads=["ptk"], writes=["ktok"])
        P.dma("pool", Va[:, :, 0:128], vtok[h].rearrange("(c p) e -> p c e", p=64), writes=["Va"])
        P.dma("sp", osg[:], otok[h].rearrange("(c p) e -> p c e", p=64), writes=["osg"])
        P.op("act", lambda e: e.activation(out=osg[:], in_=osg[:], func=AF.Sigmoid), reads=["osg"], writes=["osg"])
        P.op("pool", lambda e, h=h: e.tensor_tensor(out=Vp[:], in0=Va[:], in1=T["uew"][:, h * 64:(h + 1) * 64].unsqueeze(2).to_broadcast([64, NCH, 129]), op=ALU.mult),
             reads=["Va", "T_uew"], writes=["Vp%d" % c for c in range(NCH)])
        P.op("dve", lambda e: e.memset(Cf[:], 0.0), writes=["Cf"])
        P.op("dve", lambda e: e.memset(Cbs[0][:], 0.0), writes=["Cb0"])
        P.op("dve", lambda e: e.memset(Cbs[1][:], 0.0), writes=["Cb1"])

        def emit_gkv(c):
            b = c % 2
            csl = slice(c * 64, (c + 1) * 64)
            P.op("pe", lambda e: e.matmul(pG[b], lhsT=kTb[:, csl], rhs=qTb[:, csl], start=True, stop=True),
                 reads=["kTb", "qTb"], writes=["pG%d" % b])
            P.op("pe", lambda e: e.matmul(pKV[b][:], lhsT=ktok[:, c, :], rhs=Vp[:, c, :], start=True, stop=True),
                 reads=["ktok", "Vp%d" % c], writes=["pKV%d" % b])

        emit_gkv(0)
        for c in range(NCH):
            b = c % 2
            col = h * 64 + c
            csl = slice(c * 64, (c + 1) * 64)
            if c + 1 < NCH:
                emit_gkv(c + 1)
            if c + 1 < NCH:
                nb_ = (c + 1) % 2
                P.op("dve", lambda e, b=b, col=col: e.scalar_tensor_tensor(out=Cf[:], in0=Cf[:], scalar=decay[:, col:col + 1], in1=pKV[b][:],
                                                                           op0=ALU.mult, op1=ALU.add), reads=["Cf", "T_decay", "pKV%d" % b], writes=["Cf"])
                P.op("act", lambda e, nb_=nb_: e.copy(out=Cbs[nb_][:], in_=Cf[:]), reads=["Cf"], writes=["Cb%d" % nb_])
            P.op("dve", lambda e, b=b, col=col: e.scalar_tensor_tensor(out=Gs[b][:], in0=pG[b], scalar=T["u"][:, col:col + 1], in1=tri[:],
                                                                       op0=ALU.mult, op1=ALU.mult),
                 reads=["pG%d" % b, "T_u", "tri"], writes=["Gs%d" % b])
            if c > 0:
                P.op("pe", lambda e, b=b, csl=csl: e.matmul(pin[b], lhsT=qTb[:, csl], rhs=Cbs[b][:], start=True, stop=True),
                     reads=["qTb", "Cb%d" % b], writes=["pin%d" % b])
            P.op("pe", lambda e, b=b, c=c: e.matmul(pit[b], lhsT=Gs[b][:], rhs=Va[:, c, :], start=True, stop=True),
                 reads=["Gs%d" % b, "Va"], writes=["pit%d" % b])
            if c > 0:
                P.op("act", lambda e, b=b, c=c, col=col: e.activation(out=hraw[:, c, :], in_=pin[b], func=AF.Copy, scale=T["inter"][:, col:col + 1]),
                     reads=["pin%d" % b, "T_inter", "hrawn"], writes=["hraw%d" % c])
                P.op("dve", lambda e, b=b, c=c, col=col: e.scalar_tensor_tensor(out=hraw[:, c, :], in0=pit[b], scalar=T["w"][:, col:col + 1],
                                                                                in1=hraw[:, c, :], op0=ALU.mult, op1=ALU.add),
                     reads=["pit%d" % b, "T_w", "hraw%d" % c], writes=["hraw%d" % c])
            else:
                P.op("dve", lambda e, b=b, c=c, col=col: e.tensor_scalar(out=hraw[:, c, :], in0=pit[b], scalar1=T["w"][:, col:col + 1], scalar2=None,
                                                                         op0=ALU.mult), reads=["pit%d" % b, "T_w", "hrawn"], writes=["hraw%d" % c])
        hk = ["hraw%d" % c for c in range(NCH)]
        hsl = slice(h * 64, (h + 1) * 64)
        P.op("act", lambda e: e.activation(out=den[:], in_=hraw[:, :, 128], func=AF.Abs), reads=hk, writes=["den"])
        P.op("dve", lambda e, hsl=hsl: e.tensor_tensor(out=den[:], in0=den[:], in1=T["ecl"][:, hsl], op=ALU.max), reads=["den", "T_ecl"], writes=["den"])
        P.op("dve", lambda e: e.reciprocal(out=den[:], in_=den[:]), reads=["den"], writes=["den"])
        P.op("dve", lambda e: e.tensor_tensor(out=hraw[:, :, 0:128], in0=hraw[:, :, 0:128], in1=den[:, :].unsqueeze(2).to_broadcast([64, NCH, 128]), op=ALU.mult),
             reads=hk + ["den"], writes=["hrawn"])
        P.op("dve", lambda e: e.tensor_tensor(out=osg[:], in0=osg[:], in1=hraw[:, :, 0:128], op=ALU.mult), reads=hk + ["hrawn", "osg"], writes=["osg"])
        for hf in range(4):
            P.op("pool", lambda e, hf=hf: e.tensor_tensor(out=sqt[:], in0=osg[:, hf * 16:(hf + 1) * 16, :], in1=osg[:, hf * 16:(hf + 1) * 16, :], op=ALU.mult),
                 reads=["osg"], writes=["sqt"])
            P.op("dve", lambda e, hf=hf: e.tensor_reduce(out=ssq[:, hf * 16:(hf + 1) * 16], in_=sqt[:], axis=AX.X, op=ALU.add), reads=["sqt"], writes=["ssq"])
        P.op("act", lambda e: e.activation(out=ssq[:], in_=ssq[:], func=AF.Sqrt, bias=C.eps_ap[0:64, :], scale=1.0 / 128), reads=["ssq", "eps"], writes=["ssq"])
        P.op("dve", lambda e: e.reciprocal(out=ssq[:], in_=ssq[:]), reads=["ssq"], writes=["ssq"])
        P.op("dve", lambda e: e.tensor_tensor(out=osg[:], in0=osg[:], in1=ssq[:, :].unsqueeze(2).to_broadcast([64, NCH, 128]), op=ALU.mult),
             reads=["osg", "ssq"], writes=["osg"])
        P.op("dve", lambda e, h=h: e.tensor_tensor(out=osg[:], in0=osg[:], in1=ng[:, h * 128:(h + 1) * 128].unsqueeze(1).to_broadcast([64, NCH, 128]), op=ALU.mult),
             reads=["osg", "ng"], writes=["osg"])
        if not fused:
            P.dma("sp", y_out[h].rearrange("(c p) e -> p c e", p=64), osg[:], reads=["osg"])
            continue
        P.op("act", lambda e: e.copy(out=Va[:, :, 0:128], in_=osg[:]), reads=["osg"], writes=["Va"])
        for c8 in range(NCH // 8):
            def trs(e, c8=c8):
                ins = None
                for j in range(8):
                    ins = e.transpose(pty[:, j * 64:(j + 1) * 64], Va[:, c8 * 8 + j, 0:128], idb[0:64, 0:64])
                return ins
            P.op("pe", trs, reads=["Va", "idb"], writes=["pty"])
            P.op("dve", lambda e, c8=c8: e.tensor_copy(out=yTs[:, c8 * 512:(c8 + 1) * 512], in_=pty[:]), reads=["pty"], writes=["yTs"])
        P.dma("sp", yT_ml[h * 128:(h + 1) * 128, :], yTs[:], reads=["yTs"], writes=["yT_d"])


DBG = {}
FMC = 1028
TMC = 652
PAIRS = [[0, 1], [2, 3], [4, 5], [6, 7]]


def emit_P1(C):
    P = C.P
    xgc = C.bind.get("xgc")
    if xgc is None:
        xg = C.inp("xg", [2 * D, 2048])
    mm_ = getattr(C, "modmgr", None)
    if mm_ is None:
        cT = C.inp("cT", [128, 8])
        adaw = C.inp("adaw", [D, 2048])
        adab = C.inp("adab", [128, 16])
    gam = C.inp("gam", [128, 8])
    wc = C.inp("wc", [D, FMC + TMC])
    bfm = C.inp("bfm", [128, 9])
    btm = C.inp("btm", [128, TMC])
    fm_d = C.bind["fm_d"]
    fmA_d = C.bind["fmA_d"]
    tm_d = C.bind["tm_d"]
    setup_consts(C)
    if mm_ is None:
        mod, mkey = emit_mod(C, cT, adaw, adab, 16, "m1")
    else:
        mod, mkey = mm_.need(C.layer, 0)
    gam_sb = C.sb("gam_sb", [128, 8])
    bfm_sb = C.sb("bfm_sb", [128, 9])
    btm_sb = C.sb("btm_sb", [128, TMC])
    scale = C.sb("scale", [128, 8])
    P.dma("sp", gam_sb[:], gam, writes=["gam"])
    P.dma("sp", bfm_sb[:], bfm, writes=["bfm"])
    P.dma("sp", btm_sb[:], btm, writes=["btm"])
    P.op("dve", lambda e: e.scalar_tensor_tensor(out=scale[:], in0=mod[:, 8:16], scalar=1.0, in1=gam_sb[:], op0=ALU.add, op1=ALU.mult),
         reads=[mkey, "gam"], writes=["scale"])
    NW = FMC + TMC
    wb = C.sb("wb", [128, 8, NW], BF16)
    for i in range(4):
        P.dma("pool", wb[:, :, i * 420:(i + 1) * 420], wc[:, i * 420:(i + 1) * 420].rearrange("(kc ki) m -> ki kc m", ki=128),
              writes=["wb%d" % i])
    wkeys = ["wb%d" % i for i in range(4)]
    xts = [C.sb("xt%d" % i, [128, 8, 512]) for i in range(2)]
    hTs = [C.sb("hT%d" % i, [128, 8, 512], BF16) for i in range(2)]
    tmpb = (C.sb("sq", [128, 8, 512], BF16), C.ps("ms", [128, 512]), C.sb("rstd", [128, 512]), C.sb("tmp", [128, 8, 512]))
    pps = [C.ps("pp%d" % i, [128, 512]) for i in range(4)]
    obs = [C.sb("ob%d" % i, [128, 512]) for i in range(4)]
    obb = [C.sb("obb%d" % i, [128, 512], BF16) for i in range(4)]
    otm = [C.sb("otm%d" % i, [128, TMC]) for i in range(2)]
    k = 0
    for tb in range(8):
        half, cb = tb // 4, (tb % 4) * 512
        xt = xts[tb % 2]
        xk = "xt%d" % (tb % 2)
        if xgc is None:
            P.dma("sp", xt[:], xg[half * D:(half + 1) * D, cb:cb + 512].rearrange("(kc ki) t -> ki kc t", ki=128), writes=[xk])
        else:
            for i in range(4):
                P.dma("sp", xt[:, 2 * i:2 * i + 2, :], xgc[i][half * 256:(half + 1) * 256, cb:cb + 512].rearrange("(kc ki) t -> ki kc t", ki=128),
                      reads=["xgc%d" % i], writes=[xk])
        hT = hTs[tb % 2]
        hk_ = "hT%d" % (tb % 2)
        emit_norm_block(C, xt[:], xk, scale, mod, "scale", C.ones_bf, hT, hk_, tmpb, "n1")
        for m in range(9):
            mw = 128 if m < 8 else FMC - 1024
            pp, ob = pps[k % 4], obs[k % 4]
            kk = k % 4
            k += 1

            def mm(e, m=m, mw=mw, pp=pp, hT=hT):
                ins = None
                for kc in range(8):
                    ins = e.matmul(pp[:mw, :], lhsT=wb[:, kc, m * 128:m * 128 + mw], rhs=hT[:, kc, :], start=(kc == 0), stop=(kc == 7))
                return ins
            P.op("pe", mm, reads=[hk_] + wkeys, writes=["pp%d" % kk])
            if m < 4:
                ob = obb[kk]
                P.op("act", lambda e, m=m, pp=pp, ob=ob: e.activation(out=ob[:], in_=pp[:], func=AF.Identity, bias=bfm_sb[:, m:m + 1], scale=1.0),
                     reads=["pp%d" % kk, "bfm"], writes=["obb%d" % kk])
                P.dma("act", fmA_d[m * 128:(m + 1) * 128, tb * 512:(tb + 1) * 512], ob[:], reads=["obb%d" % kk], writes=["fm_d"])
                continue
            P.op("act", lambda e, m=m, mw=mw, pp=pp, ob=ob: e.activation(out=ob[:mw, :], in_=pp[:mw, :], func=AF.Identity,
                                                                          bias=bfm_sb[:mw, m:m + 1], scale=1.0),
                 reads=["pp%d" % kk, "bfm"], writes=["ob%d" % kk])
            P.dma("act", fm_d[m * 128:m * 128 + mw, tb * 512:(tb + 1) * 512], ob[:mw, :], reads=["ob%d" % kk], writes=["fm_d"])
        for tt in range(4):
            ot = otm[tt % 2]
            for (c0, cw) in ((0, 512), (512, TMC - 512)):
                pp = pps[k % 4]
                kk = k % 4
                k += 1

                def mm(e, tt=tt, c0=c0, cw=cw, pp=pp, hT=hT):
                    ins = None
                    for kc in range(8):
                        ins = e.matmul(pp[:, :cw], lhsT=hT[:, kc, tt * 128:(tt + 1) * 128], rhs=wb[:, kc, FMC + c0:FMC + c0 + cw],
                                       start=(kc == 0), stop=(kc == 7))
                    return ins
                P.op("pe", mm, reads=[hk_] + wkeys, writes=["pp%d" % kk])
                P.op("dve", lambda e, c0=c0, cw=cw, pp=pp, ot=ot: e.tensor_tensor(out=ot[:, c0:c0 + cw], in0=pp[:, :cw], in1=btm_sb[:, c0:c0 + cw], op=ALU.add),
                     reads=["pp%d" % kk, "btm"], writes=["otm%d" % (tt % 2)])
            r0 = tb * 512 + tt * 128
            P.dma("pool", tm_d[r0:r0 + 128, :], ot[:], reads=["otm%d" % (tt % 2)], writes=["tm_d"])


def emit_P3b(C):
    P = C.P
    wo = C.inp("wo", [512, D])
    yT_d = C.bind["yT_d"]
    zp_g = C.bind["zp_g"]
    zs_g = C.bind["zs_g"]
    yT = C.sb("yT", [128, 4, S], BF16)
    wob = C.sb("wob", [128, 4, D], BF16)
    P.dma("pool", wob[:], wo.rearrange("(kc ki) m -> ki kc m", ki=128), writes=["wob"])
    for kc in range(4):
        P.dma("sp", yT[:, kc, :], yT_d[kc * 128:(kc + 1) * 128, :], reads=["yT_d"], writes=["yT%d" % kc])
    pps = [C.ps("pz%d" % i, [128, 512]) for i in range(4)]
    obs = [C.sb("oz%d" % i, [128, 512]) for i in range(4)]
    k = 0
    for gi in range(4):
        for m in (2 * gi, 2 * gi + 1):
            for tb in range(8):
                half, cb = tb // 4, (tb % 4) * 512
                pp, ob, kk = pps[k % 4], obs[k % 4], k % 4
                k += 1

                def mm(e, m=m, tb=tb, pp=pp):
                    ins = None
                    for kc in range(4):
                        ins = e.matmul(pp[:], lhsT=wob[:, kc, m * 128:(m + 1) * 128], rhs=yT[:, kc, tb * 512:(tb + 1) * 512], start=(kc == 0), stop=(kc == 3))
                    return ins
                P.op("pe", mm, reads=["wob"] + ["yT%d" % kc for kc in range(4)], writes=["pz%d" % kk])
                if k % 2:
                    P.op("act", lambda e, pp=pp, ob=ob: e.copy(out=ob[:], in_=pp[:]), reads=["pz%d" % kk], writes=["oz%d" % kk])
                else:
                    P.op("dve", lambda e, pp=pp, ob=ob: e.tensor_copy(out=ob[:], in_=pp[:]), reads=["pz%d" % kk], writes=["oz%d" % kk])
                r0 = half * 256 + (m % 2) * 128
                P.dma("sp", zp_g[gi].ap()[r0:r0 + 128, cb:cb + 512], ob[:], reads=["oz%d" % kk], writes=["zp%d" % gi])
        C.coll("ReduceScatter", ALU.add, PAIRS, zp_g[gi], zs_g[gi], reads=["zp%d" % gi], writes=["zs%d" % gi])


def build_fused(nlayers=2, dbg_dense=False):
    C = Ctx()
    nc = C.nc
    x_own = C.inp("x_own", [D, 2048])
    xg_in = C.inp("xg_in", [2 * D, 2048])
    out = C.outp("out", [D, 2048])
    fm_d = C.scratch("fm_d", [FMC, S]).ap()
    fmA_d = C.scratch("fmA_d", [512, S], BF16).ap()
    tm_d = C.scratch("tm_d", [S, TMC]).ap()
    yT_d = C.scratch("yT_d", [512, S], BF16).ap()
    zp_g = [C.scratch("zp_g%d" % i, [512, 2048]) for i in range(4)]
    zs_g = [C.scratch("zs_g%d" % i, [256, 2048]) for i in range(4)]
    xo_t = C.scratch("xo_d", [D, 2048])
    xoc_t = [C.scratch("xoc%d" % i, [256, 2048]) for i in range(4)]
    xgc_t = [C.scratch("xgc%d" % i, [512, 2048]) for i in range(4)]
    setup_consts(C)
    C.modmgr = ModMgr(C, nlayers)
    for l in range(nlayers):
        C.layer = l
        moe = (l % 2 == 1) and not dbg_dense
        last = (l == nlayers - 1)
        final = (l == 1)
        L = "L%d_" % l
        b1 = {"fm_d": fm_d, "fmA_d": fmA_d, "tm_d": tm_d}
        if l == 0:
            b1["xg"] = xg_in
        else:
            b1["xgc"] = [t.ap() for t in xgc_t]
        with C.phase(L + "P1_", b1):
            emit_P1(C)
        with C.phase(L + "A2_", {"qT": fmA_d[0:256].rearrange("(h d) t -> h d t", h=4),
                                 "kT": fmA_d[256:448].rearrange("(b d) t -> b d t", b=3),
                                 "vcT": fmA_d[448:512],
                                 "vtok": tm_d[:, 0:128].rearrange("t (b d) -> b t d", b=2),
                                 "gl": tm_d[:, 128:140],
                                 "yT_nsa": yT_d[0:256]}):
            emit_A2(C)
        with C.phase(L + "A3_", {"qkT": fm_d[512:1024].rearrange("(h q d) t -> h q d t", h=2, q=2),
                                 "igfg": fm_d[1024:1028],
                                 "vtok": tm_d[:, 140:396].rearrange("t (h e) -> h t e", h=2),
                                 "otok": tm_d[:, 396:652].rearrange("t (h e) -> h t e", h=2),
                                 "yT_ml": yT_d[256:512]}):
            emit_A3(C)
        with C.phase(L + "P3_", {"yT_d": yT_d, "zp_g": zp_g, "zs_g": zs_g}):
            emit_P3b(C)
        b4 = {"zT": [t.ap() for t in zs_g]}
        if l == 0:
            b4["xT"] = x_own
        else:
            b4["xT_chunks"] = [t.ap() for t in xoc_t]
        if last:
            b4["xoT"] = out
        else:
            b4["xo_chunks"] = [t.ap() for t in xoc_t]
        with C.phase(L + "A4_", b4):
            emit_A4(C, moe, final)
        if not last:
            for i in range(4):
                C.coll("AllGather", ALU.bypass, PAIRS, xoc_t[i], xgc_t[i], reads=["xoc%d" % i], writes=["xgc%d" % i])
    if DBG.get("dump_y"):
        dy = C.outp("dbg_y", [512, S])
        C.P.dma("pool", dy, yT_d, reads=["yT_d"])
    if DBG.get("dump_mod"):
        dm = C.outp("dbg_mod", [128, 48 * nlayers])
        C.P.dma("sp", dm, C.modmgr.sealed[:], reads=["MODs%d_%d" % (l, p) for l in range(nlayers) for p in range(2)])
    return C.close()


def _chunkT(v, n):
    return np.ascontiguousarray(np.asarray(v, np.float32).reshape(n, 128).T)


def _a2_inputs(proj, g, inp, l, consts):
    m = {}
    q = proj[:, 0:512].reshape(S, 8, 64)[:, 4 * g:4 * g + 4]
    m["qT"] = np.ascontiguousarray(q.transpose(1, 2, 0))
    kv = proj[:, 512:1280].reshape(S, 6, 2, 64)[:, :, g]
    m["kT"] = np.ascontiguousarray(kv[:, [0, 2, 4]].transpose(1, 2, 0))
    m["vcT"] = np.ascontiguousarray(kv[:, 1].T)
    m["vtok"] = np.ascontiguousarray(kv[:, [3, 5]].transpose(1, 0, 2))
    m["gl"] = np.ascontiguousarray(proj[:, 1280:1304].reshape(S, 8, 3)[:, 4 * g:4 * g + 4].reshape(S, 12))
    w1 = inp["cmp_w1"][l]
    m["w1"] = np.ascontiguousarray(w1.reshape(2, 32, 64, 128).transpose(0, 2, 1, 3).reshape(2, 64, 32 * 128))
    m["w2"] = np.ascontiguousarray(inp["cmp_w2"][l])
    m["peT"] = np.ascontiguousarray(inp["cmp_pe"][l].transpose(0, 2, 1))
    m.update(consts[g])
    return m


def _a3_inputs(proj, hp, inp, l):
    m = {}
    hs = [2 * hp, 2 * hp + 1]
    qk = proj[:, 1304:2328]
    qkT = np.zeros((2, 2, 128, S), np.float32)
    convw = np.zeros((128, 2, 2, 4), np.float32)
    convb = np.zeros((128, 2, 2), np.float32)
    cwl = inp["conv_w"][l]
    cbl = inp["conv_b"][l]
    for i, h in enumerate(hs):
        for j in range(2):
            cols = slice(j * 512 + h * 128, j * 512 + h * 128 + 128)
            qkT[i, j] = qk[:, cols].T
            convw[:, i, j, :] = cwl[:, cols].T
            convb[:, i, j] = cbl[cols]
    m["qkT"] = qkT
    m["convw"] = convw.reshape(128, 16)
    m["convb"] = convb.reshape(128, 4)
    v = proj[:, 2328:2840]
    o = proj[:, 2840:3352]
    ip = proj[:, 3352:3356]
    fp = proj[:, 3356:3360]
    m["vtok"] = np.ascontiguousarray(np.stack([v[:, h * 128:(h + 1) * 128] for h in hs]))
    m["otok"] = np.ascontiguousarray(np.stack([o[:, h * 128:(h + 1) * 128] for h in hs]))
    m["ig"] = np.ascontiguousarray(np.concatenate([ip[:, h].reshape(64, 64).T for h in hs], axis=1))
    m["fg"] = np.ascontiguousarray(np.concatenate([fp[:, h].reshape(64, 64).T for h in hs], axis=1))
    g = inp["mlstm_norm_g"][l]
    m["normg"] = np.ascontiguousarray(np.broadcast_to(np.concatenate([g[h * 128:(h + 1) * 128] for h in hs])[None, :], (64, 256)))
    m["tri"] = np.triu(np.ones((64, 64), np.float32))
    m["ident"] = np.eye(128, dtype=np.float32)
    return m


_PROGS = {}


def _prog(name, fn):
    if name not in _PROGS:
        _PROGS[name] = fn()
    return _PROGS[name]


def _core_cols(g):
    hs = [2 * g, 2 * g + 1]
    r = np.arange
    fm = [g * 256 + r(256)]
    for br in (0, 2, 4, 1):
        fm.append(512 + br * 128 + g * 64 + r(64))
    for h in hs:
        fm.append(1304 + h * 128 + r(128))
        fm.append(1304 + 512 + h * 128 + r(128))
    fm.append(np.array([3352 + hs[0], 3352 + hs[1], 3356 + hs[0], 3356 + hs[1]]))
    tm = [512 + 3 * 128 + g * 64 + r(64), 512 + 5 * 128 + g * 64 + r(64), 1280 + 12 * g + r(12)]
    for h in hs:
        tm.append(2328 + h * 128 + r(128))
    for h in hs:
        tm.append(2840 + h * 128 + r(128))
    fm = np.concatenate(fm)
    tm = np.concatenate(tm)
    assert fm.size == FMC and tm.size == TMC
    return fm, tm


def _fused_inputs(inp, core, nlayers, consts, sel, ident, dbg_dense=False):
    b, g = core // 2, core % 2
    x = inp["x"]
    m = {}
    xb = x[b]
    m["x_own"] = np.ascontiguousarray(xb[g * 2048:(g + 1) * 2048].T)
    m["xg_in"] = np.ascontiguousarray(xb.reshape(2, 2048, D).transpose(0, 2, 1).reshape(2 * D, 2048))
    fm, tm = _core_cols(g)
    hs = [2 * g, 2 * g + 1]
    m["MOD_cT"] = _chunkT(inp["c"][b], 8)
    m["MOD_adab"] = np.ascontiguousarray(np.concatenate([_chunkT(inp["ada_b"][l], 48) for l in range(nlayers)], axis=1))
    for l in range(nlayers):
        m["MOD_adaw%d" % l] = inp["ada_w"][l]
    for l in range(nlayers):
        L = "L%d_" % l
        p = L + "P1_"
        m[p + "gam"] = _chunkT(inp["norm_mix_g"][l], 8)
        m[p + "wc"] = np.ascontiguousarray(inp["w_in"][l][:, np.concatenate([fm, tm])])
        bf = np.zeros(9 * 128, np.float32)
        bf[:FMC] = inp["b_in"][l][fm]
        m[p + "bfm"] = _chunkT(bf, 9)
        m[p + "btm"] = np.ascontiguousarray(np.broadcast_to(inp["b_in"][l][tm][None, :], (128, TMC)))
        p = L + "A2_"
        w1 = inp["cmp_w1"][l]
        m[p + "w1"] = np.ascontiguousarray(w1.reshape(2, 32, 64, 128).transpose(0, 2, 1, 3).reshape(2, 64, 32 * 128))
        m[p + "w2"] = np.ascontiguousarray(inp["cmp_w2"][l])
        m[p + "peT"] = np.ascontiguousarray(inp["cmp_pe"][l].transpose(0, 2, 1))
        for k, v in consts[g].items():
            m[p + k] = v
        p = L + "A3_"
        convw = np.zeros((128, 2, 2, 4), np.float32)
        convb = np.zeros((128, 2, 2), np.float32)
        for i, h in enumerate(hs):
            for j in range(2):
                cols = slice(j * 512 + h * 128, j * 512 + h * 128 + 128)
                convw[:, i, j, :] = inp["conv_w"][l][:, cols].T
                convb[:, i, j] = inp["conv_b"][l][cols]
        m[p + "convw"] = convw.reshape(128, 16)
        m[p + "convb"] = convb.reshape(128, 4)
        gn = inp["mlstm_norm_g"][l]
        m[p + "normg"] = np.ascontiguousarray(np.broadcast_to(np.concatenate([gn[h * 128:(h + 1) * 128] for h in hs])[None, :], (64, 256)))
        m[p + "tri"] = np.triu(np.ones((64, 64), np.float32))
        m[p + "ident"] = ident
        p = L + "P3_"
        rows = np.concatenate([g * 256 + np.arange(256), 512 + hs[0] * 128 + np.arange(128), 512 + hs[1] * 128 + np.arange(128)])
        m[p + "wo"] = np.ascontiguousarray(inp["w_out"][l][rows])
        p = L + "A4_"
        moe = (l % 2 == 1) and not dbg_dense
        m[p + "gam"] = _chunkT(inp["norm_ffn_g"][l], 8)
        if moe:
            m.update({p + "rw": inp["router_w"][l // 2], p + "sel": sel.reshape(8, 1024), p + "ident": ident,
                      p + "wg": inp["moe_w_gate"][l // 2], p + "wu": inp["moe_w_up"][l // 2], p + "wd": inp["moe_w_down"][l // 2]})
        else:
            m.update({p + "wg": inp["ffn_w_gate"][0:1], p + "wu": inp["ffn_w_up"][0:1], p + "wd": inp["ffn_w_down"][0:1]})
        if l == 1:
            m[p + "fgam"] = _chunkT(inp["final_norm_g"], 8)
    return m


def run_fused(inputs, nlayers=2, dbg_dense=False):
    inp = {k: np.asarray(v, np.float32) for k, v in inputs.items()}
    cores = list(range(8))
    consts = [nsa_consts(0), nsa_consts(1)]
    ident = np.eye(128, dtype=np.float32)
    sel = np.zeros((8, 8, 128), np.float32)
    for e in range(8):
        sel[e, e, :] = 1
    maps = [_fused_inputs(inp, core, nlayers, consts, sel, ident, dbg_dense) for core in cores]
    nc = _prog("fused%d_%d" % (nlayers, dbg_dense), lambda: build_fused(nlayers, dbg_dense))
    if DBG.get("trace"):
        rr = run_bass_kernel_spmd(nc, maps, core_ids=cores, trace=True)
        DBG["result"] = rr
        res = rr.results
    else:
        res = run_bass_kernel_spmd(nc, maps, core_ids=cores).results
    DBG["res"] = res
    out = np.stack([np.concatenate([res[2 * b]["out"], res[2 * b + 1]["out"]], axis=1).T for b in range(NB)])
    return np.ascontiguousarray(out.astype(np.float32))


def kernel(**inputs):
    return run_fused(inputs, 2)
```
